# Optimizing a Trainium2 kernel written in Bass

```python
import jax
import jax.numpy as jnp
from jax import lax
import numpy as np

D_MODEL = 1024
BATCH = 8
SEQ = 4096
DEPTH = 4

N_ATTN_HEADS = 8
Q_RANK = 256
KV_RANK = 128
ATTN_V_DIM = 64
ATTN_WIDTH = N_ATTN_HEADS * ATTN_V_DIM
ATTN_SCALE = KV_RANK ** -0.5
IDX_HEADS = 8
IDX_DIM = 64
IDX_W_SCALE = (IDX_HEADS * IDX_DIM) ** -0.5
TOPK_MAX = 256
Q_BLOCK = 128
N_REC_HEADS = 8
REC_K_DIM = 64
REC_V_DIM = 64
REC_KEY_WIDTH = N_REC_HEADS * REC_K_DIM
REC_WIDTH = N_REC_HEADS * REC_V_DIM
CHUNK = 64
N_GROUPS = 4
EXPERTS_PER_GROUP = 8
N_EXPERTS = N_GROUPS * EXPERTS_PER_GROUP
TOP_K_IN_GROUP = 2
D_EXPERT = 512
MOE_BLOCK = 128
EPS = 1e-6
NEG = -1e30
TINY = 1e-30
SPLIT_SIZES = (Q_RANK, KV_RANK, IDX_DIM, IDX_HEADS, REC_KEY_WIDTH, REC_KEY_WIDTH, REC_WIDTH, REC_WIDTH, D_MODEL, D_MODEL)
D_IN = sum(SPLIT_SIZES)

kernel_name = 'hybrid_dsa_hgrn2_hmoe_adaln'


def rmsnorm(x, g):
    xf = x.astype(jnp.float32)
    y = xf * lax.rsqrt(jnp.mean(xf * xf, axis=-1, keepdims=True) + EPS)
    return y.astype(x.dtype) * g


def dsa_attention(c_q, c_kv, k_idx_pre, w_idx_pre, g_cq, g_ckv, g_kidx, w_q_up, w_idx_q, w_v_up):
    B, S, _ = c_q.shape
    cq = rmsnorm(c_q, g_cq)
    q = (cq @ w_q_up).reshape(B, S, N_ATTN_HEADS, KV_RANK)
    kv = rmsnorm(c_kv, g_ckv)
    q_idx = (cq @ w_idx_q).reshape(B, S, IDX_HEADS, IDX_DIM)
    k_idx = rmsnorm(k_idx_pre, g_kidx)
    w_idx = w_idx_pre * IDX_W_SCALE
    topk = min(TOPK_MAX, S // 4)
    qb = min(Q_BLOCK, S)
    nb = S // qb
    key_pos = jnp.arange(S)

    def to_blocks(a):
        return jnp.moveaxis(a.reshape(B, nb, qb, *a.shape[2:]), 1, 0)

    def block(args):
        q_b, qi_b, wi_b, b_id = args
        q_pos = b_id * qb + jnp.arange(qb)
        causal = key_pos[None, :] <= q_pos[:, None]
        logits = jnp.einsum('bqhd,bsd->bqhs', qi_b, k_idx)
        score = jnp.einsum('bqh,bqhs->bqs', wi_b, jax.nn.relu(logits)).astype(jnp.float32)
        score = jnp.where(causal[None], score, NEG)
        _, sel = lax.top_k(score, topk)
        kv_sel = jax.vmap(lambda kv_b, sel_b: kv_b[sel_b])(kv, sel)
        valid = sel <= q_pos[None, :, None]
        s = jnp.einsum('bqhr,bqkr->bqhk', q_b, kv_sel).astype(jnp.float32) * ATTN_SCALE
        s = jnp.where(valid[:, :, None, :], s, NEG)
        p = jax.nn.softmax(s, axis=-1).astype(kv.dtype)
        return jnp.einsum('bqhk,bqkr->bqhr', p, kv_sel)

    o = lax.map(block, (to_blocks(q), to_blocks(q_idx), to_blocks(w_idx), jnp.arange(nb)))
    o = jnp.moveaxis(o, 0, 1).reshape(B, S, N_ATTN_HEADS, KV_RANK)
    o = jnp.einsum('bshr,hrv->bshv', o, w_v_up)
    return o.reshape(B, S, ATTN_WIDTH)


def hgrn2(q_pre, f_pre, i_pre, o_gate_pre, lower_bound, g_out):
    B, S, _ = q_pre.shape
    nc = S // CHUNK
    z = f_pre.astype(jnp.float32)
    lb = lower_bound.astype(jnp.float32)
    sig = jax.nn.sigmoid(z)
    f = lb + (1.0 - lb) * sig
    log_f = jnp.log(jnp.maximum(f, TINY))
    k = (1.0 - lb) * (1.0 - sig)
    q = jax.nn.silu(q_pre.astype(jnp.float32))
    v = i_pre.astype(jnp.float32)

    def to_chunks(a, width):
        return a.reshape(B, nc, CHUNK, N_REC_HEADS, width).transpose(1, 0, 3, 2, 4)

    tri = jnp.tril(jnp.ones((CHUNK, CHUNK), dtype=bool))[:, :, None]

    def step(state, inp):
        q_c, k_c, v_c, a_c = inp
        A = jnp.cumsum(a_c, axis=2)
        diff = A[:, :, :, None, :] - A[:, :, None, :, :]
        decay = jnp.where(tri, jnp.exp(jnp.where(tri, diff, NEG)), 0.0)
        scores = jnp.einsum('bhtk,bhsk,bhtsk->bhts', q_c, k_c, decay)
        o = (jnp.einsum('bhts,bhsv->bhtv', scores, v_c)
             + jnp.einsum('bhtk,bhkv->bhtv', q_c * jnp.exp(A), state))
        A_end = A[:, :, -1:, :]
        state = (jnp.exp(A_end[:, :, 0, :])[..., None] * state
                 + jnp.einsum('bhsk,bhsv->bhkv', k_c * jnp.exp(A_end - A), v_c))
        return state, o

    state0 = jnp.zeros((B, N_REC_HEADS, REC_K_DIM, REC_V_DIM), jnp.float32)
    _, o = lax.scan(step, state0, (to_chunks(q, REC_K_DIM), to_chunks(k, REC_K_DIM),
                                   to_chunks(v, REC_V_DIM), to_chunks(log_f, REC_K_DIM)))
    o = o.transpose(1, 0, 3, 2, 4).reshape(B, S, N_REC_HEADS, REC_V_DIM).astype(q_pre.dtype)
    gate = jax.nn.silu(o_gate_pre).reshape(B, S, N_REC_HEADS, REC_V_DIM)
    return (rmsnorm(o, g_out) * gate).reshape(B, S, REC_WIDTH)


def hier_moe(h, w_grp, b_grp, w_exp_router, b_exp_router, w_gate, w_up, w_down):
    B, S, D = h.shape
    T = B * S
    x = h.reshape(T, D)
    grp_logits = (x @ w_grp + b_grp).astype(jnp.float32)
    grp_prob = jax.nn.softmax(grp_logits, axis=-1)
    g_sel = jnp.argmax(grp_logits, axis=-1).astype(jnp.int32)
    grp_gate = jnp.take_along_axis(grp_prob, g_sel[:, None], axis=1)[:, 0]
    exp_logits = (x @ w_exp_router + b_exp_router).astype(jnp.float32).reshape(T, N_GROUPS, EXPERTS_PER_GROUP)
    exp_logits = jnp.take_along_axis(exp_logits, g_sel[:, None, None], axis=1)[:, 0]
    top_val, top_idx = lax.top_k(exp_logits, TOP_K_IN_GROUP)
    gates = jax.nn.softmax(top_val, axis=-1) * grp_gate[:, None]
    expert_id = (g_sel[:, None] * EXPERTS_PER_GROUP + top_idx.astype(jnp.int32)).reshape(-1)
    token_id = jnp.repeat(jnp.arange(T, dtype=jnp.int32), TOP_K_IN_GROUP)
    gate = gates.reshape(-1)
    N = T * TOP_K_IN_GROUP
    order = jnp.argsort(expert_id)
    e_sorted = expert_id[order]
    counts = jnp.zeros((N_EXPERTS,), jnp.int32).at[expert_id].add(1)
    padded = (counts + MOE_BLOCK - 1) // MOE_BLOCK * MOE_BLOCK
    pad_end = jnp.cumsum(padded)
    pad_start = pad_end - padded
    start = jnp.cumsum(counts) - counts
    dest = pad_start[e_sorted] + (jnp.arange(N, dtype=jnp.int32) - start[e_sorted])
    P = N + N_EXPERTS * MOE_BLOCK
    n_blk = P // MOE_BLOCK
    buf_tok = jnp.zeros((P,), jnp.int32).at[dest].set(token_id[order])
    buf_gate = jnp.zeros((P,), h.dtype).at[dest].set(gate[order].astype(h.dtype))
    blk_expert = jnp.minimum(jnp.searchsorted(pad_end, jnp.arange(n_blk, dtype=jnp.int32) * MOE_BLOCK, side='right'),
                             N_EXPERTS - 1).astype(jnp.int32)
    xs = x[buf_tok].reshape(n_blk, MOE_BLOCK, D)

    def expert_block(args):
        xb, e = args
        hid = jax.nn.silu(xb @ w_gate[e]) * (xb @ w_up[e])
        return hid @ w_down[e]

    ys = lax.map(expert_block, (xs, blk_expert)).reshape(P, D)
    out = jnp.zeros((T, D), h.dtype).at[buf_tok].add(ys * buf_gate[:, None])
    return out.reshape(B, S, D)


def setup_inputs(seed: int = 0) -> dict:
    key = jax.random.key(seed)
    ks = jax.random.split(key, 32)
    L, D = DEPTH, D_MODEL

    def nrm(k, shape, scale):
        return jax.random.normal(k, shape, jnp.float32) * scale

    def gain(k, shape):
        return 1.0 + 0.05 * jax.random.normal(k, shape, jnp.float32)

    return {
        'x': nrm(ks[0], (BATCH, SEQ, D), 1.0),
        'c': nrm(ks[1], (BATCH, D), 1.0),
        'w_mod': nrm(ks[2], (L, D, 6 * D), 0.5 * D ** -0.5),
        'b_mod': nrm(ks[3], (L, 6 * D), 0.02),
        'g_norm1': gain(ks[4], (L, D)),
        'g_norm2': gain(ks[5], (L, D)),
        'w_in': nrm(ks[6], (L, D, D_IN), D ** -0.5),
        'g_cq': gain(ks[7], (L, Q_RANK)),
        'g_ckv': gain(ks[8], (L, KV_RANK)),
        'g_kidx': gain(ks[9], (L, IDX_DIM)),
        'w_q_up': nrm(ks[10], (L, Q_RANK, N_ATTN_HEADS * KV_RANK), Q_RANK ** -0.5),
        'w_idx_q': nrm(ks[11], (L, Q_RANK, IDX_HEADS * IDX_DIM), Q_RANK ** -0.5),
        'w_v_up': nrm(ks[12], (L, N_ATTN_HEADS, KV_RANK, ATTN_V_DIM), KV_RANK ** -0.5),
        'lb_logits': nrm(ks[13], (L, REC_KEY_WIDTH), 1.0),
        'g_rec': gain(ks[14], (L, REC_V_DIM)),
        'w_branch_a': nrm(ks[15], (L, ATTN_WIDTH, D), ATTN_WIDTH ** -0.5),
        'w_branch_r': nrm(ks[16], (L, REC_WIDTH, D), REC_WIDTH ** -0.5),
        'w_out': nrm(ks[17], (L, D, D), D ** -0.5),
        'w_grp': nrm(ks[18], (L, D, N_GROUPS), D ** -0.5),
        'b_grp': nrm(ks[19], (L, N_GROUPS), 0.01),
        'w_exp_router': nrm(ks[20], (L, D, N_EXPERTS), D ** -0.5),
        'b_exp_router': nrm(ks[21], (L, N_EXPERTS), 0.01),
        'w_gate': nrm(ks[22], (L, N_EXPERTS, D, D_EXPERT), D ** -0.5),
        'w_up': nrm(ks[23], (L, N_EXPERTS, D, D_EXPERT), D ** -0.5),
        'w_down': nrm(ks[24], (L, N_EXPERTS, D_EXPERT, D), D_EXPERT ** -0.5),
        'g_final': gain(ks[25], (D,)),
    }


def reference(x, c, w_mod, b_mod, g_norm1, g_norm2, w_in, g_cq, g_ckv, g_kidx, w_q_up, w_idx_q, w_v_up,
              lb_logits, g_rec, w_branch_a, w_branch_r, w_out, w_grp, b_grp, w_exp_router, b_exp_router,
              w_gate, w_up, w_down, g_final):
    split_at = np.cumsum(SPLIT_SIZES)[:-1].tolist()
    lb_p = jax.nn.softmax(lb_logits.astype(jnp.float32), axis=0)
    lower_bounds = jnp.clip(jnp.cumsum(lb_p, axis=0) - lb_p[0:1], 0.0, 1.0)
    c_act = jax.nn.silu(c)
    for l in range(DEPTH):
        mod = c_act @ w_mod[l] + b_mod[l]
        sh1, sc1, gt1, sh2, sc2, gt2 = jnp.split(mod[:, None, :], 6, axis=-1)
        h = rmsnorm(x, g_norm1[l]) * (1.0 + sc1) + sh1
        u = h @ w_in[l]
        c_q, c_kv, k_idx_pre, w_idx_pre, q_rec, f_rec, i_rec, og_rec, ga, gr = jnp.split(u, split_at, axis=-1)
        y_a = dsa_attention(c_q, c_kv, k_idx_pre, w_idx_pre, g_cq[l], g_ckv[l], g_kidx[l],
                            w_q_up[l], w_idx_q[l], w_v_up[l])
        y_r = hgrn2(q_rec, f_rec, i_rec, og_rec, lower_bounds[l], g_rec[l])
        merged = jax.nn.sigmoid(ga) * (y_a @ w_branch_a[l]) + jax.nn.sigmoid(gr) * (y_r @ w_branch_r[l])
        x = x + gt1 * (merged @ w_out[l])
        h2 = rmsnorm(x, g_norm2[l]) * (1.0 + sc2) + sh2
        x = x + gt2 * hier_moe(h2, w_grp[l], b_grp[l], w_exp_router[l], b_exp_router[l],
                               w_gate[l], w_up[l], w_down[l])
    return rmsnorm(x, g_final)
```

```python
import numpy as np
import concourse.bass as bass
import concourse.mybir as mybir
from contextlib import ExitStack

F32 = mybir.dt.float32
BF16 = mybir.dt.bfloat16
I32 = mybir.dt.int32
AF = mybir.ActivationFunctionType
ALU = mybir.AluOpType
AX = mybir.AxisListType


class Op:
    __slots__ = ("eng", "fn", "deps", "inc", "cnt", "dma", "key", "consumed", "idx")

    def __init__(self, eng, fn, dma=False, key=None):
        self.eng = eng
        self.fn = fn
        self.deps = []
        self.inc = dma
        self.cnt = 0
        self.dma = dma
        self.key = key
        self.consumed = False


class Prog:
    ENGS = ("pe", "act", "dve", "pool", "sp")

    def __init__(self, nc):
        self.nc = nc
        self.ops = {e: [] for e in self.ENGS}
        self.last_w = {}
        self.readers = {}
        self.since_barrier = []
        self.pending_barrier = {e: [] for e in self.ENGS}
        self.nops = 0

    def _add(self, op, reads, writes):
        deps = []
        seen = set()
        for k in list(reads) + list(writes):
            w = self.last_w.get(k)
            if w is not None and id(w) not in seen:
                seen.add(id(w)); deps.append(w)
        for k in writes:
            for r in self.readers.get(k, ()):
                if id(r) not in seen:
                    seen.add(id(r)); deps.append(r)
        pb = self.pending_barrier[op.eng]
        if pb:
            for d in pb:
                if id(d) not in seen:
                    seen.add(id(d)); deps.append(d)
            self.pending_barrier[op.eng] = []
        op.deps = [d for d in deps if d is not op]
        for d in op.deps:
            d.consumed = True
        for k in writes:
            self.last_w[k] = op
            self.readers[k] = []
        for k in reads:
            if k not in writes:
                self.readers.setdefault(k, []).append(op)
        self.ops[op.eng].append(op)
        self.since_barrier.append(op)
        self.nops += 1
        return op

    def op(self, eng, fn, reads=(), writes=()):
        return self._add(Op(eng, fn), reads, writes)

    def dma(self, eng, fn, reads=(), writes=(), key=None):
        assert key is not None
        return self._add(Op(eng, fn, dma=True, key=key), reads, writes)

    def barrier(self):
        lst = []
        last = {}
        for o in self.since_barrier:
            if o.dma:
                if not o.consumed:
                    lst.append(o)
            else:
                last[o.eng] = o
        lst.extend(last.values())
        for e in self.ENGS:
            self.pending_barrier[e] = self.pending_barrier[e] + lst
        self.since_barrier = []

    def finish(self, scratch):
        self.barrier()
        self.op("pool", lambda e: e.memset(scratch, 0.0), writes=["__fin"])

    def emit(self, es):
        nc = self.nc
        for e in self.ENGS:
            for o in self.ops[e]:
                for d in o.deps:
                    if d.dma:
                        continue
                    if d.eng == "pe" and o.eng == "pe" and not o.dma:
                        continue
                    d.inc = True
        esem = {}
        for e in self.ENGS:
            esem[e] = es.enter_context(nc.semaphore("s_" + e))
        dsem = {}
        dcnt = {}
        for e in self.ENGS:
            c = 0
            for o in self.ops[e]:
                if o.dma:
                    if o.key not in dsem:
                        dsem[o.key] = es.enter_context(nc.semaphore("d_%d" % len(dsem)))
                        dcnt[o.key] = 0
                    dcnt[o.key] += 16
                    o.cnt = dcnt[o.key]
                elif o.inc:
                    c += 1
                    o.cnt = c
        self.n_dsem = len(dsem)
        block = es.enter_context(nc.Block())

        def run(ename, h):
            waited = {}
            for o in self.ops[ename]:
                need = {}
                for d in o.deps:
                    if d.dma:
                        s = dsem[d.key]
                    else:
                        if d.eng == "pe" and ename == "pe" and not o.dma:
                            continue
                        s = esem[d.eng]
                    sid = id(s)
                    if need.get(sid, (None, 0))[1] < d.cnt:
                        need[sid] = (s, d.cnt)
                for sid, (s, v) in need.items():
                    if waited.get(sid, 0) < v:
                        h.wait_ge(s, v)
                        waited[sid] = v
                ins = o.fn(h)
                if o.dma:
                    ins.then_inc(dsem[o.key], 16)
                elif o.inc:
                    ins.then_inc(esem[ename], 1)

        @block.tensor
        def _(h):
            run("pe", h)

        @block.scalar
        def _(h):
            run("act", h)

        @block.vector
        def _(h):
            run("dve", h)

        @block.gpsimd
        def _(h):
            run("pool", h)

        @block.sync
        def _(h):
            run("sp", h)


class Arena:
    def __init__(self, nc, es, ncols, name="arena"):
        self.t = es.enter_context(nc.sbuf_tensor(name, [128, ncols], F32))
        self.ncols = ncols
        self.off = 0
        self.uid = 0

    def mark(self):
        return self.off

    def release(self, m):
        self.off = m

    def alloc(self, cols, dtype=F32, parts=128):
        if dtype == BF16:
            w = (cols + 1) // 2
        else:
            w = cols
        assert self.off + w <= self.ncols, ("arena overflow", self.off, w, self.ncols)
        v = self.t[0:parts, self.off:self.off + w]
        self.off += w
        if dtype != F32:
            v = v.bitcast(dtype)
            if dtype == BF16 and cols % 2:
                v = v[:, 0:cols]
        return v


S = 4096
D = 1024
NT = S // 128
DIN = 4552
EPS = 1e-6
O_CQ, O_CKV, O_KIDX, O_WIDX, O_QREC, O_FREC, O_IREC, O_OG, O_GA, O_GR = 0, 256, 384, 448, 456, 968, 1480, 1992, 2504, 3528


class K:
    pass


def mkctx(nc, es, debug=()):
    k = K()
    k.nc = nc
    k.es = es
    k.P = Prog(nc)
    k.A = Arena(nc, es, 52800)
    k.ps = [es.enter_context(nc.psum_tensor("bank%d" % i, [128, 512], F32)) for i in range(8)]
    k.debug = set(debug)
    k.dr = {}
    k.uid = 0
    return k


def dram(k, name, shape, dtype):
    if name in k.dr:
        return k.dr[name]
    kind = "ExternalOutput" if name in k.debug else "Internal"
    t = k.nc.dram_tensor(name, list(shape), dtype, kind=kind).ap()
    k.dr[name] = t
    return t


def setup_consts(k):
    A, P = k.A, k.P
    k.ident = A.alloc(128, F32)
    k.identb = A.alloc(128, BF16)
    k.ones_f = A.alloc(128, F32)
    k.ones_b = A.alloc(128, BF16)
    P.op("pool", lambda e: e.memset(k.ident, 0.0), writes=["ident"])
    P.op("pool", lambda e: e.affine_select(out=k.ident, in_=k.ident, pattern=[[-1, 128]], compare_op=ALU.not_equal,
                                           fill=1.0, base=0, channel_multiplier=1), reads=["ident"], writes=["ident"])
    P.op("dve", lambda e: e.tensor_copy(k.identb, k.ident), reads=["ident"], writes=["identb"])
    P.op("dve", lambda e: e.memset(k.ones_f, 1.0), writes=["ones_f"])
    k.cneg = A.alloc(128, F32)
    P.op("pool", lambda e: e.memset(k.cneg, 0.0), writes=["cneg"])
    P.op("pool", lambda e: e.affine_select(out=k.cneg, in_=k.cneg, pattern=[[-1, 128]], compare_op=ALU.is_ge,
                                           fill=-1e30, base=0, channel_multiplier=1), reads=["cneg"], writes=["cneg"])
    k.cmf = A.alloc(128, F32)
    P.op("pool", lambda e: e.memset(k.cmf, 1.0), writes=["cmf"])
    P.op("pool", lambda e: e.affine_select(out=k.cmf, in_=k.cmf, pattern=[[1, 128]], compare_op=ALU.is_ge, fill=0.0, base=0, channel_multiplier=-1), reads=["cmf"], writes=["cmf"])
    P.op("pool", lambda e: e.memset(k.cmf[0:64, 64:128], 0.0), reads=["cmf"], writes=["cmf"])
    P.op("dve", lambda e: e.memset(k.ones_b, 1.0), writes=["ones_b"])


def phase0(k, l, I):
    A, P, nc = k.A, k.P, k.nc
    ps = k.ps
    stg = A.alloc(128, F32)
    k.par = A.alloc(128, F32)
    par = k.par
    P.op("dve", lambda e: e.memset(stg, 0.0), writes=["stg"])
    rows = [
        (0, 48, I["b_mod"][l].rearrange("(r c) -> r c", c=128), 128),
        (48, 8, I["c"][0].rearrange("(r c) -> r c", c=128), 128),
        (56, 8, I["g_norm1"][l].rearrange("(r c) -> r c", c=128), 128),
        (64, 8, I["g_norm2"][l].rearrange("(r c) -> r c", c=128), 128),
        (72, 2, I["g_cq"][l].rearrange("(r c) -> r c", c=128), 128),
        (74, 1, I["g_ckv"][l].rearrange("(r c) -> r c", c=128), 128),
        (75, 1, I["g_kidx"][l].rearrange("(r c) -> r c", c=64), 64),
        (76, 16, I["lb_logits"].rearrange("l (r c) -> (l r) c", c=128), 128),
        (92, 8, I["g_final"].rearrange("(r c) -> r c", c=128), 128),
    ]
    for (r0, n, src, w) in rows:
        P.dma("sp", lambda e, r0=r0, n=n, src=src, w=w: e.dma_start(out=stg[r0:r0 + n, 0:w], in_=src),
              reads=[], writes=["stg"], key="p0stg")
    P.dma("sp", lambda e: e.dma_start(out=stg[75:76, 64:128], in_=I["g_kidx"][l].rearrange("(r c) -> r c", c=64)),
          writes=["stg"], key="p0stg")
    P.op("pe", lambda e: e.transpose(ps[0][:, 0:128], stg, k.ident), reads=["stg", "ident"], writes=["ps0"])
    P.op("dve", lambda e: e.tensor_copy(par, ps[0][:, 0:128]), reads=["ps0"], writes=["par"])
    k.g1 = par[:, 56:64]; k.g2 = par[:, 64:72]; k.gcq = par[:, 72:74]; k.gckv = par[:, 74:75]
    k.gkidx = par[:, 75:76]; k.gfin = par[:, 92:100]
    sm = A.alloc(64, F32)
    k.sm = sm
    cact = sm[:, 0:8]
    t1 = sm[:, 8:16]
    P.op("act", lambda e: e.activation(out=t1, in_=par[:, 48:56], func=AF.Exp, scale=-1.0), reads=["par"], writes=["sm"])
    P.op("dve", lambda e: e.tensor_scalar(t1, t1, 1.0, None, op0=ALU.add), reads=["sm"], writes=["sm"])
    P.op("dve", lambda e: e.reciprocal(t1, t1), reads=["sm"], writes=["sm"])
    P.op("dve", lambda e: e.tensor_tensor(cact, par[:, 48:56], t1, op=ALU.mult), reads=["sm", "par"], writes=["sm"])
    lbl = par[:, 76:92].rearrange("p (l c) -> p l c", l=4)
    el = sm[:, 16:32].rearrange("p (l c) -> p l c", l=4)
    mx = sm[:, 32:36]
    P.op("dve", lambda e: e.tensor_reduce(out=mx, in_=par[:, 76:92].rearrange("p (l c) -> p c l", l=4), axis=AX.X, op=ALU.max),
         reads=["par"], writes=["sm"])
    P.op("dve", lambda e: e.tensor_tensor(el, lbl, mx.unsqueeze(1).to_broadcast([128, 4, 4]), op=ALU.subtract),
         reads=["sm", "par"], writes=["sm"])
    P.op("act", lambda e: e.activation(out=sm[:, 16:32], in_=sm[:, 16:32], func=AF.Exp), reads=["sm"], writes=["sm"])
    ssum = sm[:, 36:40]
    P.op("dve", lambda e: e.tensor_reduce(out=ssum, in_=sm[:, 16:32].rearrange("p (l c) -> p c l", l=4), axis=AX.X, op=ALU.add),
         reads=["sm"], writes=["sm"])
    P.op("dve", lambda e: e.reciprocal(ssum, ssum), reads=["sm"], writes=["sm"])
    k.lb = sm[:, 40:44]
    k.oml = sm[:, 44:48]
    P.op("dve", lambda e: e.memset(k.lb, 0.0), reads=["sm"], writes=["sm"])
    for j in range(1, l + 1):
        P.op("dve", lambda e, j=j: e.tensor_tensor(k.lb, k.lb, el[:, j, :], op=ALU.add), reads=["sm"], writes=["sm"])
    P.op("dve", lambda e: e.tensor_tensor(k.lb, k.lb, ssum, op=ALU.mult), reads=["sm"], writes=["sm"])
    P.op("dve", lambda e: e.tensor_scalar(k.lb, k.lb, 0.0, 1.0, op0=ALU.max, op1=ALU.min), reads=["sm"], writes=["sm"])
    P.op("dve", lambda e: e.tensor_scalar(k.oml, k.lb, -1.0, 1.0, op0=ALU.mult, op1=ALU.add), reads=["sm"], writes=["sm"])
    cact_rep = A.alloc(8 * 128, F32).rearrange("p (k m) -> p k m", k=8)
    P.op("dve", lambda e: e.tensor_copy(cact_rep, cact.unsqueeze(2).to_broadcast([128, 8, 128])), reads=["sm"], writes=["crep"])
    k.gtbc = [A.alloc(1024, F32), A.alloc(1024, F32)]
    bmbc = A.alloc(1024, F32)
    modf = k.A.alloc(32, F32)
    gs = k.A.alloc(16, F32)
    m = A.mark()
    wm = [A.alloc(8 * 1024, F32).rearrange("p (k c) -> p k c", k=8) for _ in range(2)]
    groups = [(0, "fm", 0), (1, "fm", 8), (3, "fm", 16), (4, "fm", 24), (2, "tm", 0), (5, "tm", 1)]
    wsrc = I["w_mod"][l].rearrange("(k p) c -> p k c", p=128)
    for gi, (g, kind, o) in enumerate(groups):
        w = wm[gi % 2]
        wk = "wm%d" % (gi % 2)
        P.dma("sp", lambda e, w=w, g=g: e.dma_start(out=w, in_=wsrc[:, :, g * 1024:(g + 1) * 1024]), writes=[wk], key=wk)
        if kind == "fm":
            for cb in range(8):
                for kc in range(8):
                    P.op("pe", lambda e, w=w, cb=cb, kc=kc, o=o: e.matmul(ps[1][:, o + cb:o + cb + 1], lhsT=w[:, kc, cb * 128:(cb + 1) * 128],
                                                                       rhs=cact[:, kc:kc + 1], start=(kc == 0), stop=(kc == 7)),
                         reads=[wk, "sm"], writes=["ps1"])
        else:
            P.dma("sp", lambda e, g=g: e.dma_start(out=bmbc, in_=I["b_mod"][l, g * 1024:(g + 1) * 1024].partition_broadcast(128)),
                  writes=["bmbc"], key="bmbc")
            for nb in range(2):
                pk = "ps%d" % (2 + nb)
                for kc in range(8):
                    P.op("pe", lambda e, w=w, nb=nb, kc=kc: e.matmul(ps[2 + nb][:, :], lhsT=cact_rep[:, kc, :], rhs=w[:, kc, nb * 512:(nb + 1) * 512],
                                                                  start=(kc == 0), stop=(kc == 7)),
                         reads=[wk, "crep"], writes=[pk])
                P.op("dve", lambda e, nb=nb, o=o: e.tensor_tensor(k.gtbc[o][:, nb * 512:(nb + 1) * 512], ps[2 + nb][:, :], bmbc[:, nb * 512:(nb + 1) * 512], op=ALU.add),
                     reads=[pk, "bmbc"], writes=["gtbc%d" % o])
    bsel = [0, 8, 24, 32]
    for i, b0 in enumerate(bsel):
        P.op("dve", lambda e, i=i, b0=b0: e.tensor_tensor(modf[:, i * 8:(i + 1) * 8], ps[1][:, i * 8:(i + 1) * 8], par[:, b0:b0 + 8], op=ALU.add),
             reads=["ps1", "par"], writes=["modf"])
    k.sh1 = modf[:, 0:8]; k.sh2 = modf[:, 16:24]
    k.gs1 = gs[:, 0:8]; k.gs2 = gs[:, 8:16]
    P.op("dve", lambda e: e.scalar_tensor_tensor(out=k.gs1, in0=modf[:, 8:16], scalar=1.0, in1=k.g1, op0=ALU.add, op1=ALU.mult),
         reads=["modf", "par"], writes=["gs"])
    P.op("dve", lambda e: e.scalar_tensor_tensor(out=k.gs2, in0=modf[:, 24:32], scalar=1.0, in1=k.g2, op0=ALU.add, op1=ALU.mult),
         reads=["modf", "par"], writes=["gs"])
    P.barrier()
    A.release(m)


def norm_transpose(k, xsrc, t, hT, gs, sh, tag, hkey):
    A, P, ps = k.A, k.P, k.ps
    xb = k.xb[t % 2]; xk = "xb%d" % (t % 2)
    xn = k.xn[t % 2]; nk = "xn%d" % (t % 2)
    st = k.nst[t % 2]; sk = "nst%d" % (t % 2)
    P.dma("sp", lambda e: e.dma_start(out=xb, in_=xsrc[t * 128:(t + 1) * 128, :]), writes=[xk], key=xk)
    P.op("act", lambda e: e.activation(out=xn, in_=xb, func=AF.Square, accum_out=st[:, 0:1]), reads=[xk], writes=[nk, sk])
    P.op("dve", lambda e: e.tensor_scalar(st[:, 1:2], st[:, 0:1], 1.0 / D, EPS, op0=ALU.mult, op1=ALU.add), reads=[sk], writes=[sk])
    P.op("act", lambda e: e.activation(out=st[:, 1:2], in_=st[:, 1:2], func=AF.Ln), reads=[sk], writes=[sk])
    P.op("act", lambda e: e.activation(out=st[:, 1:2], in_=st[:, 1:2], func=AF.Exp, scale=-0.5), reads=[sk], writes=[sk])
    P.op("dve", lambda e: e.tensor_scalar(xn, xb, st[:, 1:2], None, op0=ALU.mult), reads=[xk, sk], writes=[nk])
    b0 = (t % 2) * 2
    for kc in range(8):
        bank = b0 + kc // 4
        P.op("pe", lambda e, kc=kc, bank=bank: e.transpose(ps[bank][:, (kc % 4) * 128:(kc % 4 + 1) * 128], xn[:, kc * 128:(kc + 1) * 128], k.ident),
             reads=[nk, "ident"], writes=["ps%d" % bank])
    return b0


def phaseA(k, l, I, xsrc):
    A, P, nc, ps = k.A, k.P, k.nc, k.ps
    m0 = A.mark()
    WC = DIN + 64
    w = A.alloc(8 * WC, BF16).rearrange("p (k c) -> p k c", k=8)
    wsrc = I["w_in"][l].rearrange("(k p) c -> p k c", p=128)
    for (d0, s0, n) in [(0, 0, 448), (448, 384, 64), (512, 448, 1024), (1536, 1472, 1024), (2560, 2496, 1024), (3584, 3520, 1032)]:
        P.dma("pool", lambda e, d0=d0, s0=s0, n=n: e.dma_start(out=w[:, :, d0:d0 + n], in_=wsrc[:, :, s0:s0 + n]), writes=["w_in"], key="w_in")
    k.xb = [A.alloc(1024, F32) for _ in range(2)]
    k.xn = [A.alloc(1024, F32) for _ in range(2)]
    k.nst = [A.alloc(2, F32) for _ in range(2)]
    hT = [A.alloc(8 * 512, BF16).rearrange("p (k t) -> p k t", k=8) for _ in range(2)]
    fo = [A.alloc(512, F32) for _ in range(4)]
    gb = [A.alloc(512, BF16) for _ in range(4)]
    to = [A.alloc(1672, F32) for _ in range(2)]
    cq_fm = dram(k, "cq_fm", [256, S], F32)
    ckv_fm = dram(k, "ckv_fm", [128, S], F32)
    kidx_fm = dram(k, "kidx_fm", [128, S], F32)
    qrec_fm = dram(k, "qrec_fm", [512, S], F32)
    frec_fm = dram(k, "frec_fm", [512, S], F32)
    gates_fm = dram(k, "gates_fm", [2048, S], BF16)
    tm_out = dram(k, "tm_out", [S, 1672], F32)
    fmb = [(0, cq_fm, 0, 0), (128, cq_fm, 128, 0), (256, ckv_fm, 0, 0), (384, kidx_fm, 0, 0)]
    for j in range(4):
        fmb.append((O_QREC + 64 + j * 128, qrec_fm, j * 128, 0))
    for j in range(4):
        fmb.append((O_FREC + 64 + j * 128, frec_fm, j * 128, 0))
    for j in range(16):
        fmb.append((O_GA + 64 + j * 128, gates_fm, j * 128, 1))
    tmg = [(256, 128, 0), (O_WIDX + 64, 8, 128), (O_IREC + 64, 512, 136), (O_OG + 64, 512, 648), (O_FREC + 64, 512, 1160)]
    fcount = 0
    for st in range(S // 512):
        h = hT[st % 2]; hk = "hT%d" % (st % 2)
        for tt in range(4):
            t = st * 4 + tt
            b0 = norm_transpose(k, xsrc, t, None, None, None, None, None)
            for kc in range(8):
                bank = b0 + kc // 4
                P.op("act", lambda e, kc=kc, bank=bank, tt=tt, h=h: e.activation(out=h[:, kc, tt * 128:(tt + 1) * 128], in_=ps[bank][:, (kc % 4) * 128:(kc % 4 + 1) * 128],
                                                                                 func=AF.Identity, scale=k.gs1[:, kc:kc + 1], bias=k.sh1[:, kc:kc + 1]),
                     reads=["ps%d" % bank, "gs", "modf"], writes=[hk])
        for bi, (c0, dst, r0, act) in enumerate(fmb):
            bank = 4 + fcount % 4
            pk = "ps%d" % bank
            for kc in range(8):
                P.op("pe", lambda e, kc=kc, c0=c0, bank=bank, h=h: e.matmul(ps[bank][:, :], lhsT=w[:, kc, c0:c0 + 128], rhs=h[:, kc, :], start=(kc == 0), stop=(kc == 7)),
                     reads=["w_in", hk], writes=[pk])
            f = fo[fcount % 4]; fk = "fo%d" % (fcount % 4)
            if act == 0:
                P.op("dve", lambda e, f=f, bank=bank: e.tensor_copy(f, ps[bank][:, :]), reads=[pk], writes=[fk])
                P.dma("sp", lambda e, f=f, dst=dst, r0=r0, st=st: e.dma_start(out=dst[r0:r0 + 128, st * 512:(st + 1) * 512], in_=f), reads=[fk], writes=[], key=fk)
            else:
                fb = gb[fcount % 4]
                P.op("act", lambda e, f=f, bank=bank: e.activation(out=f, in_=ps[bank][:, :], func=AF.Exp, scale=-1.0), reads=[pk], writes=[fk])
                P.op("pool", lambda e, f=f: e.tensor_scalar(f, f, 1.0, None, op0=ALU.add), reads=[fk], writes=[fk])
                P.op("dve", lambda e, f=f: e.reciprocal(f, f), reads=[fk], writes=[fk])
                P.op("pool", lambda e, f=f, fb=fb: e.tensor_copy(fb, f), reads=[fk], writes=[fk])
                P.dma("sp", lambda e, fb=fb, dst=dst, r0=r0, st=st: e.dma_start(out=dst[r0:r0 + 128, st * 512:(st + 1) * 512], in_=fb), reads=[fk], writes=[], key=fk)
            fcount += 1
        for tt in range(4):
            t = st * 4 + tt
            tb = to[t % 2]; tk = "to%d" % (t % 2)
            for gi, (c0, n, o0) in enumerate(tmg):
                if gi == 1:
                    continue
                bank = 4 + fcount % 4
                pk = "ps%d" % bank
                subs = [(c0, n, 0)]
                if gi == 0:
                    subs = [(c0, n, 0), (tmg[1][0], 8, 128)]
                for (cc, nn, po) in subs:
                    for kc in range(8):
                        P.op("pe", lambda e, kc=kc, cc=cc, nn=nn, po=po, bank=bank, h=h, tt=tt: e.matmul(ps[bank][:, po:po + nn], lhsT=h[:, kc, tt * 128:(tt + 1) * 128], rhs=w[:, kc, cc:cc + nn],
                                                                                                  start=(kc == 0), stop=(kc == 7)),
                             reads=["w_in", hk], writes=[pk])
                tot = n + (8 if gi == 0 else 0)
                eng = "dve" if gi % 2 == 0 else "act"
                if eng == "dve":
                    P.op("dve", lambda e, tb=tb, o0=o0, tot=tot, bank=bank: e.tensor_copy(tb[:, o0:o0 + tot], ps[bank][:, 0:tot]), reads=[pk], writes=[tk])
                else:
                    P.op("act", lambda e, tb=tb, o0=o0, tot=tot, bank=bank: e.activation(out=tb[:, o0:o0 + tot], in_=ps[bank][:, 0:tot], func=AF.Copy), reads=[pk], writes=[tk])
                fcount += 1
            P.dma("sp", lambda e, tb=tb, t=t: e.dma_start(out=tm_out[t * 128:(t + 1) * 128, :], in_=tb), reads=[tk], writes=[], key=tk)
    P.barrier()
    A.release(m0)


ATTN_SCALE = 128 ** -0.5
NEG = -1e30
MASKV = -30000.0


def phaseB(k, l, I):
    A, P, ps = k.A, k.P, k.ps
    cq_fm = k.dr["cq_fm"]; ckv_fm = k.dr["ckv_fm"]; kidx_fm = k.dr["kidx_fm"]; tm_out = k.dr["tm_out"]
    qT_d = dram(k, "qT_d", [NT, 128, 8, 128], BF16)
    qidx_d = dram(k, "qidx_d", [NT, 128, 4, 128], BF16)
    k.kvT = A.alloc(S, BF16)
    k.kidxT = A.alloc(S, BF16)
    k.kvaug = A.alloc(NT * 132, BF16).rearrange("p (t c) -> p t c", t=NT)
    k.widx = A.alloc(NT * 8, F32).rearrange("p (t c) -> p t c", t=NT)
    k.wv = A.alloc(8 * 128, BF16).rearrange("p (h c) -> p h c", h=8)
    m0 = A.mark()
    wq = A.alloc(2 * 1024, BF16).rearrange("p (k c) -> p k c", k=2)
    wi = A.alloc(2 * 512, BF16).rearrange("p (k c) -> p k c", k=2)
    P.dma("pool", lambda e: e.dma_start(out=wq, in_=I["w_q_up"][l].rearrange("(k p) c -> p k c", p=128)), writes=["wq"], key="wq")
    P.dma("pool", lambda e: e.dma_start(out=wi, in_=I["w_idx_q"][l].rearrange("(k p) c -> p k c", p=128)), writes=["wi"], key="wi")
    P.op("dve", lambda e: e.memset(k.wv, 0.0), writes=["wv"])
    for par in range(2):
        P.dma("pool", lambda e, par=par: e.dma_start(out=k.wv.rearrange("p (j two) c -> p j two c", two=2)[:, :, par, par * 64:(par + 1) * 64],
                                                     in_=I["w_v_up"][l].rearrange("(j two) r v -> r j two v", two=2)[:, :, par, :]),
              writes=["wv"], key="wvd")
    ckt = A.alloc(NT * 128, F32).rearrange("p (t c) -> p t c", t=NT)
    sq = A.alloc(NT * 128, F32).rearrange("p (t c) -> p t c", t=NT)
    gb = A.alloc(128, F32)
    st = A.alloc(64, F32)
    P.dma("sp", lambda e: e.dma_start(out=ckt, in_=tm_out[:, 0:128].rearrange("(t p) c -> p t c", p=128)), writes=["ckt"], key="ckt")
    P.dma("sp", lambda e: e.dma_start(out=k.widx, in_=tm_out[:, 128:136].rearrange("(t p) c -> p t c", p=128)), writes=["widx"], key="widx")
    P.dma("sp", lambda e: e.dma_start(out=gb, in_=I["g_ckv"][l].partition_broadcast(128)), writes=["gb"], key="gb")
    P.op("dve", lambda e: e.tensor_tensor(sq, ckt, ckt, op=ALU.mult), reads=["ckt"], writes=["sq"])
    P.op("dve", lambda e: e.tensor_reduce(out=st[:, 0:32], in_=sq, axis=AX.X, op=ALU.add), reads=["sq"], writes=["stB"])
    P.op("dve", lambda e: e.tensor_scalar(st[:, 0:32], st[:, 0:32], 1.0 / 128, EPS, op0=ALU.mult, op1=ALU.add), reads=["stB"], writes=["stB"])
    P.op("act", lambda e: e.activation(out=st[:, 0:32], in_=st[:, 0:32], func=AF.Ln), reads=["stB"], writes=["stB"])
    P.op("act", lambda e: e.activation(out=st[:, 0:32], in_=st[:, 0:32], func=AF.Exp, scale=-0.5), reads=["stB"], writes=["stB"])
    P.op("dve", lambda e: e.tensor_tensor(sq, ckt, st[:, 0:32].unsqueeze(2).to_broadcast([128, NT, 128]), op=ALU.mult), reads=["ckt", "stB", "sq"], writes=["sq"])
    P.op("dve", lambda e: e.tensor_tensor(k.kvaug[:, :, 0:128], sq, gb.unsqueeze(1).to_broadcast([128, NT, 128]), op=ALU.mult), reads=["sq", "gb"], writes=["kvaug"])
    P.op("dve", lambda e: e.memset(k.kvaug[:, :, 128:132], 1.0), writes=["kvaug"])
    xin = [A.alloc(4 * 512, F32).rearrange("p (c t) -> p c t", c=4) for _ in range(2)]
    sqb = A.alloc(4 * 512, BF16).rearrange("p (c t) -> p c t", c=4)
    rs = A.alloc(3 * 512, F32).rearrange("p (c t) -> p c t", c=3)
    cqn = A.alloc(2 * 512, BF16).rearrange("p (c t) -> p c t", c=2)
    ob = [A.alloc(512, BF16) for _ in range(4)]
    oc = 0
    for b in range(S // 512):
        x = xin[b % 2]; xk = "xinB%d" % (b % 2)
        sl = slice(b * 512, (b + 1) * 512)
        P.dma("sp", lambda e, x=x, sl=sl: e.dma_start(out=x[:, 0:2, :], in_=cq_fm[:, sl].rearrange("(c p) t -> p c t", p=128)), writes=[xk], key=xk)
        P.dma("sp", lambda e, x=x, sl=sl: e.dma_start(out=x[:, 2, :], in_=ckv_fm[:, sl]), writes=[xk], key=xk)
        P.dma("sp", lambda e, x=x, sl=sl: e.dma_start(out=x[:, 3, :], in_=kidx_fm[:, sl]), writes=[xk], key=xk)
        P.op("act", lambda e, x=x: e.activation(out=sqb, in_=x, func=AF.Square), reads=[xk], writes=["sqb"])
        P.op("pe", lambda e: e.matmul(ps[0][:, :], lhsT=k.ones_b, rhs=sqb[:, 0, :], start=True, stop=False), reads=["sqb", "ones_b"], writes=["ps0"])
        P.op("pe", lambda e: e.matmul(ps[0][:, :], lhsT=k.ones_b, rhs=sqb[:, 1, :], start=False, stop=True), reads=["sqb", "ones_b"], writes=["ps0"])
        P.op("pe", lambda e: e.matmul(ps[1][:, :], lhsT=k.ones_b, rhs=sqb[:, 2, :], start=True, stop=True), reads=["sqb", "ones_b"], writes=["ps1"])
        P.op("pe", lambda e: e.matmul(ps[2][:, :], lhsT=k.ones_b, rhs=sqb[:, 3, :], start=True, stop=True), reads=["sqb", "ones_b"], writes=["ps2"])
        for i, n in enumerate([256.0, 128.0, 128.0]):
            P.op("dve", lambda e, i=i, n=n: e.tensor_scalar(rs[:, i, :], ps[i][:, :], 1.0 / n, EPS, op0=ALU.mult, op1=ALU.add), reads=["ps%d" % i], writes=["rs"])
        P.op("act", lambda e: e.activation(out=rs, in_=rs, func=AF.Ln), reads=["rs"], writes=["rs"])
        P.op("act", lambda e: e.activation(out=rs, in_=rs, func=AF.Exp, scale=-0.5), reads=["rs"], writes=["rs"])
        for c in range(2):
            P.op("dve", lambda e, c=c, x=x: e.scalar_tensor_tensor(out=cqn[:, c, :], in0=x[:, c, :], scalar=k.gcq[:, c:c + 1], in1=rs[:, 0, :], op0=ALU.mult, op1=ALU.mult),
                 reads=[xk, "rs", "par"], writes=["cqn"])
        P.op("dve", lambda e, x=x, sl=sl: e.scalar_tensor_tensor(out=k.kvT[:, sl], in0=x[:, 2, :], scalar=k.gckv[:, 0:1], in1=rs[:, 1, :], op0=ALU.mult, op1=ALU.mult),
             reads=[xk, "rs", "par"], writes=["kvT"])
        P.op("dve", lambda e, x=x, sl=sl: e.scalar_tensor_tensor(out=k.kidxT[:, sl], in0=x[:, 3, :], scalar=k.gkidx[:, 0:1], in1=rs[:, 2, :], op0=ALU.mult, op1=ALU.mult),
             reads=[xk, "rs", "par"], writes=["kidxT"])
        for h in range(8):
            bank = 4 + oc % 4; pk = "ps%d" % bank
            for c in range(2):
                P.op("pe", lambda e, h=h, c=c, bank=bank: e.matmul(ps[bank][:, :], lhsT=wq[:, c, h * 128:(h + 1) * 128], rhs=cqn[:, c, :], start=(c == 0), stop=(c == 1)),
                     reads=["wq", "cqn"], writes=[pk])
            o = ob[oc % 4]; ok = "obB%d" % (oc % 4)
            P.op("act", lambda e, o=o, bank=bank: e.activation(out=o, in_=ps[bank][:, :], func=AF.Copy, scale=ATTN_SCALE), reads=[pk], writes=[ok])
            P.dma("sp", lambda e, o=o, h=h, b=b: e.dma_start(out=qT_d[b * 4:(b + 1) * 4, :, h, :].rearrange("t r q -> r t q"), in_=o.rearrange("p (t q) -> p t q", t=4)),
                  reads=[ok], key=ok)
            oc += 1
        for j in range(4):
            bank = 4 + oc % 4; pk = "ps%d" % bank
            for c in range(2):
                P.op("pe", lambda e, j=j, c=c, bank=bank: e.matmul(ps[bank][:, :], lhsT=wi[:, c, j * 128:(j + 1) * 128], rhs=cqn[:, c, :], start=(c == 0), stop=(c == 1)),
                     reads=["wi", "cqn"], writes=[pk])
            o = ob[oc % 4]; ok = "obB%d" % (oc % 4)
            P.op("dve", lambda e, o=o, bank=bank: e.tensor_copy(o, ps[bank][:, :]), reads=[pk], writes=[ok])
            P.dma("sp", lambda e, o=o, j=j, b=b: e.dma_start(out=qidx_d[b * 4:(b + 1) * 4, :, j, :].rearrange("t r q -> r t q"), in_=o.rearrange("p (t q) -> p t q", t=4)),
                  reads=[ok], key=ok)
            oc += 1
    P.barrier()
    A.release(m0)


def phaseC(k, l, I, NITER=16):
    A, P, ps = k.A, k.P, k.ps
    qT_d = k.dr["qT_d"]; qidx_d = k.dr["qidx_d"]
    yaT_d = dram(k, "yaT_d", [4, 128, S], BF16)
    m0 = A.mark()
    qs = [A.alloc(8 * 128, BF16) for _ in range(2)]
    qi = [A.alloc(4 * 128, BF16).rearrange("p (j q) -> p j q", j=4) for _ in range(2)]
    score = [A.alloc(S, F32) for _ in range(2)]
    mb = [A.alloc(S, BF16) for _ in range(2)]
    junk = A.alloc(S, BF16)
    rb = [A.alloc(512, F32) for _ in range(3)]
    pT = [A.alloc(512, BF16) for _ in range(4)]
    on = A.alloc(8 * 128, BF16).rearrange("p (h r) -> p h r", h=8)
    onT = A.alloc(8 * 128, BF16).rearrange("p (h q) -> p h q", h=8)
    yo = [A.alloc(4 * 128, BF16).rearrange("p (j q) -> p j q", j=4) for _ in range(2)]
    ident4 = A.alloc(512, BF16)
    bs = [A.alloc(64, F32) for _ in range(2)]
    pw = A.alloc(NITER, F32)
    rden = A.alloc(8, F32)
    cm = A.alloc(2, F32)
    P.op("dve", lambda e: e.memset(cm, MASKV), writes=["cm"])
    for i in range(4):
        P.op("dve", lambda e, i=i: e.tensor_copy(ident4[:, i * 128:(i + 1) * 128], k.identb), reads=["identb"], writes=["ident4"])
    for i in range(NITER):
        P.op("dve", lambda e, i=i: e.memset(pw[:, i:i + 1], 0.5 ** (i + 1)), writes=["pw"])
    def oacc(h):
        return ps[h // 3][:, (h % 3) * 129:(h % 3) * 129 + 129]
    rc = [0]

    def indexer(qt):
        b = qt % 2
        sc = score[b]; sk = "score%d" % b
        nk = (qt + 1) * 128
        P.dma("sp", lambda e: e.dma_start(out=qs[b], in_=qT_d[qt].rearrange("r h q -> r (h q)")), writes=["qs%d" % b], key="qs%d" % b)
        P.dma("sp", lambda e: e.dma_start(out=qi[b], in_=qidx_d[qt]), writes=["qi%d" % b], key="qi%d" % b)
        for c0 in range(0, nk, 512):
            wd = min(512, nk - c0)
            for h in range(8):
                bank = 3 + rc[0] % 2; pk = "ps%d" % bank
                p0 = (h % 2) * 64
                P.op("pe", lambda e, h=h, p0=p0, bank=bank, c0=c0, wd=wd: e.matmul(ps[bank][:, 0:wd], lhsT=qi[b][p0:p0 + 64, h // 2, :], rhs=k.kidxT[p0:p0 + 64, c0:c0 + wd], start=True, stop=True),
                     reads=["qi%d" % b, "kidxT"], writes=[pk])
                r = rb[rc[0] % 3]; rk = "rb%d" % (rc[0] % 3)
                P.op("act", lambda e, r=r, bank=bank, wd=wd: e.activation(out=r[:, 0:wd], in_=ps[bank][:, 0:wd], func=AF.Relu), reads=[pk], writes=[rk])
                if h == 0:
                    P.op("dve", lambda e, r=r, c0=c0, wd=wd, h=h: e.tensor_scalar(sc[:, c0:c0 + wd], r[:, 0:wd], k.widx[:, qt, h:h + 1], None, op0=ALU.mult),
                         reads=[rk, "widx"], writes=[sk])
                else:
                    P.op("dve", lambda e, r=r, c0=c0, wd=wd, h=h: e.scalar_tensor_tensor(out=sc[:, c0:c0 + wd], in0=r[:, 0:wd], scalar=k.widx[:, qt, h:h + 1], in1=sc[:, c0:c0 + wd], op0=ALU.mult, op1=ALU.add),
                         reads=[rk, "widx", sk], writes=[sk])
                rc[0] += 1
        P.op("pool", lambda e: e.tensor_tensor(sc[:, qt * 128:(qt + 1) * 128], sc[:, qt * 128:(qt + 1) * 128], k.cneg, op=ALU.add), reads=[sk, "cneg"], writes=[sk])
        s = bs[b]; bk = "bs%d" % b
        lo = s[:, 0:1]; w0 = s[:, 1:2]; mid = s[:, 2:3]; cnt = s[:, 3:4]; tmp = s[:, 4:5]; wi_ = s[:, 8:8 + NITER]
        if qt < 2:
            P.op("dve", lambda e: e.memset(lo, -1e29), writes=[bk])
        else:
            P.op("dve", lambda e: e.tensor_reduce(out=w0, in_=sc[:, 0:nk], axis=AX.X, op=ALU.max), reads=[sk], writes=[bk])
            P.op("dve", lambda e: e.tensor_reduce(out=lo, in_=sc[:, 0:qt * 128], axis=AX.X, op=ALU.min), reads=[sk], writes=[bk])
            P.op("dve", lambda e: e.tensor_scalar(lo, lo, -1.0, None, op0=ALU.add), reads=[bk], writes=[bk])
            P.op("dve", lambda e: e.tensor_tensor(w0, w0, lo, op=ALU.subtract), reads=[bk], writes=[bk])
            P.op("dve", lambda e: e.tensor_scalar(wi_, pw, w0, None, op0=ALU.mult), reads=[bk, "pw"], writes=[bk])
            for it in range(NITER):
                P.op("dve", lambda e, it=it: e.tensor_tensor(mid, lo, wi_[:, it:it + 1], op=ALU.add), reads=[bk], writes=[bk])
                P.op("dve", lambda e: e.tensor_scalar(junk[:, 0:nk], sc[:, 0:nk], mid, None, op0=ALU.is_gt, op1=ALU.add, accum_out=cnt), reads=[sk, bk], writes=[bk, "junk"])
                P.op("dve", lambda e, it=it: e.scalar_tensor_tensor(out=tmp, in0=cnt, scalar=256.0, in1=wi_[:, it:it + 1], op0=ALU.is_ge, op1=ALU.mult), reads=[bk], writes=[bk])
                P.op("dve", lambda e: e.tensor_tensor(lo, lo, tmp, op=ALU.add), reads=[bk], writes=[bk])
        P.op("dve", lambda e: e.tensor_scalar(mb[b][:, 0:nk], sc[:, 0:nk], lo, cm[:, 0:1], op0=ALU.is_le, op1=ALU.mult), reads=[sk, bk, "cm"], writes=["mb%d" % b])

    pc = [0]

    def attention(qt):
        b = qt % 2
        q = qs[b]
        for kb in range(qt + 1):
            for hg in range(2):
                bank = 5 + pc[0] % 2; pk = "ps%d" % bank
                P.op("pe", lambda e, kb=kb, hg=hg, bank=bank: e.matmul(ps[bank][:, :], lhsT=k.kvT[:, kb * 128:(kb + 1) * 128], rhs=q[:, hg * 512:(hg + 1) * 512], start=True, stop=False),
                     reads=["kvT", "qs%d" % b], writes=[pk])
                P.op("pe", lambda e, kb=kb, bank=bank: e.matmul(ps[bank][:, :], lhsT=mb[b][:, kb * 128:(kb + 1) * 128], rhs=ident4, start=False, stop=True),
                     reads=["mb%d" % b, "ident4"], writes=[pk])
                p = pT[pc[0] % 4]; pk2 = "pT%d" % (pc[0] % 4)
                P.op("act", lambda e, p=p, bank=bank: e.activation(out=p, in_=ps[bank][:, :], func=AF.Exp), reads=[pk], writes=[pk2])
                for hh in range(4):
                    h = hg * 4 + hh
                    P.op("pe", lambda e, p=p, hh=hh, h=h, kb=kb: e.matmul(oacc(h), lhsT=p[:, hh * 128:(hh + 1) * 128], rhs=k.kvaug[:, kb, 0:129], start=(kb == 0 and h % 3 == 0), stop=(kb == qt), skip_group_check=True),
                         reads=[pk2, "kvaug"], writes=["ps%d" % (h // 3)])
                pc[0] += 1
        for bnk in range(3):
            nh = 3 if bnk < 2 else 2
            v = ps[bnk][:, 0:nh * 129].rearrange("p (h c) -> p h c", c=129)
            P.op("dve", lambda e, v=v, bnk=bnk, nh=nh: e.reciprocal(rden[:, bnk * 3:bnk * 3 + nh], v[:, :, 128]), reads=["ps%d" % bnk], writes=["rden"])
            P.op("dve", lambda e, v=v, bnk=bnk, nh=nh: e.tensor_tensor(on[:, bnk * 3:bnk * 3 + nh, :], v[:, :, 0:128], rden[:, bnk * 3:bnk * 3 + nh].unsqueeze(2).to_broadcast([128, nh, 128]), op=ALU.mult),
                 reads=["ps%d" % bnk, "rden"], writes=["on"])
        if qt == 1 and "dbg_on" in k.debug:
            dbg = dram(k, "dbg_on", [128, 1024], BF16)
            P.dma("sp", lambda e: e.dma_start(out=dbg, in_=on.rearrange("p h r -> p (h r)")), reads=["on"], key="dbg1")
            dbg2 = dram(k, "dbg_mb", [128, 256], BF16)
            P.dma("sp", lambda e: e.dma_start(out=dbg2, in_=mb[b][:, 0:256]), reads=["mb%d" % b], key="dbg2")
            dbg3 = dram(k, "dbg_sc", [128, 256], F32)
            P.dma("sp", lambda e: e.dma_start(out=dbg3, in_=score[b][:, 0:256]), reads=["score%d" % b], key="dbg3")
            dbg4 = dram(k, "dbg_kv", [128, 32 * 132], BF16)
            P.dma("sp", lambda e: e.dma_start(out=dbg4, in_=k.kvaug.rearrange("p t c -> p (t c)")), reads=["kvaug"], key="dbg4")
            dbg5 = dram(k, "dbg_qs", [128, 1024], BF16)
            P.dma("sp", lambda e: e.dma_start(out=dbg5, in_=q), reads=["qs%d" % b], key="dbg5")
            dbg6 = dram(k, "dbg_kvT", [128, S], BF16)
            P.dma("sp", lambda e: e.dma_start(out=dbg6, in_=k.kvT), reads=["kvT"], key="dbg6")
        tb = ps[7][:, :].bitcast(BF16)
        for h in range(8):
            P.op("pe", lambda e, h=h: e.transpose(tb[:, h * 128:(h + 1) * 128], on[:, h, :], k.identb), reads=["on", "identb"], writes=["ps7"])
        P.op("act", lambda e: e.activation(out=onT.rearrange("p h q -> p (h q)"), in_=tb, func=AF.Copy), reads=["ps7"], writes=["onT"])
        for j in range(4):
            for two in range(2):
                h = j * 2 + two
                P.op("pe", lambda e, j=j, two=two, h=h: e.matmul(ps[7][:, j * 128:(j + 1) * 128], lhsT=k.wv[:, h, :], rhs=onT[:, h, :], start=(two == 0), stop=(two == 1)),
                     reads=["wv", "onT"], writes=["ps7"])
        y = yo[qt % 2]; yk = "yo%d" % (qt % 2)
        P.op("dve", lambda e: e.tensor_copy(y.rearrange("p j q -> p (j q)"), ps[7][:, :]), reads=["ps7"], writes=[yk])
        P.dma("sp", lambda e: e.dma_start(out=yaT_d[:, :, qt * 128:(qt + 1) * 128].rearrange("j p q -> p j q"), in_=y), reads=[yk], key=yk)

    indexer(0)
    for qt in range(NT):
        if qt + 1 < NT:
            indexer(qt + 1)
        attention(qt)
    P.barrier()
    A.release(m0)


def phaseD(k, l, I):
    A, P, ps = k.A, k.P, k.ps
    qrec_fm = k.dr["qrec_fm"]; frec_fm = k.dr["frec_fm"]; tm_out = k.dr["tm_out"]
    yrT_d = dram(k, "yrT_d", [4, 128, S], BF16)
    m0 = A.mark()
    NB = 512
    def fm(dt=F32):
        return A.alloc(4 * NB, dt).rearrange("p (j t) -> p j t", j=4)
    z = fm(); qr = fm(); e = fm(); t1 = fm(); t2 = fm(); Acum = fm(); kk = fm(); qq = fm()
    qt_ = fm(BF16); kt_ = fm(BF16); qhA = fm(BF16); qhB = fm(BF16); kh = fm(BF16)
    cmf = k.cmf
    grb = A.alloc(512, F32)
    vt = A.alloc(4 * 512, BF16).rearrange("p (t c) -> p t c", t=4)
    ogt = A.alloc(4 * 512, F32).rearrange("p (t c) -> p t c", t=4)
    vtf = A.alloc(4 * 512, F32).rearrange("p (t c) -> p t c", t=4)
    khT = A.alloc(512, BF16)
    Pm = A.alloc(8 * 128, BF16).rearrange("p (h t) -> p h t", h=8)
    state = A.alloc(4 * 64, F32).rearrange("p (j v) -> p j v", j=4)
    stmp = A.alloc(4 * 64, F32).rearrange("p (j v) -> p j v", j=4)
    sbf = [A.alloc(4 * 64, BF16).rearrange("p (j v) -> p j v", j=4) for _ in range(4)]
    decay = A.alloc(32, F32)
    osb = A.alloc(512, F32); osq = A.alloc(512, F32); oss = A.alloc(16, F32)
    sg = A.alloc(512, F32)
    yb = A.alloc(512, BF16)
    yT = [A.alloc(512, BF16) for _ in range(2)]
    P.dma("sp", lambda e_: e_.dma_start(out=osb[:, 0:64], in_=I["g_rec"][l].partition_broadcast(128)), writes=["osb"], key="grb")
    P.op("dve", lambda e_: e_.tensor_copy(grb.rearrange("p (h v) -> p h v", h=8), osb[:, 0:64].unsqueeze(1).to_broadcast([128, 8, 64])), reads=["osb"], writes=["grb"])
    P.op("dve", lambda e_: e_.memset(state, 0.0), writes=["state"])
    P.op("dve", lambda e_: e_.memset(sbf[0], 0.0), writes=["sbf0"])
    P.op("dve", lambda e_: e_.memset(qhA, 0.0), writes=["qhA"])
    P.op("dve", lambda e_: e_.memset(qhB, 0.0), writes=["qhB"])
    sv = [0]
    z2 = z.rearrange("p j t -> p (j t)"); e2 = e.rearrange("p j t -> p (j t)"); t12 = t1.rearrange("p j t -> p (j t)"); t22 = t2.rearrange("p j t -> p (j t)")
    A2 = Acum.rearrange("p j t -> p (j t)")
    def ch(x):
        return x.rearrange("p j (c t) -> p (j c) t", t=64)
    for b in range(S // NB):
        sl = slice(b * NB, (b + 1) * NB)
        P.dma("sp", lambda e_, sl=sl: e_.dma_start(out=z, in_=frec_fm[:, sl].rearrange("(j p) t -> p j t", p=128)), writes=["z"], key="zD")
        P.dma("sp", lambda e_, sl=sl: e_.dma_start(out=qr, in_=qrec_fm[:, sl].rearrange("(j p) t -> p j t", p=128)), writes=["qr"], key="qrD")
        P.dma("sp", lambda e_, sl=sl: e_.dma_start(out=vtf, in_=tm_out[sl, 136:648].rearrange("(t p) c -> p t c", p=128)), writes=["vtf"], key="vtD")
        P.op("pool", lambda e_: e_.tensor_copy(vt, vtf), reads=["vtf"], writes=["vt"])
        P.dma("sp", lambda e_, sl=sl: e_.dma_start(out=ogt, in_=tm_out[sl, 648:1160].rearrange("(t p) c -> p t c", p=128)), writes=["ogt"], key="ogD")
        P.op("act", lambda e_: e_.activation(out=e, in_=z, func=AF.Exp, scale=-1.0), reads=["z"], writes=["e"])
        for j in range(4):
            P.op("dve", lambda e_, j=j: e_.tensor_scalar(t1[:, j, :], e[:, j, :], k.lb[:, j:j + 1], 1.0, op0=ALU.mult, op1=ALU.add), reads=["e", "sm"], writes=["t1"])
        P.op("dve", lambda e_: e_.tensor_scalar(t2, e, 1.0, None, op0=ALU.add), reads=["e"], writes=["t2"])
        P.op("act", lambda e_: e_.activation(out=t1, in_=t1, func=AF.Ln), reads=["t1"], writes=["t1"])
        P.op("act", lambda e_: e_.activation(out=z, in_=t2, func=AF.Ln), reads=["t2", "z"], writes=["z"])
        P.op("dve", lambda e_: e_.tensor_tensor(t1, t1, z, op=ALU.subtract), reads=["t1", "z"], writes=["t1"])
        P.op("dve", lambda e_: e_.reciprocal(t2, t2), reads=["t2"], writes=["t2"])
        for j in range(4):
            P.op("dve", lambda e_, j=j: e_.scalar_tensor_tensor(out=kk[:, j, :], in0=e[:, j, :], scalar=k.oml[:, j:j + 1], in1=t2[:, j, :], op0=ALU.mult, op1=ALU.mult), reads=["e", "t2", "sm"], writes=["kk"])
        srcs = [t1, Acum]
        for si, sh in enumerate([1, 2, 4, 8, 16, 32]):
            a_ = ch(srcs[si % 2]); b_ = ch(srcs[(si + 1) % 2])
            P.op("dve", lambda e_, a_=a_, b_=b_, sh=sh: e_.tensor_tensor(b_[:, :, sh:64], a_[:, :, sh:64], a_[:, :, 0:64 - sh], op=ALU.add), reads=["t1", "Acum"], writes=["t1", "Acum"])
            P.op("pool", lambda e_, a_=a_, b_=b_, sh=sh: e_.tensor_copy(b_[:, :, 0:sh], a_[:, :, 0:sh]), reads=["t1", "Acum"], writes=["t1", "Acum"])
        P.op("pool", lambda e_: e_.tensor_copy(Acum, t1), reads=["t1", "Acum"], writes=["t1", "Acum"])
        P.op("act", lambda e_: e_.activation(out=e, in_=qr, func=AF.Exp, scale=-1.0), reads=["qr", "kk"], writes=["e"])
        P.op("dve", lambda e_: e_.tensor_scalar(e, e, 1.0, None, op0=ALU.add), reads=["e"], writes=["e"])
        P.op("dve", lambda e_: e_.reciprocal(e, e), reads=["e"], writes=["e"])
        P.op("dve", lambda e_: e_.tensor_tensor(qq, qr, e, op=ALU.mult), reads=["e", "qr"], writes=["qq"])
        Ac = ch(Acum)
        P.op("dve", lambda e_: e_.tensor_tensor(ch(t1), Ac, Ac[:, :, 31:32].to_broadcast([128, 32, 64]), op=ALU.subtract), reads=["Acum", "t1"], writes=["t1"])
        P.op("dve", lambda e_: e_.tensor_scalar(t1, t1, -40.0, 40.0, op0=ALU.max, op1=ALU.min), reads=["t1"], writes=["t1"])
        P.op("act", lambda e_: e_.activation(out=t2, in_=t1, func=AF.Exp), reads=["t1"], writes=["t2"])
        P.op("dve", lambda e_: e_.tensor_tensor(qt_, qq, t2, op=ALU.mult), reads=["qq", "t2"], writes=["qt_"])
        P.op("act", lambda e_: e_.activation(out=t2, in_=t1, func=AF.Exp, scale=-1.0), reads=["t1", "qt_"], writes=["t2"])
        P.op("dve", lambda e_: e_.tensor_tensor(kt_, kk, t2, op=ALU.mult), reads=["kk", "t2"], writes=["kt_"])
        P.op("act", lambda e_: e_.activation(out=t2, in_=Acum, func=AF.Exp), reads=["Acum", "kt_"], writes=["t2"])
        def eo(x, par):
            return x.rearrange("p j (c two t) -> p j c two t", two=2, t=64)[:, :, :, par, :]
        P.op("dve", lambda e_: e_.tensor_tensor(eo(qhA, 0), eo(qq, 0), eo(t2, 0), op=ALU.mult), reads=["qq", "t2"], writes=["qhA"])
        P.op("dve", lambda e_: e_.tensor_tensor(eo(qhB, 1), eo(qq, 1), eo(t2, 1), op=ALU.mult), reads=["qq", "t2"], writes=["qhB"])
        P.op("act", lambda e_: e_.activation(out=decay, in_=Ac[:, :, 63], func=AF.Exp), reads=["Acum"], writes=["decay"])
        P.op("dve", lambda e_: e_.tensor_tensor(ch(t1), Ac[:, :, 63:64].to_broadcast([128, 32, 64]), Ac, op=ALU.subtract), reads=["Acum", "t1"], writes=["t1"])
        P.op("act", lambda e_: e_.activation(out=t2, in_=t1, func=AF.Exp), reads=["t1", "qhA", "qhB"], writes=["t2"])
        P.op("dve", lambda e_: e_.tensor_tensor(kh, kk, t2, op=ALU.mult), reads=["kk", "t2"], writes=["kh"])
        for tt in range(4):
            tsl = slice(tt * 128, (tt + 1) * 128)
            tb = ps[7][:, :].bitcast(BF16)
            for j in range(4):
                P.op("pe", lambda e_, j=j, tsl=tsl: e_.transpose(tb[:, j * 128:(j + 1) * 128], kh[:, j, tsl], k.identb), reads=["kh", "identb"], writes=["ps7"])
            P.op("act", lambda e_: e_.activation(out=khT, in_=tb[:, 0:512], func=AF.Copy), reads=["ps7"], writes=["khT"])
            for h in range(8):
                j = h // 2; p0 = (h % 2) * 64
                bank = 5 + h % 2
                P.op("pe", lambda e_, h=h, j=j, p0=p0, bank=bank, tsl=tsl: e_.matmul(ps[bank][:, j * 128:(j + 1) * 128], lhsT=kt_[p0:p0 + 64, j, tsl], rhs=qt_[p0:p0 + 64, j, tsl], start=True, stop=True),
                     reads=["kt_", "qt_"], writes=["ps%d" % bank])
            for g in range(2):
                P.op("dve", lambda e_, g=g: e_.tensor_tensor(Pm[:, g * 4:(g + 1) * 4, :], ps[5 + g][:, :].rearrange("p (h t) -> p h t", h=4), cmf.unsqueeze(1).to_broadcast([128, 4, 128]), op=ALU.mult),
                     reads=["ps%d" % (5 + g), "cmf"], writes=["Pm"])
            svA = sv[0]
            for half in range(2):
                c = tt * 2 + half
                hs = slice(half * 64, (half + 1) * 64)
                for j in range(4):
                    P.op("pe", lambda e_, j=j, hs=hs, tt=tt: e_.matmul(ps[4][:, j * 128:(j + 1) * 128], lhsT=khT[hs, j * 128:(j + 1) * 128], rhs=vt[hs, tt, j * 128:(j + 1) * 128], start=True, stop=True),
                         reads=["khT", "vt"], writes=["ps4"])
                dcol = [jj * 8 + c for jj in range(4)]
                dv = decay.rearrange("p (j c) -> p j c", j=4)[:, :, c:c + 1]
                P.op("dve", lambda e_, dv=dv: e_.tensor_tensor(stmp, state, dv.to_broadcast([128, 4, 64]), op=ALU.mult), reads=["state", "decay"], writes=["stmp"])
                pv = ps[4][:, :].rearrange("p (j x) -> p j x", j=4)
                P.op("dve", lambda e_, pv=pv: e_.tensor_tensor(state[0:64], stmp[0:64], pv[0:64, :, 0:64], op=ALU.add), reads=["stmp", "ps4"], writes=["state"])
                P.op("dve", lambda e_, pv=pv: e_.tensor_tensor(state[64:128], stmp[64:128], pv[64:128, :, 64:128], op=ALU.add), reads=["stmp", "ps4"], writes=["state"])
                sv[0] += 1
                sb_ = sbf[sv[0] % 4]
                P.op("act", lambda e_, sb_=sb_: e_.activation(out=sb_, in_=state, func=AF.Copy), reads=["state"], writes=["sbf%d" % (sv[0] % 4)])
            s0 = sbf[svA % 4]; s0k = "sbf%d" % (svA % 4)
            s1 = sbf[(svA + 1) % 4]; s1k = "sbf%d" % ((svA + 1) % 4)
            for h in range(8):
                j = h // 2; p0 = (h % 2) * 64
                oo = ps[3][:, h * 64:(h + 1) * 64]
                P.op("pe", lambda e_, h=h, oo=oo, tt=tt: e_.matmul(oo, lhsT=Pm[:, (h % 2) * 4 + h // 2, :], rhs=vt[:, tt, h * 64:(h + 1) * 64], start=True, stop=False), reads=["Pm", "vt"], writes=["ps3"])
                P.op("pe", lambda e_, j=j, p0=p0, oo=oo, tsl=tsl, s0=s0: e_.matmul(oo, lhsT=qhA[p0:p0 + 64, j, tsl], rhs=s0[p0:p0 + 64, j, :], start=False, stop=False), reads=["qhA", s0k], writes=["ps3"])
                P.op("pe", lambda e_, j=j, p0=p0, oo=oo, tsl=tsl, s1=s1: e_.matmul(oo, lhsT=qhB[p0:p0 + 64, j, tsl], rhs=s1[p0:p0 + 64, j, :], start=False, stop=True), reads=["qhB", s1k], writes=["ps3"])
            P.op("act", lambda e_: e_.activation(out=osb, in_=ps[3][:, :], func=AF.Copy), reads=["ps3"], writes=["osb"])
            P.op("dve", lambda e_: e_.tensor_tensor(osq, osb, osb, op=ALU.mult), reads=["osb"], writes=["osq"])
            P.op("dve", lambda e_: e_.tensor_reduce(out=oss[:, 0:8], in_=osq.rearrange("p (h v) -> p h v", h=8), axis=AX.X, op=ALU.add), reads=["osq"], writes=["oss"])
            P.op("dve", lambda e_: e_.tensor_scalar(oss[:, 0:8], oss[:, 0:8], 1.0 / 64, EPS, op0=ALU.mult, op1=ALU.add), reads=["oss"], writes=["oss"])
            P.op("act", lambda e_: e_.activation(out=oss[:, 0:8], in_=oss[:, 0:8], func=AF.Ln), reads=["oss"], writes=["oss"])
            P.op("act", lambda e_: e_.activation(out=oss[:, 0:8], in_=oss[:, 0:8], func=AF.Exp, scale=-0.5), reads=["oss"], writes=["oss"])
            P.op("dve", lambda e_: e_.tensor_tensor(osq.rearrange("p (h v) -> p h v", h=8), osb.rearrange("p (h v) -> p h v", h=8), oss[:, 0:8].unsqueeze(2).to_broadcast([128, 8, 64]), op=ALU.mult),
                 reads=["osb", "oss", "osq"], writes=["osq"])
            P.op("dve", lambda e_: e_.tensor_tensor(osq, osq, grb, op=ALU.mult), reads=["osq", "grb"], writes=["osq"])
            P.op("act", lambda e_, tt=tt: e_.activation(out=sg, in_=ogt[:, tt, :], func=AF.Exp, scale=-1.0), reads=["ogt"], writes=["sg"])
            P.op("dve", lambda e_: e_.tensor_scalar(sg, sg, 1.0, None, op0=ALU.add), reads=["sg"], writes=["sg"])
            P.op("dve", lambda e_: e_.reciprocal(sg, sg), reads=["sg"], writes=["sg"])
            P.op("dve", lambda e_, tt=tt: e_.tensor_tensor(sg, sg, ogt[:, tt, :], op=ALU.mult), reads=["sg", "ogt"], writes=["sg"])
            P.op("dve", lambda e_: e_.tensor_tensor(yb, osq, sg, op=ALU.mult), reads=["osq", "sg"], writes=["yb"])
            for j in range(4):
                P.op("pe", lambda e_, j=j: e_.transpose(tb[:, 512 + j * 128:512 + (j + 1) * 128], yb[:, j * 128:(j + 1) * 128], k.identb), reads=["yb", "identb"], writes=["ps7"])
            t = b * 4 + tt
            y = yT[t % 2]; yk = "yTD%d" % (t % 2)
            P.op("act", lambda e_, y=y: e_.activation(out=y, in_=tb[:, 512:1024], func=AF.Copy), reads=["ps7"], writes=[yk])
            P.dma("sp", lambda e_, y=y, t=t: e_.dma_start(out=yrT_d[:, :, t * 128:(t + 1) * 128].rearrange("j p q -> p j q"), in_=y.rearrange("p (j q) -> p j q", j=4)), reads=[yk], key=yk)
    P.barrier()
    A.release(m0)


def phaseE(k, l, I, xsrc, xdst):
    A, P, ps = k.A, k.P, k.ps
    yaT_d = k.dr["yaT_d"]; yrT_d = k.dr["yrT_d"]; gates_fm = k.dr["gates_fm"]
    m0 = A.mark()
    wa = A.alloc(4 * 1024, BF16).rearrange("p (k c) -> p k c", k=4)
    wr = A.alloc(4 * 1024, BF16).rearrange("p (k c) -> p k c", k=4)
    wo = A.alloc(8 * 1024, BF16).rearrange("p (k c) -> p k c", k=8)
    P.dma("pool", lambda e: e.dma_start(out=wa, in_=I["w_branch_a"][l].rearrange("(k p) c -> p k c", p=128)), writes=["wa"], key="wa")
    P.dma("pool", lambda e: e.dma_start(out=wr, in_=I["w_branch_r"][l].rearrange("(k p) c -> p k c", p=128)), writes=["wr"], key="wr")
    P.dma("pool", lambda e: e.dma_start(out=wo, in_=I["w_out"][l].rearrange("(k p) c -> p k c", p=128)), writes=["wo"], key="wo")
    ya = [A.alloc(4 * 512, BF16).rearrange("p (k t) -> p k t", k=4) for _ in range(2)]
    yr = [A.alloc(4 * 512, BF16).rearrange("p (k t) -> p k t", k=4) for _ in range(2)]
    gt = [A.alloc(16 * 512, BF16).rearrange("p (k t) -> p k t", k=16) for _ in range(2)]
    mT = A.alloc(8 * 512, BF16).rearrange("p (k t) -> p k t", k=8)
    m1 = [A.alloc(512, F32) for _ in range(2)]
    m2 = [A.alloc(512, F32) for _ in range(2)]
    xb = [A.alloc(1024, F32) for _ in range(2)]
    tb_ = [A.alloc(1024, F32) for _ in range(2)]
    cnt = 0
    for b in range(S // 512):
        sl = slice(b * 512, (b + 1) * 512)
        i2 = b % 2
        P.dma("sp", lambda e, i2=i2, sl=sl: e.dma_start(out=ya[i2], in_=yaT_d[:, :, sl].rearrange("j p t -> p j t")), writes=["yaE%d" % i2], key="yaE%d" % i2)
        P.dma("sp", lambda e, i2=i2, sl=sl: e.dma_start(out=yr[i2], in_=yrT_d[:, :, sl].rearrange("j p t -> p j t")), writes=["yrE%d" % i2], key="yrE%d" % i2)
        P.dma("sp", lambda e, i2=i2, sl=sl: e.dma_start(out=gt[i2], in_=gates_fm[:, sl].rearrange("(j p) t -> p j t", p=128)), writes=["gtE%d" % i2], key="gtE%d" % i2)
        for cb in range(8):
            ba = cnt % 2; bb = 2 + cnt % 2
            for kc in range(4):
                P.op("pe", lambda e, kc=kc, cb=cb, ba=ba, i2=i2: e.matmul(ps[ba][:, :], lhsT=wa[:, kc, cb * 128:(cb + 1) * 128], rhs=ya[i2][:, kc, :], start=(kc == 0), stop=(kc == 3)),
                     reads=["wa", "yaE%d" % i2], writes=["ps%d" % ba])
            for kc in range(4):
                P.op("pe", lambda e, kc=kc, cb=cb, bb=bb, i2=i2: e.matmul(ps[bb][:, :], lhsT=wr[:, kc, cb * 128:(cb + 1) * 128], rhs=yr[i2][:, kc, :], start=(kc == 0), stop=(kc == 3)),
                     reads=["wr", "yrE%d" % i2], writes=["ps%d" % bb])
            a1 = m1[cnt % 2]; a2 = m2[cnt % 2]
            P.op("dve", lambda e, a1=a1, ba=ba, cb=cb, i2=i2: e.tensor_tensor(a1, ps[ba][:, :], gt[i2][:, cb, :], op=ALU.mult), reads=["ps%d" % ba, "gtE%d" % i2], writes=["m1%d" % (cnt % 2)])
            P.op("dve", lambda e, a2=a2, bb=bb, cb=cb, i2=i2: e.tensor_tensor(a2, ps[bb][:, :], gt[i2][:, 8 + cb, :], op=ALU.mult), reads=["ps%d" % bb, "gtE%d" % i2], writes=["m2%d" % (cnt % 2)])
            P.op("pool", lambda e, a1=a1, a2=a2, cb=cb: e.tensor_tensor(mT[:, cb, :], a1, a2, op=ALU.add), reads=["m1%d" % (cnt % 2), "m2%d" % (cnt % 2)], writes=["mT"])
            cnt += 1
        for tt in range(4):
            t = b * 4 + tt
            x = xb[t % 2]; xk = "xbE%d" % (t % 2)
            tq = tb_[t % 2]; tk = "tbE%d" % (t % 2)
            P.dma("sp", lambda e, x=x, t=t: e.dma_start(out=x, in_=xsrc[t * 128:(t + 1) * 128, :]), writes=[xk], key=xk)
            for half in range(2):
                bank = 4 + (t * 2 + half) % 4
                for kc in range(8):
                    P.op("pe", lambda e, kc=kc, half=half, bank=bank, tt=tt: e.matmul(ps[bank][:, :], lhsT=mT[:, kc, tt * 128:(tt + 1) * 128], rhs=wo[:, kc, half * 512:(half + 1) * 512], start=(kc == 0), stop=(kc == 7)),
                         reads=["mT", "wo"], writes=["ps%d" % bank])
                P.op("dve", lambda e, tq=tq, half=half, bank=bank: e.tensor_tensor(tq[:, half * 512:(half + 1) * 512], ps[bank][:, :], k.gtbc[0][:, half * 512:(half + 1) * 512], op=ALU.mult),
                     reads=["ps%d" % bank, "gtbc0"], writes=[tk])
            P.op("pool", lambda e, tq=tq, x=x: e.tensor_tensor(tq, tq, x, op=ALU.add), reads=[tk, xk], writes=[tk])
            P.dma("sp", lambda e, tq=tq, t=t: e.dma_start(out=xdst[t * 128:(t + 1) * 128, :], in_=tq), reads=[tk], key=tk)
    P.barrier()
    A.release(m0)


def phaseF(k, l, I, xsrc, xdst):
    A, P, ps = k.A, k.P, k.ps
    m0 = A.mark()
    h2T = A.alloc(8 * S, BF16).rearrange("p (k t) -> p k t", k=8)
    gate = A.alloc(NT * 32, F32).rearrange("p (t c) -> p t c", t=NT)
    k.xb = [A.alloc(1024, F32) for _ in range(2)]
    m1_ = A.mark()
    hf = [A.alloc(8 * 128, F32).rearrange("p (k t) -> p k t", k=8) for _ in range(2)]
    wrt = A.alloc(8 * 36, F32).rearrange("p (k c) -> p k c", k=8)
    rb = A.alloc(36, F32)
    lg = A.alloc(NT * 36, F32).rearrange("p (t c) -> p t c", t=NT)
    k.xn = [A.alloc(1024, F32) for _ in range(2)]
    k.nst = [A.alloc(2, F32) for _ in range(2)]
    P.dma("sp", lambda e: e.dma_start(out=wrt[:, :, 0:4], in_=I["w_grp"][l].rearrange("(k p) c -> p k c", p=128)), writes=["wrt"], key="wrt")
    P.dma("sp", lambda e: e.dma_start(out=wrt[:, :, 4:36], in_=I["w_exp_router"][l].rearrange("(k p) c -> p k c", p=128)), writes=["wrt"], key="wrt")
    P.dma("sp", lambda e: e.dma_start(out=rb[:, 0:4], in_=I["b_grp"][l].partition_broadcast(128)), writes=["rbF"], key="rbF")
    P.dma("sp", lambda e: e.dma_start(out=rb[:, 4:36], in_=I["b_exp_router"][l].partition_broadcast(128)), writes=["rbF"], key="rbF")
    for t in range(NT):
        b0 = norm_transpose(k, xsrc, t, None, None, None, None, None)
        f = hf[t % 2]; fk = "hfF%d" % (t % 2)
        for kc in range(8):
            bank = b0 + kc // 4
            P.op("act", lambda e, kc=kc, bank=bank, f=f: e.activation(out=f[:, kc, :], in_=ps[bank][:, (kc % 4) * 128:(kc % 4 + 1) * 128], func=AF.Identity, scale=k.gs2[:, kc:kc + 1], bias=k.sh2[:, kc:kc + 1]),
                 reads=["ps%d" % bank, "gs", "modf"], writes=[fk])
        P.op("dve", lambda e, f=f, t=t: e.tensor_copy(h2T[:, :, t * 128:(t + 1) * 128], f), reads=[fk], writes=["h2T"])
        bank = 4 + t % 2
        for kc in range(8):
            P.op("pe", lambda e, kc=kc, f=f, bank=bank: e.matmul(ps[bank][:, 0:36], lhsT=f[:, kc, :], rhs=wrt[:, kc, :], start=(kc == 0), stop=(kc == 7)), reads=[fk, "wrt"], writes=["ps%d" % bank])
        P.op("dve", lambda e, t=t, bank=bank: e.tensor_tensor(lg[:, t, :], ps[bank][:, 0:36], rb, op=ALU.add), reads=["ps%d" % bank, "rbF"], writes=["lg"])
    g4 = A.alloc(NT * 4, F32).rearrange("p (t c) -> p t c", t=NT)
    oh = A.alloc(NT * 4, F32).rearrange("p (t c) -> p t c", t=NT)
    s1 = A.alloc(NT * 8, F32)
    le = A.alloc(NT * 32, F32).rearrange("p (t c) -> p t c", t=NT)
    o1 = A.alloc(NT * 32, F32).rearrange("p (t c) -> p t c", t=NT)
    o2 = A.alloc(NT * 32, F32).rearrange("p (t c) -> p t c", t=NT)
    mx = s1[:, 0:NT]; gs_ = s1[:, NT:2 * NT]; mA = s1[:, 2 * NT:3 * NT]; mB = s1[:, 3 * NT:4 * NT]; w1 = s1[:, 4 * NT:5 * NT]; w2 = s1[:, 5 * NT:6 * NT]
    R = ["lg", "g4", "oh", "s1", "le", "o1", "o2", "gate"]
    def D(fn):
        P.op("dve", fn, reads=R, writes=R)
    bc4 = lambda v: v.unsqueeze(2).to_broadcast([128, NT, 4])
    bc32 = lambda v: v.unsqueeze(2).to_broadcast([128, NT, 32])
    D(lambda e: e.tensor_reduce(out=mx, in_=lg[:, :, 0:4], axis=AX.X, op=ALU.max))
    D(lambda e: e.tensor_tensor(oh, lg[:, :, 0:4], bc4(mx), op=ALU.is_ge))
    D(lambda e: e.tensor_tensor(g4, lg[:, :, 0:4], bc4(mx), op=ALU.subtract))
    P.op("act", lambda e: e.activation(out=g4, in_=g4, func=AF.Exp), reads=R, writes=R)
    D(lambda e: e.tensor_reduce(out=gs_, in_=g4, axis=AX.X, op=ALU.add))
    D(lambda e: e.reciprocal(gs_, gs_))
    lev = le.rearrange("p t (g x) -> p t g x", g=4)
    D(lambda e: e.tensor_tensor(lev, lg[:, :, 4:36].rearrange("p t (g x) -> p t g x", g=4), oh.unsqueeze(3).to_broadcast([128, NT, 4, 8]), op=ALU.mult))
    D(lambda e: e.tensor_scalar(o1.rearrange("p t (g x) -> p t g x", g=4), oh.unsqueeze(3).to_broadcast([128, NT, 4, 8]), -1.0, 1e30, op0=ALU.add, op1=ALU.mult))
    D(lambda e: e.tensor_tensor(le, le, o1, op=ALU.add))
    D(lambda e: e.tensor_reduce(out=mA, in_=le, axis=AX.X, op=ALU.max))
    D(lambda e: e.tensor_tensor(o1, le, bc32(mA), op=ALU.is_ge))
    D(lambda e: e.scalar_tensor_tensor(out=le, in0=o1, scalar=-1e30, in1=le, op0=ALU.mult, op1=ALU.add))
    D(lambda e: e.tensor_reduce(out=mB, in_=le, axis=AX.X, op=ALU.max))
    D(lambda e: e.tensor_tensor(o2, le, bc32(mB), op=ALU.is_ge))
    D(lambda e: e.tensor_tensor(w1, mB, mA, op=ALU.subtract))
    P.op("act", lambda e: e.activation(out=w1, in_=w1, func=AF.Exp), reads=R, writes=R)
    D(lambda e: e.tensor_scalar(w1, w1, 1.0, None, op0=ALU.add))
    D(lambda e: e.reciprocal(w1, w1))
    D(lambda e: e.tensor_scalar(w2, w1, -1.0, 1.0, op0=ALU.mult, op1=ALU.add))
    D(lambda e: e.tensor_tensor(w1, w1, gs_, op=ALU.mult))
    D(lambda e: e.tensor_tensor(w2, w2, gs_, op=ALU.mult))
    D(lambda e: e.tensor_tensor(o1, o1, bc32(w1), op=ALU.mult))
    D(lambda e: e.tensor_tensor(o2, o2, bc32(w2), op=ALU.mult))
    D(lambda e: e.tensor_tensor(gate, o1, o2, op=ALU.add))
    P.barrier()
    A.release(m1_)
    TB = 1024
    acc = A.alloc(8 * 1024, F32).rearrange("p (t c) -> p t c", t=8)
    wg = [A.alloc(8 * 512, BF16).rearrange("p (k c) -> p k c", k=8) for _ in range(2)]
    wu = [A.alloc(8 * 512, BF16).rearrange("p (k c) -> p k c", k=8) for _ in range(2)]
    wd = [A.alloc(4 * 1024, BF16).rearrange("p (k c) -> p k c", k=4) for _ in range(2)]
    hid = [A.alloc(4 * 512, BF16).rearrange("p (k t) -> p k t", k=4) for _ in range(2)]
    sg = [A.alloc(512, F32) for _ in range(2)]
    ec = 0; hc_ = 0; sc_ = 0; yc = 0
    for tb in range(S // TB):
        P.op("pool", lambda e: e.memset(acc, 0.0), writes=["acc"])
        for ex in range(32):
            i2 = ec % 2
            P.dma("pool", lambda e, i2=i2, ex=ex: e.dma_start(out=wg[i2], in_=I["w_gate"][l, ex].rearrange("(k p) c -> p k c", p=128)), writes=["wg%d" % i2], key="wg%d" % i2)
            P.dma("pool", lambda e, i2=i2, ex=ex: e.dma_start(out=wu[i2], in_=I["w_up"][l, ex].rearrange("(k p) c -> p k c", p=128)), writes=["wu%d" % i2], key="wu%d" % i2)
            P.dma("pool", lambda e, i2=i2, ex=ex: e.dma_start(out=wd[i2], in_=I["w_down"][l, ex].rearrange("(k p) c -> p k c", p=128)), writes=["wd%d" % i2], key="wd%d" % i2)
            for hb in range(TB // 512):
                tok0 = tb * TB + hb * 512
                hd = hid[hc_ % 2]; hk = "hid%d" % (hc_ % 2)
                for cb in range(4):
                    bg = (sc_ % 2) * 2; bu = bg + 1
                    for kc in range(8):
                        P.op("pe", lambda e, kc=kc, cb=cb, bg=bg, i2=i2, tok0=tok0: e.matmul(ps[bg][:, :], lhsT=wg[i2][:, kc, cb * 128:(cb + 1) * 128], rhs=h2T[:, kc, tok0:tok0 + 512], start=(kc == 0), stop=(kc == 7)),
                             reads=["wg%d" % i2, "h2T"], writes=["ps%d" % bg])
                    for kc in range(8):
                        P.op("pe", lambda e, kc=kc, cb=cb, bu=bu, i2=i2, tok0=tok0: e.matmul(ps[bu][:, :], lhsT=wu[i2][:, kc, cb * 128:(cb + 1) * 128], rhs=h2T[:, kc, tok0:tok0 + 512], start=(kc == 0), stop=(kc == 7)),
                             reads=["wu%d" % i2, "h2T"], writes=["ps%d" % bu])
                    s = sg[sc_ % 2]; sk = "sgF%d" % (sc_ % 2)
                    P.op("act", lambda e, s=s, bg=bg: e.activation(out=s, in_=ps[bg][:, :], func=AF.Exp, scale=-1.0), reads=["ps%d" % bg], writes=[sk])
                    P.op("pool", lambda e, s=s: e.tensor_scalar(s, s, 1.0, None, op0=ALU.add), reads=[sk], writes=[sk])
                    P.op("dve", lambda e, s=s: e.reciprocal(s, s), reads=[sk], writes=[sk])
                    P.op("dve", lambda e, s=s, bg=bg: e.tensor_tensor(s, s, ps[bg][:, :], op=ALU.mult), reads=[sk, "ps%d" % bg], writes=[sk])
                    P.op("dve", lambda e, s=s, bu=bu, hd=hd, cb=cb: e.tensor_tensor(hd[:, cb, :], s, ps[bu][:, :], op=ALU.mult), reads=[sk, "ps%d" % bu], writes=[hk])
                    sc_ += 1
                for tt in range(4):
                    tl = hb * 4 + tt
                    tg = tb * 8 + tl
                    for half in range(2):
                        bank = 4 + yc % 4
                        for kc in range(4):
                            P.op("pe", lambda e, kc=kc, half=half, bank=bank, tt=tt, hd=hd, i2=i2: e.matmul(ps[bank][:, :], lhsT=hd[:, kc, tt * 128:(tt + 1) * 128], rhs=wd[i2][:, kc, half * 512:(half + 1) * 512], start=(kc == 0), stop=(kc == 3)),
                                 reads=[hk, "wd%d" % i2], writes=["ps%d" % bank])
                        P.op("dve", lambda e, bank=bank, tl=tl, tg=tg, half=half, ex=ex: e.scalar_tensor_tensor(out=acc[:, tl, half * 512:(half + 1) * 512], in0=ps[bank][:, :], scalar=gate[:, tg, ex:ex + 1], in1=acc[:, tl, half * 512:(half + 1) * 512], op0=ALU.mult, op1=ALU.add),
                             reads=["ps%d" % bank, "gate", "acc"], writes=["acc"])
                        yc += 1
                hc_ += 1
            ec += 1
        for tl in range(8):
            tg = tb * 8 + tl
            x = k.xb[tg % 2]; xk = "xb%d" % (tg % 2)
            P.dma("sp", lambda e, x=x, tg=tg: e.dma_start(out=x, in_=xsrc[tg * 128:(tg + 1) * 128, :]), writes=[xk], key=xk)
            P.op("pool", lambda e, tl=tl: e.tensor_tensor(acc[:, tl, :], acc[:, tl, :], k.gtbc[1], op=ALU.mult), reads=["acc", "gtbc1"], writes=["acc"])
            P.op("pool", lambda e, tl=tl, x=x: e.tensor_tensor(x, x, acc[:, tl, :], op=ALU.add), reads=["acc", xk], writes=[xk])
            P.dma("sp", lambda e, x=x, tg=tg: e.dma_start(out=xdst[tg * 128:(tg + 1) * 128, :], in_=x), reads=[xk], key=xk)
    P.barrier()
    A.release(m0)


def phaseG(k, I, xsrc, out):
    A, P, ps = k.A, k.P, k.ps
    m0 = A.mark()
    gb = A.alloc(1024, F32)
    P.dma("sp", lambda e: e.dma_start(out=gb, in_=I["g_final"].partition_broadcast(128)), writes=["gbG"], key="gbG")
    xb = [A.alloc(1024, F32) for _ in range(2)]
    xn = [A.alloc(1024, F32) for _ in range(2)]
    st = [A.alloc(2, F32) for _ in range(2)]
    for t in range(NT):
        x = xb[t % 2]; xk = "xbG%d" % (t % 2); n = xn[t % 2]; nk = "xnG%d" % (t % 2); s = st[t % 2]; sk = "stG%d" % (t % 2)
        P.dma("sp", lambda e, x=x, t=t: e.dma_start(out=x, in_=xsrc[t * 128:(t + 1) * 128, :]), writes=[xk], key=xk)
        P.op("act", lambda e, x=x, n=n, s=s: e.activation(out=n, in_=x, func=AF.Square, accum_out=s[:, 0:1]), reads=[xk], writes=[nk, sk])
        P.op("dve", lambda e, s=s: e.tensor_scalar(s[:, 1:2], s[:, 0:1], 1.0 / D, EPS, op0=ALU.mult, op1=ALU.add), reads=[sk], writes=[sk])
        P.op("act", lambda e, s=s: e.activation(out=s[:, 1:2], in_=s[:, 1:2], func=AF.Ln), reads=[sk], writes=[sk])
        P.op("act", lambda e, s=s: e.activation(out=s[:, 1:2], in_=s[:, 1:2], func=AF.Exp, scale=-0.5), reads=[sk], writes=[sk])
        P.op("dve", lambda e, x=x, n=n, s=s: e.scalar_tensor_tensor(out=n, in0=x, scalar=s[:, 1:2], in1=gb, op0=ALU.mult, op1=ALU.mult), reads=[xk, sk, "gbG"], writes=[nk])
        P.dma("sp", lambda e, n=n, t=t: e.dma_start(out=out[t * 128:(t + 1) * 128, :], in_=n), reads=[nk], key=nk)
    P.barrier()
    A.release(m0)


from concourse.bass_utils import run_bass_kernel_spmd

_WNAMES = ["w_mod", "b_mod", "g_norm1", "g_norm2", "w_in", "g_cq", "g_ckv", "g_kidx", "w_q_up", "w_idx_q", "w_v_up",
           "lb_logits", "g_rec", "w_branch_a", "w_branch_r", "w_out", "w_grp", "b_grp", "w_exp_router", "b_exp_router",
           "w_gate", "w_up", "w_down", "g_final"]


def _build(shapes, depth=4):
    nc = bass.Bass("TRN2", target_bir_lowering=False)
    es = ExitStack()
    with es:
        I = {}
        I["x"] = nc.dram_tensor("x", [S, D], F32, kind="ExternalInput").ap()
        I["c"] = nc.dram_tensor("c", [1, D], F32, kind="ExternalInput").ap()
        for n in _WNAMES:
            I[n] = nc.dram_tensor(n, list(shapes[n]), F32, kind="ExternalInput").ap()
        out = nc.dram_tensor("out", [S, D], F32, kind="ExternalOutput").ap()
        k = mkctx(nc, es)
        setup_consts(k)
        pm = k.A.mark()
        xa = dram(k, "xres_a", [S, D], F32)
        xb = dram(k, "xres_b", [S, D], F32)
        xcur = I["x"]
        for l in range(depth):
            k.A.release(pm)
            phase0(k, l, I)
            m_after0 = k.A.mark()
            phaseA(k, l, I, xcur)
            phaseB(k, l, I)
            phaseC(k, l, I)
            k.A.release(m_after0)
            phaseD(k, l, I)
            phaseE(k, l, I, xcur, xa)
            phaseF(k, l, I, xa, xb)
            xcur = xb
        phaseG(k, I, xcur, out)
        k.P.finish(k.A.alloc(2, F32))
        k.P.emit(es)
    return nc


def kernel(**inputs):
    x = np.ascontiguousarray(inputs["x"], dtype=np.float32)
    c = np.ascontiguousarray(inputs["c"], dtype=np.float32)
    shapes = {n: inputs[n].shape for n in _WNAMES}
    nc = _build(shapes)
    w = {n: np.ascontiguousarray(inputs[n], dtype=np.float32) for n in _WNAMES}
    in_maps = []
    for b in range(8):
        m = {"x": x[b], "c": c[b:b + 1]}
        m.update(w)
        in_maps.append(m)
    res = run_bass_kernel_spmd(nc, in_maps, core_ids=list(range(8)))
    return np.stack([np.asarray(r["out"], dtype=np.float32) for r in res.results], axis=0)
```

```python
import numpy as np
import concourse.bass as bass
import concourse.mybir as mybir
from contextlib import ExitStack

F32 = mybir.dt.float32
BF16 = mybir.dt.bfloat16
I32 = mybir.dt.int32
AF = mybir.ActivationFunctionType
ALU = mybir.AluOpType
AX = mybir.AxisListType


class Op:
    __slots__ = ("eng", "fn", "deps", "inc", "cnt", "dma", "key", "consumed", "idx")

    def __init__(self, eng, fn, dma=False, key=None):
        self.eng = eng
        self.fn = fn
        self.deps = []
        self.inc = dma
        self.cnt = 0
        self.dma = dma
        self.key = key
        self.consumed = False


class Prog:
    ENGS = ("pe", "act", "dve", "pool", "sp")

    def __init__(self, nc):
        self.nc = nc
        self.ops = {e: [] for e in self.ENGS}
        self.last_w = {}
        self.readers = {}
        self.since_barrier = []
        self.pending_barrier = {e: [] for e in self.ENGS}
        self.nops = 0

    def _add(self, op, reads, writes):
        deps = []
        seen = set()
        for k in list(reads) + list(writes):
            w = self.last_w.get(k)
            if w is not None and id(w) not in seen:
                seen.add(id(w)); deps.append(w)
        for k in writes:
            for r in self.readers.get(k, ()):
                if id(r) not in seen:
                    seen.add(id(r)); deps.append(r)
        pb = self.pending_barrier[op.eng]
        if pb:
            for d in pb:
                if id(d) not in seen:
                    seen.add(id(d)); deps.append(d)
            self.pending_barrier[op.eng] = []
        op.deps = [d for d in deps if d is not op]
        for d in op.deps:
            d.consumed = True
        for k in writes:
            self.last_w[k] = op
            self.readers[k] = []
        for k in reads:
            if k not in writes:
                self.readers.setdefault(k, []).append(op)
        self.ops[op.eng].append(op)
        self.since_barrier.append(op)
        self.nops += 1
        return op

    def op(self, eng, fn, reads=(), writes=()):
        return self._add(Op(eng, fn), reads, writes)

    def dma(self, eng, fn, reads=(), writes=(), key=None):
        assert key is not None
        return self._add(Op(eng, fn, dma=True, key=key), reads, writes)

    def barrier(self):
        lst = []
        last = {}
        for o in self.since_barrier:
            if o.dma:
                if not o.consumed:
                    lst.append(o)
            else:
                last[o.eng] = o
        lst.extend(last.values())
        for e in self.ENGS:
            self.pending_barrier[e] = self.pending_barrier[e] + lst
        self.since_barrier = []

    def finish(self, scratch):
        self.barrier()
        self.op("pool", lambda e: e.memset(scratch, 0.0), writes=["__fin"])

    def emit(self, es):
        nc = self.nc
        for e in self.ENGS:
            for o in self.ops[e]:
                for d in o.deps:
                    if d.dma:
                        continue
                    if d.eng == "pe" and o.eng == "pe" and not o.dma:
                        continue
                    d.inc = True
        esem = {}
        for e in self.ENGS:
            esem[e] = es.enter_context(nc.semaphore("s_" + e))
        dsem = {}
        dcnt = {}
        for e in self.ENGS:
            c = 0
            for o in self.ops[e]:
                if o.dma:
                    if o.key not in dsem:
                        dsem[o.key] = es.enter_context(nc.semaphore("d_%d" % len(dsem)))
                        dcnt[o.key] = 0
                    dcnt[o.key] += 16
                    o.cnt = dcnt[o.key]
                elif o.inc:
                    c += 1
                    o.cnt = c
        self.n_dsem = len(dsem)
        block = es.enter_context(nc.Block())

        def run(ename, h):
            waited = {}
            for o in self.ops[ename]:
                need = {}
                for d in o.deps:
                    if d.dma:
                        s = dsem[d.key]
                    else:
                        if d.eng == "pe" and ename == "pe" and not o.dma:
                            continue
                        s = esem[d.eng]
                    sid = id(s)
                    if need.get(sid, (None, 0))[1] < d.cnt:
                        need[sid] = (s, d.cnt)
                for sid, (s, v) in need.items():
                    if waited.get(sid, 0) < v:
                        h.wait_ge(s, v)
                        waited[sid] = v
                ins = o.fn(h)
                if o.dma:
                    ins.then_inc(dsem[o.key], 16)
                elif o.inc:
                    ins.then_inc(esem[ename], 1)

        @block.tensor
        def _(h):
            run("pe", h)

        @block.scalar
        def _(h):
            run("act", h)

        @block.vector
        def _(h):
            run("dve", h)

        @block.gpsimd
        def _(h):
            run("pool", h)

        @block.sync
        def _(h):
            run("sp", h)


class Arena:
    def __init__(self, nc, es, ncols, name="arena"):
        self.t = es.enter_context(nc.sbuf_tensor(name, [128, ncols], F32))
        self.ncols = ncols
        self.off = 0
        self.uid = 0

    def mark(self):
        return self.off

    def release(self, m):
        self.off = m

    def alloc(self, cols, dtype=F32, parts=128):
        if dtype == BF16:
            w = (cols + 1) // 2
        else:
            w = cols
        assert self.off + w <= self.ncols, ("arena overflow", self.off, w, self.ncols)
        v = self.t[0:parts, self.off:self.off + w]
        self.off += w
        if dtype != F32:
            v = v.bitcast(dtype)
            if dtype == BF16 and cols % 2:
                v = v[:, 0:cols]
        return v


S = 4096
D = 1024
NT = S // 128
DIN = 4552
EPS = 1e-6
O_CQ, O_CKV, O_KIDX, O_WIDX, O_QREC, O_FREC, O_IREC, O_OG, O_GA, O_GR = 0, 256, 384, 448, 456, 968, 1480, 1992, 2504, 3528


class K:
    pass


def mkctx(nc, es, debug=()):
    k = K()
    k.nc = nc
    k.es = es
    k.P = Prog(nc)
    k.A = Arena(nc, es, 52800)
    k.ps = [es.enter_context(nc.psum_tensor("bank%d" % i, [128, 512], F32)) for i in range(8)]
    k.debug = set(debug)
    k.dr = {}
    k.uid = 0
    return k


def dram(k, name, shape, dtype):
    if name in k.dr:
        return k.dr[name]
    kind = "ExternalOutput" if name in k.debug else "Internal"
    t = k.nc.dram_tensor(name, list(shape), dtype, kind=kind).ap()
    k.dr[name] = t
    return t


def setup_consts(k):
    A, P = k.A, k.P
    k.ident = A.alloc(128, F32)
    k.identb = A.alloc(128, BF16)
    k.ones_f = A.alloc(128, F32)
    k.ones_b = A.alloc(128, BF16)
    P.op("pool", lambda e: e.memset(k.ident, 0.0), writes=["ident"])
    P.op("pool", lambda e: e.affine_select(out=k.ident, in_=k.ident, pattern=[[-1, 128]], compare_op=ALU.not_equal,
                                           fill=1.0, base=0, channel_multiplier=1), reads=["ident"], writes=["ident"])
    P.op("dve", lambda e: e.tensor_copy(k.identb, k.ident), reads=["ident"], writes=["identb"])
    P.op("dve", lambda e: e.memset(k.ones_f, 1.0), writes=["ones_f"])
    k.cneg = A.alloc(128, F32)
    P.op("pool", lambda e: e.memset(k.cneg, 0.0), writes=["cneg"])
    P.op("pool", lambda e: e.affine_select(out=k.cneg, in_=k.cneg, pattern=[[-1, 128]], compare_op=ALU.is_ge,
                                           fill=-1e30, base=0, channel_multiplier=1), reads=["cneg"], writes=["cneg"])
    k.ecap = A.alloc(32, F32)
    for e_ in range(32):
        P.op("dve", lambda e, e_=e_: e.memset(k.ecap[:, e_:e_ + 1], float(e_ * 768)), writes=["ecap"])
    k.cneg_u = A.alloc(128, F32)
    P.op("pool", lambda e: e.memset(k.cneg_u, 1.0), writes=["cneg_u"])
    P.op("pool", lambda e: e.affine_select(out=k.cneg_u, in_=k.cneg_u, pattern=[[1, 128]], compare_op=ALU.is_gt, fill=0.0, base=0, channel_multiplier=-1), reads=["cneg_u"], writes=["cneg_u"])
    k.trash = A.alloc(2, F32)
    P.op("dve", lambda e: e.tensor_reduce(out=k.trash[:, 0:1], in_=k.cneg_u, axis=AX.X, op=ALU.add), reads=["cneg_u"], writes=["trash"])
    P.op("dve", lambda e: e.tensor_scalar(k.trash[:, 0:1], k.trash[:, 0:1], 1.0, -(32.0 * 768 + 127.0), op0=ALU.mult, op1=ALU.add), reads=["trash"], writes=["trash"])
    k.cmf = A.alloc(128, F32)
    P.op("pool", lambda e: e.memset(k.cmf, 1.0), writes=["cmf"])
    P.op("pool", lambda e: e.affine_select(out=k.cmf, in_=k.cmf, pattern=[[1, 128]], compare_op=ALU.is_ge, fill=0.0, base=0, channel_multiplier=-1), reads=["cmf"], writes=["cmf"])
    P.op("pool", lambda e: e.memset(k.cmf[0:64, 64:128], 0.0), reads=["cmf"], writes=["cmf"])
    P.op("dve", lambda e: e.memset(k.ones_b, 1.0), writes=["ones_b"])


def phase0(k, l, I):
    A, P, nc = k.A, k.P, k.nc
    ps = k.ps
    stg = A.alloc(128, F32)
    k.par = A.alloc(128, F32)
    par = k.par
    P.op("dve", lambda e: e.memset(stg, 0.0), writes=["stg"])
    rows = [
        (0, 48, I["b_mod"][l].rearrange("(r c) -> r c", c=128), 128),
        (48, 8, I["c"][0].rearrange("(r c) -> r c", c=128), 128),
        (56, 8, I["g_norm1"][l].rearrange("(r c) -> r c", c=128), 128),
        (64, 8, I["g_norm2"][l].rearrange("(r c) -> r c", c=128), 128),
        (72, 2, I["g_cq"][l].rearrange("(r c) -> r c", c=128), 128),
        (74, 1, I["g_ckv"][l].rearrange("(r c) -> r c", c=128), 128),
        (75, 1, I["g_kidx"][l].rearrange("(r c) -> r c", c=64), 64),
        (76, 16, I["lb_logits"].rearrange("l (r c) -> (l r) c", c=128), 128),
        (92, 8, I["g_final"].rearrange("(r c) -> r c", c=128), 128),
    ]
    for (r0, n, src, w) in rows:
        P.dma("sp", lambda e, r0=r0, n=n, src=src, w=w: e.dma_start(out=stg[r0:r0 + n, 0:w], in_=src),
              reads=[], writes=["stg"], key="p0stg")
    P.dma("sp", lambda e: e.dma_start(out=stg[75:76, 64:128], in_=I["g_kidx"][l].rearrange("(r c) -> r c", c=64)),
          writes=["stg"], key="p0stg")
    P.op("pe", lambda e: e.transpose(ps[0][:, 0:128], stg, k.ident), reads=["stg", "ident"], writes=["ps0"])
    P.op("dve", lambda e: e.tensor_copy(par, ps[0][:, 0:128]), reads=["ps0"], writes=["par"])
    k.g1 = par[:, 56:64]; k.g2 = par[:, 64:72]; k.gcq = par[:, 72:74]; k.gckv = par[:, 74:75]
    k.gkidx = par[:, 75:76]; k.gfin = par[:, 92:100]
    sm = A.alloc(64, F32)
    k.sm = sm
    cact = sm[:, 0:8]
    t1 = sm[:, 8:16]
    P.op("act", lambda e: e.activation(out=t1, in_=par[:, 48:56], func=AF.Exp, scale=-1.0), reads=["par"], writes=["sm"])
    P.op("dve", lambda e: e.tensor_scalar(t1, t1, 1.0, None, op0=ALU.add), reads=["sm"], writes=["sm"])
    P.op("dve", lambda e: e.reciprocal(t1, t1), reads=["sm"], writes=["sm"])
    P.op("dve", lambda e: e.tensor_tensor(cact, par[:, 48:56], t1, op=ALU.mult), reads=["sm", "par"], writes=["sm"])
    lbl = par[:, 76:92].rearrange("p (l c) -> p l c", l=4)
    el = sm[:, 16:32].rearrange("p (l c) -> p l c", l=4)
    mx = sm[:, 32:36]
    P.op("dve", lambda e: e.tensor_reduce(out=mx, in_=par[:, 76:92].rearrange("p (l c) -> p c l", l=4), axis=AX.X, op=ALU.max),
         reads=["par"], writes=["sm"])
    P.op("dve", lambda e: e.tensor_tensor(el, lbl, mx.unsqueeze(1).to_broadcast([128, 4, 4]), op=ALU.subtract),
         reads=["sm", "par"], writes=["sm"])
    P.op("act", lambda e: e.activation(out=sm[:, 16:32], in_=sm[:, 16:32], func=AF.Exp), reads=["sm"], writes=["sm"])
    ssum = sm[:, 36:40]
    P.op("dve", lambda e: e.tensor_reduce(out=ssum, in_=sm[:, 16:32].rearrange("p (l c) -> p c l", l=4), axis=AX.X, op=ALU.add),
         reads=["sm"], writes=["sm"])
    P.op("dve", lambda e: e.reciprocal(ssum, ssum), reads=["sm"], writes=["sm"])
    k.lb = sm[:, 40:44]
    k.oml = sm[:, 44:48]
    P.op("dve", lambda e: e.memset(k.lb, 0.0), reads=["sm"], writes=["sm"])
    for j in range(1, l + 1):
        P.op("dve", lambda e, j=j: e.tensor_tensor(k.lb, k.lb, el[:, j, :], op=ALU.add), reads=["sm"], writes=["sm"])
    P.op("dve", lambda e: e.tensor_tensor(k.lb, k.lb, ssum, op=ALU.mult), reads=["sm"], writes=["sm"])
    P.op("dve", lambda e: e.tensor_scalar(k.lb, k.lb, 0.0, 1.0, op0=ALU.max, op1=ALU.min), reads=["sm"], writes=["sm"])
    P.op("dve", lambda e: e.tensor_scalar(k.oml, k.lb, -1.0, 1.0, op0=ALU.mult, op1=ALU.add), reads=["sm"], writes=["sm"])
    cact_rep = A.alloc(8 * 128, F32).rearrange("p (k m) -> p k m", k=8)
    P.op("dve", lambda e: e.tensor_copy(cact_rep, cact.unsqueeze(2).to_broadcast([128, 8, 128])), reads=["sm"], writes=["crep"])
    k.gtbc = [A.alloc(1024, F32), A.alloc(1024, F32)]
    bmbc = A.alloc(1024, F32)
    modf = k.A.alloc(32, F32)
    gs = k.A.alloc(16, F32)
    m = A.mark()
    wm = [A.alloc(8 * 1024, F32).rearrange("p (k c) -> p k c", k=8) for _ in range(2)]
    groups = [(0, "fm", 0), (1, "fm", 8), (3, "fm", 16), (4, "fm", 24), (2, "tm", 0), (5, "tm", 1)]
    wsrc = I["w_mod"][l].rearrange("(k p) c -> p k c", p=128)
    for gi, (g, kind, o) in enumerate(groups):
        w = wm[gi % 2]
        wk = "wm%d" % (gi % 2)
        P.dma("sp", lambda e, w=w, g=g: e.dma_start(out=w, in_=wsrc[:, :, g * 1024:(g + 1) * 1024]), writes=[wk], key=wk)
        if kind == "fm":
            for cb in range(8):
                for kc in range(8):
                    P.op("pe", lambda e, w=w, cb=cb, kc=kc, o=o: e.matmul(ps[1][:, o + cb:o + cb + 1], lhsT=w[:, kc, cb * 128:(cb + 1) * 128],
                                                                       rhs=cact[:, kc:kc + 1], start=(kc == 0), stop=(kc == 7)),
                         reads=[wk, "sm"], writes=["ps1"])
        else:
            P.dma("sp", lambda e, g=g: e.dma_start(out=bmbc, in_=I["b_mod"][l, g * 1024:(g + 1) * 1024].partition_broadcast(128)),
                  writes=["bmbc"], key="bmbc")
            for nb in range(2):
                pk = "ps%d" % (2 + nb)
                for kc in range(8):
                    P.op("pe", lambda e, w=w, nb=nb, kc=kc: e.matmul(ps[2 + nb][:, :], lhsT=cact_rep[:, kc, :], rhs=w[:, kc, nb * 512:(nb + 1) * 512],
                                                                  start=(kc == 0), stop=(kc == 7)),
                         reads=[wk, "crep"], writes=[pk])
                P.op("dve", lambda e, nb=nb, o=o: e.tensor_tensor(k.gtbc[o][:, nb * 512:(nb + 1) * 512], ps[2 + nb][:, :], bmbc[:, nb * 512:(nb + 1) * 512], op=ALU.add),
                     reads=[pk, "bmbc"], writes=["gtbc%d" % o])
    bsel = [0, 8, 24, 32]
    for i, b0 in enumerate(bsel):
        P.op("dve", lambda e, i=i, b0=b0: e.tensor_tensor(modf[:, i * 8:(i + 1) * 8], ps[1][:, i * 8:(i + 1) * 8], par[:, b0:b0 + 8], op=ALU.add),
             reads=["ps1", "par"], writes=["modf"])
    k.sh1 = modf[:, 0:8]; k.sh2 = modf[:, 16:24]
    k.gs1 = gs[:, 0:8]; k.gs2 = gs[:, 8:16]
    P.op("dve", lambda e: e.scalar_tensor_tensor(out=k.gs1, in0=modf[:, 8:16], scalar=1.0, in1=k.g1, op0=ALU.add, op1=ALU.mult),
         reads=["modf", "par"], writes=["gs"])
    P.op("dve", lambda e: e.scalar_tensor_tensor(out=k.gs2, in0=modf[:, 24:32], scalar=1.0, in1=k.g2, op0=ALU.add, op1=ALU.mult),
         reads=["modf", "par"], writes=["gs"])
    P.barrier()
    A.release(m)


def norm_transpose(k, xsrc, t, hT, gs, sh, tag, hkey):
    A, P, ps = k.A, k.P, k.ps
    xb = k.xb[t % 2]; xk = "xb%d" % (t % 2)
    xn = k.xn[t % 2]; nk = "xn%d" % (t % 2)
    st = k.nst[t % 2]; sk = "nst%d" % (t % 2)
    P.dma("sp", lambda e: e.dma_start(out=xb, in_=xsrc[t * 128:(t + 1) * 128, :]), writes=[xk], key=xk)
    P.op("act", lambda e: e.activation(out=xn, in_=xb, func=AF.Square, accum_out=st[:, 0:1]), reads=[xk], writes=[nk, sk])
    P.op("dve", lambda e: e.tensor_scalar(st[:, 1:2], st[:, 0:1], 1.0 / D, EPS, op0=ALU.mult, op1=ALU.add), reads=[sk], writes=[sk])
    P.op("act", lambda e: e.activation(out=st[:, 1:2], in_=st[:, 1:2], func=AF.Ln), reads=[sk], writes=[sk])
    P.op("act", lambda e: e.activation(out=st[:, 1:2], in_=st[:, 1:2], func=AF.Exp, scale=-0.5), reads=[sk], writes=[sk])
    P.op("dve", lambda e: e.tensor_scalar(xn, xb, st[:, 1:2], None, op0=ALU.mult), reads=[xk, sk], writes=[nk])
    b0 = (t % 2) * 2
    for kc in range(8):
        bank = b0 + kc // 4
        P.op("pe", lambda e, kc=kc, bank=bank: e.transpose(ps[bank][:, (kc % 4) * 128:(kc % 4 + 1) * 128], xn[:, kc * 128:(kc + 1) * 128], k.ident),
             reads=[nk, "ident"], writes=["ps%d" % bank])
    return b0


def phaseA(k, l, I, xsrc):
    A, P, nc, ps = k.A, k.P, k.nc, k.ps
    m0 = A.mark()
    WC = DIN + 64
    w = A.alloc(8 * WC, BF16).rearrange("p (k c) -> p k c", k=8)
    wsrc = I["w_in"][l].rearrange("(k p) c -> p k c", p=128)
    for (d0, s0, n) in [(0, 0, 448), (448, 384, 64), (512, 448, 1024), (1536, 1472, 1024), (2560, 2496, 1024), (3584, 3520, 1032)]:
        P.dma("pool", lambda e, d0=d0, s0=s0, n=n: e.dma_start(out=w[:, :, d0:d0 + n], in_=wsrc[:, :, s0:s0 + n]), writes=["w_in"], key="w_in")
    k.xb = [A.alloc(1024, F32) for _ in range(2)]
    k.xn = [A.alloc(1024, F32) for _ in range(2)]
    k.nst = [A.alloc(2, F32) for _ in range(2)]
    hT = [A.alloc(8 * 512, BF16).rearrange("p (k t) -> p k t", k=8) for _ in range(2)]
    fo = [A.alloc(512, F32) for _ in range(4)]
    gb = [A.alloc(512, BF16) for _ in range(4)]
    to = [A.alloc(1672, F32) for _ in range(2)]
    cq_fm = dram(k, "cq_fm", [256, S], F32)
    ckv_fm = dram(k, "ckv_fm", [128, S], F32)
    kidx_fm = dram(k, "kidx_fm", [128, S], F32)
    qrec_fm = dram(k, "qrec_fm", [512, S], F32)
    frec_fm = dram(k, "frec_fm", [512, S], F32)
    gates_fm = dram(k, "gates_fm", [2048, S], BF16)
    tm_out = dram(k, "tm_out", [S, 1672], F32)
    fmb = [(0, cq_fm, 0, 0), (128, cq_fm, 128, 0), (256, ckv_fm, 0, 0), (384, kidx_fm, 0, 0)]
    for j in range(4):
        fmb.append((O_QREC + 64 + j * 128, qrec_fm, j * 128, 0))
    for j in range(4):
        fmb.append((O_FREC + 64 + j * 128, frec_fm, j * 128, 0))
    for j in range(16):
        fmb.append((O_GA + 64 + j * 128, gates_fm, j * 128, 1))
    tmg = [(256, 128, 0), (O_WIDX + 64, 8, 128), (O_IREC + 64, 512, 136), (O_OG + 64, 512, 648), (O_FREC + 64, 512, 1160)]
    fcount = 0
    for st in range(S // 512):
        h = hT[st % 2]; hk = "hT%d" % (st % 2)
        for tt in range(4):
            t = st * 4 + tt
            b0 = norm_transpose(k, xsrc, t, None, None, None, None, None)
            for kc in range(8):
                bank = b0 + kc // 4
                P.op("act", lambda e, kc=kc, bank=bank, tt=tt, h=h: e.activation(out=h[:, kc, tt * 128:(tt + 1) * 128], in_=ps[bank][:, (kc % 4) * 128:(kc % 4 + 1) * 128],
                                                                                 func=AF.Identity, scale=k.gs1[:, kc:kc + 1], bias=k.sh1[:, kc:kc + 1]),
                     reads=["ps%d" % bank, "gs", "modf"], writes=[hk])
        for bi, (c0, dst, r0, act) in enumerate(fmb):
            bank = 4 + fcount % 4
            pk = "ps%d" % bank
            for kc in range(8):
                P.op("pe", lambda e, kc=kc, c0=c0, bank=bank, h=h: e.matmul(ps[bank][:, :], lhsT=w[:, kc, c0:c0 + 128], rhs=h[:, kc, :], start=(kc == 0), stop=(kc == 7)),
                     reads=["w_in", hk], writes=[pk])
            f = fo[fcount % 4]; fk = "fo%d" % (fcount % 4)
            if act == 0:
                P.op("dve", lambda e, f=f, bank=bank: e.tensor_copy(f, ps[bank][:, :]), reads=[pk], writes=[fk])
                P.dma("sp", lambda e, f=f, dst=dst, r0=r0, st=st: e.dma_start(out=dst[r0:r0 + 128, st * 512:(st + 1) * 512], in_=f), reads=[fk], writes=[], key=fk)
            else:
                fb = gb[fcount % 4]
                P.op("act", lambda e, f=f, bank=bank: e.activation(out=f, in_=ps[bank][:, :], func=AF.Exp, scale=-1.0), reads=[pk], writes=[fk])
                P.op("pool", lambda e, f=f: e.tensor_scalar(f, f, 1.0, None, op0=ALU.add), reads=[fk], writes=[fk])
                P.op("dve", lambda e, f=f: e.reciprocal(f, f), reads=[fk], writes=[fk])
                P.op("pool", lambda e, f=f, fb=fb: e.tensor_copy(fb, f), reads=[fk], writes=[fk])
                P.dma("sp", lambda e, fb=fb, dst=dst, r0=r0, st=st: e.dma_start(out=dst[r0:r0 + 128, st * 512:(st + 1) * 512], in_=fb), reads=[fk], writes=[], key=fk)
            fcount += 1
        for tt in range(4):
            t = st * 4 + tt
            tb = to[t % 2]; tk = "to%d" % (t % 2)
            for gi, (c0, n, o0) in enumerate(tmg):
                if gi == 1:
                    continue
                bank = 4 + fcount % 4
                pk = "ps%d" % bank
                subs = [(c0, n, 0)]
                if gi == 0:
                    subs = [(c0, n, 0), (tmg[1][0], 8, 128)]
                for (cc, nn, po) in subs:
                    for kc in range(8):
                        P.op("pe", lambda e, kc=kc, cc=cc, nn=nn, po=po, bank=bank, h=h, tt=tt: e.matmul(ps[bank][:, po:po + nn], lhsT=h[:, kc, tt * 128:(tt + 1) * 128], rhs=w[:, kc, cc:cc + nn],
                                                                                                  start=(kc == 0), stop=(kc == 7)),
                             reads=["w_in", hk], writes=[pk])
                tot = n + (8 if gi == 0 else 0)
                eng = "dve" if gi % 2 == 0 else "act"
                if eng == "dve":
                    P.op("dve", lambda e, tb=tb, o0=o0, tot=tot, bank=bank: e.tensor_copy(tb[:, o0:o0 + tot], ps[bank][:, 0:tot]), reads=[pk], writes=[tk])
                else:
                    P.op("act", lambda e, tb=tb, o0=o0, tot=tot, bank=bank: e.activation(out=tb[:, o0:o0 + tot], in_=ps[bank][:, 0:tot], func=AF.Copy), reads=[pk], writes=[tk])
                fcount += 1
            P.dma("sp", lambda e, tb=tb, t=t: e.dma_start(out=tm_out[t * 128:(t + 1) * 128, :], in_=tb), reads=[tk], writes=[], key=tk)
    P.barrier()
    A.release(m0)


ATTN_SCALE = 128 ** -0.5
NEG = -1e30
MASKV = -30000.0


def phaseB(k, l, I):
    A, P, ps = k.A, k.P, k.ps
    cq_fm = k.dr["cq_fm"]; ckv_fm = k.dr["ckv_fm"]; kidx_fm = k.dr["kidx_fm"]; tm_out = k.dr["tm_out"]
    qT_d = dram(k, "qT_d", [NT, 128, 8, 128], BF16)
    qidx_d = dram(k, "qidx_d", [NT, 128, 4, 128], BF16)
    k.kvT = A.alloc(S, BF16)
    k.kidxT = A.alloc(S, BF16)
    k.kvaug = A.alloc(NT * 132, BF16).rearrange("p (t c) -> p t c", t=NT)
    k.widx = A.alloc(NT * 8, F32).rearrange("p (t c) -> p t c", t=NT)
    k.wv = A.alloc(8 * 128, BF16).rearrange("p (h c) -> p h c", h=8)
    m0 = A.mark()
    wq = A.alloc(2 * 1024, BF16).rearrange("p (k c) -> p k c", k=2)
    wi = A.alloc(2 * 512, BF16).rearrange("p (k c) -> p k c", k=2)
    P.dma("pool", lambda e: e.dma_start(out=wq, in_=I["w_q_up"][l].rearrange("(k p) c -> p k c", p=128)), writes=["wq"], key="wq")
    P.dma("pool", lambda e: e.dma_start(out=wi, in_=I["w_idx_q"][l].rearrange("(k p) c -> p k c", p=128)), writes=["wi"], key="wi")
    P.op("dve", lambda e: e.memset(k.wv, 0.0), writes=["wv"])
    for par in range(2):
        P.dma("pool", lambda e, par=par: e.dma_start(out=k.wv.rearrange("p (j two) c -> p j two c", two=2)[:, :, par, par * 64:(par + 1) * 64],
                                                     in_=I["w_v_up"][l].rearrange("(j two) r v -> r j two v", two=2)[:, :, par, :]),
              writes=["wv"], key="wvd")
    ckt = A.alloc(NT * 128, F32).rearrange("p (t c) -> p t c", t=NT)
    sq = A.alloc(NT * 128, F32).rearrange("p (t c) -> p t c", t=NT)
    gb = A.alloc(128, F32)
    st = A.alloc(64, F32)
    P.dma("sp", lambda e: e.dma_start(out=ckt, in_=tm_out[:, 0:128].rearrange("(t p) c -> p t c", p=128)), writes=["ckt"], key="ckt")
    P.dma("sp", lambda e: e.dma_start(out=k.widx, in_=tm_out[:, 128:136].rearrange("(t p) c -> p t c", p=128)), writes=["widx"], key="widx")
    P.dma("sp", lambda e: e.dma_start(out=gb, in_=I["g_ckv"][l].partition_broadcast(128)), writes=["gb"], key="gb")
    P.op("dve", lambda e: e.tensor_tensor(sq, ckt, ckt, op=ALU.mult), reads=["ckt"], writes=["sq"])
    P.op("dve", lambda e: e.tensor_reduce(out=st[:, 0:32], in_=sq, axis=AX.X, op=ALU.add), reads=["sq"], writes=["stB"])
    P.op("dve", lambda e: e.tensor_scalar(st[:, 0:32], st[:, 0:32], 1.0 / 128, EPS, op0=ALU.mult, op1=ALU.add), reads=["stB"], writes=["stB"])
    P.op("act", lambda e: e.activation(out=st[:, 0:32], in_=st[:, 0:32], func=AF.Ln), reads=["stB"], writes=["stB"])
    P.op("act", lambda e: e.activation(out=st[:, 0:32], in_=st[:, 0:32], func=AF.Exp, scale=-0.5), reads=["stB"], writes=["stB"])
    P.op("dve", lambda e: e.tensor_tensor(sq, ckt, st[:, 0:32].unsqueeze(2).to_broadcast([128, NT, 128]), op=ALU.mult), reads=["ckt", "stB", "sq"], writes=["sq"])
    P.op("dve", lambda e: e.tensor_tensor(k.kvaug[:, :, 0:128], sq, gb.unsqueeze(1).to_broadcast([128, NT, 128]), op=ALU.mult), reads=["sq", "gb"], writes=["kvaug"])
    P.op("dve", lambda e: e.memset(k.kvaug[:, :, 128:132], 1.0), writes=["kvaug"])
    xin = [A.alloc(4 * 512, F32).rearrange("p (c t) -> p c t", c=4) for _ in range(2)]
    sqb = A.alloc(4 * 512, BF16).rearrange("p (c t) -> p c t", c=4)
    rs = A.alloc(3 * 512, F32).rearrange("p (c t) -> p c t", c=3)
    cqn = A.alloc(2 * 512, BF16).rearrange("p (c t) -> p c t", c=2)
    ob = [A.alloc(512, BF16) for _ in range(4)]
    oc = 0
    for b in range(S // 512):
        x = xin[b % 2]; xk = "xinB%d" % (b % 2)
        sl = slice(b * 512, (b + 1) * 512)
        P.dma("sp", lambda e, x=x, sl=sl: e.dma_start(out=x[:, 0:2, :], in_=cq_fm[:, sl].rearrange("(c p) t -> p c t", p=128)), writes=[xk], key=xk)
        P.dma("sp", lambda e, x=x, sl=sl: e.dma_start(out=x[:, 2, :], in_=ckv_fm[:, sl]), writes=[xk], key=xk)
        P.dma("sp", lambda e, x=x, sl=sl: e.dma_start(out=x[:, 3, :], in_=kidx_fm[:, sl]), writes=[xk], key=xk)
        P.op("act", lambda e, x=x: e.activation(out=sqb, in_=x, func=AF.Square), reads=[xk], writes=["sqb"])
        P.op("pe", lambda e: e.matmul(ps[0][:, :], lhsT=k.ones_b, rhs=sqb[:, 0, :], start=True, stop=False), reads=["sqb", "ones_b"], writes=["ps0"])
        P.op("pe", lambda e: e.matmul(ps[0][:, :], lhsT=k.ones_b, rhs=sqb[:, 1, :], start=False, stop=True), reads=["sqb", "ones_b"], writes=["ps0"])
        P.op("pe", lambda e: e.matmul(ps[1][:, :], lhsT=k.ones_b, rhs=sqb[:, 2, :], start=True, stop=True), reads=["sqb", "ones_b"], writes=["ps1"])
        P.op("pe", lambda e: e.matmul(ps[2][:, :], lhsT=k.ones_b, rhs=sqb[:, 3, :], start=True, stop=True), reads=["sqb", "ones_b"], writes=["ps2"])
        for i, n in enumerate([256.0, 128.0, 128.0]):
            P.op("dve", lambda e, i=i, n=n: e.tensor_scalar(rs[:, i, :], ps[i][:, :], 1.0 / n, EPS, op0=ALU.mult, op1=ALU.add), reads=["ps%d" % i], writes=["rs"])
        P.op("act", lambda e: e.activation(out=rs, in_=rs, func=AF.Ln), reads=["rs"], writes=["rs"])
        P.op("act", lambda e: e.activation(out=rs, in_=rs, func=AF.Exp, scale=-0.5), reads=["rs"], writes=["rs"])
        for c in range(2):
            P.op("dve", lambda e, c=c, x=x: e.scalar_tensor_tensor(out=cqn[:, c, :], in0=x[:, c, :], scalar=k.gcq[:, c:c + 1], in1=rs[:, 0, :], op0=ALU.mult, op1=ALU.mult),
                 reads=[xk, "rs", "par"], writes=["cqn"])
        P.op("dve", lambda e, x=x, sl=sl: e.scalar_tensor_tensor(out=k.kvT[:, sl], in0=x[:, 2, :], scalar=k.gckv[:, 0:1], in1=rs[:, 1, :], op0=ALU.mult, op1=ALU.mult),
             reads=[xk, "rs", "par"], writes=["kvT"])
        P.op("dve", lambda e, x=x, sl=sl: e.scalar_tensor_tensor(out=k.kidxT[:, sl], in0=x[:, 3, :], scalar=k.gkidx[:, 0:1], in1=rs[:, 2, :], op0=ALU.mult, op1=ALU.mult),
             reads=[xk, "rs", "par"], writes=["kidxT"])
        for h in range(8):
            bank = 4 + oc % 4; pk = "ps%d" % bank
            for c in range(2):
                P.op("pe", lambda e, h=h, c=c, bank=bank: e.matmul(ps[bank][:, :], lhsT=wq[:, c, h * 128:(h + 1) * 128], rhs=cqn[:, c, :], start=(c == 0), stop=(c == 1)),
                     reads=["wq", "cqn"], writes=[pk])
            o = ob[oc % 4]; ok = "obB%d" % (oc % 4)
            P.op("act", lambda e, o=o, bank=bank: e.activation(out=o, in_=ps[bank][:, :], func=AF.Copy, scale=ATTN_SCALE), reads=[pk], writes=[ok])
            P.dma("sp", lambda e, o=o, h=h, b=b: e.dma_start(out=qT_d[b * 4:(b + 1) * 4, :, h, :].rearrange("t r q -> r t q"), in_=o.rearrange("p (t q) -> p t q", t=4)),
                  reads=[ok], key=ok)
            oc += 1
        for j in range(4):
            bank = 4 + oc % 4; pk = "ps%d" % bank
            for c in range(2):
                P.op("pe", lambda e, j=j, c=c, bank=bank: e.matmul(ps[bank][:, :], lhsT=wi[:, c, j * 128:(j + 1) * 128], rhs=cqn[:, c, :], start=(c == 0), stop=(c == 1)),
                     reads=["wi", "cqn"], writes=[pk])
            o = ob[oc % 4]; ok = "obB%d" % (oc % 4)
            P.op("dve", lambda e, o=o, bank=bank: e.tensor_copy(o, ps[bank][:, :]), reads=[pk], writes=[ok])
            P.dma("sp", lambda e, o=o, j=j, b=b: e.dma_start(out=qidx_d[b * 4:(b + 1) * 4, :, j, :].rearrange("t r q -> r t q"), in_=o.rearrange("p (t q) -> p t q", t=4)),
                  reads=[ok], key=ok)
            oc += 1
    P.barrier()
    A.release(m0)


def phaseC(k, l, I, NITER=16):
    A, P, ps = k.A, k.P, k.ps
    qT_d = k.dr["qT_d"]; qidx_d = k.dr["qidx_d"]
    yaT_d = dram(k, "yaT_d", [4, 128, S], BF16)
    m0 = A.mark()
    qs = [A.alloc(8 * 128, BF16) for _ in range(2)]
    qi = [A.alloc(4 * 128, BF16).rearrange("p (j q) -> p j q", j=4) for _ in range(2)]
    score = [A.alloc(S, F32) for _ in range(2)]
    mb = [A.alloc(S, BF16) for _ in range(2)]
    junk = A.alloc(S, BF16)
    rb = [A.alloc(512, F32) for _ in range(3)]
    pT = [A.alloc(512, BF16) for _ in range(4)]
    on = A.alloc(8 * 128, BF16).rearrange("p (h r) -> p h r", h=8)
    onT = A.alloc(8 * 128, BF16).rearrange("p (h q) -> p h q", h=8)
    yo = [A.alloc(4 * 128, BF16).rearrange("p (j q) -> p j q", j=4) for _ in range(2)]
    ident4 = A.alloc(512, BF16)
    bs = [A.alloc(64, F32) for _ in range(2)]
    pw = A.alloc(NITER, F32)
    rden = A.alloc(8, F32)
    cm = A.alloc(2, F32)
    P.op("dve", lambda e: e.memset(cm, MASKV), writes=["cm"])
    for i in range(4):
        P.op("dve", lambda e, i=i: e.tensor_copy(ident4[:, i * 128:(i + 1) * 128], k.identb), reads=["identb"], writes=["ident4"])
    for i in range(NITER):
        P.op("dve", lambda e, i=i: e.memset(pw[:, i:i + 1], 0.5 ** (i + 1)), writes=["pw"])
    def oacc(h):
        return ps[h // 3][:, (h % 3) * 129:(h % 3) * 129 + 129]
    rc = [0]

    def indexer(qt):
        b = qt % 2
        sc = score[b]; sk = "score%d" % b
        nk = (qt + 1) * 128
        P.dma("sp", lambda e: e.dma_start(out=qs[b], in_=qT_d[qt].rearrange("r h q -> r (h q)")), writes=["qs%d" % b], key="qs%d" % b)
        P.dma("sp", lambda e: e.dma_start(out=qi[b], in_=qidx_d[qt]), writes=["qi%d" % b], key="qi%d" % b)
        for c0 in range(0, nk, 512):
            wd = min(512, nk - c0)
            for h in range(8):
                bank = 3 + rc[0] % 2; pk = "ps%d" % bank
                p0 = (h % 2) * 64
                P.op("pe", lambda e, h=h, p0=p0, bank=bank, c0=c0, wd=wd: e.matmul(ps[bank][:, 0:wd], lhsT=qi[b][p0:p0 + 64, h // 2, :], rhs=k.kidxT[p0:p0 + 64, c0:c0 + wd], start=True, stop=True),
                     reads=["qi%d" % b, "kidxT"], writes=[pk])
                r = rb[rc[0] % 3]; rk = "rb%d" % (rc[0] % 3)
                P.op("act", lambda e, r=r, bank=bank, wd=wd: e.activation(out=r[:, 0:wd], in_=ps[bank][:, 0:wd], func=AF.Relu), reads=[pk], writes=[rk])
                if h == 0:
                    P.op("dve", lambda e, r=r, c0=c0, wd=wd, h=h: e.tensor_scalar(sc[:, c0:c0 + wd], r[:, 0:wd], k.widx[:, qt, h:h + 1], None, op0=ALU.mult),
                         reads=[rk, "widx"], writes=[sk])
                else:
                    P.op("dve", lambda e, r=r, c0=c0, wd=wd, h=h: e.scalar_tensor_tensor(out=sc[:, c0:c0 + wd], in0=r[:, 0:wd], scalar=k.widx[:, qt, h:h + 1], in1=sc[:, c0:c0 + wd], op0=ALU.mult, op1=ALU.add),
                         reads=[rk, "widx", sk], writes=[sk])
                rc[0] += 1
        P.op("pool", lambda e: e.tensor_tensor(sc[:, qt * 128:(qt + 1) * 128], sc[:, qt * 128:(qt + 1) * 128], k.cneg, op=ALU.add), reads=[sk, "cneg"], writes=[sk])
        s = bs[b]; bk = "bs%d" % b
        lo = s[:, 0:1]; w0 = s[:, 1:2]; mid = s[:, 2:3]; cnt = s[:, 3:4]; tmp = s[:, 4:5]; wi_ = s[:, 8:8 + NITER]
        if qt < 2:
            P.op("dve", lambda e: e.memset(lo, -1e29), writes=[bk])
        else:
            P.op("dve", lambda e: e.tensor_reduce(out=w0, in_=sc[:, 0:nk], axis=AX.X, op=ALU.max), reads=[sk], writes=[bk])
            P.op("dve", lambda e: e.tensor_reduce(out=lo, in_=sc[:, 0:qt * 128], axis=AX.X, op=ALU.min), reads=[sk], writes=[bk])
            P.op("dve", lambda e: e.tensor_scalar(lo, lo, -1.0, None, op0=ALU.add), reads=[bk], writes=[bk])
            P.op("dve", lambda e: e.tensor_tensor(w0, w0, lo, op=ALU.subtract), reads=[bk], writes=[bk])
            P.op("dve", lambda e: e.tensor_scalar(wi_, pw, w0, None, op0=ALU.mult), reads=[bk, "pw"], writes=[bk])
            for it in range(NITER):
                P.op("dve", lambda e, it=it: e.tensor_tensor(mid, lo, wi_[:, it:it + 1], op=ALU.add), reads=[bk], writes=[bk])
                P.op("dve", lambda e: e.tensor_scalar(junk[:, 0:nk], sc[:, 0:nk], mid, None, op0=ALU.is_gt, op1=ALU.add, accum_out=cnt), reads=[sk, bk], writes=[bk, "junk"])
                P.op("dve", lambda e, it=it: e.scalar_tensor_tensor(out=tmp, in0=cnt, scalar=256.0, in1=wi_[:, it:it + 1], op0=ALU.is_ge, op1=ALU.mult), reads=[bk], writes=[bk])
                P.op("dve", lambda e: e.tensor_tensor(lo, lo, tmp, op=ALU.add), reads=[bk], writes=[bk])
        P.op("dve", lambda e: e.tensor_scalar(mb[b][:, 0:nk], sc[:, 0:nk], lo, cm[:, 0:1], op0=ALU.is_le, op1=ALU.mult), reads=[sk, bk, "cm"], writes=["mb%d" % b])

    pc = [0]

    def attention(qt):
        b = qt % 2
        q = qs[b]
        for kb in range(qt + 1):
            for hg in range(2):
                bank = 5 + pc[0] % 2; pk = "ps%d" % bank
                P.op("pe", lambda e, kb=kb, hg=hg, bank=bank: e.matmul(ps[bank][:, :], lhsT=k.kvT[:, kb * 128:(kb + 1) * 128], rhs=q[:, hg * 512:(hg + 1) * 512], start=True, stop=False),
                     reads=["kvT", "qs%d" % b], writes=[pk])
                P.op("pe", lambda e, kb=kb, bank=bank: e.matmul(ps[bank][:, :], lhsT=mb[b][:, kb * 128:(kb + 1) * 128], rhs=ident4, start=False, stop=True),
                     reads=["mb%d" % b, "ident4"], writes=[pk])
                p = pT[pc[0] % 4]; pk2 = "pT%d" % (pc[0] % 4)
                P.op("act", lambda e, p=p, bank=bank: e.activation(out=p, in_=ps[bank][:, :], func=AF.Exp), reads=[pk], writes=[pk2])
                for hh in range(4):
                    h = hg * 4 + hh
                    P.op("pe", lambda e, p=p, hh=hh, h=h, kb=kb: e.matmul(oacc(h), lhsT=p[:, hh * 128:(hh + 1) * 128], rhs=k.kvaug[:, kb, 0:129], start=(kb == 0 and h % 3 == 0), stop=(kb == qt), skip_group_check=True),
                         reads=[pk2, "kvaug"], writes=["ps%d" % (h // 3)])
                pc[0] += 1
        for bnk in range(3):
            nh = 3 if bnk < 2 else 2
            v = ps[bnk][:, 0:nh * 129].rearrange("p (h c) -> p h c", c=129)
            P.op("dve", lambda e, v=v, bnk=bnk, nh=nh: e.reciprocal(rden[:, bnk * 3:bnk * 3 + nh], v[:, :, 128]), reads=["ps%d" % bnk], writes=["rden"])
            P.op("dve", lambda e, v=v, bnk=bnk, nh=nh: e.tensor_tensor(on[:, bnk * 3:bnk * 3 + nh, :], v[:, :, 0:128], rden[:, bnk * 3:bnk * 3 + nh].unsqueeze(2).to_broadcast([128, nh, 128]), op=ALU.mult),
                 reads=["ps%d" % bnk, "rden"], writes=["on"])
        if qt == 1 and "dbg_on" in k.debug:
            dbg = dram(k, "dbg_on", [128, 1024], BF16)
            P.dma("sp", lambda e: e.dma_start(out=dbg, in_=on.rearrange("p h r -> p (h r)")), reads=["on"], key="dbg1")
            dbg2 = dram(k, "dbg_mb", [128, 256], BF16)
            P.dma("sp", lambda e: e.dma_start(out=dbg2, in_=mb[b][:, 0:256]), reads=["mb%d" % b], key="dbg2")
            dbg3 = dram(k, "dbg_sc", [128, 256], F32)
            P.dma("sp", lambda e: e.dma_start(out=dbg3, in_=score[b][:, 0:256]), reads=["score%d" % b], key="dbg3")
            dbg4 = dram(k, "dbg_kv", [128, 32 * 132], BF16)
            P.dma("sp", lambda e: e.dma_start(out=dbg4, in_=k.kvaug.rearrange("p t c -> p (t c)")), reads=["kvaug"], key="dbg4")
            dbg5 = dram(k, "dbg_qs", [128, 1024], BF16)
            P.dma("sp", lambda e: e.dma_start(out=dbg5, in_=q), reads=["qs%d" % b], key="dbg5")
            dbg6 = dram(k, "dbg_kvT", [128, S], BF16)
            P.dma("sp", lambda e: e.dma_start(out=dbg6, in_=k.kvT), reads=["kvT"], key="dbg6")
        tb = ps[7][:, :].bitcast(BF16)
        for h in range(8):
            P.op("pe", lambda e, h=h: e.transpose(tb[:, h * 128:(h + 1) * 128], on[:, h, :], k.identb), reads=["on", "identb"], writes=["ps7"])
        P.op("act", lambda e: e.activation(out=onT.rearrange("p h q -> p (h q)"), in_=tb, func=AF.Copy), reads=["ps7"], writes=["onT"])
        for j in range(4):
            for two in range(2):
                h = j * 2 + two
                P.op("pe", lambda e, j=j, two=two, h=h: e.matmul(ps[7][:, j * 128:(j + 1) * 128], lhsT=k.wv[:, h, :], rhs=onT[:, h, :], start=(two == 0), stop=(two == 1)),
                     reads=["wv", "onT"], writes=["ps7"])
        y = yo[qt % 2]; yk = "yo%d" % (qt % 2)
        P.op("dve", lambda e: e.tensor_copy(y.rearrange("p j q -> p (j q)"), ps[7][:, :]), reads=["ps7"], writes=[yk])
        P.dma("sp", lambda e: e.dma_start(out=yaT_d[:, :, qt * 128:(qt + 1) * 128].rearrange("j p q -> p j q"), in_=y), reads=[yk], key=yk)

    indexer(0)
    for qt in range(NT):
        if qt + 1 < NT:
            indexer(qt + 1)
        attention(qt)
    P.barrier()
    A.release(m0)


def phaseD(k, l, I):
    A, P, ps = k.A, k.P, k.ps
    qrec_fm = k.dr["qrec_fm"]; frec_fm = k.dr["frec_fm"]; tm_out = k.dr["tm_out"]
    yrT_d = dram(k, "yrT_d", [4, 128, S], BF16)
    m0 = A.mark()
    NB = 512
    def fm(dt=F32):
        return A.alloc(4 * NB, dt).rearrange("p (j t) -> p j t", j=4)
    z = fm(); qr = fm(); e = fm(); t1 = fm(); t2 = fm(); Acum = fm(); kk = fm(); qq = fm()
    qt_ = fm(BF16); kt_ = fm(BF16); qhA = fm(BF16); qhB = fm(BF16); kh = fm(BF16)
    cmf = k.cmf
    grb = A.alloc(512, F32)
    vt = A.alloc(4 * 512, BF16).rearrange("p (t c) -> p t c", t=4)
    ogt = A.alloc(4 * 512, F32).rearrange("p (t c) -> p t c", t=4)
    vtf = A.alloc(4 * 512, F32).rearrange("p (t c) -> p t c", t=4)
    khT = A.alloc(512, BF16)
    Pm = A.alloc(8 * 128, BF16).rearrange("p (h t) -> p h t", h=8)
    state = A.alloc(4 * 64, F32).rearrange("p (j v) -> p j v", j=4)
    stmp = A.alloc(4 * 64, F32).rearrange("p (j v) -> p j v", j=4)
    sbf = [A.alloc(4 * 64, BF16).rearrange("p (j v) -> p j v", j=4) for _ in range(4)]
    decay = A.alloc(32, F32)
    osb = A.alloc(512, F32); osq = A.alloc(512, F32); oss = A.alloc(16, F32)
    sg = A.alloc(512, F32)
    yb = A.alloc(512, BF16)
    yT = [A.alloc(512, BF16) for _ in range(2)]
    P.dma("sp", lambda e_: e_.dma_start(out=osb[:, 0:64], in_=I["g_rec"][l].partition_broadcast(128)), writes=["osb"], key="grb")
    P.op("dve", lambda e_: e_.tensor_copy(grb.rearrange("p (h v) -> p h v", h=8), osb[:, 0:64].unsqueeze(1).to_broadcast([128, 8, 64])), reads=["osb"], writes=["grb"])
    P.op("dve", lambda e_: e_.memset(state, 0.0), writes=["state"])
    P.op("dve", lambda e_: e_.memset(sbf[0], 0.0), writes=["sbf0"])
    P.op("dve", lambda e_: e_.memset(qhA, 0.0), writes=["qhA"])
    P.op("dve", lambda e_: e_.memset(qhB, 0.0), writes=["qhB"])
    sv = [0]
    z2 = z.rearrange("p j t -> p (j t)"); e2 = e.rearrange("p j t -> p (j t)"); t12 = t1.rearrange("p j t -> p (j t)"); t22 = t2.rearrange("p j t -> p (j t)")
    A2 = Acum.rearrange("p j t -> p (j t)")
    def ch(x):
        return x.rearrange("p j (c t) -> p (j c) t", t=64)
    for b in range(S // NB):
        sl = slice(b * NB, (b + 1) * NB)
        P.dma("sp", lambda e_, sl=sl: e_.dma_start(out=z, in_=frec_fm[:, sl].rearrange("(j p) t -> p j t", p=128)), writes=["z"], key="zD")
        P.dma("sp", lambda e_, sl=sl: e_.dma_start(out=qr, in_=qrec_fm[:, sl].rearrange("(j p) t -> p j t", p=128)), writes=["qr"], key="qrD")
        P.dma("sp", lambda e_, sl=sl: e_.dma_start(out=vtf, in_=tm_out[sl, 136:648].rearrange("(t p) c -> p t c", p=128)), writes=["vtf"], key="vtD")
        P.op("pool", lambda e_: e_.tensor_copy(vt, vtf), reads=["vtf"], writes=["vt"])
        P.dma("sp", lambda e_, sl=sl: e_.dma_start(out=ogt, in_=tm_out[sl, 648:1160].rearrange("(t p) c -> p t c", p=128)), writes=["ogt"], key="ogD")
        P.op("act", lambda e_: e_.activation(out=e, in_=z, func=AF.Exp, scale=-1.0), reads=["z"], writes=["e"])
        for j in range(4):
            P.op("dve", lambda e_, j=j: e_.tensor_scalar(t1[:, j, :], e[:, j, :], k.lb[:, j:j + 1], 1.0, op0=ALU.mult, op1=ALU.add), reads=["e", "sm"], writes=["t1"])
        P.op("dve", lambda e_: e_.tensor_scalar(t2, e, 1.0, None, op0=ALU.add), reads=["e"], writes=["t2"])
        P.op("act", lambda e_: e_.activation(out=t1, in_=t1, func=AF.Ln), reads=["t1"], writes=["t1"])
        P.op("act", lambda e_: e_.activation(out=z, in_=t2, func=AF.Ln), reads=["t2", "z"], writes=["z"])
        P.op("dve", lambda e_: e_.tensor_tensor(t1, t1, z, op=ALU.subtract), reads=["t1", "z"], writes=["t1"])
        P.op("dve", lambda e_: e_.reciprocal(t2, t2), reads=["t2"], writes=["t2"])
        for j in range(4):
            P.op("dve", lambda e_, j=j: e_.scalar_tensor_tensor(out=kk[:, j, :], in0=e[:, j, :], scalar=k.oml[:, j:j + 1], in1=t2[:, j, :], op0=ALU.mult, op1=ALU.mult), reads=["e", "t2", "sm"], writes=["kk"])
        srcs = [t1, Acum]
        for si, sh in enumerate([1, 2, 4, 8, 16, 32]):
            a_ = ch(srcs[si % 2]); b_ = ch(srcs[(si + 1) % 2])
            P.op("dve", lambda e_, a_=a_, b_=b_, sh=sh: e_.tensor_tensor(b_[:, :, sh:64], a_[:, :, sh:64], a_[:, :, 0:64 - sh], op=ALU.add), reads=["t1", "Acum"], writes=["t1", "Acum"])
            P.op("pool", lambda e_, a_=a_, b_=b_, sh=sh: e_.tensor_copy(b_[:, :, 0:sh], a_[:, :, 0:sh]), reads=["t1", "Acum"], writes=["t1", "Acum"])
        P.op("pool", lambda e_: e_.tensor_copy(Acum, t1), reads=["t1", "Acum"], writes=["t1", "Acum"])
        P.op("act", lambda e_: e_.activation(out=e, in_=qr, func=AF.Exp, scale=-1.0), reads=["qr", "kk"], writes=["e"])
        P.op("dve", lambda e_: e_.tensor_scalar(e, e, 1.0, None, op0=ALU.add), reads=["e"], writes=["e"])
        P.op("dve", lambda e_: e_.reciprocal(e, e), reads=["e"], writes=["e"])
        P.op("dve", lambda e_: e_.tensor_tensor(qq, qr, e, op=ALU.mult), reads=["e", "qr"], writes=["qq"])
        Ac = ch(Acum)
        P.op("dve", lambda e_: e_.tensor_tensor(ch(t1), Ac, Ac[:, :, 31:32].to_broadcast([128, 32, 64]), op=ALU.subtract), reads=["Acum", "t1"], writes=["t1"])
        P.op("dve", lambda e_: e_.tensor_scalar(t1, t1, -40.0, 40.0, op0=ALU.max, op1=ALU.min), reads=["t1"], writes=["t1"])
        P.op("act", lambda e_: e_.activation(out=t2, in_=t1, func=AF.Exp), reads=["t1"], writes=["t2"])
        P.op("dve", lambda e_: e_.tensor_tensor(qt_, qq, t2, op=ALU.mult), reads=["qq", "t2"], writes=["qt_"])
        P.op("act", lambda e_: e_.activation(out=t2, in_=t1, func=AF.Exp, scale=-1.0), reads=["t1", "qt_"], writes=["t2"])
        P.op("dve", lambda e_: e_.tensor_tensor(kt_, kk, t2, op=ALU.mult), reads=["kk", "t2"], writes=["kt_"])
        P.op("act", lambda e_: e_.activation(out=t2, in_=Acum, func=AF.Exp), reads=["Acum", "kt_"], writes=["t2"])
        def eo(x, par):
            return x.rearrange("p j (c two t) -> p j c two t", two=2, t=64)[:, :, :, par, :]
        P.op("dve", lambda e_: e_.tensor_tensor(eo(qhA, 0), eo(qq, 0), eo(t2, 0), op=ALU.mult), reads=["qq", "t2"], writes=["qhA"])
        P.op("dve", lambda e_: e_.tensor_tensor(eo(qhB, 1), eo(qq, 1), eo(t2, 1), op=ALU.mult), reads=["qq", "t2"], writes=["qhB"])
        P.op("act", lambda e_: e_.activation(out=decay, in_=Ac[:, :, 63], func=AF.Exp), reads=["Acum"], writes=["decay"])
        P.op("dve", lambda e_: e_.tensor_tensor(ch(t1), Ac[:, :, 63:64].to_broadcast([128, 32, 64]), Ac, op=ALU.subtract), reads=["Acum", "t1"], writes=["t1"])
        P.op("act", lambda e_: e_.activation(out=t2, in_=t1, func=AF.Exp), reads=["t1", "qhA", "qhB"], writes=["t2"])
        P.op("dve", lambda e_: e_.tensor_tensor(kh, kk, t2, op=ALU.mult), reads=["kk", "t2"], writes=["kh"])
        for tt in range(4):
            tsl = slice(tt * 128, (tt + 1) * 128)
            tb = ps[7][:, :].bitcast(BF16)
            for j in range(4):
                P.op("pe", lambda e_, j=j, tsl=tsl: e_.transpose(tb[:, j * 128:(j + 1) * 128], kh[:, j, tsl], k.identb), reads=["kh", "identb"], writes=["ps7"])
            P.op("act", lambda e_: e_.activation(out=khT, in_=tb[:, 0:512], func=AF.Copy), reads=["ps7"], writes=["khT"])
            for h in range(8):
                j = h // 2; p0 = (h % 2) * 64
                bank = 5 + h % 2
                P.op("pe", lambda e_, h=h, j=j, p0=p0, bank=bank, tsl=tsl: e_.matmul(ps[bank][:, j * 128:(j + 1) * 128], lhsT=kt_[p0:p0 + 64, j, tsl], rhs=qt_[p0:p0 + 64, j, tsl], start=True, stop=True),
                     reads=["kt_", "qt_"], writes=["ps%d" % bank])
            for g in range(2):
                P.op("dve", lambda e_, g=g: e_.tensor_tensor(Pm[:, g * 4:(g + 1) * 4, :], ps[5 + g][:, :].rearrange("p (h t) -> p h t", h=4), cmf.unsqueeze(1).to_broadcast([128, 4, 128]), op=ALU.mult),
                     reads=["ps%d" % (5 + g), "cmf"], writes=["Pm"])
            svA = sv[0]
            for half in range(2):
                c = tt * 2 + half
                hs = slice(half * 64, (half + 1) * 64)
                for j in range(4):
                    P.op("pe", lambda e_, j=j, hs=hs, tt=tt: e_.matmul(ps[4][:, j * 128:(j + 1) * 128], lhsT=khT[hs, j * 128:(j + 1) * 128], rhs=vt[hs, tt, j * 128:(j + 1) * 128], start=True, stop=True),
                         reads=["khT", "vt"], writes=["ps4"])
                dcol = [jj * 8 + c for jj in range(4)]
                dv = decay.rearrange("p (j c) -> p j c", j=4)[:, :, c:c + 1]
                P.op("dve", lambda e_, dv=dv: e_.tensor_tensor(stmp, state, dv.to_broadcast([128, 4, 64]), op=ALU.mult), reads=["state", "decay"], writes=["stmp"])
                pv = ps[4][:, :].rearrange("p (j x) -> p j x", j=4)
                P.op("dve", lambda e_, pv=pv: e_.tensor_tensor(state[0:64], stmp[0:64], pv[0:64, :, 0:64], op=ALU.add), reads=["stmp", "ps4"], writes=["state"])
                P.op("dve", lambda e_, pv=pv: e_.tensor_tensor(state[64:128], stmp[64:128], pv[64:128, :, 64:128], op=ALU.add), reads=["stmp", "ps4"], writes=["state"])
                sv[0] += 1
                sb_ = sbf[sv[0] % 4]
                P.op("act", lambda e_, sb_=sb_: e_.activation(out=sb_, in_=state, func=AF.Copy), reads=["state"], writes=["sbf%d" % (sv[0] % 4)])
            s0 = sbf[svA % 4]; s0k = "sbf%d" % (svA % 4)
            s1 = sbf[(svA + 1) % 4]; s1k = "sbf%d" % ((svA + 1) % 4)
            for h in range(8):
                j = h // 2; p0 = (h % 2) * 64
                oo = ps[3][:, h * 64:(h + 1) * 64]
                P.op("pe", lambda e_, h=h, oo=oo, tt=tt: e_.matmul(oo, lhsT=Pm[:, (h % 2) * 4 + h // 2, :], rhs=vt[:, tt, h * 64:(h + 1) * 64], start=True, stop=False), reads=["Pm", "vt"], writes=["ps3"])
                P.op("pe", lambda e_, j=j, p0=p0, oo=oo, tsl=tsl, s0=s0: e_.matmul(oo, lhsT=qhA[p0:p0 + 64, j, tsl], rhs=s0[p0:p0 + 64, j, :], start=False, stop=False), reads=["qhA", s0k], writes=["ps3"])
                P.op("pe", lambda e_, j=j, p0=p0, oo=oo, tsl=tsl, s1=s1: e_.matmul(oo, lhsT=qhB[p0:p0 + 64, j, tsl], rhs=s1[p0:p0 + 64, j, :], start=False, stop=True), reads=["qhB", s1k], writes=["ps3"])
            P.op("act", lambda e_: e_.activation(out=osb, in_=ps[3][:, :], func=AF.Copy), reads=["ps3"], writes=["osb"])
            P.op("dve", lambda e_: e_.tensor_tensor(osq, osb, osb, op=ALU.mult), reads=["osb"], writes=["osq"])
            P.op("dve", lambda e_: e_.tensor_reduce(out=oss[:, 0:8], in_=osq.rearrange("p (h v) -> p h v", h=8), axis=AX.X, op=ALU.add), reads=["osq"], writes=["oss"])
            P.op("dve", lambda e_: e_.tensor_scalar(oss[:, 0:8], oss[:, 0:8], 1.0 / 64, EPS, op0=ALU.mult, op1=ALU.add), reads=["oss"], writes=["oss"])
            P.op("act", lambda e_: e_.activation(out=oss[:, 0:8], in_=oss[:, 0:8], func=AF.Ln), reads=["oss"], writes=["oss"])
            P.op("act", lambda e_: e_.activation(out=oss[:, 0:8], in_=oss[:, 0:8], func=AF.Exp, scale=-0.5), reads=["oss"], writes=["oss"])
            P.op("dve", lambda e_: e_.tensor_tensor(osq.rearrange("p (h v) -> p h v", h=8), osb.rearrange("p (h v) -> p h v", h=8), oss[:, 0:8].unsqueeze(2).to_broadcast([128, 8, 64]), op=ALU.mult),
                 reads=["osb", "oss", "osq"], writes=["osq"])
            P.op("dve", lambda e_: e_.tensor_tensor(osq, osq, grb, op=ALU.mult), reads=["osq", "grb"], writes=["osq"])
            P.op("act", lambda e_, tt=tt: e_.activation(out=sg, in_=ogt[:, tt, :], func=AF.Exp, scale=-1.0), reads=["ogt"], writes=["sg"])
            P.op("dve", lambda e_: e_.tensor_scalar(sg, sg, 1.0, None, op0=ALU.add), reads=["sg"], writes=["sg"])
            P.op("dve", lambda e_: e_.reciprocal(sg, sg), reads=["sg"], writes=["sg"])
            P.op("dve", lambda e_, tt=tt: e_.tensor_tensor(sg, sg, ogt[:, tt, :], op=ALU.mult), reads=["sg", "ogt"], writes=["sg"])
            P.op("dve", lambda e_: e_.tensor_tensor(yb, osq, sg, op=ALU.mult), reads=["osq", "sg"], writes=["yb"])
            for j in range(4):
                P.op("pe", lambda e_, j=j: e_.transpose(tb[:, 512 + j * 128:512 + (j + 1) * 128], yb[:, j * 128:(j + 1) * 128], k.identb), reads=["yb", "identb"], writes=["ps7"])
            t = b * 4 + tt
            y = yT[t % 2]; yk = "yTD%d" % (t % 2)
            P.op("act", lambda e_, y=y: e_.activation(out=y, in_=tb[:, 512:1024], func=AF.Copy), reads=["ps7"], writes=[yk])
            P.dma("sp", lambda e_, y=y, t=t: e_.dma_start(out=yrT_d[:, :, t * 128:(t + 1) * 128].rearrange("j p q -> p j q"), in_=y.rearrange("p (j q) -> p j q", j=4)), reads=[yk], key=yk)
    P.barrier()
    A.release(m0)


def phaseE(k, l, I, xsrc, xdst):
    A, P, ps = k.A, k.P, k.ps
    yaT_d = k.dr["yaT_d"]; yrT_d = k.dr["yrT_d"]; gates_fm = k.dr["gates_fm"]
    m0 = A.mark()
    wa = A.alloc(4 * 1024, BF16).rearrange("p (k c) -> p k c", k=4)
    wr = A.alloc(4 * 1024, BF16).rearrange("p (k c) -> p k c", k=4)
    wo = A.alloc(8 * 1024, BF16).rearrange("p (k c) -> p k c", k=8)
    P.dma("pool", lambda e: e.dma_start(out=wa, in_=I["w_branch_a"][l].rearrange("(k p) c -> p k c", p=128)), writes=["wa"], key="wa")
    P.dma("pool", lambda e: e.dma_start(out=wr, in_=I["w_branch_r"][l].rearrange("(k p) c -> p k c", p=128)), writes=["wr"], key="wr")
    P.dma("pool", lambda e: e.dma_start(out=wo, in_=I["w_out"][l].rearrange("(k p) c -> p k c", p=128)), writes=["wo"], key="wo")
    ya = [A.alloc(4 * 512, BF16).rearrange("p (k t) -> p k t", k=4) for _ in range(2)]
    yr = [A.alloc(4 * 512, BF16).rearrange("p (k t) -> p k t", k=4) for _ in range(2)]
    gt = [A.alloc(16 * 512, BF16).rearrange("p (k t) -> p k t", k=16) for _ in range(2)]
    mT = A.alloc(8 * 512, BF16).rearrange("p (k t) -> p k t", k=8)
    m1 = [A.alloc(512, F32) for _ in range(2)]
    m2 = [A.alloc(512, F32) for _ in range(2)]
    xb = [A.alloc(1024, F32) for _ in range(2)]
    tb_ = [A.alloc(1024, F32) for _ in range(2)]
    cnt = 0
    for b in range(S // 512):
        sl = slice(b * 512, (b + 1) * 512)
        i2 = b % 2
        P.dma("sp", lambda e, i2=i2, sl=sl: e.dma_start(out=ya[i2], in_=yaT_d[:, :, sl].rearrange("j p t -> p j t")), writes=["yaE%d" % i2], key="yaE%d" % i2)
        P.dma("sp", lambda e, i2=i2, sl=sl: e.dma_start(out=yr[i2], in_=yrT_d[:, :, sl].rearrange("j p t -> p j t")), writes=["yrE%d" % i2], key="yrE%d" % i2)
        P.dma("sp", lambda e, i2=i2, sl=sl: e.dma_start(out=gt[i2], in_=gates_fm[:, sl].rearrange("(j p) t -> p j t", p=128)), writes=["gtE%d" % i2], key="gtE%d" % i2)
        for cb in range(8):
            ba = cnt % 2; bb = 2 + cnt % 2
            for kc in range(4):
                P.op("pe", lambda e, kc=kc, cb=cb, ba=ba, i2=i2: e.matmul(ps[ba][:, :], lhsT=wa[:, kc, cb * 128:(cb + 1) * 128], rhs=ya[i2][:, kc, :], start=(kc == 0), stop=(kc == 3)),
                     reads=["wa", "yaE%d" % i2], writes=["ps%d" % ba])
            for kc in range(4):
                P.op("pe", lambda e, kc=kc, cb=cb, bb=bb, i2=i2: e.matmul(ps[bb][:, :], lhsT=wr[:, kc, cb * 128:(cb + 1) * 128], rhs=yr[i2][:, kc, :], start=(kc == 0), stop=(kc == 3)),
                     reads=["wr", "yrE%d" % i2], writes=["ps%d" % bb])
            a1 = m1[cnt % 2]; a2 = m2[cnt % 2]
            P.op("dve", lambda e, a1=a1, ba=ba, cb=cb, i2=i2: e.tensor_tensor(a1, ps[ba][:, :], gt[i2][:, cb, :], op=ALU.mult), reads=["ps%d" % ba, "gtE%d" % i2], writes=["m1%d" % (cnt % 2)])
            P.op("dve", lambda e, a2=a2, bb=bb, cb=cb, i2=i2: e.tensor_tensor(a2, ps[bb][:, :], gt[i2][:, 8 + cb, :], op=ALU.mult), reads=["ps%d" % bb, "gtE%d" % i2], writes=["m2%d" % (cnt % 2)])
            P.op("pool", lambda e, a1=a1, a2=a2, cb=cb: e.tensor_tensor(mT[:, cb, :], a1, a2, op=ALU.add), reads=["m1%d" % (cnt % 2), "m2%d" % (cnt % 2)], writes=["mT"])
            cnt += 1
        for tt in range(4):
            t = b * 4 + tt
            x = xb[t % 2]; xk = "xbE%d" % (t % 2)
            tq = tb_[t % 2]; tk = "tbE%d" % (t % 2)
            P.dma("sp", lambda e, x=x, t=t: e.dma_start(out=x, in_=xsrc[t * 128:(t + 1) * 128, :]), writes=[xk], key=xk)
            for half in range(2):
                bank = 4 + (t * 2 + half) % 4
                for kc in range(8):
                    P.op("pe", lambda e, kc=kc, half=half, bank=bank, tt=tt: e.matmul(ps[bank][:, :], lhsT=mT[:, kc, tt * 128:(tt + 1) * 128], rhs=wo[:, kc, half * 512:(half + 1) * 512], start=(kc == 0), stop=(kc == 7)),
                         reads=["mT", "wo"], writes=["ps%d" % bank])
                P.op("dve", lambda e, tq=tq, half=half, bank=bank: e.tensor_tensor(tq[:, half * 512:(half + 1) * 512], ps[bank][:, :], k.gtbc[0][:, half * 512:(half + 1) * 512], op=ALU.mult),
                     reads=["ps%d" % bank, "gtbc0"], writes=[tk])
            P.op("pool", lambda e, tq=tq, x=x: e.tensor_tensor(tq, tq, x, op=ALU.add), reads=[tk, xk], writes=[tk])
            P.dma("sp", lambda e, tq=tq, t=t: e.dma_start(out=xdst[t * 128:(t + 1) * 128, :], in_=tq), reads=[tk], key=tk)
    P.barrier()
    A.release(m0)


def phaseF(k, l, I, xsrc, xdst):
    A, P, ps = k.A, k.P, k.ps
    m0 = A.mark()
    h2T = A.alloc(8 * S, BF16).rearrange("p (k t) -> p k t", k=8)
    gate = A.alloc(NT * 32, F32).rearrange("p (t c) -> p t c", t=NT)
    k.xb = [A.alloc(1024, F32) for _ in range(2)]
    m1_ = A.mark()
    hf = [A.alloc(8 * 128, F32).rearrange("p (k t) -> p k t", k=8) for _ in range(2)]
    wrt = A.alloc(8 * 36, F32).rearrange("p (k c) -> p k c", k=8)
    rb = A.alloc(36, F32)
    lg = A.alloc(NT * 36, F32).rearrange("p (t c) -> p t c", t=NT)
    k.xn = [A.alloc(1024, F32) for _ in range(2)]
    k.nst = [A.alloc(2, F32) for _ in range(2)]
    P.dma("sp", lambda e: e.dma_start(out=wrt[:, :, 0:4], in_=I["w_grp"][l].rearrange("(k p) c -> p k c", p=128)), writes=["wrt"], key="wrt")
    P.dma("sp", lambda e: e.dma_start(out=wrt[:, :, 4:36], in_=I["w_exp_router"][l].rearrange("(k p) c -> p k c", p=128)), writes=["wrt"], key="wrt")
    P.dma("sp", lambda e: e.dma_start(out=rb[:, 0:4], in_=I["b_grp"][l].partition_broadcast(128)), writes=["rbF"], key="rbF")
    P.dma("sp", lambda e: e.dma_start(out=rb[:, 4:36], in_=I["b_exp_router"][l].partition_broadcast(128)), writes=["rbF"], key="rbF")
    for t in range(NT):
        b0 = norm_transpose(k, xsrc, t, None, None, None, None, None)
        f = hf[t % 2]; fk = "hfF%d" % (t % 2)
        for kc in range(8):
            bank = b0 + kc // 4
            P.op("act", lambda e, kc=kc, bank=bank, f=f: e.activation(out=f[:, kc, :], in_=ps[bank][:, (kc % 4) * 128:(kc % 4 + 1) * 128], func=AF.Identity, scale=k.gs2[:, kc:kc + 1], bias=k.sh2[:, kc:kc + 1]),
                 reads=["ps%d" % bank, "gs", "modf"], writes=[fk])
        P.op("dve", lambda e, f=f, t=t: e.tensor_copy(h2T[:, :, t * 128:(t + 1) * 128], f), reads=[fk], writes=["h2T"])
        bank = 4 + t % 2
        for kc in range(8):
            P.op("pe", lambda e, kc=kc, f=f, bank=bank: e.matmul(ps[bank][:, 0:36], lhsT=f[:, kc, :], rhs=wrt[:, kc, :], start=(kc == 0), stop=(kc == 7)), reads=[fk, "wrt"], writes=["ps%d" % bank])
        P.op("dve", lambda e, t=t, bank=bank: e.tensor_tensor(lg[:, t, :], ps[bank][:, 0:36], rb, op=ALU.add), reads=["ps%d" % bank, "rbF"], writes=["lg"])
    g4 = A.alloc(NT * 4, F32).rearrange("p (t c) -> p t c", t=NT)
    oh = A.alloc(NT * 4, F32).rearrange("p (t c) -> p t c", t=NT)
    s1 = A.alloc(NT * 8, F32)
    le = A.alloc(NT * 32, F32).rearrange("p (t c) -> p t c", t=NT)
    o1 = A.alloc(NT * 32, F32).rearrange("p (t c) -> p t c", t=NT)
    o2 = A.alloc(NT * 32, F32).rearrange("p (t c) -> p t c", t=NT)
    mx = s1[:, 0:NT]; gs_ = s1[:, NT:2 * NT]; mA = s1[:, 2 * NT:3 * NT]; mB = s1[:, 3 * NT:4 * NT]; w1 = s1[:, 4 * NT:5 * NT]; w2 = s1[:, 5 * NT:6 * NT]
    R = ["lg", "g4", "oh", "s1", "le", "o1", "o2", "gate"]
    def D(fn):
        P.op("dve", fn, reads=R, writes=R)
    bc4 = lambda v: v.unsqueeze(2).to_broadcast([128, NT, 4])
    bc32 = lambda v: v.unsqueeze(2).to_broadcast([128, NT, 32])
    D(lambda e: e.tensor_reduce(out=mx, in_=lg[:, :, 0:4], axis=AX.X, op=ALU.max))
    D(lambda e: e.tensor_tensor(oh, lg[:, :, 0:4], bc4(mx), op=ALU.is_ge))
    D(lambda e: e.tensor_tensor(g4, lg[:, :, 0:4], bc4(mx), op=ALU.subtract))
    P.op("act", lambda e: e.activation(out=g4, in_=g4, func=AF.Exp), reads=R, writes=R)
    D(lambda e: e.tensor_reduce(out=gs_, in_=g4, axis=AX.X, op=ALU.add))
    D(lambda e: e.reciprocal(gs_, gs_))
    lev = le.rearrange("p t (g x) -> p t g x", g=4)
    D(lambda e: e.tensor_tensor(lev, lg[:, :, 4:36].rearrange("p t (g x) -> p t g x", g=4), oh.unsqueeze(3).to_broadcast([128, NT, 4, 8]), op=ALU.mult))
    D(lambda e: e.tensor_scalar(o1.rearrange("p t (g x) -> p t g x", g=4), oh.unsqueeze(3).to_broadcast([128, NT, 4, 8]), -1.0, 1e30, op0=ALU.add, op1=ALU.mult))
    D(lambda e: e.tensor_tensor(le, le, o1, op=ALU.add))
    D(lambda e: e.tensor_reduce(out=mA, in_=le, axis=AX.X, op=ALU.max))
    D(lambda e: e.tensor_tensor(o1, le, bc32(mA), op=ALU.is_ge))
    D(lambda e: e.scalar_tensor_tensor(out=le, in0=o1, scalar=-1e30, in1=le, op0=ALU.mult, op1=ALU.add))
    D(lambda e: e.tensor_reduce(out=mB, in_=le, axis=AX.X, op=ALU.max))
    D(lambda e: e.tensor_tensor(o2, le, bc32(mB), op=ALU.is_ge))
    D(lambda e: e.tensor_tensor(w1, mB, mA, op=ALU.subtract))
    P.op("act", lambda e: e.activation(out=w1, in_=w1, func=AF.Exp), reads=R, writes=R)
    D(lambda e: e.tensor_scalar(w1, w1, 1.0, None, op0=ALU.add))
    D(lambda e: e.reciprocal(w1, w1))
    D(lambda e: e.tensor_scalar(w2, w1, -1.0, 1.0, op0=ALU.mult, op1=ALU.add))
    D(lambda e: e.tensor_tensor(w1, w1, gs_, op=ALU.mult))
    D(lambda e: e.tensor_tensor(w2, w2, gs_, op=ALU.mult))
    D(lambda e: e.tensor_tensor(o1, o1, bc32(w1), op=ALU.mult))
    D(lambda e: e.tensor_tensor(o2, o2, bc32(w2), op=ALU.mult))
    D(lambda e: e.tensor_tensor(gate, o1, o2, op=ALU.add))
    P.barrier()
    A.release(m1_)
    TB = 1024
    acc = A.alloc(8 * 1024, F32).rearrange("p (t c) -> p t c", t=8)
    wg = [A.alloc(8 * 512, BF16).rearrange("p (k c) -> p k c", k=8) for _ in range(2)]
    wu = [A.alloc(8 * 512, BF16).rearrange("p (k c) -> p k c", k=8) for _ in range(2)]
    wd = [A.alloc(4 * 1024, BF16).rearrange("p (k c) -> p k c", k=4) for _ in range(2)]
    hid = [A.alloc(4 * 512, BF16).rearrange("p (k t) -> p k t", k=4) for _ in range(2)]
    sg = [A.alloc(512, F32) for _ in range(2)]
    ec = 0; hc_ = 0; sc_ = 0; yc = 0
    for tb in range(S // TB):
        P.op("pool", lambda e: e.memset(acc, 0.0), writes=["acc"])
        for ex in range(32):
            i2 = ec % 2
            P.dma("pool", lambda e, i2=i2, ex=ex: e.dma_start(out=wg[i2], in_=I["w_gate"][l, ex].rearrange("(k p) c -> p k c", p=128)), writes=["wg%d" % i2], key="wg%d" % i2)
            P.dma("pool", lambda e, i2=i2, ex=ex: e.dma_start(out=wu[i2], in_=I["w_up"][l, ex].rearrange("(k p) c -> p k c", p=128)), writes=["wu%d" % i2], key="wu%d" % i2)
            P.dma("pool", lambda e, i2=i2, ex=ex: e.dma_start(out=wd[i2], in_=I["w_down"][l, ex].rearrange("(k p) c -> p k c", p=128)), writes=["wd%d" % i2], key="wd%d" % i2)
            for hb in range(TB // 512):
                tok0 = tb * TB + hb * 512
                hd = hid[hc_ % 2]; hk = "hid%d" % (hc_ % 2)
                for cb in range(4):
                    bg = (sc_ % 2) * 2; bu = bg + 1
                    for kc in range(8):
                        P.op("pe", lambda e, kc=kc, cb=cb, bg=bg, i2=i2, tok0=tok0: e.matmul(ps[bg][:, :], lhsT=wg[i2][:, kc, cb * 128:(cb + 1) * 128], rhs=h2T[:, kc, tok0:tok0 + 512], start=(kc == 0), stop=(kc == 7)),
                             reads=["wg%d" % i2, "h2T"], writes=["ps%d" % bg])
                    for kc in range(8):
                        P.op("pe", lambda e, kc=kc, cb=cb, bu=bu, i2=i2, tok0=tok0: e.matmul(ps[bu][:, :], lhsT=wu[i2][:, kc, cb * 128:(cb + 1) * 128], rhs=h2T[:, kc, tok0:tok0 + 512], start=(kc == 0), stop=(kc == 7)),
                             reads=["wu%d" % i2, "h2T"], writes=["ps%d" % bu])
                    s = sg[sc_ % 2]; sk = "sgF%d" % (sc_ % 2)
                    P.op("act", lambda e, s=s, bg=bg: e.activation(out=s, in_=ps[bg][:, :], func=AF.Exp, scale=-1.0), reads=["ps%d" % bg], writes=[sk])
                    P.op("pool", lambda e, s=s: e.tensor_scalar(s, s, 1.0, None, op0=ALU.add), reads=[sk], writes=[sk])
                    P.op("dve", lambda e, s=s: e.reciprocal(s, s), reads=[sk], writes=[sk])
                    P.op("dve", lambda e, s=s, bg=bg: e.tensor_tensor(s, s, ps[bg][:, :], op=ALU.mult), reads=[sk, "ps%d" % bg], writes=[sk])
                    P.op("dve", lambda e, s=s, bu=bu, hd=hd, cb=cb: e.tensor_tensor(hd[:, cb, :], s, ps[bu][:, :], op=ALU.mult), reads=[sk, "ps%d" % bu], writes=[hk])
                    sc_ += 1
                for tt in range(4):
                    tl = hb * 4 + tt
                    tg = tb * 8 + tl
                    for half in range(2):
                        bank = 4 + yc % 4
                        for kc in range(4):
                            P.op("pe", lambda e, kc=kc, half=half, bank=bank, tt=tt, hd=hd, i2=i2: e.matmul(ps[bank][:, :], lhsT=hd[:, kc, tt * 128:(tt + 1) * 128], rhs=wd[i2][:, kc, half * 512:(half + 1) * 512], start=(kc == 0), stop=(kc == 3)),
                                 reads=[hk, "wd%d" % i2], writes=["ps%d" % bank])
                        P.op("dve", lambda e, bank=bank, tl=tl, tg=tg, half=half, ex=ex: e.scalar_tensor_tensor(out=acc[:, tl, half * 512:(half + 1) * 512], in0=ps[bank][:, :], scalar=gate[:, tg, ex:ex + 1], in1=acc[:, tl, half * 512:(half + 1) * 512], op0=ALU.mult, op1=ALU.add),
                             reads=["ps%d" % bank, "gate", "acc"], writes=["acc"])
                        yc += 1
                hc_ += 1
            ec += 1
        for tl in range(8):
            tg = tb * 8 + tl
            x = k.xb[tg % 2]; xk = "xb%d" % (tg % 2)
            P.dma("sp", lambda e, x=x, tg=tg: e.dma_start(out=x, in_=xsrc[tg * 128:(tg + 1) * 128, :]), writes=[xk], key=xk)
            P.op("pool", lambda e, tl=tl: e.tensor_tensor(acc[:, tl, :], acc[:, tl, :], k.gtbc[1], op=ALU.mult), reads=["acc", "gtbc1"], writes=["acc"])
            P.op("pool", lambda e, tl=tl, x=x: e.tensor_tensor(x, x, acc[:, tl, :], op=ALU.add), reads=["acc", xk], writes=[xk])
            P.dma("sp", lambda e, x=x, tg=tg: e.dma_start(out=xdst[tg * 128:(tg + 1) * 128, :], in_=x), reads=[xk], key=xk)
    P.barrier()
    A.release(m0)


def phaseG(k, I, xsrc, out):
    A, P, ps = k.A, k.P, k.ps
    m0 = A.mark()
    gb = A.alloc(1024, F32)
    P.dma("sp", lambda e: e.dma_start(out=gb, in_=I["g_final"].partition_broadcast(128)), writes=["gbG"], key="gbG")
    xb = [A.alloc(1024, F32) for _ in range(2)]
    xn = [A.alloc(1024, F32) for _ in range(2)]
    st = [A.alloc(2, F32) for _ in range(2)]
    for t in range(NT):
        x = xb[t % 2]; xk = "xbG%d" % (t % 2); n = xn[t % 2]; nk = "xnG%d" % (t % 2); s = st[t % 2]; sk = "stG%d" % (t % 2)
        P.dma("sp", lambda e, x=x, t=t: e.dma_start(out=x, in_=xsrc[t * 128:(t + 1) * 128, :]), writes=[xk], key=xk)
        P.op("act", lambda e, x=x, n=n, s=s: e.activation(out=n, in_=x, func=AF.Square, accum_out=s[:, 0:1]), reads=[xk], writes=[nk, sk])
        P.op("dve", lambda e, s=s: e.tensor_scalar(s[:, 1:2], s[:, 0:1], 1.0 / D, EPS, op0=ALU.mult, op1=ALU.add), reads=[sk], writes=[sk])
        P.op("act", lambda e, s=s: e.activation(out=s[:, 1:2], in_=s[:, 1:2], func=AF.Ln), reads=[sk], writes=[sk])
        P.op("act", lambda e, s=s: e.activation(out=s[:, 1:2], in_=s[:, 1:2], func=AF.Exp, scale=-0.5), reads=[sk], writes=[sk])
        P.op("dve", lambda e, x=x, n=n, s=s: e.scalar_tensor_tensor(out=n, in0=x, scalar=s[:, 1:2], in1=gb, op0=ALU.mult, op1=ALU.mult), reads=[xk, sk, "gbG"], writes=[nk])
        P.dma("sp", lambda e, n=n, t=t: e.dma_start(out=out[t * 128:(t + 1) * 128, :], in_=n), reads=[nk], key=nk)
    P.barrier()
    A.release(m0)


CAP = 768
NSL = 32 * CAP


def phaseF2(k, l, I, xsrc, xdst):
    A, P, ps = k.A, k.P, k.ps
    Xbuf = dram(k, "Xbuf", [NSL + 128, D], BF16)
    Ybuf = dram(k, "Ybuf", [NSL + 128, D], F32)
    m0 = A.mark()
    idx1 = A.alloc(NT, I32); idx2 = A.alloc(NT, I32)
    g12 = A.alloc(2 * NT, F32)
    g1 = g12[:, 0:NT]; g2 = g12[:, NT:2 * NT]
    k.xb = [A.alloc(1024, F32) for _ in range(2)]
    mH = A.mark()
    h2tm = A.alloc(NT * 1024, BF16).rearrange("p (t c) -> p t c", t=NT)
    m1_ = A.mark()
    hf = [A.alloc(8 * 128, F32).rearrange("p (k t) -> p k t", k=8) for _ in range(2)]
    wrt = A.alloc(8 * 36, F32).rearrange("p (k c) -> p k c", k=8)
    rb = A.alloc(36, F32)
    lg = A.alloc(NT * 36, F32).rearrange("p (t c) -> p t c", t=NT)
    k.xn = [A.alloc(1024, F32) for _ in range(2)]
    k.nst = [A.alloc(2, F32) for _ in range(2)]
    P.dma("sp", lambda e: e.dma_start(out=wrt[:, :, 0:4], in_=I["w_grp"][l].rearrange("(k p) c -> p k c", p=128)), writes=["wrt"], key="wrt")
    P.dma("sp", lambda e: e.dma_start(out=wrt[:, :, 4:36], in_=I["w_exp_router"][l].rearrange("(k p) c -> p k c", p=128)), writes=["wrt"], key="wrt")
    P.dma("sp", lambda e: e.dma_start(out=rb[:, 0:4], in_=I["b_grp"][l].partition_broadcast(128)), writes=["rbF"], key="rbF")
    P.dma("sp", lambda e: e.dma_start(out=rb[:, 4:36], in_=I["b_exp_router"][l].partition_broadcast(128)), writes=["rbF"], key="rbF")
    for t in range(NT):
        b0 = norm_transpose(k, xsrc, t, None, None, None, None, None)
        f = hf[t % 2]; fk = "hfF%d" % (t % 2)
        for kc in range(8):
            bank = b0 + kc // 4
            P.op("act", lambda e, kc=kc, bank=bank, f=f: e.activation(out=f[:, kc, :], in_=ps[bank][:, (kc % 4) * 128:(kc % 4 + 1) * 128], func=AF.Identity, scale=k.gs2[:, kc:kc + 1], bias=k.sh2[:, kc:kc + 1]),
                 reads=["ps%d" % bank, "gs", "modf"], writes=[fk])
        bank = 4 + t % 2
        for kc in range(8):
            P.op("pe", lambda e, kc=kc, f=f, bank=bank: e.matmul(ps[bank][:, 0:36], lhsT=f[:, kc, :], rhs=wrt[:, kc, :], start=(kc == 0), stop=(kc == 7)), reads=[fk, "wrt"], writes=["ps%d" % bank])
        P.op("dve", lambda e, t=t, bank=bank: e.tensor_tensor(lg[:, t, :], ps[bank][:, 0:36], rb, op=ALU.add), reads=["ps%d" % bank, "rbF"], writes=["lg"])
        for kc in range(8):
            bank = 6 + kc // 4
            P.op("pe", lambda e, kc=kc, f=f, bank=bank: e.transpose(ps[bank][:, (kc % 4) * 128:(kc % 4 + 1) * 128], f[:, kc, :], k.ident), reads=[fk, "ident"], writes=["ps%d" % bank])
        P.op("dve", lambda e, t=t: e.tensor_copy(h2tm[:, t, 0:512], ps[6][:, :]), reads=["ps6"], writes=["h2tm"])
        P.op("pool" if False else "dve", lambda e, t=t: e.tensor_copy(h2tm[:, t, 512:1024], ps[7][:, :]), reads=["ps7"], writes=["h2tm"])
    def T32():
        return A.alloc(NT * 32, F32).rearrange("p (t c) -> p t c", t=NT)
    g4 = A.alloc(NT * 4, F32).rearrange("p (t c) -> p t c", t=NT)
    oh = A.alloc(NT * 4, F32).rearrange("p (t c) -> p t c", t=NT)
    s1 = A.alloc(NT * 8, F32)
    le = T32(); o1 = T32(); o2 = T32(); pos = T32(); tmp = T32(); offs = T32()
    indb = A.alloc(NT * 32, BF16)
    SU = A.alloc(128, BF16)
    suf = A.alloc(128, F32)
    mx = s1[:, 0:NT]; gs_ = s1[:, NT:2 * NT]; mA = s1[:, 2 * NT:3 * NT]; mB = s1[:, 3 * NT:4 * NT]; w1 = s1[:, 4 * NT:5 * NT]; w2 = s1[:, 5 * NT:6 * NT]
    v1 = s1[:, 6 * NT:7 * NT]; v2 = s1[:, 7 * NT:8 * NT]
    R = ["lg", "g4", "oh", "s1", "le", "o1", "o2", "pos", "tmp", "offs", "g12", "idx"]
    def Dv(fn):
        P.op("dve", fn, reads=R, writes=R)
    bc4 = lambda v: v.unsqueeze(2).to_broadcast([128, NT, 4])
    bc32 = lambda v: v.unsqueeze(2).to_broadcast([128, NT, 32])
    P.op("pool", lambda e: e.memset(suf, 1.0), writes=["suf"])
    P.op("pool", lambda e: e.tensor_tensor(suf, k.cneg_u, k.cneg_u, op=ALU.mult), reads=["cneg_u"], writes=["suf"])
    P.op("dve", lambda e: e.tensor_copy(SU, suf), reads=["suf"], writes=["SU"])
    Dv(lambda e: e.tensor_reduce(out=mx, in_=lg[:, :, 0:4], axis=AX.X, op=ALU.max))
    Dv(lambda e: e.tensor_tensor(oh, lg[:, :, 0:4], bc4(mx), op=ALU.is_ge))
    Dv(lambda e: e.tensor_tensor(g4, lg[:, :, 0:4], bc4(mx), op=ALU.subtract))
    P.op("act", lambda e: e.activation(out=g4, in_=g4, func=AF.Exp), reads=R, writes=R)
    Dv(lambda e: e.tensor_reduce(out=gs_, in_=g4, axis=AX.X, op=ALU.add))
    Dv(lambda e: e.reciprocal(gs_, gs_))
    lev = le.rearrange("p t (g x) -> p t g x", g=4)
    Dv(lambda e: e.tensor_tensor(lev, lg[:, :, 4:36].rearrange("p t (g x) -> p t g x", g=4), oh.unsqueeze(3).to_broadcast([128, NT, 4, 8]), op=ALU.mult))
    Dv(lambda e: e.tensor_scalar(o1.rearrange("p t (g x) -> p t g x", g=4), oh.unsqueeze(3).to_broadcast([128, NT, 4, 8]), -1.0, 1e30, op0=ALU.add, op1=ALU.mult))
    Dv(lambda e: e.tensor_tensor(le, le, o1, op=ALU.add))
    Dv(lambda e: e.tensor_reduce(out=mA, in_=le, axis=AX.X, op=ALU.max))
    Dv(lambda e: e.tensor_tensor(o1, le, bc32(mA), op=ALU.is_ge))
    Dv(lambda e: e.scalar_tensor_tensor(out=le, in0=o1, scalar=-1e30, in1=le, op0=ALU.mult, op1=ALU.add))
    Dv(lambda e: e.tensor_reduce(out=mB, in_=le, axis=AX.X, op=ALU.max))
    Dv(lambda e: e.tensor_tensor(o2, le, bc32(mB), op=ALU.is_ge))
    Dv(lambda e: e.tensor_tensor(w1, mB, mA, op=ALU.subtract))
    P.op("act", lambda e: e.activation(out=w1, in_=w1, func=AF.Exp), reads=R, writes=R)
    Dv(lambda e: e.tensor_scalar(w1, w1, 1.0, None, op0=ALU.add))
    Dv(lambda e: e.reciprocal(w1, w1))
    Dv(lambda e: e.tensor_scalar(w2, w1, -1.0, 1.0, op0=ALU.mult, op1=ALU.add))
    Dv(lambda e: e.tensor_tensor(w1, w1, gs_, op=ALU.mult))
    Dv(lambda e: e.tensor_tensor(w2, w2, gs_, op=ALU.mult))
    Dv(lambda e: e.tensor_tensor(tmp, o1, o2, op=ALU.add))
    P.op("dve", lambda e: e.tensor_copy(indb, tmp.rearrange("p t c -> p (t c)")), reads=R, writes=["indb"])
    for c in range(2):
        P.op("pe", lambda e, c=c: e.matmul(ps[c][:, :], lhsT=SU, rhs=indb[:, c * 512:(c + 1) * 512], start=True, stop=True), reads=["SU", "indb"], writes=["ps%d" % c])
        P.op("pe", lambda e, c=c: e.matmul(ps[2 + c][:, :], lhsT=k.ones_b, rhs=indb[:, c * 512:(c + 1) * 512], start=True, stop=True), reads=["ones_b", "indb"], writes=["ps%d" % (2 + c)])
    for c in range(2):
        P.op("dve", lambda e, c=c: e.tensor_copy(pos.rearrange("p t c -> p (t c)")[:, c * 512:(c + 1) * 512], ps[c][:, :]), reads=["ps%d" % c] + R, writes=R)
        P.op("dve", lambda e, c=c: e.tensor_copy(tmp.rearrange("p t c -> p (t c)")[:, c * 512:(c + 1) * 512], ps[2 + c][:, :]), reads=["ps%d" % (2 + c)] + R, writes=R)
    Dv(lambda e: e.memset(offs[:, 0, :], 0.0))
    for t in range(1, NT):
        Dv(lambda e, t=t: e.tensor_tensor(offs[:, t, :], offs[:, t - 1, :], tmp[:, t - 1, :], op=ALU.add))
    Dv(lambda e: e.tensor_tensor(pos, pos, offs, op=ALU.add))
    Dv(lambda e: e.tensor_scalar(tmp, pos, float(CAP), None, op0=ALU.is_lt))
    Dv(lambda e: e.tensor_tensor(pos, pos, k.ecap.unsqueeze(1).to_broadcast([128, NT, 32]), op=ALU.add))
    Dv(lambda e: e.tensor_tensor(pos, pos, tmp, op=ALU.mult))
    Dv(lambda e: e.tensor_scalar(offs, tmp, -1.0, k.trash[:, 0:1], op0=ALU.add, op1=ALU.mult))
    Dv(lambda e: e.tensor_tensor(pos, pos, offs, op=ALU.add))
    Dv(lambda e: e.tensor_tensor(offs, o1, pos, op=ALU.mult))
    Dv(lambda e: e.tensor_reduce(out=v1, in_=offs, axis=AX.X, op=ALU.add))
    P.op("dve", lambda e: e.tensor_copy(idx1, v1), reads=R, writes=R)
    Dv(lambda e: e.tensor_tensor(offs, o2, pos, op=ALU.mult))
    Dv(lambda e: e.tensor_reduce(out=v2, in_=offs, axis=AX.X, op=ALU.add))
    P.op("dve", lambda e: e.tensor_copy(idx2, v2), reads=R, writes=R)
    Dv(lambda e: e.tensor_tensor(offs, o1, tmp, op=ALU.mult))
    Dv(lambda e: e.tensor_reduce(out=v1, in_=offs, axis=AX.X, op=ALU.add))
    Dv(lambda e: e.tensor_tensor(g1, w1, v1, op=ALU.mult))
    Dv(lambda e: e.tensor_tensor(offs, o2, tmp, op=ALU.mult))
    Dv(lambda e: e.tensor_reduce(out=v2, in_=offs, axis=AX.X, op=ALU.add))
    Dv(lambda e: e.tensor_tensor(g2, w2, v2, op=ALU.mult))
    P.op("pool", lambda e: e.memset(k.xb[0], 0.0), writes=["xb0"])
    P.dma("sp", lambda e: e.dma_start(out=Ybuf[NSL:NSL + 128, :], in_=k.xb[0]), reads=["xb0"], key="xb0")
    if l == 0:
        zb = k.xb[0].bitcast(BF16)[:, 0:1024]
        nrt = (NSL + 128) // 128
        Xv = Xbuf.rearrange("(n p) c -> p n c", p=128)
        for n0 in range(0, nrt, 16):
            nn = min(16, nrt - n0)
            P.dma("sp", lambda e, n0=n0, nn=nn: e.dma_start(out=Xv[:, n0:n0 + nn, :], in_=zb.unsqueeze(1).to_broadcast([128, nn, 1024])), reads=["xb0"], writes=["XbufZ"], key="xbz")
    if "dbg_idx" in k.debug:
        dbi = dram(k, "dbg_idx", [128, 2 * NT], I32)
        P.dma("sp", lambda e: e.dma_start(out=dbi[:, 0:NT], in_=idx1), reads=R, key="dbgi")
        P.dma("sp", lambda e: e.dma_start(out=dbi[:, NT:2 * NT], in_=idx2), reads=R, key="dbgi")
        dbg_ = dram(k, "dbg_g", [128, 2 * NT], F32)
        P.dma("sp", lambda e: e.dma_start(out=dbg_, in_=g12), reads=R, key="dbgg")
    for t in range(NT):
        for (ix, nm) in ((idx1, "a"), (idx2, "b")):
            P.dma("pool", lambda e, t=t, ix=ix: e.indirect_dma_start(out=Xbuf, out_offset=bass.IndirectOffsetOnAxis(ap=ix[:, t:t + 1], axis=0), in_=h2tm[:, t, :], in_offset=None),
                  reads=["h2tm", "XbufZ"] + R, writes=[], key="sc%s%d" % (nm, t % 4))
    P.barrier()
    A.release(mH)
    wg = [A.alloc(8 * 512, BF16).rearrange("p (k c) -> p k c", k=8) for _ in range(2)]
    wu = [A.alloc(8 * 512, BF16).rearrange("p (k c) -> p k c", k=8) for _ in range(2)]
    wd = [A.alloc(4 * 1024, BF16).rearrange("p (k c) -> p k c", k=4) for _ in range(2)]
    XT = [A.alloc(8 * CAP, BF16).rearrange("p (k s) -> p k s", k=8) for _ in range(2)]
    xr = [A.alloc(1024, BF16) for _ in range(3)]
    hid = [A.alloc(4 * CAP, BF16).rearrange("p (k s) -> p k s", k=4) for _ in range(2)]
    sg = [A.alloc(512, F32) for _ in range(2)]
    yrow = [A.alloc(1024, F32) for _ in range(3)]
    NST = CAP // 128
    if "dbg_X0" in k.debug:
        dx = dram(k, "dbg_X0", [128, 1024], BF16)
        P.dma("sp", lambda e: e.dma_start(out=xr[2], in_=Xbuf[0:128, :]), writes=["xr2"], key="xr2")
        P.dma("sp", lambda e: e.dma_start(out=dx, in_=xr[2]), reads=["xr2"], key="dbgx0")
    chunks = [(0, 512), (512, CAP - 512)] if CAP > 512 else [(0, CAP)]
    xc = 0; cc = 0; yc = 0; scn = 0
    for ex in range(32):
        i2 = ex % 2
        P.dma("pool", lambda e, i2=i2, ex=ex: e.dma_start(out=wg[i2], in_=I["w_gate"][l, ex].rearrange("(k p) c -> p k c", p=128)), writes=["wg%d" % i2], key="wg%d" % i2)
        P.dma("pool", lambda e, i2=i2, ex=ex: e.dma_start(out=wu[i2], in_=I["w_up"][l, ex].rearrange("(k p) c -> p k c", p=128)), writes=["wu%d" % i2], key="wu%d" % i2)
        P.dma("pool", lambda e, i2=i2, ex=ex: e.dma_start(out=wd[i2], in_=I["w_down"][l, ex].rearrange("(k p) c -> p k c", p=128)), writes=["wd%d" % i2], key="wd%d" % i2)
        xt = XT[i2]; xtk = "XT%d" % i2
        for st in range(NST):
            r = xr[xc % 3]; rk = "xr%d" % (xc % 3)
            P.dma("sp", lambda e, r=r, ex=ex, st=st: e.dma_start(out=r, in_=Xbuf[ex * CAP + st * 128:ex * CAP + (st + 1) * 128, :]), writes=[rk], key=rk)
            bank = 6 + xc % 2
            tb = ps[bank][:, :].bitcast(BF16)
            for kc in range(8):
                P.op("pe", lambda e, kc=kc, r=r, tb=tb: e.transpose(tb[:, kc * 128:(kc + 1) * 128], r[:, kc * 128:(kc + 1) * 128], k.identb), reads=[rk, "identb"], writes=["ps%d" % bank])
            eng = "act" if xc % 2 == 0 else "dve"
            if eng == "act":
                P.op("act", lambda e, tb=tb, xt=xt, st=st: e.activation(out=xt[:, :, st * 128:(st + 1) * 128], in_=tb.rearrange("p (k s) -> p k s", k=8), func=AF.Copy), reads=["ps%d" % bank], writes=[xtk])
            else:
                P.op("dve", lambda e, tb=tb, xt=xt, st=st: e.tensor_copy(xt[:, :, st * 128:(st + 1) * 128], tb.rearrange("p (k s) -> p k s", k=8)), reads=["ps%d" % bank], writes=[xtk])
            xc += 1
        hd = hid[i2]; hk = "hid%d" % i2
        for hb in range(4):
            for (c0, cn) in chunks:
                bg = (cc % 2) * 2; bu = bg + 1
                for kc in range(8):
                    P.op("pe", lambda e, kc=kc, hb=hb, bg=bg, c0=c0, cn=cn, i2=i2, xt=xt: e.matmul(ps[bg][:, 0:cn], lhsT=wg[i2][:, kc, hb * 128:(hb + 1) * 128], rhs=xt[:, kc, c0:c0 + cn], start=(kc == 0), stop=(kc == 7)),
                         reads=["wg%d" % i2, xtk], writes=["ps%d" % bg])
                for kc in range(8):
                    P.op("pe", lambda e, kc=kc, hb=hb, bu=bu, c0=c0, cn=cn, i2=i2, xt=xt: e.matmul(ps[bu][:, 0:cn], lhsT=wu[i2][:, kc, hb * 128:(hb + 1) * 128], rhs=xt[:, kc, c0:c0 + cn], start=(kc == 0), stop=(kc == 7)),
                         reads=["wu%d" % i2, xtk], writes=["ps%d" % bu])
                s = sg[scn % 2]; sk = "sgF%d" % (scn % 2)
                P.op("act", lambda e, s=s, bg=bg, cn=cn: e.activation(out=s[:, 0:cn], in_=ps[bg][:, 0:cn], func=AF.Silu), reads=["ps%d" % bg], writes=[sk])
                P.op("dve", lambda e, s=s, bu=bu, hb=hb, c0=c0, cn=cn, hd=hd: e.tensor_tensor(hd[:, hb, c0:c0 + cn], s[:, 0:cn], ps[bu][:, 0:cn], op=ALU.mult), reads=[sk, "ps%d" % bu], writes=[hk])
                scn += 1; cc += 1
        if ex == 0 and "dbg_XT" in k.debug:
            d1 = dram(k, "dbg_XT", [128, 8 * CAP], BF16)
            P.dma("sp", lambda e: e.dma_start(out=d1, in_=xt.rearrange("p k s -> p (k s)")), reads=[xtk], key="dbgxt")
            d2 = dram(k, "dbg_hid", [128, 4 * CAP], BF16)
            P.dma("sp", lambda e: e.dma_start(out=d2, in_=hd.rearrange("p k s -> p (k s)")), reads=[hk], key="dbghd")
            d3 = dram(k, "dbg_wg", [128, 8 * 512], BF16)
            P.dma("sp", lambda e: e.dma_start(out=d3, in_=wg[i2].rearrange("p k s -> p (k s)")), reads=["wg%d" % i2], key="dbgwg")
        for st in range(NST):
            y = yrow[yc % 3]; yk = "yrow%d" % (yc % 3)
            for half in range(2):
                bank = 4 + half
                for kc in range(4):
                    P.op("pe", lambda e, kc=kc, half=half, bank=bank, st=st, hd=hd, i2=i2: e.matmul(ps[bank][:, :], lhsT=hd[:, kc, st * 128:(st + 1) * 128], rhs=wd[i2][:, kc, half * 512:(half + 1) * 512], start=(kc == 0), stop=(kc == 3)),
                         reads=[hk, "wd%d" % i2], writes=["ps%d" % bank])
                if half == 0:
                    P.op("act", lambda e, y=y, bank=bank: e.activation(out=y[:, 0:512], in_=ps[bank][:, :], func=AF.Copy), reads=["ps%d" % bank], writes=[yk])
                else:
                    P.op("dve", lambda e, y=y, bank=bank: e.tensor_copy(y[:, 512:1024], ps[bank][:, :]), reads=["ps%d" % bank], writes=[yk])
            P.dma("sp", lambda e, y=y, ex=ex, st=st: e.dma_start(out=Ybuf[ex * CAP + st * 128:ex * CAP + (st + 1) * 128, :], in_=y), reads=[yk], key=yk)
            yc += 1
    P.barrier()
    A.release(mH)
    Y1 = [A.alloc(1024, F32) for _ in range(2)]
    Y2 = [A.alloc(1024, F32) for _ in range(2)]
    for t in range(NT):
        b = t % 2
        x = k.xb[b]; xk = "xb%d" % b
        P.dma("sp", lambda e, x=x, t=t: e.dma_start(out=x, in_=xsrc[t * 128:(t + 1) * 128, :]), writes=[xk], key=xk)
        P.dma("pool", lambda e, t=t, b=b: e.indirect_dma_start(out=Y1[b], out_offset=None, in_=Ybuf, in_offset=bass.IndirectOffsetOnAxis(ap=idx1[:, t:t + 1], axis=0)), reads=R, writes=["Y1%d" % b], key="Y1%d" % b)
        P.dma("pool", lambda e, t=t, b=b: e.indirect_dma_start(out=Y2[b], out_offset=None, in_=Ybuf, in_offset=bass.IndirectOffsetOnAxis(ap=idx2[:, t:t + 1], axis=0)), reads=R, writes=["Y2%d" % b], key="Y2%d" % b)
        P.op("dve", lambda e, t=t, b=b: e.tensor_scalar(Y1[b], Y1[b], g1[:, t:t + 1], None, op0=ALU.mult), reads=["Y1%d" % b] + R, writes=["Y1%d" % b])
        P.op("dve", lambda e, t=t, b=b: e.scalar_tensor_tensor(out=Y1[b], in0=Y2[b], scalar=g2[:, t:t + 1], in1=Y1[b], op0=ALU.mult, op1=ALU.add), reads=["Y1%d" % b, "Y2%d" % b] + R, writes=["Y1%d" % b])
        P.op("pool", lambda e, b=b: e.tensor_tensor(Y1[b], Y1[b], k.gtbc[1], op=ALU.mult), reads=["Y1%d" % b, "gtbc1"], writes=["Y1%d" % b])
        P.op("pool", lambda e, x=x, b=b: e.tensor_tensor(x, x, Y1[b], op=ALU.add), reads=["Y1%d" % b, xk], writes=[xk])
        P.dma("sp", lambda e, x=x, t=t: e.dma_start(out=xdst[t * 128:(t + 1) * 128, :], in_=x), reads=[xk], key=xk)
    P.barrier()
    A.release(m0)


from concourse.bass_utils import run_bass_kernel_spmd

_WNAMES = ["w_mod", "b_mod", "g_norm1", "g_norm2", "w_in", "g_cq", "g_ckv", "g_kidx", "w_q_up", "w_idx_q", "w_v_up",
           "lb_logits", "g_rec", "w_branch_a", "w_branch_r", "w_out", "w_grp", "b_grp", "w_exp_router", "b_exp_router",
           "w_gate", "w_up", "w_down", "g_final"]


def _build(shapes, depth=4):
    nc = bass.Bass("TRN2", target_bir_lowering=False)
    es = ExitStack()
    with es:
        I = {}
        I["x"] = nc.dram_tensor("x", [S, D], F32, kind="ExternalInput").ap()
        I["c"] = nc.dram_tensor("c", [1, D], F32, kind="ExternalInput").ap()
        for n in _WNAMES:
            I[n] = nc.dram_tensor(n, list(shapes[n]), F32, kind="ExternalInput").ap()
        out = nc.dram_tensor("out", [S, D], F32, kind="ExternalOutput").ap()
        k = mkctx(nc, es)
        setup_consts(k)
        pm = k.A.mark()
        xa = dram(k, "xres_a", [S, D], F32)
        xb = dram(k, "xres_b", [S, D], F32)
        xcur = I["x"]
        for l in range(depth):
            k.A.release(pm)
            phase0(k, l, I)
            m_after0 = k.A.mark()
            phaseA(k, l, I, xcur)
            phaseB(k, l, I)
            phaseC(k, l, I)
            k.A.release(m_after0)
            phaseD(k, l, I)
            phaseE(k, l, I, xcur, xa)
            phaseF2(k, l, I, xa, xb)
            xcur = xb
        phaseG(k, I, xcur, out)
        k.P.finish(k.A.alloc(2, F32))
        k.P.emit(es)
    return nc


def kernel(**inputs):
    x = np.ascontiguousarray(inputs["x"], dtype=np.float32)
    c = np.ascontiguousarray(inputs["c"], dtype=np.float32)
    shapes = {n: inputs[n].shape for n in _WNAMES}
    nc = _build(shapes)
    w = {n: np.ascontiguousarray(inputs[n], dtype=np.float32) for n in _WNAMES}
    in_maps = []
    for b in range(8):
        m = {"x": x[b], "c": c[b:b + 1]}
        m.update(w)
        in_maps.append(m)
    res = run_bass_kernel_spmd(nc, in_maps, core_ids=list(range(8)))
    return np.stack([np.asarray(r["out"], dtype=np.float32) for r in res.results], axis=0)
```

```python
import numpy as np
import concourse.bass as bass
import concourse.mybir as mybir
from contextlib import ExitStack

F32 = mybir.dt.float32
BF16 = mybir.dt.bfloat16
I32 = mybir.dt.int32
AF = mybir.ActivationFunctionType
ALU = mybir.AluOpType
AX = mybir.AxisListType


class Op:
    __slots__ = ("eng", "fn", "deps", "inc", "cnt", "dma", "key", "consumed", "idx")

    def __init__(self, eng, fn, dma=False, key=None):
        self.eng = eng
        self.fn = fn
        self.deps = []
        self.inc = dma
        self.cnt = 0
        self.dma = dma
        self.key = key
        self.consumed = False


class Prog:
    ENGS = ("pe", "act", "dve", "pool", "sp")

    def __init__(self, nc):
        self.nc = nc
        self.ops = {e: [] for e in self.ENGS}
        self.last_w = {}
        self.readers = {}
        self.since_barrier = []
        self.pending_barrier = {e: [] for e in self.ENGS}
        self.nops = 0

    def _add(self, op, reads, writes):
        deps = []
        seen = set()
        for k in list(reads) + list(writes):
            w = self.last_w.get(k)
            if w is not None and id(w) not in seen:
                seen.add(id(w)); deps.append(w)
        for k in writes:
            for r in self.readers.get(k, ()):
                if id(r) not in seen:
                    seen.add(id(r)); deps.append(r)
        pb = self.pending_barrier[op.eng]
        if pb:
            for d in pb:
                if id(d) not in seen:
                    seen.add(id(d)); deps.append(d)
            self.pending_barrier[op.eng] = []
        op.deps = [d for d in deps if d is not op]
        for d in op.deps:
            d.consumed = True
        for k in writes:
            self.last_w[k] = op
            self.readers[k] = []
        for k in reads:
            if k not in writes:
                self.readers.setdefault(k, []).append(op)
        self.ops[op.eng].append(op)
        self.since_barrier.append(op)
        self.nops += 1
        return op

    def op(self, eng, fn, reads=(), writes=()):
        return self._add(Op(eng, fn), reads, writes)

    def dma(self, eng, fn, reads=(), writes=(), key=None):
        assert key is not None
        return self._add(Op(eng, fn, dma=True, key=key), reads, writes)

    def barrier(self):
        lst = []
        last = {}
        for o in self.since_barrier:
            if o.dma:
                if not o.consumed:
                    lst.append(o)
            else:
                last[o.eng] = o
        lst.extend(last.values())
        for e in self.ENGS:
            self.pending_barrier[e] = self.pending_barrier[e] + lst
        self.since_barrier = []

    def finish(self, scratch):
        self.barrier()
        self.op("pool", lambda e: e.memset(scratch, 0.0), writes=["__fin"])

    def emit(self, es):
        nc = self.nc
        for e in self.ENGS:
            for o in self.ops[e]:
                for d in o.deps:
                    if d.dma:
                        continue
                    if d.eng == "pe" and o.eng == "pe" and not o.dma:
                        continue
                    d.inc = True
        esem = {}
        for e in self.ENGS:
            esem[e] = es.enter_context(nc.semaphore("s_" + e))
        dsem = {}
        dcnt = {}
        for e in self.ENGS:
            c = 0
            for o in self.ops[e]:
                if o.dma:
                    if o.key not in dsem:
                        dsem[o.key] = es.enter_context(nc.semaphore("d_%d" % len(dsem)))
                        dcnt[o.key] = 0
                    dcnt[o.key] += 16
                    o.cnt = dcnt[o.key]
                elif o.inc:
                    c += 1
                    o.cnt = c
        self.n_dsem = len(dsem)
        block = es.enter_context(nc.Block())

        def run(ename, h):
            waited = {}
            for o in self.ops[ename]:
                need = {}
                for d in o.deps:
                    if d.dma:
                        s = dsem[d.key]
                    else:
                        if d.eng == "pe" and ename == "pe" and not o.dma:
                            continue
                        s = esem[d.eng]
                    sid = id(s)
                    if need.get(sid, (None, 0))[1] < d.cnt:
                        need[sid] = (s, d.cnt)
                for sid, (s, v) in need.items():
                    if waited.get(sid, 0) < v:
                        h.wait_ge(s, v)
                        waited[sid] = v
                ins = o.fn(h)
                if o.dma:
                    ins.then_inc(dsem[o.key], 16)
                elif o.inc:
                    ins.then_inc(esem[ename], 1)

        @block.tensor
        def _(h):
            run("pe", h)

        @block.scalar
        def _(h):
            run("act", h)

        @block.vector
        def _(h):
            run("dve", h)

        @block.gpsimd
        def _(h):
            run("pool", h)

        @block.sync
        def _(h):
            run("sp", h)


class Arena:
    def __init__(self, nc, es, ncols, name="arena"):
        self.t = es.enter_context(nc.sbuf_tensor(name, [128, ncols], F32))
        self.ncols = ncols
        self.off = 0
        self.uid = 0

    def mark(self):
        return self.off

    def release(self, m):
        self.off = m

    def alloc(self, cols, dtype=F32, parts=128):
        if dtype == BF16:
            w = (cols + 1) // 2
        else:
            w = cols
        assert self.off + w <= self.ncols, ("arena overflow", self.off, w, self.ncols)
        v = self.t[0:parts, self.off:self.off + w]
        self.off += w
        if dtype != F32:
            v = v.bitcast(dtype)
            if dtype == BF16 and cols % 2:
                v = v[:, 0:cols]
        return v


S = 4096
D = 1024
NT = S // 128
DIN = 4552
EPS = 1e-6
O_CQ, O_CKV, O_KIDX, O_WIDX, O_QREC, O_FREC, O_IREC, O_OG, O_GA, O_GR = 0, 256, 384, 448, 456, 968, 1480, 1992, 2504, 3528


class K:
    pass


def mkctx(nc, es, debug=()):
    k = K()
    k.nc = nc
    k.es = es
    k.P = Prog(nc)
    k.A = Arena(nc, es, 52800)
    k.ps = [es.enter_context(nc.psum_tensor("bank%d" % i, [128, 512], F32)) for i in range(8)]
    k.debug = set(debug)
    k.dr = {}
    k.uid = 0
    return k


def dram(k, name, shape, dtype):
    if name in k.dr:
        return k.dr[name]
    kind = "ExternalOutput" if name in k.debug else "Internal"
    t = k.nc.dram_tensor(name, list(shape), dtype, kind=kind).ap()
    k.dr[name] = t
    return t


def setup_consts(k):
    A, P = k.A, k.P
    k.ident = A.alloc(128, F32)
    k.identb = A.alloc(128, BF16)
    k.ones_f = A.alloc(128, F32)
    k.ones_b = A.alloc(128, BF16)
    P.op("pool", lambda e: e.memset(k.ident, 0.0), writes=["ident"])
    P.op("pool", lambda e: e.affine_select(out=k.ident, in_=k.ident, pattern=[[-1, 128]], compare_op=ALU.not_equal,
                                           fill=1.0, base=0, channel_multiplier=1), reads=["ident"], writes=["ident"])
    P.op("dve", lambda e: e.tensor_copy(k.identb, k.ident), reads=["ident"], writes=["identb"])
    P.op("dve", lambda e: e.memset(k.ones_f, 1.0), writes=["ones_f"])
    k.cneg = A.alloc(128, F32)
    P.op("pool", lambda e: e.memset(k.cneg, 0.0), writes=["cneg"])
    P.op("pool", lambda e: e.affine_select(out=k.cneg, in_=k.cneg, pattern=[[-1, 128]], compare_op=ALU.is_ge,
                                           fill=-1e30, base=0, channel_multiplier=1), reads=["cneg"], writes=["cneg"])
    k.ecap = A.alloc(32, F32)
    for e_ in range(32):
        P.op("dve", lambda e, e_=e_: e.memset(k.ecap[:, e_:e_ + 1], float(e_ * 768)), writes=["ecap"])
    k.cneg_u = A.alloc(128, F32)
    P.op("pool", lambda e: e.memset(k.cneg_u, 1.0), writes=["cneg_u"])
    P.op("pool", lambda e: e.affine_select(out=k.cneg_u, in_=k.cneg_u, pattern=[[1, 128]], compare_op=ALU.is_gt, fill=0.0, base=0, channel_multiplier=-1), reads=["cneg_u"], writes=["cneg_u"])
    k.trash = A.alloc(2, F32)
    P.op("dve", lambda e: e.tensor_reduce(out=k.trash[:, 0:1], in_=k.cneg_u, axis=AX.X, op=ALU.add), reads=["cneg_u"], writes=["trash"])
    P.op("dve", lambda e: e.tensor_scalar(k.trash[:, 0:1], k.trash[:, 0:1], 1.0, -(32.0 * 768 + 127.0), op0=ALU.mult, op1=ALU.add), reads=["trash"], writes=["trash"])
    k.cmf = A.alloc(128, F32)
    P.op("pool", lambda e: e.memset(k.cmf, 1.0), writes=["cmf"])
    P.op("pool", lambda e: e.affine_select(out=k.cmf, in_=k.cmf, pattern=[[1, 128]], compare_op=ALU.is_ge, fill=0.0, base=0, channel_multiplier=-1), reads=["cmf"], writes=["cmf"])
    P.op("pool", lambda e: e.memset(k.cmf[0:64, 64:128], 0.0), reads=["cmf"], writes=["cmf"])
    P.op("dve", lambda e: e.memset(k.ones_b, 1.0), writes=["ones_b"])


def phase0(k, l, I):
    A, P, nc = k.A, k.P, k.nc
    ps = k.ps
    stg = A.alloc(128, F32)
    k.par = A.alloc(128, F32)
    par = k.par
    P.op("dve", lambda e: e.memset(stg, 0.0), writes=["stg"])
    rows = [
        (0, 48, I["b_mod"][l].rearrange("(r c) -> r c", c=128), 128),
        (48, 8, I["c"][0].rearrange("(r c) -> r c", c=128), 128),
        (56, 8, I["g_norm1"][l].rearrange("(r c) -> r c", c=128), 128),
        (64, 8, I["g_norm2"][l].rearrange("(r c) -> r c", c=128), 128),
        (72, 2, I["g_cq"][l].rearrange("(r c) -> r c", c=128), 128),
        (74, 1, I["g_ckv"][l].rearrange("(r c) -> r c", c=128), 128),
        (75, 1, I["g_kidx"][l].rearrange("(r c) -> r c", c=64), 64),
        (76, 16, I["lb_logits"].rearrange("l (r c) -> (l r) c", c=128), 128),
        (92, 8, I["g_final"].rearrange("(r c) -> r c", c=128), 128),
    ]
    for (r0, n, src, w) in rows:
        P.dma("sp", lambda e, r0=r0, n=n, src=src, w=w: e.dma_start(out=stg[r0:r0 + n, 0:w], in_=src),
              reads=[], writes=["stg"], key="p0stg")
    P.dma("sp", lambda e: e.dma_start(out=stg[75:76, 64:128], in_=I["g_kidx"][l].rearrange("(r c) -> r c", c=64)),
          writes=["stg"], key="p0stg")
    P.op("pe", lambda e: e.transpose(ps[0][:, 0:128], stg, k.ident), reads=["stg", "ident"], writes=["ps0"])
    P.op("dve", lambda e: e.tensor_copy(par, ps[0][:, 0:128]), reads=["ps0"], writes=["par"])
    k.g1 = par[:, 56:64]; k.g2 = par[:, 64:72]; k.gcq = par[:, 72:74]; k.gckv = par[:, 74:75]
    k.gkidx = par[:, 75:76]; k.gfin = par[:, 92:100]
    sm = A.alloc(64, F32)
    k.sm = sm
    cact = sm[:, 0:8]
    t1 = sm[:, 8:16]
    P.op("act", lambda e: e.activation(out=t1, in_=par[:, 48:56], func=AF.Exp, scale=-1.0), reads=["par"], writes=["sm"])
    P.op("dve", lambda e: e.tensor_scalar(t1, t1, 1.0, None, op0=ALU.add), reads=["sm"], writes=["sm"])
    P.op("dve", lambda e: e.reciprocal(t1, t1), reads=["sm"], writes=["sm"])
    P.op("dve", lambda e: e.tensor_tensor(cact, par[:, 48:56], t1, op=ALU.mult), reads=["sm", "par"], writes=["sm"])
    lbl = par[:, 76:92].rearrange("p (l c) -> p l c", l=4)
    el = sm[:, 16:32].rearrange("p (l c) -> p l c", l=4)
    mx = sm[:, 32:36]
    P.op("dve", lambda e: e.tensor_reduce(out=mx, in_=par[:, 76:92].rearrange("p (l c) -> p c l", l=4), axis=AX.X, op=ALU.max),
         reads=["par"], writes=["sm"])
    P.op("dve", lambda e: e.tensor_tensor(el, lbl, mx.unsqueeze(1).to_broadcast([128, 4, 4]), op=ALU.subtract),
         reads=["sm", "par"], writes=["sm"])
    P.op("act", lambda e: e.activation(out=sm[:, 16:32], in_=sm[:, 16:32], func=AF.Exp), reads=["sm"], writes=["sm"])
    ssum = sm[:, 36:40]
    P.op("dve", lambda e: e.tensor_reduce(out=ssum, in_=sm[:, 16:32].rearrange("p (l c) -> p c l", l=4), axis=AX.X, op=ALU.add),
         reads=["sm"], writes=["sm"])
    P.op("dve", lambda e: e.reciprocal(ssum, ssum), reads=["sm"], writes=["sm"])
    k.lb = sm[:, 40:44]
    k.oml = sm[:, 44:48]
    P.op("dve", lambda e: e.memset(k.lb, 0.0), reads=["sm"], writes=["sm"])
    for j in range(1, l + 1):
        P.op("dve", lambda e, j=j: e.tensor_tensor(k.lb, k.lb, el[:, j, :], op=ALU.add), reads=["sm"], writes=["sm"])
    P.op("dve", lambda e: e.tensor_tensor(k.lb, k.lb, ssum, op=ALU.mult), reads=["sm"], writes=["sm"])
    P.op("dve", lambda e: e.tensor_scalar(k.lb, k.lb, 0.0, 1.0, op0=ALU.max, op1=ALU.min), reads=["sm"], writes=["sm"])
    P.op("dve", lambda e: e.tensor_scalar(k.oml, k.lb, -1.0, 1.0, op0=ALU.mult, op1=ALU.add), reads=["sm"], writes=["sm"])
    cact_rep = A.alloc(8 * 128, F32).rearrange("p (k m) -> p k m", k=8)
    P.op("dve", lambda e: e.tensor_copy(cact_rep, cact.unsqueeze(2).to_broadcast([128, 8, 128])), reads=["sm"], writes=["crep"])
    k.gtbc = [A.alloc(1024, F32), A.alloc(1024, F32)]
    bmbc = A.alloc(1024, F32)
    modf = k.A.alloc(32, F32)
    gs = k.A.alloc(16, F32)
    m = A.mark()
    wm = [A.alloc(8 * 1024, F32).rearrange("p (k c) -> p k c", k=8) for _ in range(2)]
    groups = [(0, "fm", 0), (1, "fm", 8), (3, "fm", 16), (4, "fm", 24), (2, "tm", 0), (5, "tm", 1)]
    wsrc = I["w_mod"][l].rearrange("(k p) c -> p k c", p=128)
    for gi, (g, kind, o) in enumerate(groups):
        w = wm[gi % 2]
        wk = "wm%d" % (gi % 2)
        P.dma("sp", lambda e, w=w, g=g: e.dma_start(out=w, in_=wsrc[:, :, g * 1024:(g + 1) * 1024]), writes=[wk], key=wk)
        if kind == "fm":
            for cb in range(8):
                for kc in range(8):
                    P.op("pe", lambda e, w=w, cb=cb, kc=kc, o=o: e.matmul(ps[1][:, o + cb:o + cb + 1], lhsT=w[:, kc, cb * 128:(cb + 1) * 128],
                                                                       rhs=cact[:, kc:kc + 1], start=(kc == 0), stop=(kc == 7)),
                         reads=[wk, "sm"], writes=["ps1"])
        else:
            P.dma("sp", lambda e, g=g: e.dma_start(out=bmbc, in_=I["b_mod"][l, g * 1024:(g + 1) * 1024].partition_broadcast(128)),
                  writes=["bmbc"], key="bmbc")
            for nb in range(2):
                pk = "ps%d" % (2 + nb)
                for kc in range(8):
                    P.op("pe", lambda e, w=w, nb=nb, kc=kc: e.matmul(ps[2 + nb][:, :], lhsT=cact_rep[:, kc, :], rhs=w[:, kc, nb * 512:(nb + 1) * 512],
                                                                  start=(kc == 0), stop=(kc == 7)),
                         reads=[wk, "crep"], writes=[pk])
                P.op("dve", lambda e, nb=nb, o=o: e.tensor_tensor(k.gtbc[o][:, nb * 512:(nb + 1) * 512], ps[2 + nb][:, :], bmbc[:, nb * 512:(nb + 1) * 512], op=ALU.add),
                     reads=[pk, "bmbc"], writes=["gtbc%d" % o])
    bsel = [0, 8, 24, 32]
    for i, b0 in enumerate(bsel):
        P.op("dve", lambda e, i=i, b0=b0: e.tensor_tensor(modf[:, i * 8:(i + 1) * 8], ps[1][:, i * 8:(i + 1) * 8], par[:, b0:b0 + 8], op=ALU.add),
             reads=["ps1", "par"], writes=["modf"])
    k.sh1 = modf[:, 0:8]; k.sh2 = modf[:, 16:24]
    k.gs1 = gs[:, 0:8]; k.gs2 = gs[:, 8:16]
    P.op("dve", lambda e: e.scalar_tensor_tensor(out=k.gs1, in0=modf[:, 8:16], scalar=1.0, in1=k.g1, op0=ALU.add, op1=ALU.mult),
         reads=["modf", "par"], writes=["gs"])
    P.op("dve", lambda e: e.scalar_tensor_tensor(out=k.gs2, in0=modf[:, 24:32], scalar=1.0, in1=k.g2, op0=ALU.add, op1=ALU.mult),
         reads=["modf", "par"], writes=["gs"])
    P.barrier()
    A.release(m)


def norm_transpose(k, xsrc, t, hT, gs, sh, tag, hkey):
    A, P, ps = k.A, k.P, k.ps
    xb = k.xb[t % 2]; xk = "xb%d" % (t % 2)
    xn = k.xn[t % 2]; nk = "xn%d" % (t % 2)
    st = k.nst[t % 2]; sk = "nst%d" % (t % 2)
    P.dma("sp", lambda e: e.dma_start(out=xb, in_=xsrc[t * 128:(t + 1) * 128, :]), writes=[xk], key=xk)
    P.op("act", lambda e: e.activation(out=xn, in_=xb, func=AF.Square, accum_out=st[:, 0:1]), reads=[xk], writes=[nk, sk])
    P.op("dve", lambda e: e.tensor_scalar(st[:, 1:2], st[:, 0:1], 1.0 / D, EPS, op0=ALU.mult, op1=ALU.add), reads=[sk], writes=[sk])
    P.op("act", lambda e: e.activation(out=st[:, 1:2], in_=st[:, 1:2], func=AF.Ln), reads=[sk], writes=[sk])
    P.op("act", lambda e: e.activation(out=st[:, 1:2], in_=st[:, 1:2], func=AF.Exp, scale=-0.5), reads=[sk], writes=[sk])
    P.op("dve", lambda e: e.tensor_scalar(xn, xb, st[:, 1:2], None, op0=ALU.mult), reads=[xk, sk], writes=[nk])
    b0 = (t % 2) * 2
    for kc in range(8):
        bank = b0 + kc // 4
        P.op("pe", lambda e, kc=kc, bank=bank: e.transpose(ps[bank][:, (kc % 4) * 128:(kc % 4 + 1) * 128], xn[:, kc * 128:(kc + 1) * 128], k.ident),
             reads=[nk, "ident"], writes=["ps%d" % bank])
    return b0


def phaseA(k, l, I, xsrc):
    A, P, nc, ps = k.A, k.P, k.nc, k.ps
    m0 = A.mark()
    WC = DIN + 64
    w = A.alloc(8 * WC, BF16).rearrange("p (k c) -> p k c", k=8)
    wsrc = I["w_in"][l].rearrange("(k p) c -> p k c", p=128)
    for (d0, s0, n) in [(0, 0, 448), (448, 384, 64), (512, 448, 1024), (1536, 1472, 1024), (2560, 2496, 1024), (3584, 3520, 1032)]:
        P.dma("pool", lambda e, d0=d0, s0=s0, n=n: e.dma_start(out=w[:, :, d0:d0 + n], in_=wsrc[:, :, s0:s0 + n]), writes=["w_in"], key="w_in")
    xbA = [A.alloc(1024, F32) for _ in range(4)]
    xnA = [A.alloc(1024, F32) for _ in range(2)]
    nstA = [A.alloc(8, F32) for _ in range(2)]
    hT = [A.alloc(8 * 512, BF16).rearrange("p (k t) -> p k t", k=8) for _ in range(2)]
    fo = [A.alloc(512, F32) for _ in range(4)]
    gb = [A.alloc(512, BF16) for _ in range(4)]
    to = [A.alloc(1672, F32) for _ in range(2)]
    cq_fm = dram(k, "cq_fm", [256, S], F32)
    ckv_fm = dram(k, "ckv_fm", [128, S], F32)
    kidx_fm = dram(k, "kidx_fm", [128, S], F32)
    qrec_fm = dram(k, "qrec_fm", [512, S], F32)
    frec_fm = dram(k, "frec_fm", [512, S], F32)
    gates_fm = dram(k, "gates_fm", [2048, S], BF16)
    tm_out = dram(k, "tm_out", [S, 1672], F32)
    fmb = [(0, cq_fm, 0, 0), (128, cq_fm, 128, 0), (256, ckv_fm, 0, 0), (384, kidx_fm, 0, 0)]
    for j in range(4):
        fmb.append((O_QREC + 64 + j * 128, qrec_fm, j * 128, 0))
    for j in range(4):
        fmb.append((O_FREC + 64 + j * 128, frec_fm, j * 128, 0))
    for j in range(16):
        fmb.append((O_GA + 64 + j * 128, gates_fm, j * 128, 1))
    tmg = [(256, 128, 0), (O_WIDX + 64, 8, 128), (O_IREC + 64, 512, 136), (O_OG + 64, 512, 648), (O_FREC + 64, 512, 1160)]
    fcount = 0
    for st in range(S // 512):
        h = hT[st % 2]; hk = "hT%d" % (st % 2)
        sA = nstA[st % 2]; sAk = "nstA%d" % (st % 2)
        for tt in range(4):
            t = st * 4 + tt
            xb_ = xbA[tt]; xk_ = "xbA%d" % tt
            P.dma("sp", lambda e, xb_=xb_, t=t: e.dma_start(out=xb_, in_=xsrc[t * 128:(t + 1) * 128, :]), writes=[xk_], key=xk_)
            xn_ = xnA[tt % 2]; nk_ = "xnA%d" % (tt % 2)
            P.op("act", lambda e, xb_=xb_, xn_=xn_, sA=sA, tt=tt: e.activation(out=xn_, in_=xb_, func=AF.Square, accum_out=sA[:, tt:tt + 1]), reads=[xk_], writes=[nk_, sAk])
        P.op("dve", lambda e, sA=sA: e.tensor_scalar(sA[:, 4:8], sA[:, 0:4], 1.0 / D, EPS, op0=ALU.mult, op1=ALU.add), reads=[sAk], writes=[sAk])
        P.op("act", lambda e, sA=sA: e.activation(out=sA[:, 4:8], in_=sA[:, 4:8], func=AF.Ln), reads=[sAk], writes=[sAk])
        P.op("act", lambda e, sA=sA: e.activation(out=sA[:, 4:8], in_=sA[:, 4:8], func=AF.Exp, scale=-0.5), reads=[sAk], writes=[sAk])
        for tt in range(4):
            t = st * 4 + tt
            xb_ = xbA[tt]; xk_ = "xbA%d" % tt
            xn_ = xnA[tt % 2]; nk_ = "xnA%d" % (tt % 2)
            P.op("dve", lambda e, xb_=xb_, xn_=xn_, sA=sA, tt=tt: e.tensor_scalar(xn_, xb_, sA[:, 4 + tt:5 + tt], None, op0=ALU.mult), reads=[xk_, sAk], writes=[nk_])
            b0 = (t % 2) * 2
            for kc in range(8):
                bank = b0 + kc // 4
                P.op("pe", lambda e, kc=kc, bank=bank, xn_=xn_: e.transpose(ps[bank][:, (kc % 4) * 128:(kc % 4 + 1) * 128], xn_[:, kc * 128:(kc + 1) * 128], k.ident),
                     reads=[nk_, "ident"], writes=["ps%d" % bank])
            for kc in range(8):
                bank = b0 + kc // 4
                P.op("act", lambda e, kc=kc, bank=bank, tt=tt, h=h: e.activation(out=h[:, kc, tt * 128:(tt + 1) * 128], in_=ps[bank][:, (kc % 4) * 128:(kc % 4 + 1) * 128],
                                                                                 func=AF.Identity, scale=k.gs1[:, kc:kc + 1], bias=k.sh1[:, kc:kc + 1]),
                     reads=["ps%d" % bank, "gs", "modf"], writes=[hk])
        for bi, (c0, dst, r0, act) in enumerate(fmb):
            bank = 4 + fcount % 4
            pk = "ps%d" % bank
            for kc in range(8):
                P.op("pe", lambda e, kc=kc, c0=c0, bank=bank, h=h: e.matmul(ps[bank][:, :], lhsT=w[:, kc, c0:c0 + 128], rhs=h[:, kc, :], start=(kc == 0), stop=(kc == 7)),
                     reads=["w_in", hk], writes=[pk])
            f = fo[fcount % 4]; fk = "fo%d" % (fcount % 4)
            if act == 0:
                P.op("dve", lambda e, f=f, bank=bank: e.tensor_copy(f, ps[bank][:, :]), reads=[pk], writes=[fk])
                P.dma("sp", lambda e, f=f, dst=dst, r0=r0, st=st: e.dma_start(out=dst[r0:r0 + 128, st * 512:(st + 1) * 512], in_=f), reads=[fk], writes=[], key=fk)
            else:
                fb = gb[fcount % 4]
                P.op("act", lambda e, fb=fb, bank=bank: e.activation(out=fb, in_=ps[bank][:, :], func=AF.Sigmoid), reads=[pk], writes=[fk])
                P.dma("sp", lambda e, fb=fb, dst=dst, r0=r0, st=st: e.dma_start(out=dst[r0:r0 + 128, st * 512:(st + 1) * 512], in_=fb), reads=[fk], writes=[], key=fk)
            fcount += 1
        for tt in range(4):
            t = st * 4 + tt
            tb = to[t % 2]; tk = "to%d" % (t % 2)
            for gi, (c0, n, o0) in enumerate(tmg):
                if gi == 1:
                    continue
                bank = 4 + fcount % 4
                pk = "ps%d" % bank
                subs = [(c0, n, 0)]
                if gi == 0:
                    subs = [(c0, n, 0), (tmg[1][0], 8, 128)]
                for (cc, nn, po) in subs:
                    for kc in range(8):
                        P.op("pe", lambda e, kc=kc, cc=cc, nn=nn, po=po, bank=bank, h=h, tt=tt: e.matmul(ps[bank][:, po:po + nn], lhsT=h[:, kc, tt * 128:(tt + 1) * 128], rhs=w[:, kc, cc:cc + nn],
                                                                                                  start=(kc == 0), stop=(kc == 7)),
                             reads=["w_in", hk], writes=[pk])
                tot = n + (8 if gi == 0 else 0)
                eng = "dve" if gi % 2 == 0 else "act"
                if eng == "dve":
                    P.op("dve", lambda e, tb=tb, o0=o0, tot=tot, bank=bank: e.tensor_copy(tb[:, o0:o0 + tot], ps[bank][:, 0:tot]), reads=[pk], writes=[tk])
                else:
                    P.op("act", lambda e, tb=tb, o0=o0, tot=tot, bank=bank: e.activation(out=tb[:, o0:o0 + tot], in_=ps[bank][:, 0:tot], func=AF.Copy), reads=[pk], writes=[tk])
                fcount += 1
            P.dma("sp", lambda e, tb=tb, t=t: e.dma_start(out=tm_out[t * 128:(t + 1) * 128, :], in_=tb), reads=[tk], writes=[], key=tk)
    P.barrier()
    A.release(m0)


ATTN_SCALE = 128 ** -0.5
NEG = -1e30
MASKV = -30000.0


def phaseB(k, l, I):
    A, P, ps = k.A, k.P, k.ps
    cq_fm = k.dr["cq_fm"]; ckv_fm = k.dr["ckv_fm"]; kidx_fm = k.dr["kidx_fm"]; tm_out = k.dr["tm_out"]
    qT_d = dram(k, "qT_d", [NT, 128, 8, 128], BF16)
    qidx_d = dram(k, "qidx_d", [NT, 128, 4, 128], BF16)
    k.kvT = A.alloc(S, BF16)
    k.kidxT = A.alloc(S, BF16)
    k.kvaug = A.alloc(NT * 132, BF16).rearrange("p (t c) -> p t c", t=NT)
    k.widx = A.alloc(NT * 8, F32).rearrange("p (t c) -> p t c", t=NT)
    k.wv = A.alloc(8 * 128, BF16).rearrange("p (h c) -> p h c", h=8)
    m0 = A.mark()
    wq = A.alloc(2 * 1024, BF16).rearrange("p (k c) -> p k c", k=2)
    wi = A.alloc(2 * 512, BF16).rearrange("p (k c) -> p k c", k=2)
    P.dma("pool", lambda e: e.dma_start(out=wq, in_=I["w_q_up"][l].rearrange("(k p) c -> p k c", p=128)), writes=["wq"], key="wq")
    P.dma("pool", lambda e: e.dma_start(out=wi, in_=I["w_idx_q"][l].rearrange("(k p) c -> p k c", p=128)), writes=["wi"], key="wi")
    P.op("dve", lambda e: e.memset(k.wv, 0.0), writes=["wv"])
    for par in range(2):
        P.dma("pool", lambda e, par=par: e.dma_start(out=k.wv.rearrange("p (j two) c -> p j two c", two=2)[:, :, par, par * 64:(par + 1) * 64],
                                                     in_=I["w_v_up"][l].rearrange("(j two) r v -> r j two v", two=2)[:, :, par, :]),
              writes=["wv"], key="wvd")
    ckt = A.alloc(NT * 128, F32).rearrange("p (t c) -> p t c", t=NT)
    sq = A.alloc(NT * 128, F32).rearrange("p (t c) -> p t c", t=NT)
    gb = A.alloc(128, F32)
    st = A.alloc(64, F32)
    P.dma("sp", lambda e: e.dma_start(out=ckt, in_=tm_out[:, 0:128].rearrange("(t p) c -> p t c", p=128)), writes=["ckt"], key="ckt")
    P.dma("sp", lambda e: e.dma_start(out=k.widx, in_=tm_out[:, 128:136].rearrange("(t p) c -> p t c", p=128)), writes=["widx"], key="widx")
    P.dma("sp", lambda e: e.dma_start(out=gb, in_=I["g_ckv"][l].partition_broadcast(128)), writes=["gb"], key="gb")
    P.op("dve", lambda e: e.tensor_tensor(sq, ckt, ckt, op=ALU.mult), reads=["ckt"], writes=["sq"])
    P.op("dve", lambda e: e.tensor_reduce(out=st[:, 0:32], in_=sq, axis=AX.X, op=ALU.add), reads=["sq"], writes=["stB"])
    P.op("dve", lambda e: e.tensor_scalar(st[:, 0:32], st[:, 0:32], 1.0 / 128, EPS, op0=ALU.mult, op1=ALU.add), reads=["stB"], writes=["stB"])
    P.op("act", lambda e: e.activation(out=st[:, 0:32], in_=st[:, 0:32], func=AF.Ln), reads=["stB"], writes=["stB"])
    P.op("act", lambda e: e.activation(out=st[:, 0:32], in_=st[:, 0:32], func=AF.Exp, scale=-0.5), reads=["stB"], writes=["stB"])
    P.op("dve", lambda e: e.tensor_tensor(sq, ckt, st[:, 0:32].unsqueeze(2).to_broadcast([128, NT, 128]), op=ALU.mult), reads=["ckt", "stB", "sq"], writes=["sq"])
    P.op("dve", lambda e: e.tensor_tensor(k.kvaug[:, :, 0:128], sq, gb.unsqueeze(1).to_broadcast([128, NT, 128]), op=ALU.mult), reads=["sq", "gb"], writes=["kvaug"])
    P.op("dve", lambda e: e.memset(k.kvaug[:, :, 128:132], 1.0), writes=["kvaug"])
    xin = [A.alloc(4 * 512, F32).rearrange("p (c t) -> p c t", c=4) for _ in range(2)]
    sqb = A.alloc(4 * 512, BF16).rearrange("p (c t) -> p c t", c=4)
    rs = A.alloc(3 * 512, F32).rearrange("p (c t) -> p c t", c=3)
    cqn = A.alloc(2 * 512, BF16).rearrange("p (c t) -> p c t", c=2)
    ob = [A.alloc(512, BF16) for _ in range(4)]
    oc = 0
    for b in range(S // 512):
        x = xin[b % 2]; xk = "xinB%d" % (b % 2)
        sl = slice(b * 512, (b + 1) * 512)
        P.dma("sp", lambda e, x=x, sl=sl: e.dma_start(out=x[:, 0:2, :], in_=cq_fm[:, sl].rearrange("(c p) t -> p c t", p=128)), writes=[xk], key=xk)
        P.dma("sp", lambda e, x=x, sl=sl: e.dma_start(out=x[:, 2, :], in_=ckv_fm[:, sl]), writes=[xk], key=xk)
        P.dma("sp", lambda e, x=x, sl=sl: e.dma_start(out=x[:, 3, :], in_=kidx_fm[:, sl]), writes=[xk], key=xk)
        P.op("act", lambda e, x=x: e.activation(out=sqb, in_=x, func=AF.Square), reads=[xk], writes=["sqb"])
        P.op("pe", lambda e: e.matmul(ps[0][:, :], lhsT=k.ones_b, rhs=sqb[:, 0, :], start=True, stop=False), reads=["sqb", "ones_b"], writes=["ps0"])
        P.op("pe", lambda e: e.matmul(ps[0][:, :], lhsT=k.ones_b, rhs=sqb[:, 1, :], start=False, stop=True), reads=["sqb", "ones_b"], writes=["ps0"])
        P.op("pe", lambda e: e.matmul(ps[1][:, :], lhsT=k.ones_b, rhs=sqb[:, 2, :], start=True, stop=True), reads=["sqb", "ones_b"], writes=["ps1"])
        P.op("pe", lambda e: e.matmul(ps[2][:, :], lhsT=k.ones_b, rhs=sqb[:, 3, :], start=True, stop=True), reads=["sqb", "ones_b"], writes=["ps2"])
        for i, n in enumerate([256.0, 128.0, 128.0]):
            P.op("dve", lambda e, i=i, n=n: e.tensor_scalar(rs[:, i, :], ps[i][:, :], 1.0 / n, EPS, op0=ALU.mult, op1=ALU.add), reads=["ps%d" % i], writes=["rs"])
        P.op("act", lambda e: e.activation(out=rs, in_=rs, func=AF.Ln), reads=["rs"], writes=["rs"])
        P.op("act", lambda e: e.activation(out=rs, in_=rs, func=AF.Exp, scale=-0.5), reads=["rs"], writes=["rs"])
        for c in range(2):
            P.op("dve", lambda e, c=c, x=x: e.scalar_tensor_tensor(out=cqn[:, c, :], in0=x[:, c, :], scalar=k.gcq[:, c:c + 1], in1=rs[:, 0, :], op0=ALU.mult, op1=ALU.mult),
                 reads=[xk, "rs", "par"], writes=["cqn"])
        P.op("dve", lambda e, x=x, sl=sl: e.scalar_tensor_tensor(out=k.kvT[:, sl], in0=x[:, 2, :], scalar=k.gckv[:, 0:1], in1=rs[:, 1, :], op0=ALU.mult, op1=ALU.mult),
             reads=[xk, "rs", "par"], writes=["kvT"])
        P.op("dve", lambda e, x=x, sl=sl: e.scalar_tensor_tensor(out=k.kidxT[:, sl], in0=x[:, 3, :], scalar=k.gkidx[:, 0:1], in1=rs[:, 2, :], op0=ALU.mult, op1=ALU.mult),
             reads=[xk, "rs", "par"], writes=["kidxT"])
        for h in range(8):
            bank = 4 + oc % 4; pk = "ps%d" % bank
            for c in range(2):
                P.op("pe", lambda e, h=h, c=c, bank=bank: e.matmul(ps[bank][:, :], lhsT=wq[:, c, h * 128:(h + 1) * 128], rhs=cqn[:, c, :], start=(c == 0), stop=(c == 1)),
                     reads=["wq", "cqn"], writes=[pk])
            o = ob[oc % 4]; ok = "obB%d" % (oc % 4)
            P.op("act", lambda e, o=o, bank=bank: e.activation(out=o, in_=ps[bank][:, :], func=AF.Copy, scale=ATTN_SCALE), reads=[pk], writes=[ok])
            P.dma("sp", lambda e, o=o, h=h, b=b: e.dma_start(out=qT_d[b * 4:(b + 1) * 4, :, h, :].rearrange("t r q -> r t q"), in_=o.rearrange("p (t q) -> p t q", t=4)),
                  reads=[ok], key=ok)
            oc += 1
        for j in range(4):
            bank = 4 + oc % 4; pk = "ps%d" % bank
            for c in range(2):
                P.op("pe", lambda e, j=j, c=c, bank=bank: e.matmul(ps[bank][:, :], lhsT=wi[:, c, j * 128:(j + 1) * 128], rhs=cqn[:, c, :], start=(c == 0), stop=(c == 1)),
                     reads=["wi", "cqn"], writes=[pk])
            o = ob[oc % 4]; ok = "obB%d" % (oc % 4)
            P.op("dve", lambda e, o=o, bank=bank: e.tensor_copy(o, ps[bank][:, :]), reads=[pk], writes=[ok])
            P.dma("sp", lambda e, o=o, j=j, b=b: e.dma_start(out=qidx_d[b * 4:(b + 1) * 4, :, j, :].rearrange("t r q -> r t q"), in_=o.rearrange("p (t q) -> p t q", t=4)),
                  reads=[ok], key=ok)
            oc += 1
    P.barrier()
    A.release(m0)


def phaseC(k, l, I, NITER=16):
    A, P, ps = k.A, k.P, k.ps
    qT_d = k.dr["qT_d"]; qidx_d = k.dr["qidx_d"]
    yaT_d = dram(k, "yaT_d", [4, 128, S], BF16)
    m0 = A.mark()
    qs = [A.alloc(8 * 128, BF16) for _ in range(2)]
    qi = [A.alloc(4 * 128, BF16).rearrange("p (j q) -> p j q", j=4) for _ in range(2)]
    score = [A.alloc(S, F32) for _ in range(2)]
    mb = [A.alloc(S, BF16) for _ in range(2)]
    junk = A.alloc(S, BF16)
    rb = [A.alloc(512, F32) for _ in range(3)]
    pT = [A.alloc(512, BF16) for _ in range(4)]
    on = A.alloc(8 * 128, BF16).rearrange("p (h r) -> p h r", h=8)
    onT = A.alloc(8 * 128, BF16).rearrange("p (h q) -> p h q", h=8)
    yo = [A.alloc(4 * 128, BF16).rearrange("p (j q) -> p j q", j=4) for _ in range(2)]
    ident4 = A.alloc(512, BF16)
    bs = [A.alloc(64, F32) for _ in range(2)]
    pw = A.alloc(NITER, F32)
    rden = A.alloc(8, F32)
    cm = A.alloc(2, F32)
    P.op("dve", lambda e: e.memset(cm, MASKV), writes=["cm"])
    for i in range(4):
        P.op("dve", lambda e, i=i: e.tensor_copy(ident4[:, i * 128:(i + 1) * 128], k.identb), reads=["identb"], writes=["ident4"])
    for i in range(NITER):
        P.op("dve", lambda e, i=i: e.memset(pw[:, i:i + 1], 0.5 ** (i + 1)), writes=["pw"])
    def oacc(h):
        return ps[h // 3][:, (h % 3) * 129:(h % 3) * 129 + 129]
    rc = [0]

    def indexer(qt):
        b = qt % 2
        sc = score[b]; sk = "score%d" % b
        nk = (qt + 1) * 128
        P.dma("sp", lambda e: e.dma_start(out=qs[b], in_=qT_d[qt].rearrange("r h q -> r (h q)")), writes=["qs%d" % b], key="qs%d" % b)
        P.dma("sp", lambda e: e.dma_start(out=qi[b], in_=qidx_d[qt]), writes=["qi%d" % b], key="qi%d" % b)
        for c0 in range(0, nk, 512):
            wd = min(512, nk - c0)
            for h in range(8):
                bank = 3 + rc[0] % 2; pk = "ps%d" % bank
                p0 = (h % 2) * 64
                P.op("pe", lambda e, h=h, p0=p0, bank=bank, c0=c0, wd=wd: e.matmul(ps[bank][:, 0:wd], lhsT=qi[b][p0:p0 + 64, h // 2, :], rhs=k.kidxT[p0:p0 + 64, c0:c0 + wd], start=True, stop=True),
                     reads=["qi%d" % b, "kidxT"], writes=[pk])
                r = rb[rc[0] % 3]; rk = "rb%d" % (rc[0] % 3)
                P.op("act", lambda e, r=r, bank=bank, wd=wd: e.activation(out=r[:, 0:wd], in_=ps[bank][:, 0:wd], func=AF.Relu), reads=[pk], writes=[rk])
                if h == 0:
                    P.op("dve", lambda e, r=r, c0=c0, wd=wd, h=h: e.tensor_scalar(sc[:, c0:c0 + wd], r[:, 0:wd], k.widx[:, qt, h:h + 1], None, op0=ALU.mult),
                         reads=[rk, "widx"], writes=[sk])
                else:
                    P.op("dve", lambda e, r=r, c0=c0, wd=wd, h=h: e.scalar_tensor_tensor(out=sc[:, c0:c0 + wd], in0=r[:, 0:wd], scalar=k.widx[:, qt, h:h + 1], in1=sc[:, c0:c0 + wd], op0=ALU.mult, op1=ALU.add),
                         reads=[rk, "widx", sk], writes=[sk])
                rc[0] += 1
        P.op("pool", lambda e: e.tensor_tensor(sc[:, qt * 128:(qt + 1) * 128], sc[:, qt * 128:(qt + 1) * 128], k.cneg, op=ALU.add), reads=[sk, "cneg"], writes=[sk])
        s = bs[b]; bk = "bs%d" % b
        lo = s[:, 0:1]; w0 = s[:, 1:2]; mid = s[:, 2:3]; cnt = s[:, 3:4]; tmp = s[:, 4:5]; wi_ = s[:, 8:8 + NITER]
        if qt < 2:
            P.op("dve", lambda e: e.memset(lo, -1e29), writes=[bk])
        else:
            P.op("dve", lambda e: e.tensor_reduce(out=w0, in_=sc[:, 0:nk], axis=AX.X, op=ALU.max), reads=[sk], writes=[bk])
            P.op("dve", lambda e: e.tensor_reduce(out=lo, in_=sc[:, 0:qt * 128], axis=AX.X, op=ALU.min), reads=[sk], writes=[bk])
            P.op("dve", lambda e: e.tensor_scalar(lo, lo, -1.0, None, op0=ALU.add), reads=[bk], writes=[bk])
            P.op("dve", lambda e: e.tensor_tensor(w0, w0, lo, op=ALU.subtract), reads=[bk], writes=[bk])
            P.op("dve", lambda e: e.tensor_scalar(wi_, pw, w0, None, op0=ALU.mult), reads=[bk, "pw"], writes=[bk])
            for it in range(NITER):
                P.op("dve", lambda e, it=it: e.tensor_tensor(mid, lo, wi_[:, it:it + 1], op=ALU.add), reads=[bk], writes=[bk])
                P.op("dve", lambda e: e.tensor_scalar(junk[:, 0:nk], sc[:, 0:nk], mid, None, op0=ALU.is_gt, op1=ALU.add, accum_out=cnt), reads=[sk, bk], writes=[bk, "junk"])
                P.op("dve", lambda e, it=it: e.scalar_tensor_tensor(out=tmp, in0=cnt, scalar=256.0, in1=wi_[:, it:it + 1], op0=ALU.is_ge, op1=ALU.mult), reads=[bk], writes=[bk])
                P.op("dve", lambda e: e.tensor_tensor(lo, lo, tmp, op=ALU.add), reads=[bk], writes=[bk])
        P.op("dve", lambda e: e.tensor_scalar(mb[b][:, 0:nk], sc[:, 0:nk], lo, cm[:, 0:1], op0=ALU.is_le, op1=ALU.mult), reads=[sk, bk, "cm"], writes=["mb%d" % b])

    pc = [0]

    def attention(qt):
        b = qt % 2
        q = qs[b]
        for kb in range(qt + 1):
            for hg in range(2):
                bank = 5 + pc[0] % 2; pk = "ps%d" % bank
                P.op("pe", lambda e, kb=kb, hg=hg, bank=bank: e.matmul(ps[bank][:, :], lhsT=k.kvT[:, kb * 128:(kb + 1) * 128], rhs=q[:, hg * 512:(hg + 1) * 512], start=True, stop=False),
                     reads=["kvT", "qs%d" % b], writes=[pk])
                P.op("pe", lambda e, kb=kb, bank=bank: e.matmul(ps[bank][:, :], lhsT=mb[b][:, kb * 128:(kb + 1) * 128], rhs=ident4, start=False, stop=True),
                     reads=["mb%d" % b, "ident4"], writes=[pk])
                p = pT[pc[0] % 4]; pk2 = "pT%d" % (pc[0] % 4)
                P.op("act", lambda e, p=p, bank=bank: e.activation(out=p, in_=ps[bank][:, :], func=AF.Exp), reads=[pk], writes=[pk2])
                for hh in range(4):
                    h = hg * 4 + hh
                    P.op("pe", lambda e, p=p, hh=hh, h=h, kb=kb: e.matmul(oacc(h), lhsT=p[:, hh * 128:(hh + 1) * 128], rhs=k.kvaug[:, kb, 0:129], start=(kb == 0 and h % 3 == 0), stop=(kb == qt), skip_group_check=True),
                         reads=[pk2, "kvaug"], writes=["ps%d" % (h // 3)])
                pc[0] += 1
        for bnk in range(3):
            nh = 3 if bnk < 2 else 2
            v = ps[bnk][:, 0:nh * 129].rearrange("p (h c) -> p h c", c=129)
            P.op("dve", lambda e, v=v, bnk=bnk, nh=nh: e.reciprocal(rden[:, bnk * 3:bnk * 3 + nh], v[:, :, 128]), reads=["ps%d" % bnk], writes=["rden"])
            P.op("dve", lambda e, v=v, bnk=bnk, nh=nh: e.tensor_tensor(on[:, bnk * 3:bnk * 3 + nh, :], v[:, :, 0:128], rden[:, bnk * 3:bnk * 3 + nh].unsqueeze(2).to_broadcast([128, nh, 128]), op=ALU.mult),
                 reads=["ps%d" % bnk, "rden"], writes=["on"])
        if qt == 1 and "dbg_on" in k.debug:
            dbg = dram(k, "dbg_on", [128, 1024], BF16)
            P.dma("sp", lambda e: e.dma_start(out=dbg, in_=on.rearrange("p h r -> p (h r)")), reads=["on"], key="dbg1")
            dbg2 = dram(k, "dbg_mb", [128, 256], BF16)
            P.dma("sp", lambda e: e.dma_start(out=dbg2, in_=mb[b][:, 0:256]), reads=["mb%d" % b], key="dbg2")
            dbg3 = dram(k, "dbg_sc", [128, 256], F32)
            P.dma("sp", lambda e: e.dma_start(out=dbg3, in_=score[b][:, 0:256]), reads=["score%d" % b], key="dbg3")
            dbg4 = dram(k, "dbg_kv", [128, 32 * 132], BF16)
            P.dma("sp", lambda e: e.dma_start(out=dbg4, in_=k.kvaug.rearrange("p t c -> p (t c)")), reads=["kvaug"], key="dbg4")
            dbg5 = dram(k, "dbg_qs", [128, 1024], BF16)
            P.dma("sp", lambda e: e.dma_start(out=dbg5, in_=q), reads=["qs%d" % b], key="dbg5")
            dbg6 = dram(k, "dbg_kvT", [128, S], BF16)
            P.dma("sp", lambda e: e.dma_start(out=dbg6, in_=k.kvT), reads=["kvT"], key="dbg6")
        tb = ps[7][:, :].bitcast(BF16)
        for h in range(8):
            P.op("pe", lambda e, h=h: e.transpose(tb[:, h * 128:(h + 1) * 128], on[:, h, :], k.identb), reads=["on", "identb"], writes=["ps7"])
        P.op("act", lambda e: e.activation(out=onT.rearrange("p h q -> p (h q)"), in_=tb, func=AF.Copy), reads=["ps7"], writes=["onT"])
        for j in range(4):
            for two in range(2):
                h = j * 2 + two
                P.op("pe", lambda e, j=j, two=two, h=h: e.matmul(ps[7][:, j * 128:(j + 1) * 128], lhsT=k.wv[:, h, :], rhs=onT[:, h, :], start=(two == 0), stop=(two == 1)),
                     reads=["wv", "onT"], writes=["ps7"])
        y = yo[qt % 2]; yk = "yo%d" % (qt % 2)
        P.op("dve", lambda e: e.tensor_copy(y.rearrange("p j q -> p (j q)"), ps[7][:, :]), reads=["ps7"], writes=[yk])
        P.dma("sp", lambda e: e.dma_start(out=yaT_d[:, :, qt * 128:(qt + 1) * 128].rearrange("j p q -> p j q"), in_=y), reads=[yk], key=yk)

    indexer(0)
    for qt in range(NT):
        if qt + 1 < NT:
            indexer(qt + 1)
        attention(qt)
    P.barrier()
    A.release(m0)


def phaseD(k, l, I):
    A, P, ps = k.A, k.P, k.ps
    qrec_fm = k.dr["qrec_fm"]; frec_fm = k.dr["frec_fm"]; tm_out = k.dr["tm_out"]
    yrT_d = dram(k, "yrT_d", [4, 128, S], BF16)
    m0 = A.mark()
    NB = 512
    def fm(dt=F32):
        return A.alloc(4 * NB, dt).rearrange("p (j t) -> p j t", j=4)
    z = fm(); qr = fm(); e = fm(); t1 = fm(); t2 = fm(); Acum = fm(); kk = fm(); qq = fm()
    qt_ = fm(BF16); kt_ = fm(BF16); qhA = fm(BF16); qhB = fm(BF16); kh = fm(BF16)
    cmf = k.cmf
    grb = A.alloc(512, F32)
    vt = A.alloc(4 * 512, BF16).rearrange("p (t c) -> p t c", t=4)
    ogt = A.alloc(4 * 512, F32).rearrange("p (t c) -> p t c", t=4)
    vtf = A.alloc(4 * 512, F32).rearrange("p (t c) -> p t c", t=4)
    sog = A.alloc(4 * 512, F32).rearrange("p (t c) -> p t c", t=4)
    khT = A.alloc(512, BF16)
    Pm = A.alloc(8 * 128, BF16).rearrange("p (h t) -> p h t", h=8)
    state = A.alloc(4 * 64, F32).rearrange("p (j v) -> p j v", j=4)
    stmp = A.alloc(4 * 64, F32).rearrange("p (j v) -> p j v", j=4)
    sbf = [A.alloc(4 * 64, BF16).rearrange("p (j v) -> p j v", j=4) for _ in range(4)]
    decay = A.alloc(32, F32)
    osb = A.alloc(512, F32); osq = A.alloc(512, F32); oss = A.alloc(16, F32)
    sg = A.alloc(512, F32)
    yb = A.alloc(512, BF16)
    yT = [A.alloc(512, BF16) for _ in range(2)]
    P.dma("sp", lambda e_: e_.dma_start(out=osb[:, 0:64], in_=I["g_rec"][l].partition_broadcast(128)), writes=["osb"], key="grb")
    P.op("dve", lambda e_: e_.tensor_copy(grb.rearrange("p (h v) -> p h v", h=8), osb[:, 0:64].unsqueeze(1).to_broadcast([128, 8, 64])), reads=["osb"], writes=["grb"])
    P.op("dve", lambda e_: e_.memset(state, 0.0), writes=["state"])
    P.op("dve", lambda e_: e_.memset(sbf[0], 0.0), writes=["sbf0"])
    P.op("dve", lambda e_: e_.memset(qhA, 0.0), writes=["qhA"])
    P.op("dve", lambda e_: e_.memset(qhB, 0.0), writes=["qhB"])
    sv = [0]
    z2 = z.rearrange("p j t -> p (j t)"); e2 = e.rearrange("p j t -> p (j t)"); t12 = t1.rearrange("p j t -> p (j t)"); t22 = t2.rearrange("p j t -> p (j t)")
    A2 = Acum.rearrange("p j t -> p (j t)")
    def ch(x):
        return x.rearrange("p j (c t) -> p (j c) t", t=64)
    for b in range(S // NB):
        sl = slice(b * NB, (b + 1) * NB)
        P.dma("sp", lambda e_, sl=sl: e_.dma_start(out=z, in_=frec_fm[:, sl].rearrange("(j p) t -> p j t", p=128)), writes=["z"], key="zD")
        P.dma("sp", lambda e_, sl=sl: e_.dma_start(out=qr, in_=qrec_fm[:, sl].rearrange("(j p) t -> p j t", p=128)), writes=["qr"], key="qrD")
        P.dma("sp", lambda e_, sl=sl: e_.dma_start(out=vtf, in_=tm_out[sl, 136:648].rearrange("(t p) c -> p t c", p=128)), writes=["vtf"], key="vtD")
        P.op("act", lambda e_: e_.activation(out=vt, in_=vtf, func=AF.Copy), reads=["vtf"], writes=["vt"])
        P.dma("sp", lambda e_, sl=sl: e_.dma_start(out=ogt, in_=tm_out[sl, 648:1160].rearrange("(t p) c -> p t c", p=128)), writes=["ogt"], key="ogD")
        P.op("act", lambda e_: e_.activation(out=kk, in_=z, func=AF.Sigmoid, scale=-1.0), reads=["z"], writes=["kk"])
        P.op("act", lambda e_: e_.activation(out=qq, in_=qr, func=AF.Sigmoid), reads=["qr"], writes=["qq"])
        P.op("act", lambda e_: e_.activation(out=sog, in_=ogt, func=AF.Sigmoid), reads=["ogt"], writes=["sog"])
        P.op("act", lambda e_: e_.activation(out=e, in_=z, func=AF.Exp, scale=-1.0), reads=["z"], writes=["e"])
        for j in range(4):
            P.op("dve", lambda e_, j=j: e_.tensor_scalar(t1[:, j, :], e[:, j, :], k.lb[:, j:j + 1], 1.0, op0=ALU.mult, op1=ALU.add), reads=["e", "sm"], writes=["t1"])
        P.op("dve", lambda e_: e_.tensor_scalar(t2, e, 1.0, None, op0=ALU.add), reads=["e"], writes=["t2"])
        P.op("act", lambda e_: e_.activation(out=t1, in_=t1, func=AF.Ln), reads=["t1"], writes=["t1"])
        P.op("act", lambda e_: e_.activation(out=z, in_=t2, func=AF.Ln), reads=["t2", "z"], writes=["z"])
        P.op("dve", lambda e_: e_.tensor_tensor(t1, t1, z, op=ALU.subtract), reads=["t1", "z"], writes=["t1"])
        for j in range(4):
            P.op("dve", lambda e_, j=j: e_.tensor_scalar(kk[:, j, :], kk[:, j, :], k.oml[:, j:j + 1], None, op0=ALU.mult), reads=["kk", "sm"], writes=["kk"])
        srcs = [t1, Acum]
        for si, sh in enumerate([1, 2, 4, 8, 16, 32]):
            a_ = ch(srcs[si % 2]); b_ = ch(srcs[(si + 1) % 2])
            P.op("dve", lambda e_, a_=a_, b_=b_, sh=sh: e_.tensor_tensor(b_[:, :, sh:64], a_[:, :, sh:64], a_[:, :, 0:64 - sh], op=ALU.add), reads=["t1", "Acum"], writes=["t1", "Acum"])
            P.op("act", lambda e_, a_=a_, b_=b_, sh=sh: e_.activation(out=b_[:, :, 0:sh], in_=a_[:, :, 0:sh], func=AF.Copy), reads=["t1", "Acum"], writes=["t1", "Acum"])
        P.op("act", lambda e_: e_.activation(out=Acum, in_=t1, func=AF.Copy), reads=["t1", "Acum"], writes=["t1", "Acum"])
        P.op("dve", lambda e_: e_.tensor_tensor(qq, qq, qr, op=ALU.mult), reads=["qq", "qr"], writes=["qq"])
        Ac = ch(Acum)
        P.op("dve", lambda e_: e_.tensor_tensor(ch(t1), Ac, Ac[:, :, 31:32].to_broadcast([128, 32, 64]), op=ALU.subtract), reads=["Acum", "t1"], writes=["t1"])
        P.op("dve", lambda e_: e_.tensor_scalar(t1, t1, -40.0, 40.0, op0=ALU.max, op1=ALU.min), reads=["t1"], writes=["t1"])
        P.op("act", lambda e_: e_.activation(out=t2, in_=t1, func=AF.Exp), reads=["t1"], writes=["t2"])
        P.op("dve", lambda e_: e_.tensor_tensor(qt_, qq, t2, op=ALU.mult), reads=["qq", "t2"], writes=["qt_"])
        P.op("act", lambda e_: e_.activation(out=t2, in_=t1, func=AF.Exp, scale=-1.0), reads=["t1", "qt_"], writes=["t2"])
        P.op("dve", lambda e_: e_.tensor_tensor(kt_, kk, t2, op=ALU.mult), reads=["kk", "t2"], writes=["kt_"])
        P.op("act", lambda e_: e_.activation(out=t2, in_=Acum, func=AF.Exp), reads=["Acum", "kt_"], writes=["t2"])
        def eo(x, par):
            return x.rearrange("p j (c two t) -> p j c two t", two=2, t=64)[:, :, :, par, :]
        P.op("dve", lambda e_: e_.tensor_tensor(eo(qhA, 0), eo(qq, 0), eo(t2, 0), op=ALU.mult), reads=["qq", "t2"], writes=["qhA"])
        P.op("dve", lambda e_: e_.tensor_tensor(eo(qhB, 1), eo(qq, 1), eo(t2, 1), op=ALU.mult), reads=["qq", "t2"], writes=["qhB"])
        P.op("act", lambda e_: e_.activation(out=decay, in_=Ac[:, :, 63], func=AF.Exp), reads=["Acum"], writes=["decay"])
        P.op("dve", lambda e_: e_.tensor_tensor(ch(t1), Ac[:, :, 63:64].to_broadcast([128, 32, 64]), Ac, op=ALU.subtract), reads=["Acum", "t1"], writes=["t1"])
        P.op("act", lambda e_: e_.activation(out=t2, in_=t1, func=AF.Exp), reads=["t1", "qhA", "qhB"], writes=["t2"])
        P.op("dve", lambda e_: e_.tensor_tensor(kh, kk, t2, op=ALU.mult), reads=["kk", "t2"], writes=["kh"])
        for tt in range(4):
            tsl = slice(tt * 128, (tt + 1) * 128)
            tb = ps[7][:, :].bitcast(BF16)
            for j in range(4):
                P.op("pe", lambda e_, j=j, tsl=tsl: e_.transpose(tb[:, j * 128:(j + 1) * 128], kh[:, j, tsl], k.identb), reads=["kh", "identb"], writes=["ps7"])
            P.op("act", lambda e_: e_.activation(out=khT, in_=tb[:, 0:512], func=AF.Copy), reads=["ps7"], writes=["khT"])
            for h in range(8):
                j = h // 2; p0 = (h % 2) * 64
                bank = 5 + h % 2
                P.op("pe", lambda e_, h=h, j=j, p0=p0, bank=bank, tsl=tsl: e_.matmul(ps[bank][:, j * 128:(j + 1) * 128], lhsT=kt_[p0:p0 + 64, j, tsl], rhs=qt_[p0:p0 + 64, j, tsl], start=True, stop=True),
                     reads=["kt_", "qt_"], writes=["ps%d" % bank])
            for g in range(2):
                P.op("dve", lambda e_, g=g: e_.tensor_tensor(Pm[:, g * 4:(g + 1) * 4, :], ps[5 + g][:, :].rearrange("p (h t) -> p h t", h=4), cmf.unsqueeze(1).to_broadcast([128, 4, 128]), op=ALU.mult),
                     reads=["ps%d" % (5 + g), "cmf"], writes=["Pm"])
            svA = sv[0]
            for half in range(2):
                c = tt * 2 + half
                hs = slice(half * 64, (half + 1) * 64)
                for j in range(4):
                    P.op("pe", lambda e_, j=j, hs=hs, tt=tt: e_.matmul(ps[4][:, j * 128:(j + 1) * 128], lhsT=khT[hs, j * 128:(j + 1) * 128], rhs=vt[hs, tt, j * 128:(j + 1) * 128], start=True, stop=True),
                         reads=["khT", "vt"], writes=["ps4"])
                dcol = [jj * 8 + c for jj in range(4)]
                dv = decay.rearrange("p (j c) -> p j c", j=4)[:, :, c:c + 1]
                P.op("dve", lambda e_, dv=dv: e_.tensor_tensor(stmp, state, dv.to_broadcast([128, 4, 64]), op=ALU.mult), reads=["state", "decay"], writes=["stmp"])
                pv = ps[4][:, :].rearrange("p (j x) -> p j x", j=4)
                P.op("dve", lambda e_, pv=pv: e_.tensor_tensor(state[0:64], stmp[0:64], pv[0:64, :, 0:64], op=ALU.add), reads=["stmp", "ps4"], writes=["state"])
                P.op("dve", lambda e_, pv=pv: e_.tensor_tensor(state[64:128], stmp[64:128], pv[64:128, :, 64:128], op=ALU.add), reads=["stmp", "ps4"], writes=["state"])
                sv[0] += 1
                sb_ = sbf[sv[0] % 4]
                P.op("act", lambda e_, sb_=sb_: e_.activation(out=sb_, in_=state, func=AF.Copy), reads=["state"], writes=["sbf%d" % (sv[0] % 4)])
            s0 = sbf[svA % 4]; s0k = "sbf%d" % (svA % 4)
            s1 = sbf[(svA + 1) % 4]; s1k = "sbf%d" % ((svA + 1) % 4)
            for h in range(8):
                j = h // 2; p0 = (h % 2) * 64
                oo = ps[3][:, h * 64:(h + 1) * 64]
                P.op("pe", lambda e_, h=h, oo=oo, tt=tt: e_.matmul(oo, lhsT=Pm[:, (h % 2) * 4 + h // 2, :], rhs=vt[:, tt, h * 64:(h + 1) * 64], start=True, stop=False), reads=["Pm", "vt"], writes=["ps3"])
                P.op("pe", lambda e_, j=j, p0=p0, oo=oo, tsl=tsl, s0=s0: e_.matmul(oo, lhsT=qhA[p0:p0 + 64, j, tsl], rhs=s0[p0:p0 + 64, j, :], start=False, stop=False), reads=["qhA", s0k], writes=["ps3"])
                P.op("pe", lambda e_, j=j, p0=p0, oo=oo, tsl=tsl, s1=s1: e_.matmul(oo, lhsT=qhB[p0:p0 + 64, j, tsl], rhs=s1[p0:p0 + 64, j, :], start=False, stop=True), reads=["qhB", s1k], writes=["ps3"])
            P.op("act", lambda e_: e_.activation(out=osb, in_=ps[3][:, :], func=AF.Copy), reads=["ps3"], writes=["osb"])
            P.op("dve", lambda e_: e_.tensor_tensor(osq, osb, osb, op=ALU.mult), reads=["osb"], writes=["osq"])
            P.op("dve", lambda e_: e_.tensor_reduce(out=oss[:, 0:8], in_=osq.rearrange("p (h v) -> p h v", h=8), axis=AX.X, op=ALU.add), reads=["osq"], writes=["oss"])
            P.op("dve", lambda e_: e_.tensor_scalar(oss[:, 0:8], oss[:, 0:8], 1.0 / 64, EPS, op0=ALU.mult, op1=ALU.add), reads=["oss"], writes=["oss"])
            P.op("act", lambda e_: e_.activation(out=oss[:, 0:8], in_=oss[:, 0:8], func=AF.Ln), reads=["oss"], writes=["oss"])
            P.op("act", lambda e_: e_.activation(out=oss[:, 0:8], in_=oss[:, 0:8], func=AF.Exp, scale=-0.5), reads=["oss"], writes=["oss"])
            P.op("dve", lambda e_: e_.tensor_tensor(osq.rearrange("p (h v) -> p h v", h=8), osb.rearrange("p (h v) -> p h v", h=8), oss[:, 0:8].unsqueeze(2).to_broadcast([128, 8, 64]), op=ALU.mult),
                 reads=["osb", "oss", "osq"], writes=["osq"])
            P.op("dve", lambda e_: e_.tensor_tensor(osq, osq, grb, op=ALU.mult), reads=["osq", "grb"], writes=["osq"])
            P.op("dve", lambda e_, tt=tt: e_.tensor_tensor(sg, sog[:, tt, :], ogt[:, tt, :], op=ALU.mult), reads=["sog", "ogt"], writes=["sg"])
            P.op("dve", lambda e_: e_.tensor_tensor(yb, osq, sg, op=ALU.mult), reads=["osq", "sg"], writes=["yb"])
            for j in range(4):
                P.op("pe", lambda e_, j=j: e_.transpose(tb[:, 512 + j * 128:512 + (j + 1) * 128], yb[:, j * 128:(j + 1) * 128], k.identb), reads=["yb", "identb"], writes=["ps7"])
            t = b * 4 + tt
            y = yT[t % 2]; yk = "yTD%d" % (t % 2)
            P.op("act", lambda e_, y=y: e_.activation(out=y, in_=tb[:, 512:1024], func=AF.Copy), reads=["ps7"], writes=[yk])
            P.dma("sp", lambda e_, y=y, t=t: e_.dma_start(out=yrT_d[:, :, t * 128:(t + 1) * 128].rearrange("j p q -> p j q"), in_=y.rearrange("p (j q) -> p j q", j=4)), reads=[yk], key=yk)
    P.barrier()
    A.release(m0)


def phaseE(k, l, I, xsrc, xdst):
    A, P, ps = k.A, k.P, k.ps
    yaT_d = k.dr["yaT_d"]; yrT_d = k.dr["yrT_d"]; gates_fm = k.dr["gates_fm"]
    m0 = A.mark()
    wa = A.alloc(4 * 1024, BF16).rearrange("p (k c) -> p k c", k=4)
    wr = A.alloc(4 * 1024, BF16).rearrange("p (k c) -> p k c", k=4)
    wo = A.alloc(8 * 1024, BF16).rearrange("p (k c) -> p k c", k=8)
    P.dma("pool", lambda e: e.dma_start(out=wa, in_=I["w_branch_a"][l].rearrange("(k p) c -> p k c", p=128)), writes=["wa"], key="wa")
    P.dma("pool", lambda e: e.dma_start(out=wr, in_=I["w_branch_r"][l].rearrange("(k p) c -> p k c", p=128)), writes=["wr"], key="wr")
    P.dma("pool", lambda e: e.dma_start(out=wo, in_=I["w_out"][l].rearrange("(k p) c -> p k c", p=128)), writes=["wo"], key="wo")
    ya = [A.alloc(4 * 512, BF16).rearrange("p (k t) -> p k t", k=4) for _ in range(2)]
    yr = [A.alloc(4 * 512, BF16).rearrange("p (k t) -> p k t", k=4) for _ in range(2)]
    gt = [A.alloc(16 * 512, BF16).rearrange("p (k t) -> p k t", k=16) for _ in range(2)]
    mT = A.alloc(8 * 512, BF16).rearrange("p (k t) -> p k t", k=8)
    m1 = [A.alloc(512, F32) for _ in range(2)]
    m2 = [A.alloc(512, F32) for _ in range(2)]
    xb = [A.alloc(1024, F32) for _ in range(2)]
    tb_ = [A.alloc(1024, F32) for _ in range(2)]
    cnt = 0
    for b in range(S // 512):
        sl = slice(b * 512, (b + 1) * 512)
        i2 = b % 2
        P.dma("sp", lambda e, i2=i2, sl=sl: e.dma_start(out=ya[i2], in_=yaT_d[:, :, sl].rearrange("j p t -> p j t")), writes=["yaE%d" % i2], key="yaE%d" % i2)
        P.dma("sp", lambda e, i2=i2, sl=sl: e.dma_start(out=yr[i2], in_=yrT_d[:, :, sl].rearrange("j p t -> p j t")), writes=["yrE%d" % i2], key="yrE%d" % i2)
        P.dma("sp", lambda e, i2=i2, sl=sl: e.dma_start(out=gt[i2], in_=gates_fm[:, sl].rearrange("(j p) t -> p j t", p=128)), writes=["gtE%d" % i2], key="gtE%d" % i2)
        for cb in range(8):
            ba = cnt % 2; bb = 2 + cnt % 2
            for kc in range(4):
                P.op("pe", lambda e, kc=kc, cb=cb, ba=ba, i2=i2: e.matmul(ps[ba][:, :], lhsT=wa[:, kc, cb * 128:(cb + 1) * 128], rhs=ya[i2][:, kc, :], start=(kc == 0), stop=(kc == 3)),
                     reads=["wa", "yaE%d" % i2], writes=["ps%d" % ba])
            for kc in range(4):
                P.op("pe", lambda e, kc=kc, cb=cb, bb=bb, i2=i2: e.matmul(ps[bb][:, :], lhsT=wr[:, kc, cb * 128:(cb + 1) * 128], rhs=yr[i2][:, kc, :], start=(kc == 0), stop=(kc == 3)),
                     reads=["wr", "yrE%d" % i2], writes=["ps%d" % bb])
            a1 = m1[cnt % 2]; a2 = m2[cnt % 2]
            P.op("dve", lambda e, a1=a1, ba=ba, cb=cb, i2=i2: e.tensor_tensor(a1, ps[ba][:, :], gt[i2][:, cb, :], op=ALU.mult), reads=["ps%d" % ba, "gtE%d" % i2], writes=["m1%d" % (cnt % 2)])
            P.op("dve", lambda e, a2=a2, bb=bb, cb=cb, i2=i2: e.tensor_tensor(a2, ps[bb][:, :], gt[i2][:, 8 + cb, :], op=ALU.mult), reads=["ps%d" % bb, "gtE%d" % i2], writes=["m2%d" % (cnt % 2)])
            P.op("dve", lambda e, a1=a1, a2=a2, cb=cb: e.tensor_tensor(mT[:, cb, :], a1, a2, op=ALU.add), reads=["m1%d" % (cnt % 2), "m2%d" % (cnt % 2)], writes=["mT"])
            cnt += 1
        for tt in range(4):
            t = b * 4 + tt
            x = xb[t % 2]; xk = "xbE%d" % (t % 2)
            tq = tb_[t % 2]; tk = "tbE%d" % (t % 2)
            P.dma("sp", lambda e, x=x, t=t: e.dma_start(out=x, in_=xsrc[t * 128:(t + 1) * 128, :]), writes=[xk], key=xk)
            for half in range(2):
                bank = 4 + (t * 2 + half) % 4
                for kc in range(8):
                    P.op("pe", lambda e, kc=kc, half=half, bank=bank, tt=tt: e.matmul(ps[bank][:, :], lhsT=mT[:, kc, tt * 128:(tt + 1) * 128], rhs=wo[:, kc, half * 512:(half + 1) * 512], start=(kc == 0), stop=(kc == 7)),
                         reads=["mT", "wo"], writes=["ps%d" % bank])
                P.op("dve", lambda e, tq=tq, half=half, bank=bank: e.tensor_tensor(tq[:, half * 512:(half + 1) * 512], ps[bank][:, :], k.gtbc[0][:, half * 512:(half + 1) * 512], op=ALU.mult),
                     reads=["ps%d" % bank, "gtbc0"], writes=[tk])
            P.op("dve", lambda e, tq=tq, x=x: e.tensor_tensor(tq, tq, x, op=ALU.add), reads=[tk, xk], writes=[tk])
            P.dma("sp", lambda e, tq=tq, t=t: e.dma_start(out=xdst[t * 128:(t + 1) * 128, :], in_=tq), reads=[tk], key=tk)
    P.barrier()
    A.release(m0)


def phaseF(k, l, I, xsrc, xdst):
    A, P, ps = k.A, k.P, k.ps
    m0 = A.mark()
    h2T = A.alloc(8 * S, BF16).rearrange("p (k t) -> p k t", k=8)
    gate = A.alloc(NT * 32, F32).rearrange("p (t c) -> p t c", t=NT)
    k.xb = [A.alloc(1024, F32) for _ in range(2)]
    m1_ = A.mark()
    hf = [A.alloc(8 * 128, F32).rearrange("p (k t) -> p k t", k=8) for _ in range(2)]
    wrt = A.alloc(8 * 36, F32).rearrange("p (k c) -> p k c", k=8)
    rb = A.alloc(36, F32)
    lg = A.alloc(NT * 36, F32).rearrange("p (t c) -> p t c", t=NT)
    k.xn = [A.alloc(1024, F32) for _ in range(2)]
    k.nst = [A.alloc(2, F32) for _ in range(2)]
    P.dma("sp", lambda e: e.dma_start(out=wrt[:, :, 0:4], in_=I["w_grp"][l].rearrange("(k p) c -> p k c", p=128)), writes=["wrt"], key="wrt")
    P.dma("sp", lambda e: e.dma_start(out=wrt[:, :, 4:36], in_=I["w_exp_router"][l].rearrange("(k p) c -> p k c", p=128)), writes=["wrt"], key="wrt")
    P.dma("sp", lambda e: e.dma_start(out=rb[:, 0:4], in_=I["b_grp"][l].partition_broadcast(128)), writes=["rbF"], key="rbF")
    P.dma("sp", lambda e: e.dma_start(out=rb[:, 4:36], in_=I["b_exp_router"][l].partition_broadcast(128)), writes=["rbF"], key="rbF")
    for t in range(NT):
        b0 = norm_transpose(k, xsrc, t, None, None, None, None, None)
        f = hf[t % 2]; fk = "hfF%d" % (t % 2)
        for kc in range(8):
            bank = b0 + kc // 4
            P.op("act", lambda e, kc=kc, bank=bank, f=f: e.activation(out=f[:, kc, :], in_=ps[bank][:, (kc % 4) * 128:(kc % 4 + 1) * 128], func=AF.Identity, scale=k.gs2[:, kc:kc + 1], bias=k.sh2[:, kc:kc + 1]),
                 reads=["ps%d" % bank, "gs", "modf"], writes=[fk])
        P.op("dve", lambda e, f=f, t=t: e.tensor_copy(h2T[:, :, t * 128:(t + 1) * 128], f), reads=[fk], writes=["h2T"])
        bank = 4 + t % 2
        for kc in range(8):
            P.op("pe", lambda e, kc=kc, f=f, bank=bank: e.matmul(ps[bank][:, 0:36], lhsT=f[:, kc, :], rhs=wrt[:, kc, :], start=(kc == 0), stop=(kc == 7)), reads=[fk, "wrt"], writes=["ps%d" % bank])
        P.op("dve", lambda e, t=t, bank=bank: e.tensor_tensor(lg[:, t, :], ps[bank][:, 0:36], rb, op=ALU.add), reads=["ps%d" % bank, "rbF"], writes=["lg"])
    g4 = A.alloc(NT * 4, F32).rearrange("p (t c) -> p t c", t=NT)
    oh = A.alloc(NT * 4, F32).rearrange("p (t c) -> p t c", t=NT)
    s1 = A.alloc(NT * 8, F32)
    le = A.alloc(NT * 32, F32).rearrange("p (t c) -> p t c", t=NT)
    o1 = A.alloc(NT * 32, F32).rearrange("p (t c) -> p t c", t=NT)
    o2 = A.alloc(NT * 32, F32).rearrange("p (t c) -> p t c", t=NT)
    mx = s1[:, 0:NT]; gs_ = s1[:, NT:2 * NT]; mA = s1[:, 2 * NT:3 * NT]; mB = s1[:, 3 * NT:4 * NT]; w1 = s1[:, 4 * NT:5 * NT]; w2 = s1[:, 5 * NT:6 * NT]
    R = ["lg", "g4", "oh", "s1", "le", "o1", "o2", "gate"]
    def D(fn):
        P.op("dve", fn, reads=R, writes=R)
    bc4 = lambda v: v.unsqueeze(2).to_broadcast([128, NT, 4])
    bc32 = lambda v: v.unsqueeze(2).to_broadcast([128, NT, 32])
    D(lambda e: e.tensor_reduce(out=mx, in_=lg[:, :, 0:4], axis=AX.X, op=ALU.max))
    D(lambda e: e.tensor_tensor(oh, lg[:, :, 0:4], bc4(mx), op=ALU.is_ge))
    D(lambda e: e.tensor_tensor(g4, lg[:, :, 0:4], bc4(mx), op=ALU.subtract))
    P.op("act", lambda e: e.activation(out=g4, in_=g4, func=AF.Exp), reads=R, writes=R)
    D(lambda e: e.tensor_reduce(out=gs_, in_=g4, axis=AX.X, op=ALU.add))
    D(lambda e: e.reciprocal(gs_, gs_))
    lev = le.rearrange("p t (g x) -> p t g x", g=4)
    D(lambda e: e.tensor_tensor(lev, lg[:, :, 4:36].rearrange("p t (g x) -> p t g x", g=4), oh.unsqueeze(3).to_broadcast([128, NT, 4, 8]), op=ALU.mult))
    D(lambda e: e.tensor_scalar(o1.rearrange("p t (g x) -> p t g x", g=4), oh.unsqueeze(3).to_broadcast([128, NT, 4, 8]), -1.0, 1e30, op0=ALU.add, op1=ALU.mult))
    D(lambda e: e.tensor_tensor(le, le, o1, op=ALU.add))
    D(lambda e: e.tensor_reduce(out=mA, in_=le, axis=AX.X, op=ALU.max))
    D(lambda e: e.tensor_tensor(o1, le, bc32(mA), op=ALU.is_ge))
    D(lambda e: e.scalar_tensor_tensor(out=le, in0=o1, scalar=-1e30, in1=le, op0=ALU.mult, op1=ALU.add))
    D(lambda e: e.tensor_reduce(out=mB, in_=le, axis=AX.X, op=ALU.max))
    D(lambda e: e.tensor_tensor(o2, le, bc32(mB), op=ALU.is_ge))
    D(lambda e: e.tensor_tensor(w1, mB, mA, op=ALU.subtract))
    P.op("act", lambda e: e.activation(out=w1, in_=w1, func=AF.Exp), reads=R, writes=R)
    D(lambda e: e.tensor_scalar(w1, w1, 1.0, None, op0=ALU.add))
    D(lambda e: e.reciprocal(w1, w1))
    D(lambda e: e.tensor_scalar(w2, w1, -1.0, 1.0, op0=ALU.mult, op1=ALU.add))
    D(lambda e: e.tensor_tensor(w1, w1, gs_, op=ALU.mult))
    D(lambda e: e.tensor_tensor(w2, w2, gs_, op=ALU.mult))
    D(lambda e: e.tensor_tensor(o1, o1, bc32(w1), op=ALU.mult))
    D(lambda e: e.tensor_tensor(o2, o2, bc32(w2), op=ALU.mult))
    D(lambda e: e.tensor_tensor(gate, o1, o2, op=ALU.add))
    P.barrier()
    A.release(m1_)
    TB = 1024
    acc = A.alloc(8 * 1024, F32).rearrange("p (t c) -> p t c", t=8)
    wg = [A.alloc(8 * 512, BF16).rearrange("p (k c) -> p k c", k=8) for _ in range(2)]
    wu = [A.alloc(8 * 512, BF16).rearrange("p (k c) -> p k c", k=8) for _ in range(2)]
    wd = [A.alloc(4 * 1024, BF16).rearrange("p (k c) -> p k c", k=4) for _ in range(2)]
    hid = [A.alloc(4 * 512, BF16).rearrange("p (k t) -> p k t", k=4) for _ in range(2)]
    sg = [A.alloc(512, F32) for _ in range(2)]
    ec = 0; hc_ = 0; sc_ = 0; yc = 0
    for tb in range(S // TB):
        P.op("pool", lambda e: e.memset(acc, 0.0), writes=["acc"])
        for ex in range(32):
            i2 = ec % 2
            P.dma("pool", lambda e, i2=i2, ex=ex: e.dma_start(out=wg[i2], in_=I["w_gate"][l, ex].rearrange("(k p) c -> p k c", p=128)), writes=["wg%d" % i2], key="wg%d" % i2)
            P.dma("pool", lambda e, i2=i2, ex=ex: e.dma_start(out=wu[i2], in_=I["w_up"][l, ex].rearrange("(k p) c -> p k c", p=128)), writes=["wu%d" % i2], key="wu%d" % i2)
            P.dma("pool", lambda e, i2=i2, ex=ex: e.dma_start(out=wd[i2], in_=I["w_down"][l, ex].rearrange("(k p) c -> p k c", p=128)), writes=["wd%d" % i2], key="wd%d" % i2)
            for hb in range(TB // 512):
                tok0 = tb * TB + hb * 512
                hd = hid[hc_ % 2]; hk = "hid%d" % (hc_ % 2)
                for cb in range(4):
                    bg = (sc_ % 2) * 2; bu = bg + 1
                    for kc in range(8):
                        P.op("pe", lambda e, kc=kc, cb=cb, bg=bg, i2=i2, tok0=tok0: e.matmul(ps[bg][:, :], lhsT=wg[i2][:, kc, cb * 128:(cb + 1) * 128], rhs=h2T[:, kc, tok0:tok0 + 512], start=(kc == 0), stop=(kc == 7)),
                             reads=["wg%d" % i2, "h2T"], writes=["ps%d" % bg])
                    for kc in range(8):
                        P.op("pe", lambda e, kc=kc, cb=cb, bu=bu, i2=i2, tok0=tok0: e.matmul(ps[bu][:, :], lhsT=wu[i2][:, kc, cb * 128:(cb + 1) * 128], rhs=h2T[:, kc, tok0:tok0 + 512], start=(kc == 0), stop=(kc == 7)),
                             reads=["wu%d" % i2, "h2T"], writes=["ps%d" % bu])
                    s = sg[sc_ % 2]; sk = "sgF%d" % (sc_ % 2)
                    P.op("act", lambda e, s=s, bg=bg: e.activation(out=s, in_=ps[bg][:, :], func=AF.Exp, scale=-1.0), reads=["ps%d" % bg], writes=[sk])
                    P.op("pool", lambda e, s=s: e.tensor_scalar(s, s, 1.0, None, op0=ALU.add), reads=[sk], writes=[sk])
                    P.op("dve", lambda e, s=s: e.reciprocal(s, s), reads=[sk], writes=[sk])
                    P.op("dve", lambda e, s=s, bg=bg: e.tensor_tensor(s, s, ps[bg][:, :], op=ALU.mult), reads=[sk, "ps%d" % bg], writes=[sk])
                    P.op("dve", lambda e, s=s, bu=bu, hd=hd, cb=cb: e.tensor_tensor(hd[:, cb, :], s, ps[bu][:, :], op=ALU.mult), reads=[sk, "ps%d" % bu], writes=[hk])
                    sc_ += 1
                for tt in range(4):
                    tl = hb * 4 + tt
                    tg = tb * 8 + tl
                    for half in range(2):
                        bank = 4 + yc % 4
                        for kc in range(4):
                            P.op("pe", lambda e, kc=kc, half=half, bank=bank, tt=tt, hd=hd, i2=i2: e.matmul(ps[bank][:, :], lhsT=hd[:, kc, tt * 128:(tt + 1) * 128], rhs=wd[i2][:, kc, half * 512:(half + 1) * 512], start=(kc == 0), stop=(kc == 3)),
                                 reads=[hk, "wd%d" % i2], writes=["ps%d" % bank])
                        P.op("dve", lambda e, bank=bank, tl=tl, tg=tg, half=half, ex=ex: e.scalar_tensor_tensor(out=acc[:, tl, half * 512:(half + 1) * 512], in0=ps[bank][:, :], scalar=gate[:, tg, ex:ex + 1], in1=acc[:, tl, half * 512:(half + 1) * 512], op0=ALU.mult, op1=ALU.add),
                             reads=["ps%d" % bank, "gate", "acc"], writes=["acc"])
                        yc += 1
                hc_ += 1
            ec += 1
        for tl in range(8):
            tg = tb * 8 + tl
            x = k.xb[tg % 2]; xk = "xb%d" % (tg % 2)
            P.dma("sp", lambda e, x=x, tg=tg: e.dma_start(out=x, in_=xsrc[tg * 128:(tg + 1) * 128, :]), writes=[xk], key=xk)
            P.op("pool", lambda e, tl=tl: e.tensor_tensor(acc[:, tl, :], acc[:, tl, :], k.gtbc[1], op=ALU.mult), reads=["acc", "gtbc1"], writes=["acc"])
            P.op("pool", lambda e, tl=tl, x=x: e.tensor_tensor(x, x, acc[:, tl, :], op=ALU.add), reads=["acc", xk], writes=[xk])
            P.dma("sp", lambda e, x=x, tg=tg: e.dma_start(out=xdst[tg * 128:(tg + 1) * 128, :], in_=x), reads=[xk], key=xk)
    P.barrier()
    A.release(m0)


def phaseG(k, I, xsrc, out):
    A, P, ps = k.A, k.P, k.ps
    m0 = A.mark()
    gb = A.alloc(1024, F32)
    P.dma("sp", lambda e: e.dma_start(out=gb, in_=I["g_final"].partition_broadcast(128)), writes=["gbG"], key="gbG")
    xb = [A.alloc(1024, F32) for _ in range(2)]
    xn = [A.alloc(1024, F32) for _ in range(2)]
    st = [A.alloc(2, F32) for _ in range(2)]
    for t in range(NT):
        x = xb[t % 2]; xk = "xbG%d" % (t % 2); n = xn[t % 2]; nk = "xnG%d" % (t % 2); s = st[t % 2]; sk = "stG%d" % (t % 2)
        P.dma("sp", lambda e, x=x, t=t: e.dma_start(out=x, in_=xsrc[t * 128:(t + 1) * 128, :]), writes=[xk], key=xk)
        P.op("act", lambda e, x=x, n=n, s=s: e.activation(out=n, in_=x, func=AF.Square, accum_out=s[:, 0:1]), reads=[xk], writes=[nk, sk])
        P.op("dve", lambda e, s=s: e.tensor_scalar(s[:, 1:2], s[:, 0:1], 1.0 / D, EPS, op0=ALU.mult, op1=ALU.add), reads=[sk], writes=[sk])
        P.op("act", lambda e, s=s: e.activation(out=s[:, 1:2], in_=s[:, 1:2], func=AF.Ln), reads=[sk], writes=[sk])
        P.op("act", lambda e, s=s: e.activation(out=s[:, 1:2], in_=s[:, 1:2], func=AF.Exp, scale=-0.5), reads=[sk], writes=[sk])
        P.op("dve", lambda e, x=x, n=n, s=s: e.scalar_tensor_tensor(out=n, in0=x, scalar=s[:, 1:2], in1=gb, op0=ALU.mult, op1=ALU.mult), reads=[xk, sk, "gbG"], writes=[nk])
        P.dma("sp", lambda e, n=n, t=t: e.dma_start(out=out[t * 128:(t + 1) * 128, :], in_=n), reads=[nk], key=nk)
    P.barrier()
    A.release(m0)


CAP = 768
NSL = 32 * CAP


def phaseF2(k, l, I, xsrc, xdst):
    A, P, ps = k.A, k.P, k.ps
    Xbuf = dram(k, "Xbuf", [NSL + 128, D], BF16)
    Ybuf = dram(k, "Ybuf", [NSL + 128, D], F32)
    m0 = A.mark()
    idx1 = A.alloc(NT, I32); idx2 = A.alloc(NT, I32)
    g12 = A.alloc(2 * NT, F32)
    g1 = g12[:, 0:NT]; g2 = g12[:, NT:2 * NT]
    k.xb = [A.alloc(1024, F32) for _ in range(2)]
    mH = A.mark()
    h2tm = A.alloc(NT * 1024, BF16).rearrange("p (t c) -> p t c", t=NT)
    m1_ = A.mark()
    hf = [A.alloc(8 * 128, F32).rearrange("p (k t) -> p k t", k=8) for _ in range(2)]
    wrt = A.alloc(8 * 36, F32).rearrange("p (k c) -> p k c", k=8)
    rb = A.alloc(36, F32)
    lg = A.alloc(NT * 36, F32).rearrange("p (t c) -> p t c", t=NT)
    k.xn = [A.alloc(1024, F32) for _ in range(2)]
    k.nst = [A.alloc(2, F32) for _ in range(2)]
    P.dma("sp", lambda e: e.dma_start(out=wrt[:, :, 0:4], in_=I["w_grp"][l].rearrange("(k p) c -> p k c", p=128)), writes=["wrt"], key="wrt")
    P.dma("sp", lambda e: e.dma_start(out=wrt[:, :, 4:36], in_=I["w_exp_router"][l].rearrange("(k p) c -> p k c", p=128)), writes=["wrt"], key="wrt")
    P.dma("sp", lambda e: e.dma_start(out=rb[:, 0:4], in_=I["b_grp"][l].partition_broadcast(128)), writes=["rbF"], key="rbF")
    P.dma("sp", lambda e: e.dma_start(out=rb[:, 4:36], in_=I["b_exp_router"][l].partition_broadcast(128)), writes=["rbF"], key="rbF")
    for t in range(NT):
        b0 = norm_transpose(k, xsrc, t, None, None, None, None, None)
        f = hf[t % 2]; fk = "hfF%d" % (t % 2)
        for kc in range(8):
            bank = b0 + kc // 4
            P.op("act", lambda e, kc=kc, bank=bank, f=f: e.activation(out=f[:, kc, :], in_=ps[bank][:, (kc % 4) * 128:(kc % 4 + 1) * 128], func=AF.Identity, scale=k.gs2[:, kc:kc + 1], bias=k.sh2[:, kc:kc + 1]),
                 reads=["ps%d" % bank, "gs", "modf"], writes=[fk])
        bank = 4 + t % 2
        for kc in range(8):
            P.op("pe", lambda e, kc=kc, f=f, bank=bank: e.matmul(ps[bank][:, 0:36], lhsT=f[:, kc, :], rhs=wrt[:, kc, :], start=(kc == 0), stop=(kc == 7)), reads=[fk, "wrt"], writes=["ps%d" % bank])
        P.op("dve", lambda e, t=t, bank=bank: e.tensor_tensor(lg[:, t, :], ps[bank][:, 0:36], rb, op=ALU.add), reads=["ps%d" % bank, "rbF"], writes=["lg"])
        for kc in range(8):
            bank = 6 + kc // 4
            P.op("pe", lambda e, kc=kc, f=f, bank=bank: e.transpose(ps[bank][:, (kc % 4) * 128:(kc % 4 + 1) * 128], f[:, kc, :], k.ident), reads=[fk, "ident"], writes=["ps%d" % bank])
        P.op("dve", lambda e, t=t: e.tensor_copy(h2tm[:, t, 0:512], ps[6][:, :]), reads=["ps6"], writes=["h2tm"])
        P.op("pool" if False else "dve", lambda e, t=t: e.tensor_copy(h2tm[:, t, 512:1024], ps[7][:, :]), reads=["ps7"], writes=["h2tm"])
    def T32():
        return A.alloc(NT * 32, F32).rearrange("p (t c) -> p t c", t=NT)
    g4 = A.alloc(NT * 4, F32).rearrange("p (t c) -> p t c", t=NT)
    oh = A.alloc(NT * 4, F32).rearrange("p (t c) -> p t c", t=NT)
    s1 = A.alloc(NT * 8, F32)
    le = T32(); o1 = T32(); o2 = T32(); pos = T32(); tmp = T32(); offs = T32()
    indb = A.alloc(NT * 32, BF16)
    SU = A.alloc(128, BF16)
    suf = A.alloc(128, F32)
    mx = s1[:, 0:NT]; gs_ = s1[:, NT:2 * NT]; mA = s1[:, 2 * NT:3 * NT]; mB = s1[:, 3 * NT:4 * NT]; w1 = s1[:, 4 * NT:5 * NT]; w2 = s1[:, 5 * NT:6 * NT]
    v1 = s1[:, 6 * NT:7 * NT]; v2 = s1[:, 7 * NT:8 * NT]
    R = ["lg", "g4", "oh", "s1", "le", "o1", "o2", "pos", "tmp", "offs", "g12", "idx"]
    def Dv(fn):
        P.op("dve", fn, reads=R, writes=R)
    bc4 = lambda v: v.unsqueeze(2).to_broadcast([128, NT, 4])
    bc32 = lambda v: v.unsqueeze(2).to_broadcast([128, NT, 32])
    P.op("pool", lambda e: e.memset(suf, 1.0), writes=["suf"])
    P.op("pool", lambda e: e.tensor_tensor(suf, k.cneg_u, k.cneg_u, op=ALU.mult), reads=["cneg_u"], writes=["suf"])
    P.op("dve", lambda e: e.tensor_copy(SU, suf), reads=["suf"], writes=["SU"])
    Dv(lambda e: e.tensor_reduce(out=mx, in_=lg[:, :, 0:4], axis=AX.X, op=ALU.max))
    Dv(lambda e: e.tensor_tensor(oh, lg[:, :, 0:4], bc4(mx), op=ALU.is_ge))
    Dv(lambda e: e.tensor_tensor(g4, lg[:, :, 0:4], bc4(mx), op=ALU.subtract))
    P.op("act", lambda e: e.activation(out=g4, in_=g4, func=AF.Exp), reads=R, writes=R)
    Dv(lambda e: e.tensor_reduce(out=gs_, in_=g4, axis=AX.X, op=ALU.add))
    Dv(lambda e: e.reciprocal(gs_, gs_))
    lev = le.rearrange("p t (g x) -> p t g x", g=4)
    Dv(lambda e: e.tensor_tensor(lev, lg[:, :, 4:36].rearrange("p t (g x) -> p t g x", g=4), oh.unsqueeze(3).to_broadcast([128, NT, 4, 8]), op=ALU.mult))
    Dv(lambda e: e.tensor_scalar(o1.rearrange("p t (g x) -> p t g x", g=4), oh.unsqueeze(3).to_broadcast([128, NT, 4, 8]), -1.0, 1e30, op0=ALU.add, op1=ALU.mult))
    Dv(lambda e: e.tensor_tensor(le, le, o1, op=ALU.add))
    Dv(lambda e: e.tensor_reduce(out=mA, in_=le, axis=AX.X, op=ALU.max))
    Dv(lambda e: e.tensor_tensor(o1, le, bc32(mA), op=ALU.is_ge))
    Dv(lambda e: e.scalar_tensor_tensor(out=le, in0=o1, scalar=-1e30, in1=le, op0=ALU.mult, op1=ALU.add))
    Dv(lambda e: e.tensor_reduce(out=mB, in_=le, axis=AX.X, op=ALU.max))
    Dv(lambda e: e.tensor_tensor(o2, le, bc32(mB), op=ALU.is_ge))
    Dv(lambda e: e.tensor_tensor(w1, mB, mA, op=ALU.subtract))
    P.op("act", lambda e: e.activation(out=w1, in_=w1, func=AF.Exp), reads=R, writes=R)
    Dv(lambda e: e.tensor_scalar(w1, w1, 1.0, None, op0=ALU.add))
    Dv(lambda e: e.reciprocal(w1, w1))
    Dv(lambda e: e.tensor_scalar(w2, w1, -1.0, 1.0, op0=ALU.mult, op1=ALU.add))
    Dv(lambda e: e.tensor_tensor(w1, w1, gs_, op=ALU.mult))
    Dv(lambda e: e.tensor_tensor(w2, w2, gs_, op=ALU.mult))
    Dv(lambda e: e.tensor_tensor(tmp, o1, o2, op=ALU.add))
    P.op("dve", lambda e: e.tensor_copy(indb, tmp.rearrange("p t c -> p (t c)")), reads=R, writes=["indb"])
    for c in range(2):
        P.op("pe", lambda e, c=c: e.matmul(ps[c][:, :], lhsT=SU, rhs=indb[:, c * 512:(c + 1) * 512], start=True, stop=True), reads=["SU", "indb"], writes=["ps%d" % c])
        P.op("pe", lambda e, c=c: e.matmul(ps[2 + c][:, :], lhsT=k.ones_b, rhs=indb[:, c * 512:(c + 1) * 512], start=True, stop=True), reads=["ones_b", "indb"], writes=["ps%d" % (2 + c)])
    for c in range(2):
        P.op("dve", lambda e, c=c: e.tensor_copy(pos.rearrange("p t c -> p (t c)")[:, c * 512:(c + 1) * 512], ps[c][:, :]), reads=["ps%d" % c] + R, writes=R)
        P.op("dve", lambda e, c=c: e.tensor_copy(tmp.rearrange("p t c -> p (t c)")[:, c * 512:(c + 1) * 512], ps[2 + c][:, :]), reads=["ps%d" % (2 + c)] + R, writes=R)
    Dv(lambda e: e.memset(offs[:, 0, :], 0.0))
    for t in range(1, NT):
        Dv(lambda e, t=t: e.tensor_tensor(offs[:, t, :], offs[:, t - 1, :], tmp[:, t - 1, :], op=ALU.add))
    Dv(lambda e: e.tensor_tensor(pos, pos, offs, op=ALU.add))
    Dv(lambda e: e.tensor_scalar(tmp, pos, float(CAP), None, op0=ALU.is_lt))
    Dv(lambda e: e.tensor_tensor(pos, pos, k.ecap.unsqueeze(1).to_broadcast([128, NT, 32]), op=ALU.add))
    Dv(lambda e: e.tensor_tensor(pos, pos, tmp, op=ALU.mult))
    Dv(lambda e: e.tensor_scalar(offs, tmp, -1.0, k.trash[:, 0:1], op0=ALU.add, op1=ALU.mult))
    Dv(lambda e: e.tensor_tensor(pos, pos, offs, op=ALU.add))
    Dv(lambda e: e.tensor_tensor(offs, o1, pos, op=ALU.mult))
    Dv(lambda e: e.tensor_reduce(out=v1, in_=offs, axis=AX.X, op=ALU.add))
    P.op("dve", lambda e: e.tensor_copy(idx1, v1), reads=R, writes=R)
    Dv(lambda e: e.tensor_tensor(offs, o2, pos, op=ALU.mult))
    Dv(lambda e: e.tensor_reduce(out=v2, in_=offs, axis=AX.X, op=ALU.add))
    P.op("dve", lambda e: e.tensor_copy(idx2, v2), reads=R, writes=R)
    Dv(lambda e: e.tensor_tensor(offs, o1, tmp, op=ALU.mult))
    Dv(lambda e: e.tensor_reduce(out=v1, in_=offs, axis=AX.X, op=ALU.add))
    Dv(lambda e: e.tensor_tensor(g1, w1, v1, op=ALU.mult))
    Dv(lambda e: e.tensor_tensor(offs, o2, tmp, op=ALU.mult))
    Dv(lambda e: e.tensor_reduce(out=v2, in_=offs, axis=AX.X, op=ALU.add))
    Dv(lambda e: e.tensor_tensor(g2, w2, v2, op=ALU.mult))
    P.op("pool", lambda e: e.memset(k.xb[0], 0.0), writes=["xb0"])
    P.dma("sp", lambda e: e.dma_start(out=Ybuf[NSL:NSL + 128, :], in_=k.xb[0]), reads=["xb0"], key="xb0")
    if l == 0:
        zb = k.xb[0].bitcast(BF16)[:, 0:1024]
        nrt = (NSL + 128) // 128
        Xv = Xbuf.rearrange("(n p) c -> p n c", p=128)
        for n0 in range(0, nrt, 16):
            nn = min(16, nrt - n0)
            P.dma("sp", lambda e, n0=n0, nn=nn: e.dma_start(out=Xv[:, n0:n0 + nn, :], in_=zb.unsqueeze(1).to_broadcast([128, nn, 1024])), reads=["xb0"], writes=["XbufZ"], key="xbz")
    if "dbg_idx" in k.debug:
        dbi = dram(k, "dbg_idx", [128, 2 * NT], I32)
        P.dma("sp", lambda e: e.dma_start(out=dbi[:, 0:NT], in_=idx1), reads=R, key="dbgi")
        P.dma("sp", lambda e: e.dma_start(out=dbi[:, NT:2 * NT], in_=idx2), reads=R, key="dbgi")
        dbg_ = dram(k, "dbg_g", [128, 2 * NT], F32)
        P.dma("sp", lambda e: e.dma_start(out=dbg_, in_=g12), reads=R, key="dbgg")
    for t in range(NT):
        for (ix, nm) in ((idx1, "a"), (idx2, "b")):
            P.dma("pool", lambda e, t=t, ix=ix: e.indirect_dma_start(out=Xbuf, out_offset=bass.IndirectOffsetOnAxis(ap=ix[:, t:t + 1], axis=0), in_=h2tm[:, t, :], in_offset=None),
                  reads=["h2tm", "XbufZ"] + R, writes=[], key="sc%s%d" % (nm, t % 4))
    P.barrier()
    A.release(mH)
    wg = [A.alloc(8 * 512, BF16).rearrange("p (k c) -> p k c", k=8) for _ in range(2)]
    wu = [A.alloc(8 * 512, BF16).rearrange("p (k c) -> p k c", k=8) for _ in range(2)]
    wd = [A.alloc(4 * 1024, BF16).rearrange("p (k c) -> p k c", k=4) for _ in range(2)]
    XT = [A.alloc(8 * CAP, BF16).rearrange("p (k s) -> p k s", k=8) for _ in range(2)]
    xr = [A.alloc(1024, BF16) for _ in range(3)]
    hid = [A.alloc(4 * CAP, BF16).rearrange("p (k s) -> p k s", k=4) for _ in range(2)]
    sg = [A.alloc(512, F32) for _ in range(2)]
    yrow = [A.alloc(1024, F32) for _ in range(3)]
    NST = CAP // 128
    if "dbg_X0" in k.debug:
        dx = dram(k, "dbg_X0", [128, 1024], BF16)
        P.dma("sp", lambda e: e.dma_start(out=xr[2], in_=Xbuf[0:128, :]), writes=["xr2"], key="xr2")
        P.dma("sp", lambda e: e.dma_start(out=dx, in_=xr[2]), reads=["xr2"], key="dbgx0")
    chunks = [(0, 512), (512, CAP - 512)] if CAP > 512 else [(0, CAP)]
    cnts = {"xc": 0, "cc": 0, "yc": 0, "scn": 0}
    NEXP = 32

    def Wload(ex):
        i2 = ex % 2
        P.dma("pool", lambda e: e.dma_start(out=wg[i2], in_=I["w_gate"][l, ex].rearrange("(k p) c -> p k c", p=128)), writes=["wg%d" % i2], key="wg%d" % i2)
        P.dma("pool", lambda e: e.dma_start(out=wu[i2], in_=I["w_up"][l, ex].rearrange("(k p) c -> p k c", p=128)), writes=["wu%d" % i2], key="wu%d" % i2)
        P.dma("pool", lambda e: e.dma_start(out=wd[i2], in_=I["w_down"][l, ex].rearrange("(k p) c -> p k c", p=128)), writes=["wd%d" % i2], key="wd%d" % i2)

    def Tphase(ex):
        i2 = ex % 2
        xt = XT[i2]; xtk = "XT%d" % i2
        for st in range(NST):
            xc = cnts["xc"]
            r = xr[xc % 3]; rk = "xr%d" % (xc % 3)
            P.dma("sp", lambda e, r=r, st=st: e.dma_start(out=r, in_=Xbuf[ex * CAP + st * 128:ex * CAP + (st + 1) * 128, :]), writes=[rk], key=rk)
            bank = 6 + xc % 2
            tb = ps[bank][:, :].bitcast(BF16)
            for kc in range(8):
                P.op("pe", lambda e, kc=kc, r=r, tb=tb: e.transpose(tb[:, kc * 128:(kc + 1) * 128], r[:, kc * 128:(kc + 1) * 128], k.identb), reads=[rk, "identb"], writes=["ps%d" % bank])
            if xc % 2 == 0:
                P.op("act", lambda e, tb=tb, st=st: e.activation(out=xt[:, :, st * 128:(st + 1) * 128], in_=tb.rearrange("p (k s) -> p k s", k=8), func=AF.Copy), reads=["ps%d" % bank], writes=[xtk])
            else:
                P.op("dve", lambda e, tb=tb, st=st: e.tensor_copy(xt[:, :, st * 128:(st + 1) * 128], tb.rearrange("p (k s) -> p k s", k=8)), reads=["ps%d" % bank], writes=[xtk])
            cnts["xc"] += 1

    def Hphase(ex):
        i2 = ex % 2
        xt = XT[i2]; xtk = "XT%d" % i2
        hd = hid[i2]; hk = "hid%d" % i2
        for hb in range(4):
            for (c0, cn) in chunks:
                cc = cnts["cc"]; scn = cnts["scn"]
                bg = (cc % 2) * 2; bu = bg + 1
                for kc in range(8):
                    P.op("pe", lambda e, kc=kc, hb=hb, bg=bg, c0=c0, cn=cn: e.matmul(ps[bg][:, 0:cn], lhsT=wg[i2][:, kc, hb * 128:(hb + 1) * 128], rhs=xt[:, kc, c0:c0 + cn], start=(kc == 0), stop=(kc == 7)),
                         reads=["wg%d" % i2, xtk], writes=["ps%d" % bg])
                for kc in range(8):
                    P.op("pe", lambda e, kc=kc, hb=hb, bu=bu, c0=c0, cn=cn: e.matmul(ps[bu][:, 0:cn], lhsT=wu[i2][:, kc, hb * 128:(hb + 1) * 128], rhs=xt[:, kc, c0:c0 + cn], start=(kc == 0), stop=(kc == 7)),
                         reads=["wu%d" % i2, xtk], writes=["ps%d" % bu])
                s_ = sg[scn % 2]; sk = "sgF%d" % (scn % 2)
                P.op("act", lambda e, s_=s_, bg=bg, cn=cn: e.activation(out=s_[:, 0:cn], in_=ps[bg][:, 0:cn], func=AF.Silu), reads=["ps%d" % bg], writes=[sk])
                P.op("dve", lambda e, s_=s_, bu=bu, hb=hb, c0=c0, cn=cn: e.tensor_tensor(hd[:, hb, c0:c0 + cn], s_[:, 0:cn], ps[bu][:, 0:cn], op=ALU.mult), reads=[sk, "ps%d" % bu], writes=[hk])
                cnts["scn"] += 1; cnts["cc"] += 1

    def Dphase(ex):
        i2 = ex % 2
        hd = hid[i2]; hk = "hid%d" % i2
        for st in range(NST):
            yc = cnts["yc"]
            y = yrow[yc % 3]; yk = "yrow%d" % (yc % 3)
            for half in range(2):
                bank = 4 + half
                for kc in range(4):
                    P.op("pe", lambda e, kc=kc, half=half, bank=bank, st=st: e.matmul(ps[bank][:, :], lhsT=hd[:, kc, st * 128:(st + 1) * 128], rhs=wd[i2][:, kc, half * 512:(half + 1) * 512], start=(kc == 0), stop=(kc == 3)),
                         reads=[hk, "wd%d" % i2], writes=["ps%d" % bank])
                if half == 0:
                    P.op("act", lambda e, y=y, bank=bank: e.activation(out=y[:, 0:512], in_=ps[bank][:, :], func=AF.Copy), reads=["ps%d" % bank], writes=[yk])
                else:
                    P.op("dve", lambda e, y=y, bank=bank: e.tensor_copy(y[:, 512:1024], ps[bank][:, :]), reads=["ps%d" % bank], writes=[yk])
            P.dma("sp", lambda e, y=y, st=st: e.dma_start(out=Ybuf[ex * CAP + st * 128:ex * CAP + (st + 1) * 128, :], in_=y), reads=[yk], key=yk)
            cnts["yc"] += 1

    Wload(0)
    Tphase(0)
    for ex in range(NEXP):
        if ex + 1 < NEXP:
            Wload(ex + 1)
        Hphase(ex)
        if ex + 1 < NEXP:
            Tphase(ex + 1)
        Dphase(ex)
    P.barrier()
    A.release(mH)
    Y1 = [A.alloc(1024, F32) for _ in range(2)]
    Y2 = [A.alloc(1024, F32) for _ in range(2)]
    for t in range(NT):
        b = t % 2
        x = k.xb[b]; xk = "xb%d" % b
        P.dma("sp", lambda e, x=x, t=t: e.dma_start(out=x, in_=xsrc[t * 128:(t + 1) * 128, :]), writes=[xk], key=xk)
        P.dma("pool", lambda e, t=t, b=b: e.indirect_dma_start(out=Y1[b], out_offset=None, in_=Ybuf, in_offset=bass.IndirectOffsetOnAxis(ap=idx1[:, t:t + 1], axis=0)), reads=R, writes=["Y1%d" % b], key="Y1%d" % b)
        P.dma("pool", lambda e, t=t, b=b: e.indirect_dma_start(out=Y2[b], out_offset=None, in_=Ybuf, in_offset=bass.IndirectOffsetOnAxis(ap=idx2[:, t:t + 1], axis=0)), reads=R, writes=["Y2%d" % b], key="Y2%d" % b)
        P.op("act", lambda e, t=t, b=b: e.activation(out=Y1[b], in_=Y1[b], func=AF.Copy, scale=g1[:, t:t + 1]), reads=["Y1%d" % b] + R, writes=["Y1%d" % b])
        P.op("dve", lambda e, t=t, b=b: e.scalar_tensor_tensor(out=Y1[b], in0=Y2[b], scalar=g2[:, t:t + 1], in1=Y1[b], op0=ALU.mult, op1=ALU.add), reads=["Y1%d" % b, "Y2%d" % b] + R, writes=["Y1%d" % b])
        P.op("dve", lambda e, b=b: e.tensor_tensor(Y1[b], Y1[b], k.gtbc[1], op=ALU.mult), reads=["Y1%d" % b, "gtbc1"], writes=["Y1%d" % b])
        P.op("dve", lambda e, x=x, b=b: e.tensor_tensor(x, x, Y1[b], op=ALU.add), reads=["Y1%d" % b, xk], writes=[xk])
        P.dma("sp", lambda e, x=x, t=t: e.dma_start(out=xdst[t * 128:(t + 1) * 128, :], in_=x), reads=[xk], key=xk)
    P.barrier()
    A.release(m0)


from concourse.bass_utils import run_bass_kernel_spmd

_WNAMES = ["w_mod", "b_mod", "g_norm1", "g_norm2", "w_in", "g_cq", "g_ckv", "g_kidx", "w_q_up", "w_idx_q", "w_v_up",
           "lb_logits", "g_rec", "w_branch_a", "w_branch_r", "w_out", "w_grp", "b_grp", "w_exp_router", "b_exp_router",
           "w_gate", "w_up", "w_down", "g_final"]


def _build(shapes, depth=4):
    nc = bass.Bass("TRN2", target_bir_lowering=False)
    es = ExitStack()
    with es:
        I = {}
        I["x"] = nc.dram_tensor("x", [S, D], F32, kind="ExternalInput").ap()
        I["c"] = nc.dram_tensor("c", [1, D], F32, kind="ExternalInput").ap()
        for n in _WNAMES:
            I[n] = nc.dram_tensor(n, list(shapes[n]), F32, kind="ExternalInput").ap()
        out = nc.dram_tensor("out", [S, D], F32, kind="ExternalOutput").ap()
        k = mkctx(nc, es)
        setup_consts(k)
        pm = k.A.mark()
        xa = dram(k, "xres_a", [S, D], F32)
        xb = dram(k, "xres_b", [S, D], F32)
        xcur = I["x"]
        for l in range(depth):
            k.A.release(pm)
            phase0(k, l, I)
            m_after0 = k.A.mark()
            phaseA(k, l, I, xcur)
            phaseB(k, l, I)
            phaseC(k, l, I)
            k.A.release(m_after0)
            phaseD(k, l, I)
            phaseE(k, l, I, xcur, xa)
            phaseF2(k, l, I, xa, xb)
            xcur = xb
        phaseG(k, I, xcur, out)
        k.P.finish(k.A.alloc(2, F32))
        k.P.emit(es)
    return nc


def kernel(**inputs):
    x = np.ascontiguousarray(inputs["x"], dtype=np.float32)
    c = np.ascontiguousarray(inputs["c"], dtype=np.float32)
    shapes = {n: inputs[n].shape for n in _WNAMES}
    nc = _build(shapes)
    w = {n: np.ascontiguousarray(inputs[n], dtype=np.float32) for n in _WNAMES}
    in_maps = []
    for b in range(8):
        m = {"x": x[b], "c": c[b:b + 1]}
        m.update(w)
        in_maps.append(m)
    res = run_bass_kernel_spmd(nc, in_maps, core_ids=list(range(8)))
    return np.stack([np.asarray(r["out"], dtype=np.float32) for r in res.results], axis=0)
```

```python
import numpy as np
import concourse.bass as bass
import concourse.mybir as mybir
from contextlib import ExitStack

F32 = mybir.dt.float32
BF16 = mybir.dt.bfloat16
I32 = mybir.dt.int32
AF = mybir.ActivationFunctionType
ALU = mybir.AluOpType
AX = mybir.AxisListType


class Op:
    __slots__ = ("eng", "fn", "deps", "inc", "cnt", "dma", "key", "consumed", "idx")

    def __init__(self, eng, fn, dma=False, key=None):
        self.eng = eng
        self.fn = fn
        self.deps = []
        self.inc = dma
        self.cnt = 0
        self.dma = dma
        self.key = key
        self.consumed = False


class Prog:
    ENGS = ("pe", "act", "dve", "pool", "sp")

    def __init__(self, nc):
        self.nc = nc
        self.ops = {e: [] for e in self.ENGS}
        self.last_w = {}
        self.readers = {}
        self.since_barrier = []
        self.pending_barrier = {e: [] for e in self.ENGS}
        self.nops = 0

    def _add(self, op, reads, writes):
        deps = []
        seen = set()
        for k in list(reads) + list(writes):
            w = self.last_w.get(k)
            if w is not None and id(w) not in seen:
                seen.add(id(w)); deps.append(w)
        for k in writes:
            for r in self.readers.get(k, ()):
                if id(r) not in seen:
                    seen.add(id(r)); deps.append(r)
        pb = self.pending_barrier[op.eng]
        if pb:
            for d in pb:
                if id(d) not in seen:
                    seen.add(id(d)); deps.append(d)
            self.pending_barrier[op.eng] = []
        op.deps = [d for d in deps if d is not op]
        for d in op.deps:
            d.consumed = True
        for k in writes:
            self.last_w[k] = op
            self.readers[k] = []
        for k in reads:
            if k not in writes:
                self.readers.setdefault(k, []).append(op)
        self.ops[op.eng].append(op)
        self.since_barrier.append(op)
        self.nops += 1
        return op

    def op(self, eng, fn, reads=(), writes=()):
        return self._add(Op(eng, fn), reads, writes)

    def dma(self, eng, fn, reads=(), writes=(), key=None):
        assert key is not None
        return self._add(Op(eng, fn, dma=True, key=key), reads, writes)

    def barrier(self):
        lst = []
        last = {}
        for o in self.since_barrier:
            if o.dma:
                if not o.consumed:
                    lst.append(o)
            else:
                last[o.eng] = o
        lst.extend(last.values())
        for e in self.ENGS:
            self.pending_barrier[e] = self.pending_barrier[e] + lst
        self.since_barrier = []

    def finish(self, scratch):
        self.barrier()
        self.op("pool", lambda e: e.memset(scratch, 0.0), writes=["__fin"])

    def emit(self, es):
        nc = self.nc
        for e in self.ENGS:
            for o in self.ops[e]:
                for d in o.deps:
                    if d.dma:
                        continue
                    if d.eng == "pe" and o.eng == "pe" and not o.dma:
                        continue
                    d.inc = True
        esem = {}
        for e in self.ENGS:
            esem[e] = es.enter_context(nc.semaphore("s_" + e))
        dsem = {}
        dcnt = {}
        for e in self.ENGS:
            c = 0
            for o in self.ops[e]:
                if o.dma:
                    if o.key not in dsem:
                        dsem[o.key] = es.enter_context(nc.semaphore("d_%d" % len(dsem)))
                        dcnt[o.key] = 0
                    dcnt[o.key] += 16
                    o.cnt = dcnt[o.key]
                elif o.inc:
                    c += 1
                    o.cnt = c
        self.n_dsem = len(dsem)
        block = es.enter_context(nc.Block())

        def run(ename, h):
            waited = {}
            for o in self.ops[ename]:
                need = {}
                for d in o.deps:
                    if d.dma:
                        s = dsem[d.key]
                    else:
                        if d.eng == "pe" and ename == "pe" and not o.dma:
                            continue
                        s = esem[d.eng]
                    sid = id(s)
                    if need.get(sid, (None, 0))[1] < d.cnt:
                        need[sid] = (s, d.cnt)
                for sid, (s, v) in need.items():
                    if waited.get(sid, 0) < v:
                        h.wait_ge(s, v)
                        waited[sid] = v
                ins = o.fn(h)
                if o.dma:
                    ins.then_inc(dsem[o.key], 16)
                elif o.inc:
                    ins.then_inc(esem[ename], 1)

        @block.tensor
        def _(h):
            run("pe", h)

        @block.scalar
        def _(h):
            run("act", h)

        @block.vector
        def _(h):
            run("dve", h)

        @block.gpsimd
        def _(h):
            run("pool", h)

        @block.sync
        def _(h):
            run("sp", h)


class Arena:
    def __init__(self, nc, es, ncols, name="arena"):
        self.t = es.enter_context(nc.sbuf_tensor(name, [128, ncols], F32))
        self.ncols = ncols
        self.off = 0
        self.uid = 0

    def mark(self):
        return self.off

    def release(self, m):
        self.off = m

    def alloc(self, cols, dtype=F32, parts=128):
        if dtype == BF16:
            w = (cols + 1) // 2
        else:
            w = cols
        assert self.off + w <= self.ncols, ("arena overflow", self.off, w, self.ncols)
        v = self.t[0:parts, self.off:self.off + w]
        self.off += w
        if dtype != F32:
            v = v.bitcast(dtype)
            if dtype == BF16 and cols % 2:
                v = v[:, 0:cols]
        return v


S = 4096
D = 1024
NT = S // 128
DIN = 4552
EPS = 1e-6
O_CQ, O_CKV, O_KIDX, O_WIDX, O_QREC, O_FREC, O_IREC, O_OG, O_GA, O_GR = 0, 256, 384, 448, 456, 968, 1480, 1992, 2504, 3528


class K:
    pass


def mkctx(nc, es, debug=()):
    k = K()
    k.nc = nc
    k.es = es
    k.P = Prog(nc)
    k.A = Arena(nc, es, 52800)
    k.ps = [es.enter_context(nc.psum_tensor("bank%d" % i, [128, 512], F32)) for i in range(8)]
    k.debug = set(debug)
    k.dr = {}
    k.uid = 0
    return k


def dram(k, name, shape, dtype):
    if name in k.dr:
        return k.dr[name]
    kind = "ExternalOutput" if name in k.debug else "Internal"
    t = k.nc.dram_tensor(name, list(shape), dtype, kind=kind).ap()
    k.dr[name] = t
    return t


def setup_consts(k):
    A, P = k.A, k.P
    k.ident = A.alloc(128, F32)
    k.identb = A.alloc(128, BF16)
    k.ones_f = A.alloc(128, F32)
    k.ones_b = A.alloc(128, BF16)
    P.op("pool", lambda e: e.memset(k.ident, 0.0), writes=["ident"])
    P.op("pool", lambda e: e.affine_select(out=k.ident, in_=k.ident, pattern=[[-1, 128]], compare_op=ALU.not_equal,
                                           fill=1.0, base=0, channel_multiplier=1), reads=["ident"], writes=["ident"])
    P.op("dve", lambda e: e.tensor_copy(k.identb, k.ident), reads=["ident"], writes=["identb"])
    P.op("dve", lambda e: e.memset(k.ones_f, 1.0), writes=["ones_f"])
    k.cneg = A.alloc(128, F32)
    P.op("pool", lambda e: e.memset(k.cneg, 0.0), writes=["cneg"])
    P.op("pool", lambda e: e.affine_select(out=k.cneg, in_=k.cneg, pattern=[[-1, 128]], compare_op=ALU.is_ge,
                                           fill=-1e30, base=0, channel_multiplier=1), reads=["cneg"], writes=["cneg"])
    k.ecap = A.alloc(32, F32)
    for e_ in range(32):
        P.op("dve", lambda e, e_=e_: e.memset(k.ecap[:, e_:e_ + 1], float(e_ * 768)), writes=["ecap"])
    k.cneg_u = A.alloc(128, F32)
    P.op("pool", lambda e: e.memset(k.cneg_u, 1.0), writes=["cneg_u"])
    P.op("pool", lambda e: e.affine_select(out=k.cneg_u, in_=k.cneg_u, pattern=[[1, 128]], compare_op=ALU.is_gt, fill=0.0, base=0, channel_multiplier=-1), reads=["cneg_u"], writes=["cneg_u"])
    k.trash = A.alloc(2, F32)
    P.op("dve", lambda e: e.tensor_reduce(out=k.trash[:, 0:1], in_=k.cneg_u, axis=AX.X, op=ALU.add), reads=["cneg_u"], writes=["trash"])
    P.op("dve", lambda e: e.tensor_scalar(k.trash[:, 0:1], k.trash[:, 0:1], 1.0, -(32.0 * 768 + 127.0), op0=ALU.mult, op1=ALU.add), reads=["trash"], writes=["trash"])
    k.cmf = A.alloc(128, F32)
    P.op("pool", lambda e: e.memset(k.cmf, 1.0), writes=["cmf"])
    P.op("pool", lambda e: e.affine_select(out=k.cmf, in_=k.cmf, pattern=[[1, 128]], compare_op=ALU.is_ge, fill=0.0, base=0, channel_multiplier=-1), reads=["cmf"], writes=["cmf"])
    P.op("pool", lambda e: e.memset(k.cmf[0:64, 64:128], 0.0), reads=["cmf"], writes=["cmf"])
    P.op("dve", lambda e: e.memset(k.ones_b, 1.0), writes=["ones_b"])


def phase0(k, l, I):
    A, P, nc = k.A, k.P, k.nc
    ps = k.ps
    stg = A.alloc(128, F32)
    k.par = A.alloc(128, F32)
    par = k.par
    P.op("dve", lambda e: e.memset(stg, 0.0), writes=["stg"])
    rows = [
        (0, 48, I["b_mod"][l].rearrange("(r c) -> r c", c=128), 128),
        (48, 8, I["c"][0].rearrange("(r c) -> r c", c=128), 128),
        (56, 8, I["g_norm1"][l].rearrange("(r c) -> r c", c=128), 128),
        (64, 8, I["g_norm2"][l].rearrange("(r c) -> r c", c=128), 128),
        (72, 2, I["g_cq"][l].rearrange("(r c) -> r c", c=128), 128),
        (74, 1, I["g_ckv"][l].rearrange("(r c) -> r c", c=128), 128),
        (75, 1, I["g_kidx"][l].rearrange("(r c) -> r c", c=64), 64),
        (76, 16, I["lb_logits"].rearrange("l (r c) -> (l r) c", c=128), 128),
        (92, 8, I["g_final"].rearrange("(r c) -> r c", c=128), 128),
    ]
    for (r0, n, src, w) in rows:
        P.dma("sp", lambda e, r0=r0, n=n, src=src, w=w: e.dma_start(out=stg[r0:r0 + n, 0:w], in_=src),
              reads=[], writes=["stg"], key="p0stg")
    P.dma("sp", lambda e: e.dma_start(out=stg[75:76, 64:128], in_=I["g_kidx"][l].rearrange("(r c) -> r c", c=64)),
          writes=["stg"], key="p0stg")
    P.op("pe", lambda e: e.transpose(ps[0][:, 0:128], stg, k.ident), reads=["stg", "ident"], writes=["ps0"])
    P.op("dve", lambda e: e.tensor_copy(par, ps[0][:, 0:128]), reads=["ps0"], writes=["par"])
    k.g1 = par[:, 56:64]; k.g2 = par[:, 64:72]; k.gcq = par[:, 72:74]; k.gckv = par[:, 74:75]
    k.gkidx = par[:, 75:76]; k.gfin = par[:, 92:100]
    sm = A.alloc(64, F32)
    k.sm = sm
    cact = sm[:, 0:8]
    t1 = sm[:, 8:16]
    P.op("act", lambda e: e.activation(out=t1, in_=par[:, 48:56], func=AF.Exp, scale=-1.0), reads=["par"], writes=["sm"])
    P.op("dve", lambda e: e.tensor_scalar(t1, t1, 1.0, None, op0=ALU.add), reads=["sm"], writes=["sm"])
    P.op("dve", lambda e: e.reciprocal(t1, t1), reads=["sm"], writes=["sm"])
    P.op("dve", lambda e: e.tensor_tensor(cact, par[:, 48:56], t1, op=ALU.mult), reads=["sm", "par"], writes=["sm"])
    lbl = par[:, 76:92].rearrange("p (l c) -> p l c", l=4)
    el = sm[:, 16:32].rearrange("p (l c) -> p l c", l=4)
    mx = sm[:, 32:36]
    P.op("dve", lambda e: e.tensor_reduce(out=mx, in_=par[:, 76:92].rearrange("p (l c) -> p c l", l=4), axis=AX.X, op=ALU.max),
         reads=["par"], writes=["sm"])
    P.op("dve", lambda e: e.tensor_tensor(el, lbl, mx.unsqueeze(1).to_broadcast([128, 4, 4]), op=ALU.subtract),
         reads=["sm", "par"], writes=["sm"])
    P.op("act", lambda e: e.activation(out=sm[:, 16:32], in_=sm[:, 16:32], func=AF.Exp), reads=["sm"], writes=["sm"])
    ssum = sm[:, 36:40]
    P.op("dve", lambda e: e.tensor_reduce(out=ssum, in_=sm[:, 16:32].rearrange("p (l c) -> p c l", l=4), axis=AX.X, op=ALU.add),
         reads=["sm"], writes=["sm"])
    P.op("dve", lambda e: e.reciprocal(ssum, ssum), reads=["sm"], writes=["sm"])
    k.lb = sm[:, 40:44]
    k.oml = sm[:, 44:48]
    P.op("dve", lambda e: e.memset(k.lb, 0.0), reads=["sm"], writes=["sm"])
    for j in range(1, l + 1):
        P.op("dve", lambda e, j=j: e.tensor_tensor(k.lb, k.lb, el[:, j, :], op=ALU.add), reads=["sm"], writes=["sm"])
    P.op("dve", lambda e: e.tensor_tensor(k.lb, k.lb, ssum, op=ALU.mult), reads=["sm"], writes=["sm"])
    P.op("dve", lambda e: e.tensor_scalar(k.lb, k.lb, 0.0, 1.0, op0=ALU.max, op1=ALU.min), reads=["sm"], writes=["sm"])
    P.op("dve", lambda e: e.tensor_scalar(k.oml, k.lb, -1.0, 1.0, op0=ALU.mult, op1=ALU.add), reads=["sm"], writes=["sm"])
    cact_rep = A.alloc(8 * 128, F32).rearrange("p (k m) -> p k m", k=8)
    P.op("dve", lambda e: e.tensor_copy(cact_rep, cact.unsqueeze(2).to_broadcast([128, 8, 128])), reads=["sm"], writes=["crep"])
    k.gtbc = [A.alloc(1024, F32), A.alloc(1024, F32)]
    bmbc = A.alloc(1024, F32)
    modf = k.A.alloc(32, F32)
    gs = k.A.alloc(16, F32)
    m = A.mark()
    wm = [A.alloc(8 * 1024, F32).rearrange("p (k c) -> p k c", k=8) for _ in range(2)]
    groups = [(0, "fm", 0), (1, "fm", 8), (3, "fm", 16), (4, "fm", 24), (2, "tm", 0), (5, "tm", 1)]
    wsrc = I["w_mod"][l].rearrange("(k p) c -> p k c", p=128)
    for gi, (g, kind, o) in enumerate(groups):
        w = wm[gi % 2]
        wk = "wm%d" % (gi % 2)
        P.dma("sp", lambda e, w=w, g=g: e.dma_start(out=w, in_=wsrc[:, :, g * 1024:(g + 1) * 1024]), writes=[wk], key=wk)
        if kind == "fm":
            for cb in range(8):
                for kc in range(8):
                    P.op("pe", lambda e, w=w, cb=cb, kc=kc, o=o: e.matmul(ps[1][:, o + cb:o + cb + 1], lhsT=w[:, kc, cb * 128:(cb + 1) * 128],
                                                                       rhs=cact[:, kc:kc + 1], start=(kc == 0), stop=(kc == 7)),
                         reads=[wk, "sm"], writes=["ps1"])
        else:
            P.dma("sp", lambda e, g=g: e.dma_start(out=bmbc, in_=I["b_mod"][l, g * 1024:(g + 1) * 1024].partition_broadcast(128)),
                  writes=["bmbc"], key="bmbc")
            for nb in range(2):
                pk = "ps%d" % (2 + nb)
                for kc in range(8):
                    P.op("pe", lambda e, w=w, nb=nb, kc=kc: e.matmul(ps[2 + nb][:, :], lhsT=cact_rep[:, kc, :], rhs=w[:, kc, nb * 512:(nb + 1) * 512],
                                                                  start=(kc == 0), stop=(kc == 7)),
                         reads=[wk, "crep"], writes=[pk])
                P.op("dve", lambda e, nb=nb, o=o: e.tensor_tensor(k.gtbc[o][:, nb * 512:(nb + 1) * 512], ps[2 + nb][:, :], bmbc[:, nb * 512:(nb + 1) * 512], op=ALU.add),
                     reads=[pk, "bmbc"], writes=["gtbc%d" % o])
    bsel = [0, 8, 24, 32]
    for i, b0 in enumerate(bsel):
        P.op("dve", lambda e, i=i, b0=b0: e.tensor_tensor(modf[:, i * 8:(i + 1) * 8], ps[1][:, i * 8:(i + 1) * 8], par[:, b0:b0 + 8], op=ALU.add),
             reads=["ps1", "par"], writes=["modf"])
    k.sh1 = modf[:, 0:8]; k.sh2 = modf[:, 16:24]
    k.gs1 = gs[:, 0:8]; k.gs2 = gs[:, 8:16]
    P.op("dve", lambda e: e.scalar_tensor_tensor(out=k.gs1, in0=modf[:, 8:16], scalar=1.0, in1=k.g1, op0=ALU.add, op1=ALU.mult),
         reads=["modf", "par"], writes=["gs"])
    P.op("dve", lambda e: e.scalar_tensor_tensor(out=k.gs2, in0=modf[:, 24:32], scalar=1.0, in1=k.g2, op0=ALU.add, op1=ALU.mult),
         reads=["modf", "par"], writes=["gs"])
    P.barrier()
    A.release(m)


def norm_transpose(k, xsrc, t, hT, gs, sh, tag, hkey):
    A, P, ps = k.A, k.P, k.ps
    xb = k.xb[t % 2]; xk = "xb%d" % (t % 2)
    xn = k.xn[t % 2]; nk = "xn%d" % (t % 2)
    st = k.nst[t % 2]; sk = "nst%d" % (t % 2)
    P.dma("sp", lambda e: e.dma_start(out=xb, in_=xsrc[t * 128:(t + 1) * 128, :]), writes=[xk], key=xk)
    P.op("act", lambda e: e.activation(out=xn, in_=xb, func=AF.Square, accum_out=st[:, 0:1]), reads=[xk], writes=[nk, sk])
    P.op("dve", lambda e: e.tensor_scalar(st[:, 1:2], st[:, 0:1], 1.0 / D, EPS, op0=ALU.mult, op1=ALU.add), reads=[sk], writes=[sk])
    P.op("act", lambda e: e.activation(out=st[:, 1:2], in_=st[:, 1:2], func=AF.Ln), reads=[sk], writes=[sk])
    P.op("act", lambda e: e.activation(out=st[:, 1:2], in_=st[:, 1:2], func=AF.Exp, scale=-0.5), reads=[sk], writes=[sk])
    P.op("dve", lambda e: e.tensor_scalar(xn, xb, st[:, 1:2], None, op0=ALU.mult), reads=[xk, sk], writes=[nk])
    b0 = (t % 2) * 2
    for kc in range(8):
        bank = b0 + kc // 4
        P.op("pe", lambda e, kc=kc, bank=bank: e.transpose(ps[bank][:, (kc % 4) * 128:(kc % 4 + 1) * 128], xn[:, kc * 128:(kc + 1) * 128], k.ident),
             reads=[nk, "ident"], writes=["ps%d" % bank])
    return b0


def phaseA(k, l, I, xsrc):
    A, P, nc, ps = k.A, k.P, k.nc, k.ps
    m0 = A.mark()
    WC = DIN + 64
    w = A.alloc(8 * WC, BF16).rearrange("p (k c) -> p k c", k=8)
    wsrc = I["w_in"][l].rearrange("(k p) c -> p k c", p=128)
    for (d0, s0, n) in [(0, 0, 448), (448, 384, 64), (512, 448, 1024), (1536, 1472, 1024), (2560, 2496, 1024), (3584, 3520, 1032)]:
        P.dma("pool", lambda e, d0=d0, s0=s0, n=n: e.dma_start(out=w[:, :, d0:d0 + n], in_=wsrc[:, :, s0:s0 + n]), writes=["w_in"], key="w_in")
    xbA = [A.alloc(1024, F32) for _ in range(4)]
    xnA = [A.alloc(1024, F32) for _ in range(2)]
    nstA = [A.alloc(8, F32) for _ in range(2)]
    hT = [A.alloc(8 * 512, BF16).rearrange("p (k t) -> p k t", k=8) for _ in range(2)]
    fo = [A.alloc(512, F32) for _ in range(4)]
    gb = [A.alloc(512, BF16) for _ in range(4)]
    to = [A.alloc(1672, F32) for _ in range(2)]
    cq_fm = dram(k, "cq_fm", [256, S], F32)
    ckv_fm = dram(k, "ckv_fm", [128, S], F32)
    kidx_fm = dram(k, "kidx_fm", [128, S], F32)
    qrec_fm = dram(k, "qrec_fm", [512, S], F32)
    frec_fm = dram(k, "frec_fm", [512, S], F32)
    gates_fm = dram(k, "gates_fm", [2048, S], BF16)
    tm_out = dram(k, "tm_out", [S, 1672], F32)
    fmb = [(0, cq_fm, 0, 0), (128, cq_fm, 128, 0), (256, ckv_fm, 0, 0), (384, kidx_fm, 0, 0)]
    for j in range(4):
        fmb.append((O_QREC + 64 + j * 128, qrec_fm, j * 128, 0))
    for j in range(4):
        fmb.append((O_FREC + 64 + j * 128, frec_fm, j * 128, 0))
    for j in range(16):
        fmb.append((O_GA + 64 + j * 128, gates_fm, j * 128, 1))
    tmg = [(256, 128, 0), (O_WIDX + 64, 8, 128), (O_IREC + 64, 512, 136), (O_OG + 64, 512, 648), (O_FREC + 64, 512, 1160)]
    fcount = 0
    for st in range(S // 512):
        h = hT[st % 2]; hk = "hT%d" % (st % 2)
        sA = nstA[st % 2]; sAk = "nstA%d" % (st % 2)
        for tt in range(4):
            t = st * 4 + tt
            xb_ = xbA[tt]; xk_ = "xbA%d" % tt
            P.dma("sp", lambda e, xb_=xb_, t=t: e.dma_start(out=xb_, in_=xsrc[t * 128:(t + 1) * 128, :]), writes=[xk_], key=xk_)
            xn_ = xnA[tt % 2]; nk_ = "xnA%d" % (tt % 2)
            P.op("act", lambda e, xb_=xb_, xn_=xn_, sA=sA, tt=tt: e.activation(out=xn_, in_=xb_, func=AF.Square, accum_out=sA[:, tt:tt + 1]), reads=[xk_], writes=[nk_, sAk])
        P.op("dve", lambda e, sA=sA: e.tensor_scalar(sA[:, 4:8], sA[:, 0:4], 1.0 / D, EPS, op0=ALU.mult, op1=ALU.add), reads=[sAk], writes=[sAk])
        P.op("act", lambda e, sA=sA: e.activation(out=sA[:, 4:8], in_=sA[:, 4:8], func=AF.Ln), reads=[sAk], writes=[sAk])
        P.op("act", lambda e, sA=sA: e.activation(out=sA[:, 4:8], in_=sA[:, 4:8], func=AF.Exp, scale=-0.5), reads=[sAk], writes=[sAk])
        for tt in range(4):
            t = st * 4 + tt
            xb_ = xbA[tt]; xk_ = "xbA%d" % tt
            xn_ = xnA[tt % 2]; nk_ = "xnA%d" % (tt % 2)
            P.op("dve", lambda e, xb_=xb_, xn_=xn_, sA=sA, tt=tt: e.tensor_scalar(xn_, xb_, sA[:, 4 + tt:5 + tt], None, op0=ALU.mult), reads=[xk_, sAk], writes=[nk_])
            b0 = (t % 2) * 2
            for kc in range(8):
                bank = b0 + kc // 4
                P.op("pe", lambda e, kc=kc, bank=bank, xn_=xn_: e.transpose(ps[bank][:, (kc % 4) * 128:(kc % 4 + 1) * 128], xn_[:, kc * 128:(kc + 1) * 128], k.ident),
                     reads=[nk_, "ident"], writes=["ps%d" % bank])
            for kc in range(8):
                bank = b0 + kc // 4
                P.op("act", lambda e, kc=kc, bank=bank, tt=tt, h=h: e.activation(out=h[:, kc, tt * 128:(tt + 1) * 128], in_=ps[bank][:, (kc % 4) * 128:(kc % 4 + 1) * 128],
                                                                                 func=AF.Identity, scale=k.gs1[:, kc:kc + 1], bias=k.sh1[:, kc:kc + 1]),
                     reads=["ps%d" % bank, "gs", "modf"], writes=[hk])
        for bi, (c0, dst, r0, act) in enumerate(fmb):
            bank = 4 + fcount % 4
            pk = "ps%d" % bank
            for kc in range(8):
                P.op("pe", lambda e, kc=kc, c0=c0, bank=bank, h=h: e.matmul(ps[bank][:, :], lhsT=w[:, kc, c0:c0 + 128], rhs=h[:, kc, :], start=(kc == 0), stop=(kc == 7)),
                     reads=["w_in", hk], writes=[pk])
            f = fo[fcount % 4]; fk = "fo%d" % (fcount % 4)
            if act == 0:
                P.op("dve", lambda e, f=f, bank=bank: e.tensor_copy(f, ps[bank][:, :]), reads=[pk], writes=[fk])
                P.dma("sp", lambda e, f=f, dst=dst, r0=r0, st=st: e.dma_start(out=dst[r0:r0 + 128, st * 512:(st + 1) * 512], in_=f), reads=[fk], writes=[], key=fk)
            else:
                fb = gb[fcount % 4]
                P.op("act", lambda e, fb=fb, bank=bank: e.activation(out=fb, in_=ps[bank][:, :], func=AF.Sigmoid), reads=[pk], writes=[fk])
                P.dma("sp", lambda e, fb=fb, dst=dst, r0=r0, st=st: e.dma_start(out=dst[r0:r0 + 128, st * 512:(st + 1) * 512], in_=fb), reads=[fk], writes=[], key=fk)
            fcount += 1
        for tt in range(4):
            t = st * 4 + tt
            tb = to[t % 2]; tk = "to%d" % (t % 2)
            for gi, (c0, n, o0) in enumerate(tmg):
                if gi == 1:
                    continue
                bank = 4 + fcount % 4
                pk = "ps%d" % bank
                subs = [(c0, n, 0)]
                if gi == 0:
                    subs = [(c0, n, 0), (tmg[1][0], 8, 128)]
                for (cc, nn, po) in subs:
                    for kc in range(8):
                        P.op("pe", lambda e, kc=kc, cc=cc, nn=nn, po=po, bank=bank, h=h, tt=tt: e.matmul(ps[bank][:, po:po + nn], lhsT=h[:, kc, tt * 128:(tt + 1) * 128], rhs=w[:, kc, cc:cc + nn],
                                                                                                  start=(kc == 0), stop=(kc == 7)),
                             reads=["w_in", hk], writes=[pk])
                tot = n + (8 if gi == 0 else 0)
                eng = "dve" if gi % 2 == 0 else "act"
                if eng == "dve":
                    P.op("dve", lambda e, tb=tb, o0=o0, tot=tot, bank=bank: e.tensor_copy(tb[:, o0:o0 + tot], ps[bank][:, 0:tot]), reads=[pk], writes=[tk])
                else:
                    P.op("act", lambda e, tb=tb, o0=o0, tot=tot, bank=bank: e.activation(out=tb[:, o0:o0 + tot], in_=ps[bank][:, 0:tot], func=AF.Copy), reads=[pk], writes=[tk])
                fcount += 1
            P.dma("sp", lambda e, tb=tb, t=t: e.dma_start(out=tm_out[t * 128:(t + 1) * 128, :], in_=tb), reads=[tk], writes=[], key=tk)
    P.barrier()
    A.release(m0)


ATTN_SCALE = 128 ** -0.5
NEG = -1e30
MASKV = -30000.0


def phaseB(k, l, I):
    A, P, ps = k.A, k.P, k.ps
    cq_fm = k.dr["cq_fm"]; ckv_fm = k.dr["ckv_fm"]; kidx_fm = k.dr["kidx_fm"]; tm_out = k.dr["tm_out"]
    qT_d = dram(k, "qT_d", [NT, 128, 8, 128], BF16)
    qidx_d = dram(k, "qidx_d", [NT, 128, 4, 128], BF16)
    k.kvT = A.alloc(S, BF16)
    k.kidxT = A.alloc(S, BF16)
    k.kvaug = A.alloc(NT * 132, BF16).rearrange("p (t c) -> p t c", t=NT)
    k.widx = A.alloc(NT * 8, F32).rearrange("p (t c) -> p t c", t=NT)
    k.wv = A.alloc(8 * 128, BF16).rearrange("p (h c) -> p h c", h=8)
    m0 = A.mark()
    wq = A.alloc(2 * 1024, BF16).rearrange("p (k c) -> p k c", k=2)
    wi = A.alloc(2 * 512, BF16).rearrange("p (k c) -> p k c", k=2)
    P.dma("pool", lambda e: e.dma_start(out=wq, in_=I["w_q_up"][l].rearrange("(k p) c -> p k c", p=128)), writes=["wq"], key="wq")
    P.dma("pool", lambda e: e.dma_start(out=wi, in_=I["w_idx_q"][l].rearrange("(k p) c -> p k c", p=128)), writes=["wi"], key="wi")
    P.op("dve", lambda e: e.memset(k.wv, 0.0), writes=["wv"])
    for par in range(2):
        P.dma("pool", lambda e, par=par: e.dma_start(out=k.wv.rearrange("p (j two) c -> p j two c", two=2)[:, :, par, par * 64:(par + 1) * 64],
                                                     in_=I["w_v_up"][l].rearrange("(j two) r v -> r j two v", two=2)[:, :, par, :]),
              writes=["wv"], key="wvd")
    ckt = A.alloc(NT * 128, F32).rearrange("p (t c) -> p t c", t=NT)
    sq = A.alloc(NT * 128, F32).rearrange("p (t c) -> p t c", t=NT)
    gb = A.alloc(128, F32)
    st = A.alloc(64, F32)
    P.dma("sp", lambda e: e.dma_start(out=ckt, in_=tm_out[:, 0:128].rearrange("(t p) c -> p t c", p=128)), writes=["ckt"], key="ckt")
    P.dma("sp", lambda e: e.dma_start(out=k.widx, in_=tm_out[:, 128:136].rearrange("(t p) c -> p t c", p=128)), writes=["widx"], key="widx")
    P.dma("sp", lambda e: e.dma_start(out=gb, in_=I["g_ckv"][l].partition_broadcast(128)), writes=["gb"], key="gb")
    P.op("dve", lambda e: e.tensor_tensor(sq, ckt, ckt, op=ALU.mult), reads=["ckt"], writes=["sq"])
    P.op("dve", lambda e: e.tensor_reduce(out=st[:, 0:32], in_=sq, axis=AX.X, op=ALU.add), reads=["sq"], writes=["stB"])
    P.op("dve", lambda e: e.tensor_scalar(st[:, 0:32], st[:, 0:32], 1.0 / 128, EPS, op0=ALU.mult, op1=ALU.add), reads=["stB"], writes=["stB"])
    P.op("act", lambda e: e.activation(out=st[:, 0:32], in_=st[:, 0:32], func=AF.Ln), reads=["stB"], writes=["stB"])
    P.op("act", lambda e: e.activation(out=st[:, 0:32], in_=st[:, 0:32], func=AF.Exp, scale=-0.5), reads=["stB"], writes=["stB"])
    P.op("dve", lambda e: e.tensor_tensor(sq, ckt, st[:, 0:32].unsqueeze(2).to_broadcast([128, NT, 128]), op=ALU.mult), reads=["ckt", "stB", "sq"], writes=["sq"])
    P.op("dve", lambda e: e.tensor_tensor(k.kvaug[:, :, 0:128], sq, gb.unsqueeze(1).to_broadcast([128, NT, 128]), op=ALU.mult), reads=["sq", "gb"], writes=["kvaug"])
    P.op("dve", lambda e: e.memset(k.kvaug[:, :, 128:132], 1.0), writes=["kvaug"])
    xin = [A.alloc(4 * 512, F32).rearrange("p (c t) -> p c t", c=4) for _ in range(2)]
    sqb = A.alloc(4 * 512, BF16).rearrange("p (c t) -> p c t", c=4)
    rs = A.alloc(3 * 512, F32).rearrange("p (c t) -> p c t", c=3)
    cqn = A.alloc(2 * 512, BF16).rearrange("p (c t) -> p c t", c=2)
    ob = [A.alloc(512, BF16) for _ in range(4)]
    oc = 0
    for b in range(S // 512):
        x = xin[b % 2]; xk = "xinB%d" % (b % 2)
        sl = slice(b * 512, (b + 1) * 512)
        P.dma("sp", lambda e, x=x, sl=sl: e.dma_start(out=x[:, 0:2, :], in_=cq_fm[:, sl].rearrange("(c p) t -> p c t", p=128)), writes=[xk], key=xk)
        P.dma("sp", lambda e, x=x, sl=sl: e.dma_start(out=x[:, 2, :], in_=ckv_fm[:, sl]), writes=[xk], key=xk)
        P.dma("sp", lambda e, x=x, sl=sl: e.dma_start(out=x[:, 3, :], in_=kidx_fm[:, sl]), writes=[xk], key=xk)
        P.op("act", lambda e, x=x: e.activation(out=sqb, in_=x, func=AF.Square), reads=[xk], writes=["sqb"])
        P.op("pe", lambda e: e.matmul(ps[0][:, :], lhsT=k.ones_b, rhs=sqb[:, 0, :], start=True, stop=False), reads=["sqb", "ones_b"], writes=["ps0"])
        P.op("pe", lambda e: e.matmul(ps[0][:, :], lhsT=k.ones_b, rhs=sqb[:, 1, :], start=False, stop=True), reads=["sqb", "ones_b"], writes=["ps0"])
        P.op("pe", lambda e: e.matmul(ps[1][:, :], lhsT=k.ones_b, rhs=sqb[:, 2, :], start=True, stop=True), reads=["sqb", "ones_b"], writes=["ps1"])
        P.op("pe", lambda e: e.matmul(ps[2][:, :], lhsT=k.ones_b, rhs=sqb[:, 3, :], start=True, stop=True), reads=["sqb", "ones_b"], writes=["ps2"])
        for i, n in enumerate([256.0, 128.0, 128.0]):
            P.op("dve", lambda e, i=i, n=n: e.tensor_scalar(rs[:, i, :], ps[i][:, :], 1.0 / n, EPS, op0=ALU.mult, op1=ALU.add), reads=["ps%d" % i], writes=["rs"])
        P.op("act", lambda e: e.activation(out=rs, in_=rs, func=AF.Ln), reads=["rs"], writes=["rs"])
        P.op("act", lambda e: e.activation(out=rs, in_=rs, func=AF.Exp, scale=-0.5), reads=["rs"], writes=["rs"])
        for c in range(2):
            P.op("dve", lambda e, c=c, x=x: e.scalar_tensor_tensor(out=cqn[:, c, :], in0=x[:, c, :], scalar=k.gcq[:, c:c + 1], in1=rs[:, 0, :], op0=ALU.mult, op1=ALU.mult),
                 reads=[xk, "rs", "par"], writes=["cqn"])
        P.op("dve", lambda e, x=x, sl=sl: e.scalar_tensor_tensor(out=k.kvT[:, sl], in0=x[:, 2, :], scalar=k.gckv[:, 0:1], in1=rs[:, 1, :], op0=ALU.mult, op1=ALU.mult),
             reads=[xk, "rs", "par"], writes=["kvT"])
        P.op("dve", lambda e, x=x, sl=sl: e.scalar_tensor_tensor(out=k.kidxT[:, sl], in0=x[:, 3, :], scalar=k.gkidx[:, 0:1], in1=rs[:, 2, :], op0=ALU.mult, op1=ALU.mult),
             reads=[xk, "rs", "par"], writes=["kidxT"])
        for h in range(8):
            bank = 4 + oc % 4; pk = "ps%d" % bank
            for c in range(2):
                P.op("pe", lambda e, h=h, c=c, bank=bank: e.matmul(ps[bank][:, :], lhsT=wq[:, c, h * 128:(h + 1) * 128], rhs=cqn[:, c, :], start=(c == 0), stop=(c == 1)),
                     reads=["wq", "cqn"], writes=[pk])
            o = ob[oc % 4]; ok = "obB%d" % (oc % 4)
            P.op("act", lambda e, o=o, bank=bank: e.activation(out=o, in_=ps[bank][:, :], func=AF.Copy, scale=ATTN_SCALE), reads=[pk], writes=[ok])
            P.dma("sp", lambda e, o=o, h=h, b=b: e.dma_start(out=qT_d[b * 4:(b + 1) * 4, :, h, :].rearrange("t r q -> r t q"), in_=o.rearrange("p (t q) -> p t q", t=4)),
                  reads=[ok], key=ok)
            oc += 1
        for j in range(4):
            bank = 4 + oc % 4; pk = "ps%d" % bank
            for c in range(2):
                P.op("pe", lambda e, j=j, c=c, bank=bank: e.matmul(ps[bank][:, :], lhsT=wi[:, c, j * 128:(j + 1) * 128], rhs=cqn[:, c, :], start=(c == 0), stop=(c == 1)),
                     reads=["wi", "cqn"], writes=[pk])
            o = ob[oc % 4]; ok = "obB%d" % (oc % 4)
            P.op("dve", lambda e, o=o, bank=bank: e.tensor_copy(o, ps[bank][:, :]), reads=[pk], writes=[ok])
            P.dma("sp", lambda e, o=o, j=j, b=b: e.dma_start(out=qidx_d[b * 4:(b + 1) * 4, :, j, :].rearrange("t r q -> r t q"), in_=o.rearrange("p (t q) -> p t q", t=4)),
                  reads=[ok], key=ok)
            oc += 1
    P.barrier()
    A.release(m0)


def phaseC(k, l, I, NITER=12):
    A, P, ps = k.A, k.P, k.ps
    qT_d = k.dr["qT_d"]; qidx_d = k.dr["qidx_d"]
    yaT_d = dram(k, "yaT_d", [4, 128, S], BF16)
    m0 = A.mark()
    qs = [A.alloc(8 * 128, BF16) for _ in range(4)]
    qi = [A.alloc(4 * 128, BF16).rearrange("p (j q) -> p j q", j=4) for _ in range(2)]
    score = [A.alloc(S, F32) for _ in range(2)]
    mb = [A.alloc(S, BF16) for _ in range(4)]
    junk = [A.alloc(S, BF16) for _ in range(2)]
    rb = [A.alloc(512, F32) for _ in range(3)]
    pT = [A.alloc(512, BF16) for _ in range(4)]
    on = A.alloc(8 * 128, BF16).rearrange("p (h r) -> p h r", h=8)
    onT = A.alloc(8 * 128, BF16).rearrange("p (h q) -> p h q", h=8)
    yo = [A.alloc(4 * 128, BF16).rearrange("p (j q) -> p j q", j=4) for _ in range(2)]
    ident4 = A.alloc(512, BF16)
    bs = [A.alloc(64, F32) for _ in range(2)]
    pw = A.alloc(NITER, F32)
    rden = A.alloc(8, F32)
    cm = A.alloc(2, F32)
    P.op("dve", lambda e: e.memset(cm, MASKV), writes=["cm"])
    for i in range(4):
        P.op("dve", lambda e, i=i: e.tensor_copy(ident4[:, i * 128:(i + 1) * 128], k.identb), reads=["identb"], writes=["ident4"])
    for i in range(NITER):
        P.op("dve", lambda e, i=i: e.memset(pw[:, i:i + 1], 0.5 ** (i + 1)), writes=["pw"])
    def oacc(h):
        return ps[h // 3][:, (h % 3) * 129:(h % 3) * 129 + 129]
    rc = [0]

    def prep(qt):
        b = qt % 2; b4 = qt % 4
        sc = score[b]; sk = "score%d" % b
        nk = (qt + 1) * 128
        P.dma("sp", lambda e: e.dma_start(out=qs[b4], in_=qT_d[qt].rearrange("r h q -> r (h q)")), writes=["qs%d" % b4], key="qs%d" % b4)
        P.dma("sp", lambda e: e.dma_start(out=qi[b], in_=qidx_d[qt]), writes=["qi%d" % b], key="qi%d" % b)
        for c0 in range(0, nk, 512):
            wd = min(512, nk - c0)
            for h in range(8):
                bank = 3 + rc[0] % 2; pk = "ps%d" % bank
                p0 = (h % 2) * 64
                P.op("pe", lambda e, h=h, p0=p0, bank=bank, c0=c0, wd=wd: e.matmul(ps[bank][:, 0:wd], lhsT=qi[b][p0:p0 + 64, h // 2, :], rhs=k.kidxT[p0:p0 + 64, c0:c0 + wd], start=True, stop=True),
                     reads=["qi%d" % b, "kidxT"], writes=[pk])
                r = rb[rc[0] % 3]; rk = "rb%d" % (rc[0] % 3)
                P.op("act", lambda e, r=r, bank=bank, wd=wd: e.activation(out=r[:, 0:wd], in_=ps[bank][:, 0:wd], func=AF.Relu), reads=[pk], writes=[rk])
                if h == 0:
                    P.op("dve", lambda e, r=r, c0=c0, wd=wd, h=h: e.tensor_scalar(sc[:, c0:c0 + wd], r[:, 0:wd], k.widx[:, qt, h:h + 1], None, op0=ALU.mult),
                         reads=[rk, "widx"], writes=[sk])
                else:
                    P.op("dve", lambda e, r=r, c0=c0, wd=wd, h=h: e.scalar_tensor_tensor(out=sc[:, c0:c0 + wd], in0=r[:, 0:wd], scalar=k.widx[:, qt, h:h + 1], in1=sc[:, c0:c0 + wd], op0=ALU.mult, op1=ALU.add),
                         reads=[rk, "widx", sk], writes=[sk])
                rc[0] += 1
        P.op("pool", lambda e: e.tensor_tensor(sc[:, qt * 128:(qt + 1) * 128], sc[:, qt * 128:(qt + 1) * 128], k.cneg, op=ALU.add), reads=[sk, "cneg"], writes=[sk])
        s_ = bs[b]; bk = "bs%d" % b
        lo = s_[:, 0:1]; w0 = s_[:, 1:2]; mid = s_[:, 2:3]; wi_ = s_[:, 8:8 + NITER]
        if qt < 2:
            P.op("dve", lambda e: e.memset(lo, -1e29), writes=[bk])
        else:
            P.op("dve", lambda e: e.tensor_reduce(out=w0, in_=sc[:, 0:nk], axis=AX.X, op=ALU.max), reads=[sk], writes=[bk])
            P.op("dve", lambda e: e.tensor_reduce(out=lo, in_=sc[:, 0:qt * 128], axis=AX.X, op=ALU.min), reads=[sk], writes=[bk])
            P.op("dve", lambda e: e.tensor_scalar(lo, lo, -1.0, None, op0=ALU.add), reads=[bk], writes=[bk])
            P.op("dve", lambda e: e.tensor_tensor(w0, w0, lo, op=ALU.subtract), reads=[bk], writes=[bk])
            P.op("dve", lambda e: e.tensor_scalar(wi_, pw, w0, None, op0=ALU.mult), reads=[bk, "pw"], writes=[bk])
            P.op("dve", lambda e: e.tensor_tensor(mid, lo, wi_[:, 0:1], op=ALU.add), reads=[bk], writes=[bk])

    def bis_iter(qt, it):
        b = qt % 2
        sc = score[b]; sk = "score%d" % b
        nk = (qt + 1) * 128
        s_ = bs[b]; bk = "bs%d" % b
        lo = s_[:, 0:1]; mid = s_[:, 2:3]; cnt = s_[:, 3:4]; tmp = s_[:, 4:5]; wi_ = s_[:, 8:8 + NITER]
        jk = junk[b]; jkk = "junk%d" % b
        P.op("dve", lambda e: e.tensor_scalar(jk[:, 0:nk], sc[:, 0:nk], mid, None, op0=ALU.is_gt, op1=ALU.add, accum_out=cnt), reads=[sk, bk], writes=[bk, jkk])
        P.op("dve", lambda e: e.scalar_tensor_tensor(out=tmp, in0=cnt, scalar=256.0, in1=wi_[:, it:it + 1], op0=ALU.is_ge, op1=ALU.mult), reads=[bk], writes=[bk])
        if it < NITER - 1:
            P.op("dve", lambda e: e.scalar_tensor_tensor(out=mid, in0=mid, scalar=wi_[:, it + 1:it + 2], in1=tmp, op0=ALU.subtract, op1=ALU.add), reads=[bk], writes=[bk])
        else:
            P.op("dve", lambda e: e.scalar_tensor_tensor(out=lo, in0=mid, scalar=wi_[:, it:it + 1], in1=tmp, op0=ALU.subtract, op1=ALU.add), reads=[bk], writes=[bk])

    def fin_mask(qt):
        b = qt % 2; b4 = qt % 4
        nk = (qt + 1) * 128
        lo = bs[b][:, 0:1]
        P.op("dve", lambda e: e.tensor_scalar(mb[b4][:, 0:nk], score[b][:, 0:nk], lo, cm[:, 0:1], op0=ALU.is_le, op1=ALU.mult), reads=["score%d" % b, "bs%d" % b, "cm"], writes=["mb%d" % b4])

    def bis(qts):
        for it in range(NITER):
            for qt in qts:
                if qt >= 2:
                    bis_iter(qt, it)
        for qt in qts:
            fin_mask(qt)

    pc = [0]

    def attention(qt):
        b = qt % 4
        q = qs[b]
        steps = [(kb, hg) for kb in range(qt + 1) for hg in range(2)]
        base = pc[0]

        def qk(i_):
            kb, hg = steps[i_]
            bank = 5 + (base + i_) % 2; pk = "ps%d" % bank
            P.op("pe", lambda e: e.matmul(ps[bank][:, :], lhsT=k.kvT[:, kb * 128:(kb + 1) * 128], rhs=q[:, hg * 512:(hg + 1) * 512], start=True, stop=False),
                 reads=["kvT", "qs%d" % b], writes=[pk])
            P.op("pe", lambda e: e.matmul(ps[bank][:, :], lhsT=mb[b][:, kb * 128:(kb + 1) * 128], rhs=ident4, start=False, stop=True),
                 reads=["mb%d" % b, "ident4"], writes=[pk])

        qk(0)
        for i_, (kb, hg) in enumerate(steps):
            if i_ + 1 < len(steps):
                qk(i_ + 1)
            bank = 5 + (base + i_) % 2; pk = "ps%d" % bank
            p = pT[(base + i_) % 4]; pk2 = "pT%d" % ((base + i_) % 4)
            P.op("act", lambda e, p=p, bank=bank: e.activation(out=p, in_=ps[bank][:, :], func=AF.Exp), reads=[pk], writes=[pk2])
            for hh in range(4):
                h = hg * 4 + hh
                P.op("pe", lambda e, p=p, hh=hh, h=h, kb=kb: e.matmul(oacc(h), lhsT=p[:, hh * 128:(hh + 1) * 128], rhs=k.kvaug[:, kb, 0:129], start=(kb == 0 and h % 3 == 0), stop=(kb == qt), skip_group_check=True),
                     reads=[pk2, "kvaug"], writes=["ps%d" % (h // 3)])
        pc[0] += len(steps)
        for bnk in range(3):
            nh = 3 if bnk < 2 else 2
            v = ps[bnk][:, 0:nh * 129].rearrange("p (h c) -> p h c", c=129)
            P.op("dve", lambda e, v=v, bnk=bnk, nh=nh: e.reciprocal(rden[:, bnk * 3:bnk * 3 + nh], v[:, :, 128]), reads=["ps%d" % bnk], writes=["rden"])
            P.op("dve", lambda e, v=v, bnk=bnk, nh=nh: e.tensor_tensor(on[:, bnk * 3:bnk * 3 + nh, :], v[:, :, 0:128], rden[:, bnk * 3:bnk * 3 + nh].unsqueeze(2).to_broadcast([128, nh, 128]), op=ALU.mult),
                 reads=["ps%d" % bnk, "rden"], writes=["on"])
        tb = ps[7][:, :].bitcast(BF16)
        for h in range(8):
            P.op("pe", lambda e, h=h: e.transpose(tb[:, h * 128:(h + 1) * 128], on[:, h, :], k.identb), reads=["on", "identb"], writes=["ps7"])
        P.op("act", lambda e: e.activation(out=onT.rearrange("p h q -> p (h q)"), in_=tb, func=AF.Copy), reads=["ps7"], writes=["onT"])
        for j in range(4):
            for two in range(2):
                h = j * 2 + two
                P.op("pe", lambda e, j=j, two=two, h=h: e.matmul(ps[7][:, j * 128:(j + 1) * 128], lhsT=k.wv[:, h, :], rhs=onT[:, h, :], start=(two == 0), stop=(two == 1)),
                     reads=["wv", "onT"], writes=["ps7"])
        y = yo[qt % 2]; yk = "yo%d" % (qt % 2)
        P.op("dve", lambda e: e.tensor_copy(y.rearrange("p j q -> p (j q)"), ps[7][:, :]), reads=["ps7"], writes=[yk])
        P.dma("sp", lambda e: e.dma_start(out=yaT_d[:, :, qt * 128:(qt + 1) * 128].rearrange("j p q -> p j q"), in_=y), reads=[yk], key=yk)

    prep(0); bis([0])
    for qt in range(NT):
        if qt + 1 < NT:
            prep(qt + 1); bis([qt + 1])
        attention(qt)
    P.barrier()
    A.release(m0)


def phaseD(k, l, I):
    A, P, ps = k.A, k.P, k.ps
    qrec_fm = k.dr["qrec_fm"]; frec_fm = k.dr["frec_fm"]; tm_out = k.dr["tm_out"]
    yrT_d = dram(k, "yrT_d", [4, 128, S], BF16)
    m0 = A.mark()
    NB = 512
    def fm(dt=F32):
        return A.alloc(4 * NB, dt).rearrange("p (j t) -> p j t", j=4)
    z = fm(); qr = fm(); e = fm(); t1 = fm(); t2 = fm(); Acum = fm(); kk = fm(); qq = fm()
    qt_ = fm(BF16); kt_ = fm(BF16); qhA = fm(BF16); qhB = fm(BF16); kh = fm(BF16)
    cmf = k.cmf
    grb = A.alloc(512, F32)
    vt = A.alloc(4 * 512, BF16).rearrange("p (t c) -> p t c", t=4)
    ogt = A.alloc(4 * 512, F32).rearrange("p (t c) -> p t c", t=4)
    vtf = A.alloc(4 * 512, F32).rearrange("p (t c) -> p t c", t=4)
    sog = A.alloc(4 * 512, F32).rearrange("p (t c) -> p t c", t=4)
    khT = A.alloc(512, BF16)
    Pm = A.alloc(8 * 128, BF16).rearrange("p (h t) -> p h t", h=8)
    state = A.alloc(4 * 64, F32).rearrange("p (j v) -> p j v", j=4)
    stmp = A.alloc(4 * 64, F32).rearrange("p (j v) -> p j v", j=4)
    sbf = [A.alloc(4 * 64, BF16).rearrange("p (j v) -> p j v", j=4) for _ in range(4)]
    decay = A.alloc(32, F32)
    osb = A.alloc(512, F32); osq = A.alloc(512, F32); oss = A.alloc(16, F32)
    sg = A.alloc(512, F32)
    yb = A.alloc(512, BF16)
    yT = [A.alloc(512, BF16) for _ in range(2)]
    P.dma("sp", lambda e_: e_.dma_start(out=osb[:, 0:64], in_=I["g_rec"][l].partition_broadcast(128)), writes=["osb"], key="grb")
    P.op("dve", lambda e_: e_.tensor_copy(grb.rearrange("p (h v) -> p h v", h=8), osb[:, 0:64].unsqueeze(1).to_broadcast([128, 8, 64])), reads=["osb"], writes=["grb"])
    P.op("dve", lambda e_: e_.memset(state, 0.0), writes=["state"])
    P.op("dve", lambda e_: e_.memset(sbf[0], 0.0), writes=["sbf0"])
    P.op("dve", lambda e_: e_.memset(qhA, 0.0), writes=["qhA"])
    P.op("dve", lambda e_: e_.memset(qhB, 0.0), writes=["qhB"])
    sv = [0]
    z2 = z.rearrange("p j t -> p (j t)"); e2 = e.rearrange("p j t -> p (j t)"); t12 = t1.rearrange("p j t -> p (j t)"); t22 = t2.rearrange("p j t -> p (j t)")
    A2 = Acum.rearrange("p j t -> p (j t)")
    def ch(x):
        return x.rearrange("p j (c t) -> p (j c) t", t=64)
    for b in range(S // NB):
        sl = slice(b * NB, (b + 1) * NB)
        P.dma("sp", lambda e_, sl=sl: e_.dma_start(out=z, in_=frec_fm[:, sl].rearrange("(j p) t -> p j t", p=128)), writes=["z"], key="zD")
        P.dma("sp", lambda e_, sl=sl: e_.dma_start(out=qr, in_=qrec_fm[:, sl].rearrange("(j p) t -> p j t", p=128)), writes=["qr"], key="qrD")
        P.dma("sp", lambda e_, sl=sl: e_.dma_start(out=vtf, in_=tm_out[sl, 136:648].rearrange("(t p) c -> p t c", p=128)), writes=["vtf"], key="vtD")
        P.op("act", lambda e_: e_.activation(out=vt, in_=vtf, func=AF.Copy), reads=["vtf"], writes=["vt"])
        P.dma("sp", lambda e_, sl=sl: e_.dma_start(out=ogt, in_=tm_out[sl, 648:1160].rearrange("(t p) c -> p t c", p=128)), writes=["ogt"], key="ogD")
        P.op("act", lambda e_: e_.activation(out=kk, in_=z, func=AF.Sigmoid, scale=-1.0), reads=["z"], writes=["kk"])
        P.op("act", lambda e_: e_.activation(out=qq, in_=qr, func=AF.Sigmoid), reads=["qr"], writes=["qq"])
        P.op("act", lambda e_: e_.activation(out=sog, in_=ogt, func=AF.Sigmoid), reads=["ogt"], writes=["sog"])
        P.op("act", lambda e_: e_.activation(out=e, in_=z, func=AF.Exp, scale=-1.0), reads=["z"], writes=["e"])
        for j in range(4):
            P.op("dve", lambda e_, j=j: e_.tensor_scalar(t1[:, j, :], e[:, j, :], k.lb[:, j:j + 1], 1.0, op0=ALU.mult, op1=ALU.add), reads=["e", "sm"], writes=["t1"])
        P.op("dve", lambda e_: e_.tensor_scalar(t2, e, 1.0, None, op0=ALU.add), reads=["e"], writes=["t2"])
        P.op("act", lambda e_: e_.activation(out=t1, in_=t1, func=AF.Ln), reads=["t1"], writes=["t1"])
        P.op("act", lambda e_: e_.activation(out=z, in_=t2, func=AF.Ln), reads=["t2", "z"], writes=["z"])
        P.op("dve", lambda e_: e_.tensor_tensor(t1, t1, z, op=ALU.subtract), reads=["t1", "z"], writes=["t1"])
        for j in range(4):
            P.op("dve", lambda e_, j=j: e_.tensor_scalar(kk[:, j, :], kk[:, j, :], k.oml[:, j:j + 1], None, op0=ALU.mult), reads=["kk", "sm"], writes=["kk"])
        srcs = [t1, Acum]
        for si, sh in enumerate([1, 2, 4, 8, 16, 32]):
            a_ = ch(srcs[si % 2]); b_ = ch(srcs[(si + 1) % 2])
            P.op("dve", lambda e_, a_=a_, b_=b_, sh=sh: e_.tensor_tensor(b_[:, :, sh:64], a_[:, :, sh:64], a_[:, :, 0:64 - sh], op=ALU.add), reads=["t1", "Acum"], writes=["t1", "Acum"])
            P.op("act", lambda e_, a_=a_, b_=b_, sh=sh: e_.activation(out=b_[:, :, 0:sh], in_=a_[:, :, 0:sh], func=AF.Copy), reads=["t1", "Acum"], writes=["t1", "Acum"])
        P.op("act", lambda e_: e_.activation(out=Acum, in_=t1, func=AF.Copy), reads=["t1", "Acum"], writes=["t1", "Acum"])
        P.op("dve", lambda e_: e_.tensor_tensor(qq, qq, qr, op=ALU.mult), reads=["qq", "qr"], writes=["qq"])
        Ac = ch(Acum)
        P.op("dve", lambda e_: e_.tensor_tensor(ch(t1), Ac, Ac[:, :, 31:32].to_broadcast([128, 32, 64]), op=ALU.subtract), reads=["Acum", "t1"], writes=["t1"])
        P.op("dve", lambda e_: e_.tensor_scalar(t1, t1, -40.0, 40.0, op0=ALU.max, op1=ALU.min), reads=["t1"], writes=["t1"])
        P.op("act", lambda e_: e_.activation(out=t2, in_=t1, func=AF.Exp), reads=["t1"], writes=["t2"])
        P.op("dve", lambda e_: e_.tensor_tensor(qt_, qq, t2, op=ALU.mult), reads=["qq", "t2"], writes=["qt_"])
        P.op("act", lambda e_: e_.activation(out=t2, in_=t1, func=AF.Exp, scale=-1.0), reads=["t1", "qt_"], writes=["t2"])
        P.op("dve", lambda e_: e_.tensor_tensor(kt_, kk, t2, op=ALU.mult), reads=["kk", "t2"], writes=["kt_"])
        P.op("act", lambda e_: e_.activation(out=t2, in_=Acum, func=AF.Exp), reads=["Acum", "kt_"], writes=["t2"])
        def eo(x, par):
            return x.rearrange("p j (c two t) -> p j c two t", two=2, t=64)[:, :, :, par, :]
        P.op("dve", lambda e_: e_.tensor_tensor(eo(qhA, 0), eo(qq, 0), eo(t2, 0), op=ALU.mult), reads=["qq", "t2"], writes=["qhA"])
        P.op("dve", lambda e_: e_.tensor_tensor(eo(qhB, 1), eo(qq, 1), eo(t2, 1), op=ALU.mult), reads=["qq", "t2"], writes=["qhB"])
        P.op("act", lambda e_: e_.activation(out=decay, in_=Ac[:, :, 63], func=AF.Exp), reads=["Acum"], writes=["decay"])
        P.op("dve", lambda e_: e_.tensor_tensor(ch(t1), Ac[:, :, 63:64].to_broadcast([128, 32, 64]), Ac, op=ALU.subtract), reads=["Acum", "t1"], writes=["t1"])
        P.op("act", lambda e_: e_.activation(out=t2, in_=t1, func=AF.Exp), reads=["t1", "qhA", "qhB"], writes=["t2"])
        P.op("dve", lambda e_: e_.tensor_tensor(kh, kk, t2, op=ALU.mult), reads=["kk", "t2"], writes=["kh"])
        for tt in range(4):
            tsl = slice(tt * 128, (tt + 1) * 128)
            tb = ps[7][:, :].bitcast(BF16)
            for j in range(4):
                P.op("pe", lambda e_, j=j, tsl=tsl: e_.transpose(tb[:, j * 128:(j + 1) * 128], kh[:, j, tsl], k.identb), reads=["kh", "identb"], writes=["ps7"])
            P.op("act", lambda e_: e_.activation(out=khT, in_=tb[:, 0:512], func=AF.Copy), reads=["ps7"], writes=["khT"])
            for h in range(8):
                j = h // 2; p0 = (h % 2) * 64
                bank = 5 + h % 2
                P.op("pe", lambda e_, h=h, j=j, p0=p0, bank=bank, tsl=tsl: e_.matmul(ps[bank][:, j * 128:(j + 1) * 128], lhsT=kt_[p0:p0 + 64, j, tsl], rhs=qt_[p0:p0 + 64, j, tsl], start=True, stop=True),
                     reads=["kt_", "qt_"], writes=["ps%d" % bank])
            for g in range(2):
                P.op("dve", lambda e_, g=g: e_.tensor_tensor(Pm[:, g * 4:(g + 1) * 4, :], ps[5 + g][:, :].rearrange("p (h t) -> p h t", h=4), cmf.unsqueeze(1).to_broadcast([128, 4, 128]), op=ALU.mult),
                     reads=["ps%d" % (5 + g), "cmf"], writes=["Pm"])
            svA = sv[0]
            for half in range(2):
                c = tt * 2 + half
                hs = slice(half * 64, (half + 1) * 64)
                for j in range(4):
                    P.op("pe", lambda e_, j=j, hs=hs, tt=tt: e_.matmul(ps[4][:, j * 128:(j + 1) * 128], lhsT=khT[hs, j * 128:(j + 1) * 128], rhs=vt[hs, tt, j * 128:(j + 1) * 128], start=True, stop=True),
                         reads=["khT", "vt"], writes=["ps4"])
                dcol = [jj * 8 + c for jj in range(4)]
                dv = decay.rearrange("p (j c) -> p j c", j=4)[:, :, c:c + 1]
                P.op("dve", lambda e_, dv=dv: e_.tensor_tensor(stmp, state, dv.to_broadcast([128, 4, 64]), op=ALU.mult), reads=["state", "decay"], writes=["stmp"])
                pv = ps[4][:, :].rearrange("p (j x) -> p j x", j=4)
                P.op("dve", lambda e_, pv=pv: e_.tensor_tensor(state[0:64], stmp[0:64], pv[0:64, :, 0:64], op=ALU.add), reads=["stmp", "ps4"], writes=["state"])
                P.op("dve", lambda e_, pv=pv: e_.tensor_tensor(state[64:128], stmp[64:128], pv[64:128, :, 64:128], op=ALU.add), reads=["stmp", "ps4"], writes=["state"])
                sv[0] += 1
                sb_ = sbf[sv[0] % 4]
                P.op("act", lambda e_, sb_=sb_: e_.activation(out=sb_, in_=state, func=AF.Copy), reads=["state"], writes=["sbf%d" % (sv[0] % 4)])
            s0 = sbf[svA % 4]; s0k = "sbf%d" % (svA % 4)
            s1 = sbf[(svA + 1) % 4]; s1k = "sbf%d" % ((svA + 1) % 4)
            for h in range(8):
                j = h // 2; p0 = (h % 2) * 64
                oo = ps[3][:, h * 64:(h + 1) * 64]
                P.op("pe", lambda e_, h=h, oo=oo, tt=tt: e_.matmul(oo, lhsT=Pm[:, (h % 2) * 4 + h // 2, :], rhs=vt[:, tt, h * 64:(h + 1) * 64], start=True, stop=False), reads=["Pm", "vt"], writes=["ps3"])
                P.op("pe", lambda e_, j=j, p0=p0, oo=oo, tsl=tsl, s0=s0: e_.matmul(oo, lhsT=qhA[p0:p0 + 64, j, tsl], rhs=s0[p0:p0 + 64, j, :], start=False, stop=False), reads=["qhA", s0k], writes=["ps3"])
                P.op("pe", lambda e_, j=j, p0=p0, oo=oo, tsl=tsl, s1=s1: e_.matmul(oo, lhsT=qhB[p0:p0 + 64, j, tsl], rhs=s1[p0:p0 + 64, j, :], start=False, stop=True), reads=["qhB", s1k], writes=["ps3"])
            P.op("act", lambda e_: e_.activation(out=osb, in_=ps[3][:, :], func=AF.Copy), reads=["ps3"], writes=["osb"])
            P.op("dve", lambda e_: e_.tensor_tensor(osq, osb, osb, op=ALU.mult), reads=["osb"], writes=["osq"])
            P.op("dve", lambda e_: e_.tensor_reduce(out=oss[:, 0:8], in_=osq.rearrange("p (h v) -> p h v", h=8), axis=AX.X, op=ALU.add), reads=["osq"], writes=["oss"])
            P.op("dve", lambda e_: e_.tensor_scalar(oss[:, 0:8], oss[:, 0:8], 1.0 / 64, EPS, op0=ALU.mult, op1=ALU.add), reads=["oss"], writes=["oss"])
            P.op("act", lambda e_: e_.activation(out=oss[:, 0:8], in_=oss[:, 0:8], func=AF.Ln), reads=["oss"], writes=["oss"])
            P.op("act", lambda e_: e_.activation(out=oss[:, 0:8], in_=oss[:, 0:8], func=AF.Exp, scale=-0.5), reads=["oss"], writes=["oss"])
            P.op("dve", lambda e_: e_.tensor_tensor(osq.rearrange("p (h v) -> p h v", h=8), osb.rearrange("p (h v) -> p h v", h=8), oss[:, 0:8].unsqueeze(2).to_broadcast([128, 8, 64]), op=ALU.mult),
                 reads=["osb", "oss", "osq"], writes=["osq"])
            P.op("dve", lambda e_: e_.tensor_tensor(osq, osq, grb, op=ALU.mult), reads=["osq", "grb"], writes=["osq"])
            P.op("dve", lambda e_, tt=tt: e_.tensor_tensor(sg, sog[:, tt, :], ogt[:, tt, :], op=ALU.mult), reads=["sog", "ogt"], writes=["sg"])
            P.op("dve", lambda e_: e_.tensor_tensor(yb, osq, sg, op=ALU.mult), reads=["osq", "sg"], writes=["yb"])
            for j in range(4):
                P.op("pe", lambda e_, j=j: e_.transpose(tb[:, 512 + j * 128:512 + (j + 1) * 128], yb[:, j * 128:(j + 1) * 128], k.identb), reads=["yb", "identb"], writes=["ps7"])
            t = b * 4 + tt
            y = yT[t % 2]; yk = "yTD%d" % (t % 2)
            P.op("act", lambda e_, y=y: e_.activation(out=y, in_=tb[:, 512:1024], func=AF.Copy), reads=["ps7"], writes=[yk])
            P.dma("sp", lambda e_, y=y, t=t: e_.dma_start(out=yrT_d[:, :, t * 128:(t + 1) * 128].rearrange("j p q -> p j q"), in_=y.rearrange("p (j q) -> p j q", j=4)), reads=[yk], key=yk)
    P.barrier()
    A.release(m0)


def phaseE(k, l, I, xsrc, xdst):
    A, P, ps = k.A, k.P, k.ps
    yaT_d = k.dr["yaT_d"]; yrT_d = k.dr["yrT_d"]; gates_fm = k.dr["gates_fm"]
    m0 = A.mark()
    wa = A.alloc(4 * 1024, BF16).rearrange("p (k c) -> p k c", k=4)
    wr = A.alloc(4 * 1024, BF16).rearrange("p (k c) -> p k c", k=4)
    wo = A.alloc(8 * 1024, BF16).rearrange("p (k c) -> p k c", k=8)
    P.dma("pool", lambda e: e.dma_start(out=wa, in_=I["w_branch_a"][l].rearrange("(k p) c -> p k c", p=128)), writes=["wa"], key="wa")
    P.dma("pool", lambda e: e.dma_start(out=wr, in_=I["w_branch_r"][l].rearrange("(k p) c -> p k c", p=128)), writes=["wr"], key="wr")
    P.dma("pool", lambda e: e.dma_start(out=wo, in_=I["w_out"][l].rearrange("(k p) c -> p k c", p=128)), writes=["wo"], key="wo")
    ya = [A.alloc(4 * 512, BF16).rearrange("p (k t) -> p k t", k=4) for _ in range(2)]
    yr = [A.alloc(4 * 512, BF16).rearrange("p (k t) -> p k t", k=4) for _ in range(2)]
    gt = [A.alloc(16 * 512, BF16).rearrange("p (k t) -> p k t", k=16) for _ in range(2)]
    mT = A.alloc(8 * 512, BF16).rearrange("p (k t) -> p k t", k=8)
    m1 = [A.alloc(512, F32) for _ in range(2)]
    m2 = [A.alloc(512, F32) for _ in range(2)]
    xb = [A.alloc(1024, F32) for _ in range(2)]
    tb_ = [A.alloc(1024, F32) for _ in range(2)]
    cnt = 0
    for b in range(S // 512):
        sl = slice(b * 512, (b + 1) * 512)
        i2 = b % 2
        P.dma("sp", lambda e, i2=i2, sl=sl: e.dma_start(out=ya[i2], in_=yaT_d[:, :, sl].rearrange("j p t -> p j t")), writes=["yaE%d" % i2], key="yaE%d" % i2)
        P.dma("sp", lambda e, i2=i2, sl=sl: e.dma_start(out=yr[i2], in_=yrT_d[:, :, sl].rearrange("j p t -> p j t")), writes=["yrE%d" % i2], key="yrE%d" % i2)
        P.dma("sp", lambda e, i2=i2, sl=sl: e.dma_start(out=gt[i2], in_=gates_fm[:, sl].rearrange("(j p) t -> p j t", p=128)), writes=["gtE%d" % i2], key="gtE%d" % i2)
        for cb in range(8):
            ba = cnt % 2; bb = 2 + cnt % 2
            for kc in range(4):
                P.op("pe", lambda e, kc=kc, cb=cb, ba=ba, i2=i2: e.matmul(ps[ba][:, :], lhsT=wa[:, kc, cb * 128:(cb + 1) * 128], rhs=ya[i2][:, kc, :], start=(kc == 0), stop=(kc == 3)),
                     reads=["wa", "yaE%d" % i2], writes=["ps%d" % ba])
            for kc in range(4):
                P.op("pe", lambda e, kc=kc, cb=cb, bb=bb, i2=i2: e.matmul(ps[bb][:, :], lhsT=wr[:, kc, cb * 128:(cb + 1) * 128], rhs=yr[i2][:, kc, :], start=(kc == 0), stop=(kc == 3)),
                     reads=["wr", "yrE%d" % i2], writes=["ps%d" % bb])
            a1 = m1[cnt % 2]; a2 = m2[cnt % 2]
            P.op("dve", lambda e, a1=a1, ba=ba, cb=cb, i2=i2: e.tensor_tensor(a1, ps[ba][:, :], gt[i2][:, cb, :], op=ALU.mult), reads=["ps%d" % ba, "gtE%d" % i2], writes=["m1%d" % (cnt % 2)])
            P.op("dve", lambda e, a2=a2, bb=bb, cb=cb, i2=i2: e.tensor_tensor(a2, ps[bb][:, :], gt[i2][:, 8 + cb, :], op=ALU.mult), reads=["ps%d" % bb, "gtE%d" % i2], writes=["m2%d" % (cnt % 2)])
            P.op("dve", lambda e, a1=a1, a2=a2, cb=cb: e.tensor_tensor(mT[:, cb, :], a1, a2, op=ALU.add), reads=["m1%d" % (cnt % 2), "m2%d" % (cnt % 2)], writes=["mT"])
            cnt += 1
        for tt in range(4):
            t = b * 4 + tt
            x = xb[t % 2]; xk = "xbE%d" % (t % 2)
            tq = tb_[t % 2]; tk = "tbE%d" % (t % 2)
            P.dma("sp", lambda e, x=x, t=t: e.dma_start(out=x, in_=xsrc[t * 128:(t + 1) * 128, :]), writes=[xk], key=xk)
            for half in range(2):
                bank = 4 + (t * 2 + half) % 4
                for kc in range(8):
                    P.op("pe", lambda e, kc=kc, half=half, bank=bank, tt=tt: e.matmul(ps[bank][:, :], lhsT=mT[:, kc, tt * 128:(tt + 1) * 128], rhs=wo[:, kc, half * 512:(half + 1) * 512], start=(kc == 0), stop=(kc == 7)),
                         reads=["mT", "wo"], writes=["ps%d" % bank])
                P.op("dve", lambda e, tq=tq, half=half, bank=bank: e.tensor_tensor(tq[:, half * 512:(half + 1) * 512], ps[bank][:, :], k.gtbc[0][:, half * 512:(half + 1) * 512], op=ALU.mult),
                     reads=["ps%d" % bank, "gtbc0"], writes=[tk])
            P.op("dve", lambda e, tq=tq, x=x: e.tensor_tensor(tq, tq, x, op=ALU.add), reads=[tk, xk], writes=[tk])
            P.dma("sp", lambda e, tq=tq, t=t: e.dma_start(out=xdst[t * 128:(t + 1) * 128, :], in_=tq), reads=[tk], key=tk)
    P.barrier()
    A.release(m0)


def phaseF(k, l, I, xsrc, xdst):
    A, P, ps = k.A, k.P, k.ps
    m0 = A.mark()
    h2T = A.alloc(8 * S, BF16).rearrange("p (k t) -> p k t", k=8)
    gate = A.alloc(NT * 32, F32).rearrange("p (t c) -> p t c", t=NT)
    k.xb = [A.alloc(1024, F32) for _ in range(2)]
    m1_ = A.mark()
    hf = [A.alloc(8 * 128, F32).rearrange("p (k t) -> p k t", k=8) for _ in range(2)]
    wrt = A.alloc(8 * 36, F32).rearrange("p (k c) -> p k c", k=8)
    rb = A.alloc(36, F32)
    lg = A.alloc(NT * 36, F32).rearrange("p (t c) -> p t c", t=NT)
    k.xn = [A.alloc(1024, F32) for _ in range(2)]
    k.nst = [A.alloc(2, F32) for _ in range(2)]
    P.dma("sp", lambda e: e.dma_start(out=wrt[:, :, 0:4], in_=I["w_grp"][l].rearrange("(k p) c -> p k c", p=128)), writes=["wrt"], key="wrt")
    P.dma("sp", lambda e: e.dma_start(out=wrt[:, :, 4:36], in_=I["w_exp_router"][l].rearrange("(k p) c -> p k c", p=128)), writes=["wrt"], key="wrt")
    P.dma("sp", lambda e: e.dma_start(out=rb[:, 0:4], in_=I["b_grp"][l].partition_broadcast(128)), writes=["rbF"], key="rbF")
    P.dma("sp", lambda e: e.dma_start(out=rb[:, 4:36], in_=I["b_exp_router"][l].partition_broadcast(128)), writes=["rbF"], key="rbF")
    for t in range(NT):
        b0 = norm_transpose(k, xsrc, t, None, None, None, None, None)
        f = hf[t % 2]; fk = "hfF%d" % (t % 2)
        for kc in range(8):
            bank = b0 + kc // 4
            P.op("act", lambda e, kc=kc, bank=bank, f=f: e.activation(out=f[:, kc, :], in_=ps[bank][:, (kc % 4) * 128:(kc % 4 + 1) * 128], func=AF.Identity, scale=k.gs2[:, kc:kc + 1], bias=k.sh2[:, kc:kc + 1]),
                 reads=["ps%d" % bank, "gs", "modf"], writes=[fk])
        P.op("dve", lambda e, f=f, t=t: e.tensor_copy(h2T[:, :, t * 128:(t + 1) * 128], f), reads=[fk], writes=["h2T"])
        bank = 4 + t % 2
        for kc in range(8):
            P.op("pe", lambda e, kc=kc, f=f, bank=bank: e.matmul(ps[bank][:, 0:36], lhsT=f[:, kc, :], rhs=wrt[:, kc, :], start=(kc == 0), stop=(kc == 7)), reads=[fk, "wrt"], writes=["ps%d" % bank])
        P.op("dve", lambda e, t=t, bank=bank: e.tensor_tensor(lg[:, t, :], ps[bank][:, 0:36], rb, op=ALU.add), reads=["ps%d" % bank, "rbF"], writes=["lg"])
    g4 = A.alloc(NT * 4, F32).rearrange("p (t c) -> p t c", t=NT)
    oh = A.alloc(NT * 4, F32).rearrange("p (t c) -> p t c", t=NT)
    s1 = A.alloc(NT * 8, F32)
    le = A.alloc(NT * 32, F32).rearrange("p (t c) -> p t c", t=NT)
    o1 = A.alloc(NT * 32, F32).rearrange("p (t c) -> p t c", t=NT)
    o2 = A.alloc(NT * 32, F32).rearrange("p (t c) -> p t c", t=NT)
    mx = s1[:, 0:NT]; gs_ = s1[:, NT:2 * NT]; mA = s1[:, 2 * NT:3 * NT]; mB = s1[:, 3 * NT:4 * NT]; w1 = s1[:, 4 * NT:5 * NT]; w2 = s1[:, 5 * NT:6 * NT]
    R = ["lg", "g4", "oh", "s1", "le", "o1", "o2", "gate"]
    def D(fn):
        P.op("dve", fn, reads=R, writes=R)
    bc4 = lambda v: v.unsqueeze(2).to_broadcast([128, NT, 4])
    bc32 = lambda v: v.unsqueeze(2).to_broadcast([128, NT, 32])
    D(lambda e: e.tensor_reduce(out=mx, in_=lg[:, :, 0:4], axis=AX.X, op=ALU.max))
    D(lambda e: e.tensor_tensor(oh, lg[:, :, 0:4], bc4(mx), op=ALU.is_ge))
    D(lambda e: e.tensor_tensor(g4, lg[:, :, 0:4], bc4(mx), op=ALU.subtract))
    P.op("act", lambda e: e.activation(out=g4, in_=g4, func=AF.Exp), reads=R, writes=R)
    D(lambda e: e.tensor_reduce(out=gs_, in_=g4, axis=AX.X, op=ALU.add))
    D(lambda e: e.reciprocal(gs_, gs_))
    lev = le.rearrange("p t (g x) -> p t g x", g=4)
    D(lambda e: e.tensor_tensor(lev, lg[:, :, 4:36].rearrange("p t (g x) -> p t g x", g=4), oh.unsqueeze(3).to_broadcast([128, NT, 4, 8]), op=ALU.mult))
    D(lambda e: e.tensor_scalar(o1.rearrange("p t (g x) -> p t g x", g=4), oh.unsqueeze(3).to_broadcast([128, NT, 4, 8]), -1.0, 1e30, op0=ALU.add, op1=ALU.mult))
    D(lambda e: e.tensor_tensor(le, le, o1, op=ALU.add))
    D(lambda e: e.tensor_reduce(out=mA, in_=le, axis=AX.X, op=ALU.max))
    D(lambda e: e.tensor_tensor(o1, le, bc32(mA), op=ALU.is_ge))
    D(lambda e: e.scalar_tensor_tensor(out=le, in0=o1, scalar=-1e30, in1=le, op0=ALU.mult, op1=ALU.add))
    D(lambda e: e.tensor_reduce(out=mB, in_=le, axis=AX.X, op=ALU.max))
    D(lambda e: e.tensor_tensor(o2, le, bc32(mB), op=ALU.is_ge))
    D(lambda e: e.tensor_tensor(w1, mB, mA, op=ALU.subtract))
    P.op("act", lambda e: e.activation(out=w1, in_=w1, func=AF.Exp), reads=R, writes=R)
    D(lambda e: e.tensor_scalar(w1, w1, 1.0, None, op0=ALU.add))
    D(lambda e: e.reciprocal(w1, w1))
    D(lambda e: e.tensor_scalar(w2, w1, -1.0, 1.0, op0=ALU.mult, op1=ALU.add))
    D(lambda e: e.tensor_tensor(w1, w1, gs_, op=ALU.mult))
    D(lambda e: e.tensor_tensor(w2, w2, gs_, op=ALU.mult))
    D(lambda e: e.tensor_tensor(o1, o1, bc32(w1), op=ALU.mult))
    D(lambda e: e.tensor_tensor(o2, o2, bc32(w2), op=ALU.mult))
    D(lambda e: e.tensor_tensor(gate, o1, o2, op=ALU.add))
    P.barrier()
    A.release(m1_)
    TB = 1024
    acc = A.alloc(8 * 1024, F32).rearrange("p (t c) -> p t c", t=8)
    wg = [A.alloc(8 * 512, BF16).rearrange("p (k c) -> p k c", k=8) for _ in range(2)]
    wu = [A.alloc(8 * 512, BF16).rearrange("p (k c) -> p k c", k=8) for _ in range(2)]
    wd = [A.alloc(4 * 1024, BF16).rearrange("p (k c) -> p k c", k=4) for _ in range(2)]
    hid = [A.alloc(4 * 512, BF16).rearrange("p (k t) -> p k t", k=4) for _ in range(2)]
    sg = [A.alloc(512, F32) for _ in range(2)]
    ec = 0; hc_ = 0; sc_ = 0; yc = 0
    for tb in range(S // TB):
        P.op("pool", lambda e: e.memset(acc, 0.0), writes=["acc"])
        for ex in range(32):
            i2 = ec % 2
            P.dma("pool", lambda e, i2=i2, ex=ex: e.dma_start(out=wg[i2], in_=I["w_gate"][l, ex].rearrange("(k p) c -> p k c", p=128)), writes=["wg%d" % i2], key="wg%d" % i2)
            P.dma("pool", lambda e, i2=i2, ex=ex: e.dma_start(out=wu[i2], in_=I["w_up"][l, ex].rearrange("(k p) c -> p k c", p=128)), writes=["wu%d" % i2], key="wu%d" % i2)
            P.dma("pool", lambda e, i2=i2, ex=ex: e.dma_start(out=wd[i2], in_=I["w_down"][l, ex].rearrange("(k p) c -> p k c", p=128)), writes=["wd%d" % i2], key="wd%d" % i2)
            for hb in range(TB // 512):
                tok0 = tb * TB + hb * 512
                hd = hid[hc_ % 2]; hk = "hid%d" % (hc_ % 2)
                for cb in range(4):
                    bg = (sc_ % 2) * 2; bu = bg + 1
                    for kc in range(8):
                        P.op("pe", lambda e, kc=kc, cb=cb, bg=bg, i2=i2, tok0=tok0: e.matmul(ps[bg][:, :], lhsT=wg[i2][:, kc, cb * 128:(cb + 1) * 128], rhs=h2T[:, kc, tok0:tok0 + 512], start=(kc == 0), stop=(kc == 7)),
                             reads=["wg%d" % i2, "h2T"], writes=["ps%d" % bg])
                    for kc in range(8):
                        P.op("pe", lambda e, kc=kc, cb=cb, bu=bu, i2=i2, tok0=tok0: e.matmul(ps[bu][:, :], lhsT=wu[i2][:, kc, cb * 128:(cb + 1) * 128], rhs=h2T[:, kc, tok0:tok0 + 512], start=(kc == 0), stop=(kc == 7)),
                             reads=["wu%d" % i2, "h2T"], writes=["ps%d" % bu])
                    s = sg[sc_ % 2]; sk = "sgF%d" % (sc_ % 2)
                    P.op("act", lambda e, s=s, bg=bg: e.activation(out=s, in_=ps[bg][:, :], func=AF.Exp, scale=-1.0), reads=["ps%d" % bg], writes=[sk])
                    P.op("pool", lambda e, s=s: e.tensor_scalar(s, s, 1.0, None, op0=ALU.add), reads=[sk], writes=[sk])
                    P.op("dve", lambda e, s=s: e.reciprocal(s, s), reads=[sk], writes=[sk])
                    P.op("dve", lambda e, s=s, bg=bg: e.tensor_tensor(s, s, ps[bg][:, :], op=ALU.mult), reads=[sk, "ps%d" % bg], writes=[sk])
                    P.op("dve", lambda e, s=s, bu=bu, hd=hd, cb=cb: e.tensor_tensor(hd[:, cb, :], s, ps[bu][:, :], op=ALU.mult), reads=[sk, "ps%d" % bu], writes=[hk])
                    sc_ += 1
                for tt in range(4):
                    tl = hb * 4 + tt
                    tg = tb * 8 + tl
                    for half in range(2):
                        bank = 4 + yc % 4
                        for kc in range(4):
                            P.op("pe", lambda e, kc=kc, half=half, bank=bank, tt=tt, hd=hd, i2=i2: e.matmul(ps[bank][:, :], lhsT=hd[:, kc, tt * 128:(tt + 1) * 128], rhs=wd[i2][:, kc, half * 512:(half + 1) * 512], start=(kc == 0), stop=(kc == 3)),
                                 reads=[hk, "wd%d" % i2], writes=["ps%d" % bank])
                        P.op("dve", lambda e, bank=bank, tl=tl, tg=tg, half=half, ex=ex: e.scalar_tensor_tensor(out=acc[:, tl, half * 512:(half + 1) * 512], in0=ps[bank][:, :], scalar=gate[:, tg, ex:ex + 1], in1=acc[:, tl, half * 512:(half + 1) * 512], op0=ALU.mult, op1=ALU.add),
                             reads=["ps%d" % bank, "gate", "acc"], writes=["acc"])
                        yc += 1
                hc_ += 1
            ec += 1
        for tl in range(8):
            tg = tb * 8 + tl
            x = k.xb[tg % 2]; xk = "xb%d" % (tg % 2)
            P.dma("sp", lambda e, x=x, tg=tg: e.dma_start(out=x, in_=xsrc[tg * 128:(tg + 1) * 128, :]), writes=[xk], key=xk)
            P.op("pool", lambda e, tl=tl: e.tensor_tensor(acc[:, tl, :], acc[:, tl, :], k.gtbc[1], op=ALU.mult), reads=["acc", "gtbc1"], writes=["acc"])
            P.op("pool", lambda e, tl=tl, x=x: e.tensor_tensor(x, x, acc[:, tl, :], op=ALU.add), reads=["acc", xk], writes=[xk])
            P.dma("sp", lambda e, x=x, tg=tg: e.dma_start(out=xdst[tg * 128:(tg + 1) * 128, :], in_=x), reads=[xk], key=xk)
    P.barrier()
    A.release(m0)


def phaseG(k, I, xsrc, out):
    A, P, ps = k.A, k.P, k.ps
    m0 = A.mark()
    gb = A.alloc(1024, F32)
    P.dma("sp", lambda e: e.dma_start(out=gb, in_=I["g_final"].partition_broadcast(128)), writes=["gbG"], key="gbG")
    xb = [A.alloc(1024, F32) for _ in range(2)]
    xn = [A.alloc(1024, F32) for _ in range(2)]
    st = [A.alloc(2, F32) for _ in range(2)]
    for t in range(NT):
        x = xb[t % 2]; xk = "xbG%d" % (t % 2); n = xn[t % 2]; nk = "xnG%d" % (t % 2); s = st[t % 2]; sk = "stG%d" % (t % 2)
        P.dma("sp", lambda e, x=x, t=t: e.dma_start(out=x, in_=xsrc[t * 128:(t + 1) * 128, :]), writes=[xk], key=xk)
        P.op("act", lambda e, x=x, n=n, s=s: e.activation(out=n, in_=x, func=AF.Square, accum_out=s[:, 0:1]), reads=[xk], writes=[nk, sk])
        P.op("dve", lambda e, s=s: e.tensor_scalar(s[:, 1:2], s[:, 0:1], 1.0 / D, EPS, op0=ALU.mult, op1=ALU.add), reads=[sk], writes=[sk])
        P.op("act", lambda e, s=s: e.activation(out=s[:, 1:2], in_=s[:, 1:2], func=AF.Ln), reads=[sk], writes=[sk])
        P.op("act", lambda e, s=s: e.activation(out=s[:, 1:2], in_=s[:, 1:2], func=AF.Exp, scale=-0.5), reads=[sk], writes=[sk])
        P.op("dve", lambda e, x=x, n=n, s=s: e.scalar_tensor_tensor(out=n, in0=x, scalar=s[:, 1:2], in1=gb, op0=ALU.mult, op1=ALU.mult), reads=[xk, sk, "gbG"], writes=[nk])
        P.dma("sp", lambda e, n=n, t=t: e.dma_start(out=out[t * 128:(t + 1) * 128, :], in_=n), reads=[nk], key=nk)
    P.barrier()
    A.release(m0)


CAP = 768
NSL = 32 * CAP


def phaseF2(k, l, I, xsrc, xdst):
    A, P, ps = k.A, k.P, k.ps
    Xbuf = dram(k, "Xbuf", [NSL + 128, D], BF16)
    Ybuf = dram(k, "Ybuf", [NSL + 128, D], F32)
    m0 = A.mark()
    idx1 = A.alloc(NT, I32); idx2 = A.alloc(NT, I32)
    g12 = A.alloc(2 * NT, F32)
    g1 = g12[:, 0:NT]; g2 = g12[:, NT:2 * NT]
    k.xb = [A.alloc(1024, F32) for _ in range(2)]
    mH = A.mark()
    h2tm = A.alloc(NT * 1024, BF16).rearrange("p (t c) -> p t c", t=NT)
    m1_ = A.mark()
    hf = [A.alloc(8 * 128, F32).rearrange("p (k t) -> p k t", k=8) for _ in range(2)]
    wrt = A.alloc(8 * 36, F32).rearrange("p (k c) -> p k c", k=8)
    rb = A.alloc(36, F32)
    lg = A.alloc(NT * 36, F32).rearrange("p (t c) -> p t c", t=NT)
    k.xn = [A.alloc(1024, F32) for _ in range(2)]
    k.nst = [A.alloc(2, F32) for _ in range(2)]
    P.dma("sp", lambda e: e.dma_start(out=wrt[:, :, 0:4], in_=I["w_grp"][l].rearrange("(k p) c -> p k c", p=128)), writes=["wrt"], key="wrt")
    P.dma("sp", lambda e: e.dma_start(out=wrt[:, :, 4:36], in_=I["w_exp_router"][l].rearrange("(k p) c -> p k c", p=128)), writes=["wrt"], key="wrt")
    P.dma("sp", lambda e: e.dma_start(out=rb[:, 0:4], in_=I["b_grp"][l].partition_broadcast(128)), writes=["rbF"], key="rbF")
    P.dma("sp", lambda e: e.dma_start(out=rb[:, 4:36], in_=I["b_exp_router"][l].partition_broadcast(128)), writes=["rbF"], key="rbF")
    for t in range(NT):
        b0 = norm_transpose(k, xsrc, t, None, None, None, None, None)
        f = hf[t % 2]; fk = "hfF%d" % (t % 2)
        for kc in range(8):
            bank = b0 + kc // 4
            P.op("act", lambda e, kc=kc, bank=bank, f=f: e.activation(out=f[:, kc, :], in_=ps[bank][:, (kc % 4) * 128:(kc % 4 + 1) * 128], func=AF.Identity, scale=k.gs2[:, kc:kc + 1], bias=k.sh2[:, kc:kc + 1]),
                 reads=["ps%d" % bank, "gs", "modf"], writes=[fk])
        bank = 4 + t % 2
        for kc in range(8):
            P.op("pe", lambda e, kc=kc, f=f, bank=bank: e.matmul(ps[bank][:, 0:36], lhsT=f[:, kc, :], rhs=wrt[:, kc, :], start=(kc == 0), stop=(kc == 7)), reads=[fk, "wrt"], writes=["ps%d" % bank])
        P.op("dve", lambda e, t=t, bank=bank: e.tensor_tensor(lg[:, t, :], ps[bank][:, 0:36], rb, op=ALU.add), reads=["ps%d" % bank, "rbF"], writes=["lg"])
        for kc in range(8):
            bank = 6 + kc // 4
            P.op("pe", lambda e, kc=kc, f=f, bank=bank: e.transpose(ps[bank][:, (kc % 4) * 128:(kc % 4 + 1) * 128], f[:, kc, :], k.ident), reads=[fk, "ident"], writes=["ps%d" % bank])
        P.op("dve", lambda e, t=t: e.tensor_copy(h2tm[:, t, 0:512], ps[6][:, :]), reads=["ps6"], writes=["h2tm"])
        P.op("pool" if False else "dve", lambda e, t=t: e.tensor_copy(h2tm[:, t, 512:1024], ps[7][:, :]), reads=["ps7"], writes=["h2tm"])
    def T32():
        return A.alloc(NT * 32, F32).rearrange("p (t c) -> p t c", t=NT)
    g4 = A.alloc(NT * 4, F32).rearrange("p (t c) -> p t c", t=NT)
    oh = A.alloc(NT * 4, F32).rearrange("p (t c) -> p t c", t=NT)
    s1 = A.alloc(NT * 8, F32)
    le = T32(); o1 = T32(); o2 = T32(); pos = T32(); tmp = T32(); offs = T32()
    indb = A.alloc(NT * 32, BF16)
    SU = A.alloc(128, BF16)
    suf = A.alloc(128, F32)
    mx = s1[:, 0:NT]; gs_ = s1[:, NT:2 * NT]; mA = s1[:, 2 * NT:3 * NT]; mB = s1[:, 3 * NT:4 * NT]; w1 = s1[:, 4 * NT:5 * NT]; w2 = s1[:, 5 * NT:6 * NT]
    v1 = s1[:, 6 * NT:7 * NT]; v2 = s1[:, 7 * NT:8 * NT]
    R = ["lg", "g4", "oh", "s1", "le", "o1", "o2", "pos", "tmp", "offs", "g12", "idx"]
    def Dv(fn):
        P.op("dve", fn, reads=R, writes=R)
    bc4 = lambda v: v.unsqueeze(2).to_broadcast([128, NT, 4])
    bc32 = lambda v: v.unsqueeze(2).to_broadcast([128, NT, 32])
    P.op("pool", lambda e: e.memset(suf, 1.0), writes=["suf"])
    P.op("pool", lambda e: e.tensor_tensor(suf, k.cneg_u, k.cneg_u, op=ALU.mult), reads=["cneg_u"], writes=["suf"])
    P.op("dve", lambda e: e.tensor_copy(SU, suf), reads=["suf"], writes=["SU"])
    Dv(lambda e: e.tensor_reduce(out=mx, in_=lg[:, :, 0:4], axis=AX.X, op=ALU.max))
    Dv(lambda e: e.tensor_tensor(oh, lg[:, :, 0:4], bc4(mx), op=ALU.is_ge))
    Dv(lambda e: e.tensor_tensor(g4, lg[:, :, 0:4], bc4(mx), op=ALU.subtract))
    P.op("act", lambda e: e.activation(out=g4, in_=g4, func=AF.Exp), reads=R, writes=R)
    Dv(lambda e: e.tensor_reduce(out=gs_, in_=g4, axis=AX.X, op=ALU.add))
    Dv(lambda e: e.reciprocal(gs_, gs_))
    lev = le.rearrange("p t (g x) -> p t g x", g=4)
    Dv(lambda e: e.tensor_tensor(lev, lg[:, :, 4:36].rearrange("p t (g x) -> p t g x", g=4), oh.unsqueeze(3).to_broadcast([128, NT, 4, 8]), op=ALU.mult))
    Dv(lambda e: e.tensor_scalar(o1.rearrange("p t (g x) -> p t g x", g=4), oh.unsqueeze(3).to_broadcast([128, NT, 4, 8]), -1.0, 1e30, op0=ALU.add, op1=ALU.mult))
    Dv(lambda e: e.tensor_tensor(le, le, o1, op=ALU.add))
    Dv(lambda e: e.tensor_reduce(out=mA, in_=le, axis=AX.X, op=ALU.max))
    Dv(lambda e: e.tensor_tensor(o1, le, bc32(mA), op=ALU.is_ge))
    Dv(lambda e: e.scalar_tensor_tensor(out=le, in0=o1, scalar=-1e30, in1=le, op0=ALU.mult, op1=ALU.add))
    Dv(lambda e: e.tensor_reduce(out=mB, in_=le, axis=AX.X, op=ALU.max))
    Dv(lambda e: e.tensor_tensor(o2, le, bc32(mB), op=ALU.is_ge))
    Dv(lambda e: e.tensor_tensor(w1, mB, mA, op=ALU.subtract))
    P.op("act", lambda e: e.activation(out=w1, in_=w1, func=AF.Exp), reads=R, writes=R)
    Dv(lambda e: e.tensor_scalar(w1, w1, 1.0, None, op0=ALU.add))
    Dv(lambda e: e.reciprocal(w1, w1))
    Dv(lambda e: e.tensor_scalar(w2, w1, -1.0, 1.0, op0=ALU.mult, op1=ALU.add))
    Dv(lambda e: e.tensor_tensor(w1, w1, gs_, op=ALU.mult))
    Dv(lambda e: e.tensor_tensor(w2, w2, gs_, op=ALU.mult))
    Dv(lambda e: e.tensor_tensor(tmp, o1, o2, op=ALU.add))
    P.op("dve", lambda e: e.tensor_copy(indb, tmp.rearrange("p t c -> p (t c)")), reads=R, writes=["indb"])
    for c in range(2):
        P.op("pe", lambda e, c=c: e.matmul(ps[c][:, :], lhsT=SU, rhs=indb[:, c * 512:(c + 1) * 512], start=True, stop=True), reads=["SU", "indb"], writes=["ps%d" % c])
        P.op("pe", lambda e, c=c: e.matmul(ps[2 + c][:, :], lhsT=k.ones_b, rhs=indb[:, c * 512:(c + 1) * 512], start=True, stop=True), reads=["ones_b", "indb"], writes=["ps%d" % (2 + c)])
    for c in range(2):
        P.op("dve", lambda e, c=c: e.tensor_copy(pos.rearrange("p t c -> p (t c)")[:, c * 512:(c + 1) * 512], ps[c][:, :]), reads=["ps%d" % c] + R, writes=R)
        P.op("dve", lambda e, c=c: e.tensor_copy(tmp.rearrange("p t c -> p (t c)")[:, c * 512:(c + 1) * 512], ps[2 + c][:, :]), reads=["ps%d" % (2 + c)] + R, writes=R)
    Dv(lambda e: e.memset(offs[:, 0, :], 0.0))
    for t in range(1, NT):
        Dv(lambda e, t=t: e.tensor_tensor(offs[:, t, :], offs[:, t - 1, :], tmp[:, t - 1, :], op=ALU.add))
    Dv(lambda e: e.tensor_tensor(pos, pos, offs, op=ALU.add))
    Dv(lambda e: e.tensor_scalar(tmp, pos, float(CAP), None, op0=ALU.is_lt))
    Dv(lambda e: e.tensor_tensor(pos, pos, k.ecap.unsqueeze(1).to_broadcast([128, NT, 32]), op=ALU.add))
    Dv(lambda e: e.tensor_tensor(pos, pos, tmp, op=ALU.mult))
    Dv(lambda e: e.tensor_scalar(offs, tmp, -1.0, k.trash[:, 0:1], op0=ALU.add, op1=ALU.mult))
    Dv(lambda e: e.tensor_tensor(pos, pos, offs, op=ALU.add))
    Dv(lambda e: e.tensor_tensor(offs, o1, pos, op=ALU.mult))
    Dv(lambda e: e.tensor_reduce(out=v1, in_=offs, axis=AX.X, op=ALU.add))
    P.op("dve", lambda e: e.tensor_copy(idx1, v1), reads=R, writes=R)
    Dv(lambda e: e.tensor_tensor(offs, o2, pos, op=ALU.mult))
    Dv(lambda e: e.tensor_reduce(out=v2, in_=offs, axis=AX.X, op=ALU.add))
    P.op("dve", lambda e: e.tensor_copy(idx2, v2), reads=R, writes=R)
    Dv(lambda e: e.tensor_tensor(offs, o1, tmp, op=ALU.mult))
    Dv(lambda e: e.tensor_reduce(out=v1, in_=offs, axis=AX.X, op=ALU.add))
    Dv(lambda e: e.tensor_tensor(g1, w1, v1, op=ALU.mult))
    Dv(lambda e: e.tensor_tensor(offs, o2, tmp, op=ALU.mult))
    Dv(lambda e: e.tensor_reduce(out=v2, in_=offs, axis=AX.X, op=ALU.add))
    Dv(lambda e: e.tensor_tensor(g2, w2, v2, op=ALU.mult))
    P.op("pool", lambda e: e.memset(k.xb[0], 0.0), writes=["xb0"])
    P.dma("sp", lambda e: e.dma_start(out=Ybuf[NSL:NSL + 128, :], in_=k.xb[0]), reads=["xb0"], key="xb0")
    if l == 0:
        zb = k.xb[0].bitcast(BF16)[:, 0:1024]
        nrt = (NSL + 128) // 128
        Xv = Xbuf.rearrange("(n p) c -> p n c", p=128)
        for n0 in range(0, nrt, 16):
            nn = min(16, nrt - n0)
            P.dma("sp", lambda e, n0=n0, nn=nn: e.dma_start(out=Xv[:, n0:n0 + nn, :], in_=zb.unsqueeze(1).to_broadcast([128, nn, 1024])), reads=["xb0"], writes=["XbufZ"], key="xbz")
    if "dbg_idx" in k.debug:
        dbi = dram(k, "dbg_idx", [128, 2 * NT], I32)
        P.dma("sp", lambda e: e.dma_start(out=dbi[:, 0:NT], in_=idx1), reads=R, key="dbgi")
        P.dma("sp", lambda e: e.dma_start(out=dbi[:, NT:2 * NT], in_=idx2), reads=R, key="dbgi")
        dbg_ = dram(k, "dbg_g", [128, 2 * NT], F32)
        P.dma("sp", lambda e: e.dma_start(out=dbg_, in_=g12), reads=R, key="dbgg")
    for t in range(NT):
        for (ix, nm) in ((idx1, "a"), (idx2, "b")):
            P.dma("pool", lambda e, t=t, ix=ix: e.indirect_dma_start(out=Xbuf, out_offset=bass.IndirectOffsetOnAxis(ap=ix[:, t:t + 1], axis=0), in_=h2tm[:, t, :], in_offset=None),
                  reads=["h2tm", "XbufZ"] + R, writes=[], key="sc%s%d" % (nm, t % 4))
    P.barrier()
    A.release(mH)
    wg = [A.alloc(8 * 512, BF16).rearrange("p (k c) -> p k c", k=8) for _ in range(2)]
    wu = [A.alloc(8 * 512, BF16).rearrange("p (k c) -> p k c", k=8) for _ in range(2)]
    wd = [A.alloc(4 * 1024, BF16).rearrange("p (k c) -> p k c", k=4) for _ in range(2)]
    XT = [A.alloc(8 * CAP, BF16).rearrange("p (k s) -> p k s", k=8) for _ in range(2)]
    xr = [A.alloc(1024, BF16) for _ in range(3)]
    hid = [A.alloc(4 * CAP, BF16).rearrange("p (k s) -> p k s", k=4) for _ in range(2)]
    sg = [A.alloc(512, F32) for _ in range(2)]
    yrow = [A.alloc(1024, F32) for _ in range(3)]
    NST = CAP // 128
    if "dbg_X0" in k.debug:
        dx = dram(k, "dbg_X0", [128, 1024], BF16)
        P.dma("sp", lambda e: e.dma_start(out=xr[2], in_=Xbuf[0:128, :]), writes=["xr2"], key="xr2")
        P.dma("sp", lambda e: e.dma_start(out=dx, in_=xr[2]), reads=["xr2"], key="dbgx0")
    chunks = [(0, 512), (512, CAP - 512)] if CAP > 512 else [(0, CAP)]
    cnts = {"xc": 0, "cc": 0, "yc": 0, "scn": 0}
    NEXP = 32

    def Wload(ex):
        i2 = ex % 2
        P.dma("pool", lambda e: e.dma_start(out=wg[i2], in_=I["w_gate"][l, ex].rearrange("(k p) c -> p k c", p=128)), writes=["wg%d" % i2], key="wg%d" % i2)
        P.dma("pool", lambda e: e.dma_start(out=wu[i2], in_=I["w_up"][l, ex].rearrange("(k p) c -> p k c", p=128)), writes=["wu%d" % i2], key="wu%d" % i2)
        P.dma("pool", lambda e: e.dma_start(out=wd[i2], in_=I["w_down"][l, ex].rearrange("(k p) c -> p k c", p=128)), writes=["wd%d" % i2], key="wd%d" % i2)

    def Tphase(ex):
        i2 = ex % 2
        xt = XT[i2]; xtk = "XT%d" % i2
        for st in range(NST):
            xc = cnts["xc"]
            r = xr[xc % 3]; rk = "xr%d" % (xc % 3)
            P.dma("sp", lambda e, r=r, st=st: e.dma_start(out=r, in_=Xbuf[ex * CAP + st * 128:ex * CAP + (st + 1) * 128, :]), writes=[rk], key=rk)
            bank = 6 + xc % 2
            tb = ps[bank][:, :].bitcast(BF16)
            for kc in range(8):
                P.op("pe", lambda e, kc=kc, r=r, tb=tb: e.transpose(tb[:, kc * 128:(kc + 1) * 128], r[:, kc * 128:(kc + 1) * 128], k.identb), reads=[rk, "identb"], writes=["ps%d" % bank])
            if xc % 2 == 0:
                P.op("act", lambda e, tb=tb, st=st: e.activation(out=xt[:, :, st * 128:(st + 1) * 128], in_=tb.rearrange("p (k s) -> p k s", k=8), func=AF.Copy), reads=["ps%d" % bank], writes=[xtk])
            else:
                P.op("dve", lambda e, tb=tb, st=st: e.tensor_copy(xt[:, :, st * 128:(st + 1) * 128], tb.rearrange("p (k s) -> p k s", k=8)), reads=["ps%d" % bank], writes=[xtk])
            cnts["xc"] += 1

    def Hphase(ex):
        i2 = ex % 2
        xt = XT[i2]; xtk = "XT%d" % i2
        hd = hid[i2]; hk = "hid%d" % i2
        for hb in range(4):
            for (c0, cn) in chunks:
                cc = cnts["cc"]; scn = cnts["scn"]
                bg = (cc % 2) * 2; bu = bg + 1
                for kc in range(8):
                    P.op("pe", lambda e, kc=kc, hb=hb, bg=bg, c0=c0, cn=cn: e.matmul(ps[bg][:, 0:cn], lhsT=wg[i2][:, kc, hb * 128:(hb + 1) * 128], rhs=xt[:, kc, c0:c0 + cn], start=(kc == 0), stop=(kc == 7)),
                         reads=["wg%d" % i2, xtk], writes=["ps%d" % bg])
                for kc in range(8):
                    P.op("pe", lambda e, kc=kc, hb=hb, bu=bu, c0=c0, cn=cn: e.matmul(ps[bu][:, 0:cn], lhsT=wu[i2][:, kc, hb * 128:(hb + 1) * 128], rhs=xt[:, kc, c0:c0 + cn], start=(kc == 0), stop=(kc == 7)),
                         reads=["wu%d" % i2, xtk], writes=["ps%d" % bu])
                s_ = sg[scn % 2]; sk = "sgF%d" % (scn % 2)
                P.op("act", lambda e, s_=s_, bg=bg, cn=cn: e.activation(out=s_[:, 0:cn], in_=ps[bg][:, 0:cn], func=AF.Silu), reads=["ps%d" % bg], writes=[sk])
                P.op("dve", lambda e, s_=s_, bu=bu, hb=hb, c0=c0, cn=cn: e.tensor_tensor(hd[:, hb, c0:c0 + cn], s_[:, 0:cn], ps[bu][:, 0:cn], op=ALU.mult), reads=[sk, "ps%d" % bu], writes=[hk])
                cnts["scn"] += 1; cnts["cc"] += 1

    def Dphase(ex):
        i2 = ex % 2
        hd = hid[i2]; hk = "hid%d" % i2
        for st in range(NST):
            yc = cnts["yc"]
            y = yrow[yc % 3]; yk = "yrow%d" % (yc % 3)
            for half in range(2):
                bank = 4 + half
                for kc in range(4):
                    P.op("pe", lambda e, kc=kc, half=half, bank=bank, st=st: e.matmul(ps[bank][:, :], lhsT=hd[:, kc, st * 128:(st + 1) * 128], rhs=wd[i2][:, kc, half * 512:(half + 1) * 512], start=(kc == 0), stop=(kc == 3)),
                         reads=[hk, "wd%d" % i2], writes=["ps%d" % bank])
                if half == 0:
                    P.op("act", lambda e, y=y, bank=bank: e.activation(out=y[:, 0:512], in_=ps[bank][:, :], func=AF.Copy), reads=["ps%d" % bank], writes=[yk])
                else:
                    P.op("dve", lambda e, y=y, bank=bank: e.tensor_copy(y[:, 512:1024], ps[bank][:, :]), reads=["ps%d" % bank], writes=[yk])
            P.dma("sp", lambda e, y=y, st=st: e.dma_start(out=Ybuf[ex * CAP + st * 128:ex * CAP + (st + 1) * 128, :], in_=y), reads=[yk], key=yk)
            cnts["yc"] += 1

    Wload(0)
    Tphase(0)
    for ex in range(NEXP):
        if ex + 1 < NEXP:
            Wload(ex + 1)
        Hphase(ex)
        if ex + 1 < NEXP:
            Tphase(ex + 1)
        Dphase(ex)
    P.barrier()
    A.release(mH)
    Y1 = [A.alloc(1024, F32) for _ in range(2)]
    Y2 = [A.alloc(1024, F32) for _ in range(2)]
    for t in range(NT):
        b = t % 2
        x = k.xb[b]; xk = "xb%d" % b
        P.dma("sp", lambda e, x=x, t=t: e.dma_start(out=x, in_=xsrc[t * 128:(t + 1) * 128, :]), writes=[xk], key=xk)
        P.dma("pool", lambda e, t=t, b=b: e.indirect_dma_start(out=Y1[b], out_offset=None, in_=Ybuf, in_offset=bass.IndirectOffsetOnAxis(ap=idx1[:, t:t + 1], axis=0)), reads=R, writes=["Y1%d" % b], key="Y1%d" % b)
        P.dma("pool", lambda e, t=t, b=b: e.indirect_dma_start(out=Y2[b], out_offset=None, in_=Ybuf, in_offset=bass.IndirectOffsetOnAxis(ap=idx2[:, t:t + 1], axis=0)), reads=R, writes=["Y2%d" % b], key="Y2%d" % b)
        P.op("act", lambda e, t=t, b=b: e.activation(out=Y1[b], in_=Y1[b], func=AF.Copy, scale=g1[:, t:t + 1]), reads=["Y1%d" % b] + R, writes=["Y1%d" % b])
        P.op("dve", lambda e, t=t, b=b: e.scalar_tensor_tensor(out=Y1[b], in0=Y2[b], scalar=g2[:, t:t + 1], in1=Y1[b], op0=ALU.mult, op1=ALU.add), reads=["Y1%d" % b, "Y2%d" % b] + R, writes=["Y1%d" % b])
        P.op("dve", lambda e, b=b: e.tensor_tensor(Y1[b], Y1[b], k.gtbc[1], op=ALU.mult), reads=["Y1%d" % b, "gtbc1"], writes=["Y1%d" % b])
        P.op("dve", lambda e, x=x, b=b: e.tensor_tensor(x, x, Y1[b], op=ALU.add), reads=["Y1%d" % b, xk], writes=[xk])
        P.dma("sp", lambda e, x=x, t=t: e.dma_start(out=xdst[t * 128:(t + 1) * 128, :], in_=x), reads=[xk], key=xk)
    P.barrier()
    A.release(m0)


from concourse.bass_utils import run_bass_kernel_spmd

_WNAMES = ["w_mod", "b_mod", "g_norm1", "g_norm2", "w_in", "g_cq", "g_ckv", "g_kidx", "w_q_up", "w_idx_q", "w_v_up",
           "lb_logits", "g_rec", "w_branch_a", "w_branch_r", "w_out", "w_grp", "b_grp", "w_exp_router", "b_exp_router",
           "w_gate", "w_up", "w_down", "g_final"]


def _build(shapes, depth=4):
    nc = bass.Bass("TRN2", target_bir_lowering=False)
    es = ExitStack()
    with es:
        I = {}
        I["x"] = nc.dram_tensor("x", [S, D], F32, kind="ExternalInput").ap()
        I["c"] = nc.dram_tensor("c", [1, D], F32, kind="ExternalInput").ap()
        for n in _WNAMES:
            I[n] = nc.dram_tensor(n, list(shapes[n]), F32, kind="ExternalInput").ap()
        out = nc.dram_tensor("out", [S, D], F32, kind="ExternalOutput").ap()
        k = mkctx(nc, es)
        setup_consts(k)
        pm = k.A.mark()
        xa = dram(k, "xres_a", [S, D], F32)
        xb = dram(k, "xres_b", [S, D], F32)
        xcur = I["x"]
        for l in range(depth):
            k.A.release(pm)
            phase0(k, l, I)
            m_after0 = k.A.mark()
            phaseA(k, l, I, xcur)
            phaseB(k, l, I)
            phaseC(k, l, I)
            k.A.release(m_after0)
            phaseD(k, l, I)
            phaseE(k, l, I, xcur, xa)
            phaseF2(k, l, I, xa, xb)
            xcur = xb
        phaseG(k, I, xcur, out)
        k.P.finish(k.A.alloc(2, F32))
        k.P.emit(es)
    return nc


def kernel(**inputs):
    x = np.ascontiguousarray(inputs["x"], dtype=np.float32)
    c = np.ascontiguousarray(inputs["c"], dtype=np.float32)
    shapes = {n: inputs[n].shape for n in _WNAMES}
    nc = _build(shapes)
    w = {n: np.ascontiguousarray(inputs[n], dtype=np.float32) for n in _WNAMES}
    in_maps = []
    for b in range(8):
        m = {"x": x[b], "c": c[b:b + 1]}
        m.update(w)
        in_maps.append(m)
    res = run_bass_kernel_spmd(nc, in_maps, core_ids=list(range(8)))
    return np.stack([np.asarray(r["out"], dtype=np.float32) for r in res.results], axis=0)
```

```python
import numpy as np
import concourse.bass as bass
import concourse.mybir as mybir
from contextlib import ExitStack

F32 = mybir.dt.float32
BF16 = mybir.dt.bfloat16
I32 = mybir.dt.int32
AF = mybir.ActivationFunctionType
ALU = mybir.AluOpType
AX = mybir.AxisListType


class Op:
    __slots__ = ("eng", "fn", "deps", "inc", "cnt", "dma", "key", "consumed", "idx")

    def __init__(self, eng, fn, dma=False, key=None):
        self.eng = eng
        self.fn = fn
        self.deps = []
        self.inc = dma
        self.cnt = 0
        self.dma = dma
        self.key = key
        self.consumed = False


class Prog:
    ENGS = ("pe", "act", "dve", "pool", "sp")

    def __init__(self, nc):
        self.nc = nc
        self.ops = {e: [] for e in self.ENGS}
        self.last_w = {}
        self.readers = {}
        self.since_barrier = []
        self.pending_barrier = {e: [] for e in self.ENGS}
        self.nops = 0

    def _add(self, op, reads, writes):
        deps = []
        seen = set()
        for k in list(reads) + list(writes):
            w = self.last_w.get(k)
            if w is not None and id(w) not in seen:
                seen.add(id(w)); deps.append(w)
        for k in writes:
            for r in self.readers.get(k, ()):
                if id(r) not in seen:
                    seen.add(id(r)); deps.append(r)
        pb = self.pending_barrier[op.eng]
        if pb:
            for d in pb:
                if id(d) not in seen:
                    seen.add(id(d)); deps.append(d)
            self.pending_barrier[op.eng] = []
        op.deps = [d for d in deps if d is not op]
        for d in op.deps:
            d.consumed = True
        for k in writes:
            self.last_w[k] = op
            self.readers[k] = []
        for k in reads:
            if k not in writes:
                self.readers.setdefault(k, []).append(op)
        self.ops[op.eng].append(op)
        self.since_barrier.append(op)
        self.nops += 1
        return op

    def op(self, eng, fn, reads=(), writes=()):
        return self._add(Op(eng, fn), reads, writes)

    def dma(self, eng, fn, reads=(), writes=(), key=None):
        assert key is not None
        return self._add(Op(eng, fn, dma=True, key=key), reads, writes)

    def barrier(self):
        lst = []
        last = {}
        for o in self.since_barrier:
            if o.dma:
                if not o.consumed:
                    lst.append(o)
            else:
                last[o.eng] = o
        lst.extend(last.values())
        for e in self.ENGS:
            self.pending_barrier[e] = self.pending_barrier[e] + lst
        self.since_barrier = []

    def finish(self, scratch):
        self.barrier()
        self.op("pool", lambda e: e.memset(scratch, 0.0), writes=["__fin"])

    def emit(self, es):
        nc = self.nc
        for e in self.ENGS:
            for o in self.ops[e]:
                for d in o.deps:
                    if d.dma:
                        continue
                    if d.eng == "pe" and o.eng == "pe" and not o.dma:
                        continue
                    d.inc = True
        esem = {}
        for e in self.ENGS:
            esem[e] = es.enter_context(nc.semaphore("s_" + e))
        dsem = {}
        dcnt = {}
        for e in self.ENGS:
            c = 0
            for o in self.ops[e]:
                if o.dma:
                    if o.key not in dsem:
                        dsem[o.key] = es.enter_context(nc.semaphore("d_%d" % len(dsem)))
                        dcnt[o.key] = 0
                    dcnt[o.key] += 16
                    o.cnt = dcnt[o.key]
                elif o.inc:
                    c += 1
                    o.cnt = c
        self.n_dsem = len(dsem)
        block = es.enter_context(nc.Block())

        def run(ename, h):
            waited = {}
            for o in self.ops[ename]:
                need = {}
                for d in o.deps:
                    if d.dma:
                        s = dsem[d.key]
                    else:
                        if d.eng == "pe" and ename == "pe" and not o.dma:
                            continue
                        s = esem[d.eng]
                    sid = id(s)
                    if need.get(sid, (None, 0))[1] < d.cnt:
                        need[sid] = (s, d.cnt)
                for sid, (s, v) in need.items():
                    if waited.get(sid, 0) < v:
                        h.wait_ge(s, v)
                        waited[sid] = v
                ins = o.fn(h)
                if o.dma:
                    ins.then_inc(dsem[o.key], 16)
                elif o.inc:
                    ins.then_inc(esem[ename], 1)

        @block.tensor
        def _(h):
            run("pe", h)

        @block.scalar
        def _(h):
            run("act", h)

        @block.vector
        def _(h):
            run("dve", h)

        @block.gpsimd
        def _(h):
            run("pool", h)

        @block.sync
        def _(h):
            run("sp", h)


class Arena:
    def __init__(self, nc, es, ncols, name="arena"):
        self.t = es.enter_context(nc.sbuf_tensor(name, [128, ncols], F32))
        self.ncols = ncols
        self.off = 0
        self.uid = 0

    def mark(self):
        return self.off

    def release(self, m):
        self.off = m

    def alloc(self, cols, dtype=F32, parts=128):
        if dtype == BF16:
            w = (cols + 1) // 2
        else:
            w = cols
        assert self.off + w <= self.ncols, ("arena overflow", self.off, w, self.ncols)
        v = self.t[0:parts, self.off:self.off + w]
        self.off += w
        if dtype != F32:
            v = v.bitcast(dtype)
            if dtype == BF16 and cols % 2:
                v = v[:, 0:cols]
        return v


S = 4096
D = 1024
NT = S // 128
DIN = 4552
EPS = 1e-6
O_CQ, O_CKV, O_KIDX, O_WIDX, O_QREC, O_FREC, O_IREC, O_OG, O_GA, O_GR = 0, 256, 384, 448, 456, 968, 1480, 1992, 2504, 3528


class K:
    pass


def mkctx(nc, es, debug=()):
    k = K()
    k.nc = nc
    k.es = es
    k.P = Prog(nc)
    k.A = Arena(nc, es, 52800)
    k.ps = [es.enter_context(nc.psum_tensor("bank%d" % i, [128, 512], F32)) for i in range(8)]
    k.debug = set(debug)
    k.dr = {}
    k.uid = 0
    return k


def dram(k, name, shape, dtype):
    if name in k.dr:
        return k.dr[name]
    kind = "ExternalOutput" if name in k.debug else "Internal"
    t = k.nc.dram_tensor(name, list(shape), dtype, kind=kind).ap()
    k.dr[name] = t
    return t


def setup_consts(k):
    A, P = k.A, k.P
    k.ident = A.alloc(128, F32)
    k.identb = A.alloc(128, BF16)
    k.ones_f = A.alloc(128, F32)
    k.ones_b = A.alloc(128, BF16)
    P.op("pool", lambda e: e.memset(k.ident, 0.0), writes=["ident"])
    P.op("pool", lambda e: e.affine_select(out=k.ident, in_=k.ident, pattern=[[-1, 128]], compare_op=ALU.not_equal,
                                           fill=1.0, base=0, channel_multiplier=1), reads=["ident"], writes=["ident"])
    P.op("dve", lambda e: e.tensor_copy(k.identb, k.ident), reads=["ident"], writes=["identb"])
    P.op("dve", lambda e: e.memset(k.ones_f, 1.0), writes=["ones_f"])
    k.cneg = A.alloc(128, F32)
    P.op("pool", lambda e: e.memset(k.cneg, 0.0), writes=["cneg"])
    P.op("pool", lambda e: e.affine_select(out=k.cneg, in_=k.cneg, pattern=[[-1, 128]], compare_op=ALU.is_ge,
                                           fill=-1e30, base=0, channel_multiplier=1), reads=["cneg"], writes=["cneg"])
    k.ecap = A.alloc(32, F32)
    for e_ in range(32):
        P.op("dve", lambda e, e_=e_: e.memset(k.ecap[:, e_:e_ + 1], float(e_ * 768)), writes=["ecap"])
    k.cneg_u = A.alloc(128, F32)
    P.op("pool", lambda e: e.memset(k.cneg_u, 1.0), writes=["cneg_u"])
    P.op("pool", lambda e: e.affine_select(out=k.cneg_u, in_=k.cneg_u, pattern=[[1, 128]], compare_op=ALU.is_gt, fill=0.0, base=0, channel_multiplier=-1), reads=["cneg_u"], writes=["cneg_u"])
    k.trash = A.alloc(2, F32)
    P.op("dve", lambda e: e.tensor_reduce(out=k.trash[:, 0:1], in_=k.cneg_u, axis=AX.X, op=ALU.add), reads=["cneg_u"], writes=["trash"])
    P.op("dve", lambda e: e.tensor_scalar(k.trash[:, 0:1], k.trash[:, 0:1], 1.0, -(32.0 * 768 + 127.0), op0=ALU.mult, op1=ALU.add), reads=["trash"], writes=["trash"])
    k.cmf = A.alloc(128, F32)
    P.op("pool", lambda e: e.memset(k.cmf, 1.0), writes=["cmf"])
    P.op("pool", lambda e: e.affine_select(out=k.cmf, in_=k.cmf, pattern=[[1, 128]], compare_op=ALU.is_ge, fill=0.0, base=0, channel_multiplier=-1), reads=["cmf"], writes=["cmf"])
    P.op("pool", lambda e: e.memset(k.cmf[0:64, 64:128], 0.0), reads=["cmf"], writes=["cmf"])
    P.op("dve", lambda e: e.memset(k.ones_b, 1.0), writes=["ones_b"])


def phase0(k, l, I):
    A, P, nc = k.A, k.P, k.nc
    ps = k.ps
    stg = A.alloc(128, F32)
    k.par = A.alloc(128, F32)
    par = k.par
    P.op("dve", lambda e: e.memset(stg, 0.0), writes=["stg"])
    rows = [
        (0, 48, I["b_mod"][l].rearrange("(r c) -> r c", c=128), 128),
        (48, 8, I["c"][0].rearrange("(r c) -> r c", c=128), 128),
        (56, 8, I["g_norm1"][l].rearrange("(r c) -> r c", c=128), 128),
        (64, 8, I["g_norm2"][l].rearrange("(r c) -> r c", c=128), 128),
        (72, 2, I["g_cq"][l].rearrange("(r c) -> r c", c=128), 128),
        (74, 1, I["g_ckv"][l].rearrange("(r c) -> r c", c=128), 128),
        (75, 1, I["g_kidx"][l].rearrange("(r c) -> r c", c=64), 64),
        (76, 16, I["lb_logits"].rearrange("l (r c) -> (l r) c", c=128), 128),
        (92, 8, I["g_final"].rearrange("(r c) -> r c", c=128), 128),
    ]
    for (r0, n, src, w) in rows:
        P.dma("sp", lambda e, r0=r0, n=n, src=src, w=w: e.dma_start(out=stg[r0:r0 + n, 0:w], in_=src),
              reads=[], writes=["stg"], key="p0stg")
    P.dma("sp", lambda e: e.dma_start(out=stg[75:76, 64:128], in_=I["g_kidx"][l].rearrange("(r c) -> r c", c=64)),
          writes=["stg"], key="p0stg")
    P.op("pe", lambda e: e.transpose(ps[0][:, 0:128], stg, k.ident), reads=["stg", "ident"], writes=["ps0"])
    P.op("dve", lambda e: e.tensor_copy(par, ps[0][:, 0:128]), reads=["ps0"], writes=["par"])
    k.g1 = par[:, 56:64]; k.g2 = par[:, 64:72]; k.gcq = par[:, 72:74]; k.gckv = par[:, 74:75]
    k.gkidx = par[:, 75:76]; k.gfin = par[:, 92:100]
    sm = A.alloc(64, F32)
    k.sm = sm
    cact = sm[:, 0:8]
    t1 = sm[:, 8:16]
    P.op("act", lambda e: e.activation(out=t1, in_=par[:, 48:56], func=AF.Exp, scale=-1.0), reads=["par"], writes=["sm"])
    P.op("dve", lambda e: e.tensor_scalar(t1, t1, 1.0, None, op0=ALU.add), reads=["sm"], writes=["sm"])
    P.op("dve", lambda e: e.reciprocal(t1, t1), reads=["sm"], writes=["sm"])
    P.op("dve", lambda e: e.tensor_tensor(cact, par[:, 48:56], t1, op=ALU.mult), reads=["sm", "par"], writes=["sm"])
    lbl = par[:, 76:92].rearrange("p (l c) -> p l c", l=4)
    el = sm[:, 16:32].rearrange("p (l c) -> p l c", l=4)
    mx = sm[:, 32:36]
    P.op("dve", lambda e: e.tensor_reduce(out=mx, in_=par[:, 76:92].rearrange("p (l c) -> p c l", l=4), axis=AX.X, op=ALU.max),
         reads=["par"], writes=["sm"])
    P.op("dve", lambda e: e.tensor_tensor(el, lbl, mx.unsqueeze(1).to_broadcast([128, 4, 4]), op=ALU.subtract),
         reads=["sm", "par"], writes=["sm"])
    P.op("act", lambda e: e.activation(out=sm[:, 16:32], in_=sm[:, 16:32], func=AF.Exp), reads=["sm"], writes=["sm"])
    ssum = sm[:, 36:40]
    P.op("dve", lambda e: e.tensor_reduce(out=ssum, in_=sm[:, 16:32].rearrange("p (l c) -> p c l", l=4), axis=AX.X, op=ALU.add),
         reads=["sm"], writes=["sm"])
    P.op("dve", lambda e: e.reciprocal(ssum, ssum), reads=["sm"], writes=["sm"])
    k.lb = sm[:, 40:44]
    k.oml = sm[:, 44:48]
    P.op("dve", lambda e: e.memset(k.lb, 0.0), reads=["sm"], writes=["sm"])
    for j in range(1, l + 1):
        P.op("dve", lambda e, j=j: e.tensor_tensor(k.lb, k.lb, el[:, j, :], op=ALU.add), reads=["sm"], writes=["sm"])
    P.op("dve", lambda e: e.tensor_tensor(k.lb, k.lb, ssum, op=ALU.mult), reads=["sm"], writes=["sm"])
    P.op("dve", lambda e: e.tensor_scalar(k.lb, k.lb, 0.0, 1.0, op0=ALU.max, op1=ALU.min), reads=["sm"], writes=["sm"])
    P.op("dve", lambda e: e.tensor_scalar(k.oml, k.lb, -1.0, 1.0, op0=ALU.mult, op1=ALU.add), reads=["sm"], writes=["sm"])
    cact_rep = A.alloc(8 * 128, F32).rearrange("p (k m) -> p k m", k=8)
    P.op("dve", lambda e: e.tensor_copy(cact_rep, cact.unsqueeze(2).to_broadcast([128, 8, 128])), reads=["sm"], writes=["crep"])
    k.gtbc = [A.alloc(1024, F32), A.alloc(1024, F32)]
    bmbc = A.alloc(1024, F32)
    modf = k.A.alloc(32, F32)
    gs = k.A.alloc(16, F32)
    m = A.mark()
    wm = [A.alloc(8 * 1024, F32).rearrange("p (k c) -> p k c", k=8) for _ in range(2)]
    groups = [(0, "fm", 0), (1, "fm", 8), (3, "fm", 16), (4, "fm", 24), (2, "tm", 0), (5, "tm", 1)]
    wsrc = I["w_mod"][l].rearrange("(k p) c -> p k c", p=128)
    for gi, (g, kind, o) in enumerate(groups):
        w = wm[gi % 2]
        wk = "wm%d" % (gi % 2)
        P.dma("sp", lambda e, w=w, g=g: e.dma_start(out=w, in_=wsrc[:, :, g * 1024:(g + 1) * 1024]), writes=[wk], key=wk)
        if kind == "fm":
            for cb in range(8):
                for kc in range(8):
                    P.op("pe", lambda e, w=w, cb=cb, kc=kc, o=o: e.matmul(ps[1][:, o + cb:o + cb + 1], lhsT=w[:, kc, cb * 128:(cb + 1) * 128],
                                                                       rhs=cact[:, kc:kc + 1], start=(kc == 0), stop=(kc == 7)),
                         reads=[wk, "sm"], writes=["ps1"])
        else:
            P.dma("sp", lambda e, g=g: e.dma_start(out=bmbc, in_=I["b_mod"][l, g * 1024:(g + 1) * 1024].partition_broadcast(128)),
                  writes=["bmbc"], key="bmbc")
            for nb in range(2):
                pk = "ps%d" % (2 + nb)
                for kc in range(8):
                    P.op("pe", lambda e, w=w, nb=nb, kc=kc: e.matmul(ps[2 + nb][:, :], lhsT=cact_rep[:, kc, :], rhs=w[:, kc, nb * 512:(nb + 1) * 512],
                                                                  start=(kc == 0), stop=(kc == 7)),
                         reads=[wk, "crep"], writes=[pk])
                P.op("dve", lambda e, nb=nb, o=o: e.tensor_tensor(k.gtbc[o][:, nb * 512:(nb + 1) * 512], ps[2 + nb][:, :], bmbc[:, nb * 512:(nb + 1) * 512], op=ALU.add),
                     reads=[pk, "bmbc"], writes=["gtbc%d" % o])
    bsel = [0, 8, 24, 32]
    for i, b0 in enumerate(bsel):
        P.op("dve", lambda e, i=i, b0=b0: e.tensor_tensor(modf[:, i * 8:(i + 1) * 8], ps[1][:, i * 8:(i + 1) * 8], par[:, b0:b0 + 8], op=ALU.add),
             reads=["ps1", "par"], writes=["modf"])
    k.sh1 = modf[:, 0:8]; k.sh2 = modf[:, 16:24]
    k.gs1 = gs[:, 0:8]; k.gs2 = gs[:, 8:16]
    P.op("dve", lambda e: e.scalar_tensor_tensor(out=k.gs1, in0=modf[:, 8:16], scalar=1.0, in1=k.g1, op0=ALU.add, op1=ALU.mult),
         reads=["modf", "par"], writes=["gs"])
    P.op("dve", lambda e: e.scalar_tensor_tensor(out=k.gs2, in0=modf[:, 24:32], scalar=1.0, in1=k.g2, op0=ALU.add, op1=ALU.mult),
         reads=["modf", "par"], writes=["gs"])
    P.barrier()
    A.release(m)


def norm_transpose(k, xsrc, t, hT, gs, sh, tag, hkey):
    A, P, ps = k.A, k.P, k.ps
    xb = k.xb[t % 2]; xk = "xb%d" % (t % 2)
    xn = k.xn[t % 2]; nk = "xn%d" % (t % 2)
    st = k.nst[t % 2]; sk = "nst%d" % (t % 2)
    P.dma("sp", lambda e: e.dma_start(out=xb, in_=xsrc[t * 128:(t + 1) * 128, :]), writes=[xk], key=xk)
    P.op("act", lambda e: e.activation(out=xn, in_=xb, func=AF.Square, accum_out=st[:, 0:1]), reads=[xk], writes=[nk, sk])
    P.op("dve", lambda e: e.tensor_scalar(st[:, 1:2], st[:, 0:1], 1.0 / D, EPS, op0=ALU.mult, op1=ALU.add), reads=[sk], writes=[sk])
    P.op("act", lambda e: e.activation(out=st[:, 1:2], in_=st[:, 1:2], func=AF.Ln), reads=[sk], writes=[sk])
    P.op("act", lambda e: e.activation(out=st[:, 1:2], in_=st[:, 1:2], func=AF.Exp, scale=-0.5), reads=[sk], writes=[sk])
    P.op("dve", lambda e: e.tensor_scalar(xn, xb, st[:, 1:2], None, op0=ALU.mult), reads=[xk, sk], writes=[nk])
    b0 = (t % 2) * 2
    for kc in range(8):
        bank = b0 + kc // 4
        P.op("pe", lambda e, kc=kc, bank=bank: e.transpose(ps[bank][:, (kc % 4) * 128:(kc % 4 + 1) * 128], xn[:, kc * 128:(kc + 1) * 128], k.ident),
             reads=[nk, "ident"], writes=["ps%d" % bank])
    return b0


def phaseA(k, l, I, xsrc):
    A, P, nc, ps = k.A, k.P, k.nc, k.ps
    m0 = A.mark()
    WC = DIN + 64
    w = A.alloc(8 * WC, BF16).rearrange("p (k c) -> p k c", k=8)
    wsrc = I["w_in"][l].rearrange("(k p) c -> p k c", p=128)
    for (d0, s0, n) in [(0, 0, 448), (448, 384, 64), (512, 448, 1024), (1536, 1472, 1024), (2560, 2496, 1024), (3584, 3520, 1032)]:
        P.dma("pool", lambda e, d0=d0, s0=s0, n=n: e.dma_start(out=w[:, :, d0:d0 + n], in_=wsrc[:, :, s0:s0 + n]), writes=["w_in"], key="w_in")
    xbA = [A.alloc(1024, F32) for _ in range(4)]
    xnA = [A.alloc(1024, F32) for _ in range(2)]
    nstA = [A.alloc(8, F32) for _ in range(2)]
    hT = [A.alloc(8 * 512, BF16).rearrange("p (k t) -> p k t", k=8) for _ in range(2)]
    fo = [A.alloc(512, F32) for _ in range(4)]
    gb = [A.alloc(512, BF16) for _ in range(4)]
    to = [A.alloc(1672, F32) for _ in range(2)]
    cq_fm = dram(k, "cq_fm", [256, S], F32)
    ckv_fm = dram(k, "ckv_fm", [128, S], F32)
    kidx_fm = dram(k, "kidx_fm", [128, S], F32)
    qrec_fm = dram(k, "qrec_fm", [512, S], F32)
    frec_fm = dram(k, "frec_fm", [512, S], F32)
    gates_fm = dram(k, "gates_fm", [2048, S], BF16)
    tm_out = dram(k, "tm_out", [S, 1672], F32)
    fmb = [(0, cq_fm, 0, 0), (128, cq_fm, 128, 0), (256, ckv_fm, 0, 0), (384, kidx_fm, 0, 0)]
    for j in range(4):
        fmb.append((O_QREC + 64 + j * 128, qrec_fm, j * 128, 0))
    for j in range(4):
        fmb.append((O_FREC + 64 + j * 128, frec_fm, j * 128, 0))
    for j in range(16):
        fmb.append((O_GA + 64 + j * 128, gates_fm, j * 128, 1))
    tmg = [(256, 128, 0), (O_WIDX + 64, 8, 128), (O_IREC + 64, 512, 136), (O_OG + 64, 512, 648), (O_FREC + 64, 512, 1160)]
    fcount = 0
    def NORM(st):
        h = hT[st % 2]; hk = "hT%d" % (st % 2)
        sA = nstA[st % 2]; sAk = "nstA%d" % (st % 2)
        for tt in range(4):
            t = st * 4 + tt
            xb_ = xbA[tt]; xk_ = "xbA%d" % tt
            P.dma("sp", lambda e, xb_=xb_, t=t: e.dma_start(out=xb_, in_=xsrc[t * 128:(t + 1) * 128, :]), writes=[xk_], key=xk_)
            xn_ = xnA[tt % 2]; nk_ = "xnA%d" % (tt % 2)
            P.op("act", lambda e, xb_=xb_, xn_=xn_, sA=sA, tt=tt: e.activation(out=xn_, in_=xb_, func=AF.Square, accum_out=sA[:, tt:tt + 1]), reads=[xk_], writes=[nk_, sAk])
        P.op("dve", lambda e, sA=sA: e.tensor_scalar(sA[:, 4:8], sA[:, 0:4], 1.0 / D, EPS, op0=ALU.mult, op1=ALU.add), reads=[sAk], writes=[sAk])
        P.op("act", lambda e, sA=sA: e.activation(out=sA[:, 4:8], in_=sA[:, 4:8], func=AF.Ln), reads=[sAk], writes=[sAk])
        P.op("act", lambda e, sA=sA: e.activation(out=sA[:, 4:8], in_=sA[:, 4:8], func=AF.Exp, scale=-0.5), reads=[sAk], writes=[sAk])
        for tt in range(4):
            t = st * 4 + tt
            xb_ = xbA[tt]; xk_ = "xbA%d" % tt
            xn_ = xnA[tt % 2]; nk_ = "xnA%d" % (tt % 2)
            P.op("dve", lambda e, xb_=xb_, xn_=xn_, sA=sA, tt=tt: e.tensor_scalar(xn_, xb_, sA[:, 4 + tt:5 + tt], None, op0=ALU.mult), reads=[xk_, sAk], writes=[nk_])
            b0 = (t % 2) * 2
            for kc in range(8):
                bank = b0 + kc // 4
                P.op("pe", lambda e, kc=kc, bank=bank, xn_=xn_: e.transpose(ps[bank][:, (kc % 4) * 128:(kc % 4 + 1) * 128], xn_[:, kc * 128:(kc + 1) * 128], k.ident),
                     reads=[nk_, "ident"], writes=["ps%d" % bank])
            for kc in range(8):
                bank = b0 + kc // 4
                P.op("act", lambda e, kc=kc, bank=bank, tt=tt, h=h: e.activation(out=h[:, kc, tt * 128:(tt + 1) * 128], in_=ps[bank][:, (kc % 4) * 128:(kc % 4 + 1) * 128],
                                                                                 func=AF.Identity, scale=k.gs1[:, kc:kc + 1], bias=k.sh1[:, kc:kc + 1]),
                     reads=["ps%d" % bank, "gs", "modf"], writes=[hk])
    def MMS(st):
        nonlocal fcount
        h = hT[st % 2]; hk = "hT%d" % (st % 2)
        for bi, (c0, dst, r0, act) in enumerate(fmb):
            bank = 4 + fcount % 4
            pk = "ps%d" % bank
            for kc in range(8):
                P.op("pe", lambda e, kc=kc, c0=c0, bank=bank, h=h: e.matmul(ps[bank][:, :], lhsT=w[:, kc, c0:c0 + 128], rhs=h[:, kc, :], start=(kc == 0), stop=(kc == 7)),
                     reads=["w_in", hk], writes=[pk])
            f = fo[fcount % 4]; fk = "fo%d" % (fcount % 4)
            if act == 0:
                P.op("dve", lambda e, f=f, bank=bank: e.tensor_copy(f, ps[bank][:, :]), reads=[pk], writes=[fk])
                P.dma("sp", lambda e, f=f, dst=dst, r0=r0, st=st: e.dma_start(out=dst[r0:r0 + 128, st * 512:(st + 1) * 512], in_=f), reads=[fk], writes=[], key=fk)
            else:
                fb = gb[fcount % 4]
                P.op("act", lambda e, fb=fb, bank=bank: e.activation(out=fb, in_=ps[bank][:, :], func=AF.Sigmoid), reads=[pk], writes=[fk])
                P.dma("sp", lambda e, fb=fb, dst=dst, r0=r0, st=st: e.dma_start(out=dst[r0:r0 + 128, st * 512:(st + 1) * 512], in_=fb), reads=[fk], writes=[], key=fk)
            fcount += 1
        for tt in range(4):
            t = st * 4 + tt
            tb = to[t % 2]; tk = "to%d" % (t % 2)
            for gi, (c0, n, o0) in enumerate(tmg):
                if gi == 1:
                    continue
                bank = 4 + fcount % 4
                pk = "ps%d" % bank
                subs = [(c0, n, 0)]
                if gi == 0:
                    subs = [(c0, n, 0), (tmg[1][0], 8, 128)]
                for (cc, nn, po) in subs:
                    for kc in range(8):
                        P.op("pe", lambda e, kc=kc, cc=cc, nn=nn, po=po, bank=bank, h=h, tt=tt: e.matmul(ps[bank][:, po:po + nn], lhsT=h[:, kc, tt * 128:(tt + 1) * 128], rhs=w[:, kc, cc:cc + nn],
                                                                                                  start=(kc == 0), stop=(kc == 7)),
                             reads=["w_in", hk], writes=[pk])
                tot = n + (8 if gi == 0 else 0)
                eng = "dve" if gi % 2 == 0 else "act"
                if eng == "dve":
                    P.op("dve", lambda e, tb=tb, o0=o0, tot=tot, bank=bank: e.tensor_copy(tb[:, o0:o0 + tot], ps[bank][:, 0:tot]), reads=[pk], writes=[tk])
                else:
                    P.op("act", lambda e, tb=tb, o0=o0, tot=tot, bank=bank: e.activation(out=tb[:, o0:o0 + tot], in_=ps[bank][:, 0:tot], func=AF.Copy), reads=[pk], writes=[tk])
                fcount += 1
            P.dma("sp", lambda e, tb=tb, t=t: e.dma_start(out=tm_out[t * 128:(t + 1) * 128, :], in_=tb), reads=[tk], writes=[], key=tk)
    NORM(0)
    for st in range(S // 512):
        if st + 1 < S // 512:
            NORM(st + 1)
        MMS(st)
    P.barrier()
    A.release(m0)


ATTN_SCALE = 128 ** -0.5
NEG = -1e30
MASKV = -30000.0


def phaseB(k, l, I):
    A, P, ps = k.A, k.P, k.ps
    cq_fm = k.dr["cq_fm"]; ckv_fm = k.dr["ckv_fm"]; kidx_fm = k.dr["kidx_fm"]; tm_out = k.dr["tm_out"]
    qT_d = dram(k, "qT_d", [NT, 128, 8, 128], BF16)
    qidx_d = dram(k, "qidx_d", [NT, 128, 4, 128], BF16)
    k.kvT = A.alloc(S, BF16)
    k.kidxT = A.alloc(S, BF16)
    k.kvaug = A.alloc(NT * 132, BF16).rearrange("p (t c) -> p t c", t=NT)
    k.widx = A.alloc(NT * 8, F32).rearrange("p (t c) -> p t c", t=NT)
    k.wv = A.alloc(8 * 128, BF16).rearrange("p (h c) -> p h c", h=8)
    m0 = A.mark()
    wq = A.alloc(2 * 1024, BF16).rearrange("p (k c) -> p k c", k=2)
    wi = A.alloc(2 * 512, BF16).rearrange("p (k c) -> p k c", k=2)
    P.dma("pool", lambda e: e.dma_start(out=wq, in_=I["w_q_up"][l].rearrange("(k p) c -> p k c", p=128)), writes=["wq"], key="wq")
    P.dma("pool", lambda e: e.dma_start(out=wi, in_=I["w_idx_q"][l].rearrange("(k p) c -> p k c", p=128)), writes=["wi"], key="wi")
    P.op("dve", lambda e: e.memset(k.wv, 0.0), writes=["wv"])
    for par in range(2):
        P.dma("pool", lambda e, par=par: e.dma_start(out=k.wv.rearrange("p (j two) c -> p j two c", two=2)[:, :, par, par * 64:(par + 1) * 64],
                                                     in_=I["w_v_up"][l].rearrange("(j two) r v -> r j two v", two=2)[:, :, par, :]),
              writes=["wv"], key="wvd")
    ckt = A.alloc(NT * 128, F32).rearrange("p (t c) -> p t c", t=NT)
    sq = A.alloc(NT * 128, F32).rearrange("p (t c) -> p t c", t=NT)
    gb = A.alloc(128, F32)
    st = A.alloc(64, F32)
    P.dma("sp", lambda e: e.dma_start(out=ckt, in_=tm_out[:, 0:128].rearrange("(t p) c -> p t c", p=128)), writes=["ckt"], key="ckt")
    P.dma("sp", lambda e: e.dma_start(out=k.widx, in_=tm_out[:, 128:136].rearrange("(t p) c -> p t c", p=128)), writes=["widx"], key="widx")
    P.dma("sp", lambda e: e.dma_start(out=gb, in_=I["g_ckv"][l].partition_broadcast(128)), writes=["gb"], key="gb")
    P.op("dve", lambda e: e.tensor_tensor(sq, ckt, ckt, op=ALU.mult), reads=["ckt"], writes=["sq"])
    P.op("dve", lambda e: e.tensor_reduce(out=st[:, 0:32], in_=sq, axis=AX.X, op=ALU.add), reads=["sq"], writes=["stB"])
    P.op("dve", lambda e: e.tensor_scalar(st[:, 0:32], st[:, 0:32], 1.0 / 128, EPS, op0=ALU.mult, op1=ALU.add), reads=["stB"], writes=["stB"])
    P.op("act", lambda e: e.activation(out=st[:, 0:32], in_=st[:, 0:32], func=AF.Ln), reads=["stB"], writes=["stB"])
    P.op("act", lambda e: e.activation(out=st[:, 0:32], in_=st[:, 0:32], func=AF.Exp, scale=-0.5), reads=["stB"], writes=["stB"])
    P.op("dve", lambda e: e.tensor_tensor(sq, ckt, st[:, 0:32].unsqueeze(2).to_broadcast([128, NT, 128]), op=ALU.mult), reads=["ckt", "stB", "sq"], writes=["sq"])
    P.op("dve", lambda e: e.tensor_tensor(k.kvaug[:, :, 0:128], sq, gb.unsqueeze(1).to_broadcast([128, NT, 128]), op=ALU.mult), reads=["sq", "gb"], writes=["kvaug"])
    P.op("dve", lambda e: e.memset(k.kvaug[:, :, 128:132], 1.0), writes=["kvaug"])
    xin = [A.alloc(4 * 512, F32).rearrange("p (c t) -> p c t", c=4) for _ in range(2)]
    sqb = A.alloc(4 * 512, BF16).rearrange("p (c t) -> p c t", c=4)
    rs = A.alloc(3 * 512, F32).rearrange("p (c t) -> p c t", c=3)
    cqn = A.alloc(2 * 512, BF16).rearrange("p (c t) -> p c t", c=2)
    ob = [A.alloc(512, BF16) for _ in range(4)]
    oc = 0
    for b in range(S // 512):
        x = xin[b % 2]; xk = "xinB%d" % (b % 2)
        sl = slice(b * 512, (b + 1) * 512)
        P.dma("sp", lambda e, x=x, sl=sl: e.dma_start(out=x[:, 0:2, :], in_=cq_fm[:, sl].rearrange("(c p) t -> p c t", p=128)), writes=[xk], key=xk)
        P.dma("sp", lambda e, x=x, sl=sl: e.dma_start(out=x[:, 2, :], in_=ckv_fm[:, sl]), writes=[xk], key=xk)
        P.dma("sp", lambda e, x=x, sl=sl: e.dma_start(out=x[:, 3, :], in_=kidx_fm[:, sl]), writes=[xk], key=xk)
        P.op("act", lambda e, x=x: e.activation(out=sqb, in_=x, func=AF.Square), reads=[xk], writes=["sqb"])
        P.op("pe", lambda e: e.matmul(ps[0][:, :], lhsT=k.ones_b, rhs=sqb[:, 0, :], start=True, stop=False), reads=["sqb", "ones_b"], writes=["ps0"])
        P.op("pe", lambda e: e.matmul(ps[0][:, :], lhsT=k.ones_b, rhs=sqb[:, 1, :], start=False, stop=True), reads=["sqb", "ones_b"], writes=["ps0"])
        P.op("pe", lambda e: e.matmul(ps[1][:, :], lhsT=k.ones_b, rhs=sqb[:, 2, :], start=True, stop=True), reads=["sqb", "ones_b"], writes=["ps1"])
        P.op("pe", lambda e: e.matmul(ps[2][:, :], lhsT=k.ones_b, rhs=sqb[:, 3, :], start=True, stop=True), reads=["sqb", "ones_b"], writes=["ps2"])
        for i, n in enumerate([256.0, 128.0, 128.0]):
            P.op("dve", lambda e, i=i, n=n: e.tensor_scalar(rs[:, i, :], ps[i][:, :], 1.0 / n, EPS, op0=ALU.mult, op1=ALU.add), reads=["ps%d" % i], writes=["rs"])
        P.op("act", lambda e: e.activation(out=rs, in_=rs, func=AF.Ln), reads=["rs"], writes=["rs"])
        P.op("act", lambda e: e.activation(out=rs, in_=rs, func=AF.Exp, scale=-0.5), reads=["rs"], writes=["rs"])
        for c in range(2):
            P.op("dve", lambda e, c=c, x=x: e.scalar_tensor_tensor(out=cqn[:, c, :], in0=x[:, c, :], scalar=k.gcq[:, c:c + 1], in1=rs[:, 0, :], op0=ALU.mult, op1=ALU.mult),
                 reads=[xk, "rs", "par"], writes=["cqn"])
        P.op("dve", lambda e, x=x, sl=sl: e.scalar_tensor_tensor(out=k.kvT[:, sl], in0=x[:, 2, :], scalar=k.gckv[:, 0:1], in1=rs[:, 1, :], op0=ALU.mult, op1=ALU.mult),
             reads=[xk, "rs", "par"], writes=["kvT"])
        P.op("dve", lambda e, x=x, sl=sl: e.scalar_tensor_tensor(out=k.kidxT[:, sl], in0=x[:, 3, :], scalar=k.gkidx[:, 0:1], in1=rs[:, 2, :], op0=ALU.mult, op1=ALU.mult),
             reads=[xk, "rs", "par"], writes=["kidxT"])
        for h in range(8):
            bank = 4 + oc % 4; pk = "ps%d" % bank
            for c in range(2):
                P.op("pe", lambda e, h=h, c=c, bank=bank: e.matmul(ps[bank][:, :], lhsT=wq[:, c, h * 128:(h + 1) * 128], rhs=cqn[:, c, :], start=(c == 0), stop=(c == 1)),
                     reads=["wq", "cqn"], writes=[pk])
            o = ob[oc % 4]; ok = "obB%d" % (oc % 4)
            P.op("act", lambda e, o=o, bank=bank: e.activation(out=o, in_=ps[bank][:, :], func=AF.Copy, scale=ATTN_SCALE), reads=[pk], writes=[ok])
            P.dma("sp", lambda e, o=o, h=h, b=b: e.dma_start(out=qT_d[b * 4:(b + 1) * 4, :, h, :].rearrange("t r q -> r t q"), in_=o.rearrange("p (t q) -> p t q", t=4)),
                  reads=[ok], key=ok)
            oc += 1
        for j in range(4):
            bank = 4 + oc % 4; pk = "ps%d" % bank
            for c in range(2):
                P.op("pe", lambda e, j=j, c=c, bank=bank: e.matmul(ps[bank][:, :], lhsT=wi[:, c, j * 128:(j + 1) * 128], rhs=cqn[:, c, :], start=(c == 0), stop=(c == 1)),
                     reads=["wi", "cqn"], writes=[pk])
            o = ob[oc % 4]; ok = "obB%d" % (oc % 4)
            P.op("dve", lambda e, o=o, bank=bank: e.tensor_copy(o, ps[bank][:, :]), reads=[pk], writes=[ok])
            P.dma("sp", lambda e, o=o, j=j, b=b: e.dma_start(out=qidx_d[b * 4:(b + 1) * 4, :, j, :].rearrange("t r q -> r t q"), in_=o.rearrange("p (t q) -> p t q", t=4)),
                  reads=[ok], key=ok)
            oc += 1
    P.barrier()
    A.release(m0)


def phaseC(k, l, I, NITER=12):
    A, P, ps = k.A, k.P, k.ps
    qT_d = k.dr["qT_d"]; qidx_d = k.dr["qidx_d"]
    yaT_d = dram(k, "yaT_d", [4, 128, S], BF16)
    m0 = A.mark()
    qs = [A.alloc(8 * 128, BF16) for _ in range(4)]
    qi = [A.alloc(4 * 128, BF16).rearrange("p (j q) -> p j q", j=4) for _ in range(2)]
    score = [A.alloc(S, F32) for _ in range(2)]
    mb = [A.alloc(S, BF16) for _ in range(4)]
    junk = [A.alloc(S, BF16) for _ in range(2)]
    rb = [A.alloc(512, F32) for _ in range(3)]
    pT = [A.alloc(512, BF16) for _ in range(4)]
    on = A.alloc(8 * 128, BF16).rearrange("p (h r) -> p h r", h=8)
    onT = A.alloc(8 * 128, BF16).rearrange("p (h q) -> p h q", h=8)
    yo = [A.alloc(4 * 128, BF16).rearrange("p (j q) -> p j q", j=4) for _ in range(2)]
    ident4 = A.alloc(512, BF16)
    bs = [A.alloc(64, F32) for _ in range(2)]
    pw = A.alloc(NITER, F32)
    rden = A.alloc(8, F32)
    cm = A.alloc(2, F32)
    P.op("dve", lambda e: e.memset(cm, MASKV), writes=["cm"])
    for i in range(4):
        P.op("dve", lambda e, i=i: e.tensor_copy(ident4[:, i * 128:(i + 1) * 128], k.identb), reads=["identb"], writes=["ident4"])
    for i in range(NITER):
        P.op("dve", lambda e, i=i: e.memset(pw[:, i:i + 1], 0.5 ** (i + 1)), writes=["pw"])
    def oacc(h):
        return ps[h // 3][:, (h % 3) * 129:(h % 3) * 129 + 129]
    rc = [0]

    def prep(qt):
        b = qt % 2; b4 = qt % 4
        sc = score[b]; sk = "score%d" % b
        nk = (qt + 1) * 128
        P.dma("sp", lambda e: e.dma_start(out=qs[b4], in_=qT_d[qt].rearrange("r h q -> r (h q)")), writes=["qs%d" % b4], key="qs%d" % b4)
        P.dma("sp", lambda e: e.dma_start(out=qi[b], in_=qidx_d[qt]), writes=["qi%d" % b], key="qi%d" % b)
        for c0 in range(0, nk, 512):
            wd = min(512, nk - c0)
            for h in range(8):
                bank = 3 + rc[0] % 2; pk = "ps%d" % bank
                p0 = (h % 2) * 64
                P.op("pe", lambda e, h=h, p0=p0, bank=bank, c0=c0, wd=wd: e.matmul(ps[bank][:, 0:wd], lhsT=qi[b][p0:p0 + 64, h // 2, :], rhs=k.kidxT[p0:p0 + 64, c0:c0 + wd], start=True, stop=True),
                     reads=["qi%d" % b, "kidxT"], writes=[pk])
                r = rb[rc[0] % 3]; rk = "rb%d" % (rc[0] % 3)
                P.op("act", lambda e, r=r, bank=bank, wd=wd: e.activation(out=r[:, 0:wd], in_=ps[bank][:, 0:wd], func=AF.Relu), reads=[pk], writes=[rk])
                if h == 0:
                    P.op("dve", lambda e, r=r, c0=c0, wd=wd, h=h: e.tensor_scalar(sc[:, c0:c0 + wd], r[:, 0:wd], k.widx[:, qt, h:h + 1], None, op0=ALU.mult),
                         reads=[rk, "widx"], writes=[sk])
                else:
                    P.op("dve", lambda e, r=r, c0=c0, wd=wd, h=h: e.scalar_tensor_tensor(out=sc[:, c0:c0 + wd], in0=r[:, 0:wd], scalar=k.widx[:, qt, h:h + 1], in1=sc[:, c0:c0 + wd], op0=ALU.mult, op1=ALU.add),
                         reads=[rk, "widx", sk], writes=[sk])
                rc[0] += 1
        P.op("pool", lambda e: e.tensor_tensor(sc[:, qt * 128:(qt + 1) * 128], sc[:, qt * 128:(qt + 1) * 128], k.cneg, op=ALU.add), reads=[sk, "cneg"], writes=[sk])
        s_ = bs[b]; bk = "bs%d" % b
        lo = s_[:, 0:1]; w0 = s_[:, 1:2]; mid = s_[:, 2:3]; wi_ = s_[:, 8:8 + NITER]
        if qt < 2:
            P.op("dve", lambda e: e.memset(lo, -1e29), writes=[bk])
        else:
            P.op("dve", lambda e: e.tensor_reduce(out=w0, in_=sc[:, 0:nk], axis=AX.X, op=ALU.max), reads=[sk], writes=[bk])
            P.op("dve", lambda e: e.tensor_reduce(out=lo, in_=sc[:, 0:qt * 128], axis=AX.X, op=ALU.min), reads=[sk], writes=[bk])
            P.op("dve", lambda e: e.tensor_scalar(lo, lo, -1.0, None, op0=ALU.add), reads=[bk], writes=[bk])
            P.op("dve", lambda e: e.tensor_tensor(w0, w0, lo, op=ALU.subtract), reads=[bk], writes=[bk])
            P.op("dve", lambda e: e.tensor_scalar(wi_, pw, w0, None, op0=ALU.mult), reads=[bk, "pw"], writes=[bk])
            P.op("dve", lambda e: e.tensor_tensor(mid, lo, wi_[:, 0:1], op=ALU.add), reads=[bk], writes=[bk])

    def bis_iter(qt, it):
        b = qt % 2
        sc = score[b]; sk = "score%d" % b
        nk = (qt + 1) * 128
        s_ = bs[b]; bk = "bs%d" % b
        lo = s_[:, 0:1]; mid = s_[:, 2:3]; cnt = s_[:, 3:4]; tmp = s_[:, 4:5]; wi_ = s_[:, 8:8 + NITER]
        jk = junk[b]; jkk = "junk%d" % b
        P.op("dve", lambda e: e.tensor_scalar(jk[:, 0:nk], sc[:, 0:nk], mid, None, op0=ALU.is_gt, op1=ALU.add, accum_out=cnt), reads=[sk, bk], writes=[bk, jkk])
        P.op("dve", lambda e: e.scalar_tensor_tensor(out=tmp, in0=cnt, scalar=256.0, in1=wi_[:, it:it + 1], op0=ALU.is_ge, op1=ALU.mult), reads=[bk], writes=[bk])
        if it < NITER - 1:
            P.op("dve", lambda e: e.scalar_tensor_tensor(out=mid, in0=mid, scalar=wi_[:, it + 1:it + 2], in1=tmp, op0=ALU.subtract, op1=ALU.add), reads=[bk], writes=[bk])
        else:
            P.op("dve", lambda e: e.scalar_tensor_tensor(out=lo, in0=mid, scalar=wi_[:, it:it + 1], in1=tmp, op0=ALU.subtract, op1=ALU.add), reads=[bk], writes=[bk])

    def fin_mask(qt):
        b = qt % 2; b4 = qt % 4
        nk = (qt + 1) * 128
        lo = bs[b][:, 0:1]
        P.op("dve", lambda e: e.tensor_scalar(mb[b4][:, 0:nk], score[b][:, 0:nk], lo, cm[:, 0:1], op0=ALU.is_le, op1=ALU.mult), reads=["score%d" % b, "bs%d" % b, "cm"], writes=["mb%d" % b4])

    def bis(qts):
        for it in range(NITER):
            for qt in qts:
                if qt >= 2:
                    bis_iter(qt, it)
        for qt in qts:
            fin_mask(qt)

    pc = [0]

    def attention(qt):
        b = qt % 4
        q = qs[b]
        steps = [(kb, hg) for kb in range(qt + 1) for hg in range(2)]
        base = pc[0]

        def qk(i_):
            kb, hg = steps[i_]
            bank = 5 + (base + i_) % 2; pk = "ps%d" % bank
            P.op("pe", lambda e: e.matmul(ps[bank][:, :], lhsT=k.kvT[:, kb * 128:(kb + 1) * 128], rhs=q[:, hg * 512:(hg + 1) * 512], start=True, stop=False),
                 reads=["kvT", "qs%d" % b], writes=[pk])
            P.op("pe", lambda e: e.matmul(ps[bank][:, :], lhsT=mb[b][:, kb * 128:(kb + 1) * 128], rhs=ident4, start=False, stop=True),
                 reads=["mb%d" % b, "ident4"], writes=[pk])

        qk(0)
        for i_, (kb, hg) in enumerate(steps):
            if i_ + 1 < len(steps):
                qk(i_ + 1)
            bank = 5 + (base + i_) % 2; pk = "ps%d" % bank
            p = pT[(base + i_) % 4]; pk2 = "pT%d" % ((base + i_) % 4)
            P.op("act", lambda e, p=p, bank=bank: e.activation(out=p, in_=ps[bank][:, :], func=AF.Exp), reads=[pk], writes=[pk2])
            for hh in range(4):
                h = hg * 4 + hh
                P.op("pe", lambda e, p=p, hh=hh, h=h, kb=kb: e.matmul(oacc(h), lhsT=p[:, hh * 128:(hh + 1) * 128], rhs=k.kvaug[:, kb, 0:129], start=(kb == 0 and h % 3 == 0), stop=(kb == qt), skip_group_check=True),
                     reads=[pk2, "kvaug"], writes=["ps%d" % (h // 3)])
        pc[0] += len(steps)
        for bnk in range(3):
            nh = 3 if bnk < 2 else 2
            v = ps[bnk][:, 0:nh * 129].rearrange("p (h c) -> p h c", c=129)
            P.op("dve", lambda e, v=v, bnk=bnk, nh=nh: e.reciprocal(rden[:, bnk * 3:bnk * 3 + nh], v[:, :, 128]), reads=["ps%d" % bnk], writes=["rden"])
            P.op("dve", lambda e, v=v, bnk=bnk, nh=nh: e.tensor_tensor(on[:, bnk * 3:bnk * 3 + nh, :], v[:, :, 0:128], rden[:, bnk * 3:bnk * 3 + nh].unsqueeze(2).to_broadcast([128, nh, 128]), op=ALU.mult),
                 reads=["ps%d" % bnk, "rden"], writes=["on"])
        tb = ps[7][:, :].bitcast(BF16)
        for h in range(8):
            P.op("pe", lambda e, h=h: e.transpose(tb[:, h * 128:(h + 1) * 128], on[:, h, :], k.identb), reads=["on", "identb"], writes=["ps7"])
        P.op("act", lambda e: e.activation(out=onT.rearrange("p h q -> p (h q)"), in_=tb, func=AF.Copy), reads=["ps7"], writes=["onT"])
        for j in range(4):
            for two in range(2):
                h = j * 2 + two
                P.op("pe", lambda e, j=j, two=two, h=h: e.matmul(ps[7][:, j * 128:(j + 1) * 128], lhsT=k.wv[:, h, :], rhs=onT[:, h, :], start=(two == 0), stop=(two == 1)),
                     reads=["wv", "onT"], writes=["ps7"])
        y = yo[qt % 2]; yk = "yo%d" % (qt % 2)
        P.op("dve", lambda e: e.tensor_copy(y.rearrange("p j q -> p (j q)"), ps[7][:, :]), reads=["ps7"], writes=[yk])
        P.dma("sp", lambda e: e.dma_start(out=yaT_d[:, :, qt * 128:(qt + 1) * 128].rearrange("j p q -> p j q"), in_=y), reads=[yk], key=yk)

    prep(0); bis([0])
    for qt in range(NT):
        if qt + 1 < NT:
            prep(qt + 1); bis([qt + 1])
        attention(qt)
    P.barrier()
    A.release(m0)


def phaseD(k, l, I):
    A, P, ps = k.A, k.P, k.ps
    qrec_fm = k.dr["qrec_fm"]; frec_fm = k.dr["frec_fm"]; tm_out = k.dr["tm_out"]
    yrT_d = dram(k, "yrT_d", [4, 128, S], BF16)
    m0 = A.mark()
    NB = 512
    def fm(dt=F32):
        return A.alloc(4 * NB, dt).rearrange("p (j t) -> p j t", j=4)
    z = fm(); qr = fm(); e = fm(); t1 = fm(); t2 = fm(); Acum = fm(); kk = fm(); qq = fm()
    cmf = k.cmf
    def mkset():
        return dict(qt_=fm(BF16), kt_=fm(BF16), qhA=fm(BF16), qhB=fm(BF16), kh=fm(BF16),
                    vt=A.alloc(4 * 512, BF16).rearrange("p (t c) -> p t c", t=4), ogt=A.alloc(4 * 512, F32).rearrange("p (t c) -> p t c", t=4),
                    sog=A.alloc(4 * 512, F32).rearrange("p (t c) -> p t c", t=4), decay=A.alloc(32, F32))
    sets = [mkset(), mkset()]
    SETN = ["qt_", "kt_", "qhA", "qhB", "kh", "vt", "ogt", "sog", "decay"]
    grb = A.alloc(512, F32)
    vtf = A.alloc(4 * 512, F32).rearrange("p (t c) -> p t c", t=4)
    khT2 = [A.alloc(512, BF16) for _ in range(2)]
    Pm2 = [A.alloc(8 * 128, BF16).rearrange("p (h t) -> p h t", h=8) for _ in range(2)]
    state = A.alloc(4 * 64, F32).rearrange("p (j v) -> p j v", j=4)
    stmp = A.alloc(4 * 64, F32).rearrange("p (j v) -> p j v", j=4)
    sbf = [A.alloc(4 * 64, BF16).rearrange("p (j v) -> p j v", j=4) for _ in range(4)]
    osb = A.alloc(512, F32); osq = A.alloc(512, F32); oss = A.alloc(16, F32)
    sg = A.alloc(512, F32)
    yb = A.alloc(512, BF16)
    yT = [A.alloc(512, BF16) for _ in range(2)]
    P.dma("sp", lambda e_: e_.dma_start(out=osb[:, 0:64], in_=I["g_rec"][l].partition_broadcast(128)), writes=["osb"], key="grb")
    P.op("dve", lambda e_: e_.tensor_copy(grb.rearrange("p (h v) -> p h v", h=8), osb[:, 0:64].unsqueeze(1).to_broadcast([128, 8, 64])), reads=["osb"], writes=["grb"])
    P.op("dve", lambda e_: e_.memset(state, 0.0), writes=["state"])
    P.op("dve", lambda e_: e_.memset(sbf[0], 0.0), writes=["sbf0"])
    for bp_ in range(2):
        P.op("dve", lambda e_, bp_=bp_: e_.memset(sets[bp_]["qhA"], 0.0), writes=["qhA%d" % bp_])
        P.op("dve", lambda e_, bp_=bp_: e_.memset(sets[bp_]["qhB"], 0.0), writes=["qhB%d" % bp_])
    sv = [0]
    z2 = z.rearrange("p j t -> p (j t)"); e2 = e.rearrange("p j t -> p (j t)"); t12 = t1.rearrange("p j t -> p (j t)"); t22 = t2.rearrange("p j t -> p (j t)")
    A2 = Acum.rearrange("p j t -> p (j t)")
    def ch(x):
        return x.rearrange("p j (c t) -> p (j c) t", t=64)
    def prep_gen(b):
        bp = b % 2
        qt_, kt_, qhA, qhB, kh, vt, ogt, sog, decay = (sets[bp][n_] for n_ in SETN)
        sl = slice(b * NB, (b + 1) * NB)
        P.dma("sp", lambda e_, sl=sl: e_.dma_start(out=z, in_=frec_fm[:, sl].rearrange("(j p) t -> p j t", p=128)), writes=["z"], key="zD")
        yield
        P.dma("sp", lambda e_, sl=sl: e_.dma_start(out=qr, in_=qrec_fm[:, sl].rearrange("(j p) t -> p j t", p=128)), writes=["qr"], key="qrD")
        yield
        P.dma("sp", lambda e_, sl=sl: e_.dma_start(out=vtf, in_=tm_out[sl, 136:648].rearrange("(t p) c -> p t c", p=128)), writes=["vtf"], key="vtD")
        yield
        P.op("act", lambda e_: e_.activation(out=vt, in_=vtf, func=AF.Copy), reads=["vtf"], writes=[("vt%d" % bp)])
        yield
        P.dma("sp", lambda e_, sl=sl: e_.dma_start(out=ogt, in_=tm_out[sl, 648:1160].rearrange("(t p) c -> p t c", p=128)), writes=[("ogt%d" % bp)], key="ogD")
        yield
        P.op("act", lambda e_: e_.activation(out=kk, in_=z, func=AF.Sigmoid, scale=-1.0), reads=["z"], writes=["kk"])
        yield
        P.op("act", lambda e_: e_.activation(out=qq, in_=qr, func=AF.Sigmoid), reads=["qr"], writes=["qq"])
        yield
        P.op("act", lambda e_: e_.activation(out=sog, in_=ogt, func=AF.Sigmoid), reads=[("ogt%d" % bp)], writes=[("sog%d" % bp)])
        yield
        P.op("act", lambda e_: e_.activation(out=e, in_=z, func=AF.Exp, scale=-1.0), reads=["z"], writes=["e"])
        yield
        for j in range(4):
            P.op("dve", lambda e_, j=j: e_.tensor_scalar(t1[:, j, :], e[:, j, :], k.lb[:, j:j + 1], 1.0, op0=ALU.mult, op1=ALU.add), reads=["e", "sm"], writes=["t1"])
            yield
        P.op("dve", lambda e_: e_.tensor_scalar(t2, e, 1.0, None, op0=ALU.add), reads=["e"], writes=["t2"])
        yield
        P.op("act", lambda e_: e_.activation(out=t1, in_=t1, func=AF.Ln), reads=["t1"], writes=["t1"])
        yield
        P.op("act", lambda e_: e_.activation(out=z, in_=t2, func=AF.Ln), reads=["t2", "z"], writes=["z"])
        yield
        P.op("dve", lambda e_: e_.tensor_tensor(t1, t1, z, op=ALU.subtract), reads=["t1", "z"], writes=["t1"])
        yield
        for j in range(4):
            P.op("dve", lambda e_, j=j: e_.tensor_scalar(kk[:, j, :], kk[:, j, :], k.oml[:, j:j + 1], None, op0=ALU.mult), reads=["kk", "sm"], writes=["kk"])
            yield
        srcs = [t1, Acum]
        for si, sh in enumerate([1, 2, 4, 8, 16, 32]):
            a_ = ch(srcs[si % 2]); b_ = ch(srcs[(si + 1) % 2])
            P.op("dve", lambda e_, a_=a_, b_=b_, sh=sh: e_.tensor_tensor(b_[:, :, sh:64], a_[:, :, sh:64], a_[:, :, 0:64 - sh], op=ALU.add), reads=["t1", "Acum"], writes=["t1", "Acum"])
            yield
            P.op("act", lambda e_, a_=a_, b_=b_, sh=sh: e_.activation(out=b_[:, :, 0:sh], in_=a_[:, :, 0:sh], func=AF.Copy), reads=["t1", "Acum"], writes=["t1", "Acum"])
            yield
        P.op("act", lambda e_: e_.activation(out=Acum, in_=t1, func=AF.Copy), reads=["t1", "Acum"], writes=["t1", "Acum"])
        yield
        P.op("dve", lambda e_: e_.tensor_tensor(qq, qq, qr, op=ALU.mult), reads=["qq", "qr"], writes=["qq"])
        yield
        Ac = ch(Acum)
        P.op("dve", lambda e_: e_.tensor_tensor(ch(t1), Ac, Ac[:, :, 31:32].to_broadcast([128, 32, 64]), op=ALU.subtract), reads=["Acum", "t1"], writes=["t1"])
        yield
        P.op("dve", lambda e_: e_.tensor_scalar(t1, t1, -40.0, 40.0, op0=ALU.max, op1=ALU.min), reads=["t1"], writes=["t1"])
        yield
        P.op("act", lambda e_: e_.activation(out=t2, in_=t1, func=AF.Exp), reads=["t1"], writes=["t2"])
        yield
        P.op("dve", lambda e_: e_.tensor_tensor(qt_, qq, t2, op=ALU.mult), reads=["qq", "t2"], writes=[("qt_%d" % bp)])
        yield
        P.op("act", lambda e_: e_.activation(out=t2, in_=t1, func=AF.Exp, scale=-1.0), reads=["t1", ("qt_%d" % bp)], writes=["t2"])
        yield
        P.op("dve", lambda e_: e_.tensor_tensor(kt_, kk, t2, op=ALU.mult), reads=["kk", "t2"], writes=[("kt_%d" % bp)])
        yield
        P.op("act", lambda e_: e_.activation(out=t2, in_=Acum, func=AF.Exp), reads=["Acum", ("kt_%d" % bp)], writes=["t2"])
        yield
        def eo(x, par):
            return x.rearrange("p j (c two t) -> p j c two t", two=2, t=64)[:, :, :, par, :]
        P.op("dve", lambda e_: e_.tensor_tensor(eo(qhA, 0), eo(qq, 0), eo(t2, 0), op=ALU.mult), reads=["qq", "t2"], writes=[("qhA%d" % bp)])
        yield
        P.op("dve", lambda e_: e_.tensor_tensor(eo(qhB, 1), eo(qq, 1), eo(t2, 1), op=ALU.mult), reads=["qq", "t2"], writes=[("qhB%d" % bp)])
        yield
        P.op("act", lambda e_: e_.activation(out=decay, in_=Ac[:, :, 63], func=AF.Exp), reads=["Acum"], writes=[("decay%d" % bp)])
        yield
        P.op("dve", lambda e_: e_.tensor_tensor(ch(t1), Ac[:, :, 63:64].to_broadcast([128, 32, 64]), Ac, op=ALU.subtract), reads=["Acum", "t1"], writes=["t1"])
        yield
        P.op("act", lambda e_: e_.activation(out=t2, in_=t1, func=AF.Exp), reads=["t1", ("qhA%d" % bp), ("qhB%d" % bp)], writes=["t2"])
        yield
        P.op("dve", lambda e_: e_.tensor_tensor(kh, kk, t2, op=ALU.mult), reads=["kk", "t2"], writes=[("kh%d" % bp)])
        yield
    def tiles(b, filler, quota):
        bp = b % 2
        qt_, kt_, qhA, qhB, kh, vt, ogt, sog, decay = (sets[bp][n_] for n_ in SETN)
        tb = ps[7][:, :].bitcast(BF16)

        def stage1(tt):
            tsl = slice(tt * 128, (tt + 1) * 128)
            par = tt % 2
            khT = khT2[par]; khk = "khT%d" % par
            Pm = Pm2[par]; pmk = "Pm%d" % par
            for j in range(4):
                P.op("pe", lambda e_, j=j: e_.transpose(tb[:, j * 128:(j + 1) * 128], kh[:, j, tsl], k.identb), reads=[("kh%d" % bp), "identb"], writes=["ps7"])
            P.op("act", lambda e_: e_.activation(out=khT, in_=tb[:, 0:512], func=AF.Copy), reads=["ps7"], writes=[khk])
            for h in range(8):
                j = h // 2; p0 = (h % 2) * 64
                bank = 5 + h % 2
                P.op("pe", lambda e_, j=j, p0=p0, bank=bank: e_.matmul(ps[bank][:, j * 128:(j + 1) * 128], lhsT=kt_[p0:p0 + 64, j, tsl], rhs=qt_[p0:p0 + 64, j, tsl], start=True, stop=True),
                     reads=[("kt_%d" % bp), ("qt_%d" % bp)], writes=["ps%d" % bank])
            for g in range(2):
                P.op("dve", lambda e_, g=g: e_.tensor_tensor(Pm[:, g * 4:(g + 1) * 4, :], ps[5 + g][:, :].rearrange("p (h t) -> p h t", h=4), cmf.unsqueeze(1).to_broadcast([128, 4, 128]), op=ALU.mult),
                     reads=["ps%d" % (5 + g), "cmf"], writes=[pmk])
            for half in range(2):
                hs = slice(half * 64, (half + 1) * 64)
                bank = (4, 2)[half] if par == 0 else (0, 1)[half]
                for j in range(4):
                    P.op("pe", lambda e_, j=j, hs=hs, bank=bank: e_.matmul(ps[bank][:, j * 128:(j + 1) * 128], lhsT=khT[hs, j * 128:(j + 1) * 128], rhs=vt[hs, tt, j * 128:(j + 1) * 128], start=True, stop=True),
                         reads=[khk, ("vt%d" % bp)], writes=["ps%d" % bank])

        def stage2(tt):
            tsl = slice(tt * 128, (tt + 1) * 128)
            par = tt % 2
            Pm = Pm2[par]; pmk = "Pm%d" % par
            svA = sv[0]
            for half in range(2):
                c = tt * 2 + half
                bank = (4, 2)[half] if par == 0 else (0, 1)[half]
                dv = decay.rearrange("p (j c) -> p j c", j=4)[:, :, c:c + 1]
                P.op("dve", lambda e_, dv=dv: e_.tensor_tensor(stmp, state, dv.to_broadcast([128, 4, 64]), op=ALU.mult), reads=["state", ("decay%d" % bp)], writes=["stmp"])
                pv = ps[bank][:, :].rearrange("p (j x) -> p j x", j=4)
                P.op("dve", lambda e_, pv=pv: e_.tensor_tensor(state[0:64], stmp[0:64], pv[0:64, :, 0:64], op=ALU.add), reads=["stmp", "ps%d" % bank], writes=["state"])
                P.op("dve", lambda e_, pv=pv: e_.tensor_tensor(state[64:128], stmp[64:128], pv[64:128, :, 64:128], op=ALU.add), reads=["stmp", "ps%d" % bank], writes=["state"])
                sv[0] += 1
                sb_ = sbf[sv[0] % 4]
                P.op("act", lambda e_, sb_=sb_: e_.activation(out=sb_, in_=state, func=AF.Copy), reads=["state"], writes=["sbf%d" % (sv[0] % 4)])
            s0 = sbf[svA % 4]; s0k = "sbf%d" % (svA % 4)
            s1 = sbf[(svA + 1) % 4]; s1k = "sbf%d" % ((svA + 1) % 4)
            for h in range(8):
                j = h // 2; p0 = (h % 2) * 64
                oo = ps[3][:, h * 64:(h + 1) * 64]
                P.op("pe", lambda e_, h=h, oo=oo: e_.matmul(oo, lhsT=Pm[:, (h % 2) * 4 + h // 2, :], rhs=vt[:, tt, h * 64:(h + 1) * 64], start=True, stop=False), reads=[pmk, ("vt%d" % bp)], writes=["ps3"])
                P.op("pe", lambda e_, j=j, p0=p0, oo=oo: e_.matmul(oo, lhsT=qhA[p0:p0 + 64, j, tsl], rhs=s0[p0:p0 + 64, j, :], start=False, stop=False), reads=[("qhA%d" % bp), s0k], writes=["ps3"])
                P.op("pe", lambda e_, j=j, p0=p0, oo=oo: e_.matmul(oo, lhsT=qhB[p0:p0 + 64, j, tsl], rhs=s1[p0:p0 + 64, j, :], start=False, stop=True), reads=[("qhB%d" % bp), s1k], writes=["ps3"])
            P.op("act", lambda e_: e_.activation(out=osb, in_=ps[3][:, :], func=AF.Copy), reads=["ps3"], writes=["osb"])
            P.op("dve", lambda e_: e_.tensor_tensor(osq, osb, osb, op=ALU.mult), reads=["osb"], writes=["osq"])
            P.op("dve", lambda e_: e_.tensor_reduce(out=oss[:, 0:8], in_=osq.rearrange("p (h v) -> p h v", h=8), axis=AX.X, op=ALU.add), reads=["osq"], writes=["oss"])
            P.op("dve", lambda e_: e_.tensor_scalar(oss[:, 0:8], oss[:, 0:8], 1.0 / 64, EPS, op0=ALU.mult, op1=ALU.add), reads=["oss"], writes=["oss"])
            P.op("act", lambda e_: e_.activation(out=oss[:, 0:8], in_=oss[:, 0:8], func=AF.Ln), reads=["oss"], writes=["oss"])
            P.op("act", lambda e_: e_.activation(out=oss[:, 0:8], in_=oss[:, 0:8], func=AF.Exp, scale=-0.5), reads=["oss"], writes=["oss"])
            P.op("dve", lambda e_: e_.tensor_tensor(osq.rearrange("p (h v) -> p h v", h=8), osb.rearrange("p (h v) -> p h v", h=8), oss[:, 0:8].unsqueeze(2).to_broadcast([128, 8, 64]), op=ALU.mult),
                 reads=["osb", "oss", "osq"], writes=["osq"])
            P.op("dve", lambda e_: e_.tensor_tensor(osq, osq, grb, op=ALU.mult), reads=["osq", "grb"], writes=["osq"])
            P.op("dve", lambda e_: e_.tensor_tensor(sg, sog[:, tt, :], ogt[:, tt, :], op=ALU.mult), reads=[("sog%d" % bp), ("ogt%d" % bp)], writes=["sg"])
            P.op("dve", lambda e_: e_.tensor_tensor(yb, osq, sg, op=ALU.mult), reads=["osq", "sg"], writes=["yb"])
            for j in range(4):
                P.op("pe", lambda e_, j=j: e_.transpose(tb[:, 512 + j * 128:512 + (j + 1) * 128], yb[:, j * 128:(j + 1) * 128], k.identb), reads=["yb", "identb"], writes=["ps7"])
            t = b * 4 + tt
            y = yT[t % 2]; yk = "yTD%d" % (t % 2)
            P.op("act", lambda e_, y=y: e_.activation(out=y, in_=tb[:, 512:1024], func=AF.Copy), reads=["ps7"], writes=[yk])
            P.dma("sp", lambda e_, y=y, t=t: e_.dma_start(out=yrT_d[:, :, t * 128:(t + 1) * 128].rearrange("j p q -> p j q"), in_=y.rearrange("p (j q) -> p j q", j=4)), reads=[yk], key=yk)

        stage1(0)
        for tt in range(4):
            if filler is not None:
                for _ in range(quota):
                    next(filler, None)
            if tt + 1 < 4:
                stage1(tt + 1)
            stage2(tt)

    for _ in prep_gen(0):
        pass
    for b in range(S // NB):
        filler = prep_gen(b + 1) if b + 1 < S // NB else None
        tiles(b, filler, 14)
        if filler is not None:
            for _ in filler:
                pass
    P.barrier()
    A.release(m0)


def phaseE(k, l, I, xsrc, xdst):
    A, P, ps = k.A, k.P, k.ps
    yaT_d = k.dr["yaT_d"]; yrT_d = k.dr["yrT_d"]; gates_fm = k.dr["gates_fm"]
    m0 = A.mark()
    wa = A.alloc(4 * 1024, BF16).rearrange("p (k c) -> p k c", k=4)
    wr = A.alloc(4 * 1024, BF16).rearrange("p (k c) -> p k c", k=4)
    wo = A.alloc(8 * 1024, BF16).rearrange("p (k c) -> p k c", k=8)
    P.dma("pool", lambda e: e.dma_start(out=wa, in_=I["w_branch_a"][l].rearrange("(k p) c -> p k c", p=128)), writes=["wa"], key="wa")
    P.dma("pool", lambda e: e.dma_start(out=wr, in_=I["w_branch_r"][l].rearrange("(k p) c -> p k c", p=128)), writes=["wr"], key="wr")
    P.dma("pool", lambda e: e.dma_start(out=wo, in_=I["w_out"][l].rearrange("(k p) c -> p k c", p=128)), writes=["wo"], key="wo")
    ya = [A.alloc(4 * 512, BF16).rearrange("p (k t) -> p k t", k=4) for _ in range(2)]
    yr = [A.alloc(4 * 512, BF16).rearrange("p (k t) -> p k t", k=4) for _ in range(2)]
    gt = [A.alloc(16 * 512, BF16).rearrange("p (k t) -> p k t", k=16) for _ in range(2)]
    mT2 = [A.alloc(8 * 512, BF16).rearrange("p (k t) -> p k t", k=8) for _ in range(2)]
    m1 = [A.alloc(512, F32) for _ in range(2)]
    m2 = [A.alloc(512, F32) for _ in range(2)]
    xb = [A.alloc(1024, F32) for _ in range(2)]
    tb_ = [A.alloc(1024, F32) for _ in range(2)]
    cnt = 0
    for b in range(S // 512):
        sl = slice(b * 512, (b + 1) * 512)
        i2 = b % 2
        mT = mT2[i2]; mTk = "mT%d" % i2
        P.dma("sp", lambda e, i2=i2, sl=sl: e.dma_start(out=ya[i2], in_=yaT_d[:, :, sl].rearrange("j p t -> p j t")), writes=["yaE%d" % i2], key="yaE%d" % i2)
        P.dma("sp", lambda e, i2=i2, sl=sl: e.dma_start(out=yr[i2], in_=yrT_d[:, :, sl].rearrange("j p t -> p j t")), writes=["yrE%d" % i2], key="yrE%d" % i2)
        P.dma("sp", lambda e, i2=i2, sl=sl: e.dma_start(out=gt[i2], in_=gates_fm[:, sl].rearrange("(j p) t -> p j t", p=128)), writes=["gtE%d" % i2], key="gtE%d" % i2)
        for cb in range(8):
            ba = cnt % 2; bb = 2 + cnt % 2
            for kc in range(4):
                P.op("pe", lambda e, kc=kc, cb=cb, ba=ba, i2=i2: e.matmul(ps[ba][:, :], lhsT=wa[:, kc, cb * 128:(cb + 1) * 128], rhs=ya[i2][:, kc, :], start=(kc == 0), stop=(kc == 3)),
                     reads=["wa", "yaE%d" % i2], writes=["ps%d" % ba])
            for kc in range(4):
                P.op("pe", lambda e, kc=kc, cb=cb, bb=bb, i2=i2: e.matmul(ps[bb][:, :], lhsT=wr[:, kc, cb * 128:(cb + 1) * 128], rhs=yr[i2][:, kc, :], start=(kc == 0), stop=(kc == 3)),
                     reads=["wr", "yrE%d" % i2], writes=["ps%d" % bb])
            a1 = m1[cnt % 2]; a2 = m2[cnt % 2]
            P.op("dve", lambda e, a1=a1, ba=ba, cb=cb, i2=i2: e.tensor_tensor(a1, ps[ba][:, :], gt[i2][:, cb, :], op=ALU.mult), reads=["ps%d" % ba, "gtE%d" % i2], writes=["m1%d" % (cnt % 2)])
            P.op("dve", lambda e, a2=a2, bb=bb, cb=cb, i2=i2: e.tensor_tensor(a2, ps[bb][:, :], gt[i2][:, 8 + cb, :], op=ALU.mult), reads=["ps%d" % bb, "gtE%d" % i2], writes=["m2%d" % (cnt % 2)])
            P.op("dve", lambda e, a1=a1, a2=a2, cb=cb, mT=mT: e.tensor_tensor(mT[:, cb, :], a1, a2, op=ALU.add), reads=["m1%d" % (cnt % 2), "m2%d" % (cnt % 2)], writes=[mTk])
            cnt += 1
        for tt in range(4):
            t = b * 4 + tt
            x = xb[t % 2]; xk = "xbE%d" % (t % 2)
            tq = tb_[t % 2]; tk = "tbE%d" % (t % 2)
            P.dma("sp", lambda e, x=x, t=t: e.dma_start(out=x, in_=xsrc[t * 128:(t + 1) * 128, :]), writes=[xk], key=xk)
            for half in range(2):
                bank = 4 + (t * 2 + half) % 4
                for kc in range(8):
                    P.op("pe", lambda e, kc=kc, half=half, bank=bank, tt=tt, mT=mT: e.matmul(ps[bank][:, :], lhsT=mT[:, kc, tt * 128:(tt + 1) * 128], rhs=wo[:, kc, half * 512:(half + 1) * 512], start=(kc == 0), stop=(kc == 7)),
                         reads=[mTk, "wo"], writes=["ps%d" % bank])
                P.op("dve", lambda e, tq=tq, half=half, bank=bank: e.tensor_tensor(tq[:, half * 512:(half + 1) * 512], ps[bank][:, :], k.gtbc[0][:, half * 512:(half + 1) * 512], op=ALU.mult),
                     reads=["ps%d" % bank, "gtbc0"], writes=[tk])
            P.op("dve", lambda e, tq=tq, x=x: e.tensor_tensor(tq, tq, x, op=ALU.add), reads=[tk, xk], writes=[tk])
            P.dma("sp", lambda e, tq=tq, t=t: e.dma_start(out=xdst[t * 128:(t + 1) * 128, :], in_=tq), reads=[tk], key=tk)
    P.barrier()
    A.release(m0)


def phaseF(k, l, I, xsrc, xdst):
    A, P, ps = k.A, k.P, k.ps
    m0 = A.mark()
    h2T = A.alloc(8 * S, BF16).rearrange("p (k t) -> p k t", k=8)
    gate = A.alloc(NT * 32, F32).rearrange("p (t c) -> p t c", t=NT)
    k.xb = [A.alloc(1024, F32) for _ in range(2)]
    m1_ = A.mark()
    hf = [A.alloc(8 * 128, F32).rearrange("p (k t) -> p k t", k=8) for _ in range(2)]
    wrt = A.alloc(8 * 36, F32).rearrange("p (k c) -> p k c", k=8)
    rb = A.alloc(36, F32)
    lg = A.alloc(NT * 36, F32).rearrange("p (t c) -> p t c", t=NT)
    k.xn = [A.alloc(1024, F32) for _ in range(2)]
    k.nst = [A.alloc(2, F32) for _ in range(2)]
    P.dma("sp", lambda e: e.dma_start(out=wrt[:, :, 0:4], in_=I["w_grp"][l].rearrange("(k p) c -> p k c", p=128)), writes=["wrt"], key="wrt")
    P.dma("sp", lambda e: e.dma_start(out=wrt[:, :, 4:36], in_=I["w_exp_router"][l].rearrange("(k p) c -> p k c", p=128)), writes=["wrt"], key="wrt")
    P.dma("sp", lambda e: e.dma_start(out=rb[:, 0:4], in_=I["b_grp"][l].partition_broadcast(128)), writes=["rbF"], key="rbF")
    P.dma("sp", lambda e: e.dma_start(out=rb[:, 4:36], in_=I["b_exp_router"][l].partition_broadcast(128)), writes=["rbF"], key="rbF")
    for t in range(NT):
        b0 = norm_transpose(k, xsrc, t, None, None, None, None, None)
        f = hf[t % 2]; fk = "hfF%d" % (t % 2)
        for kc in range(8):
            bank = b0 + kc // 4
            P.op("act", lambda e, kc=kc, bank=bank, f=f: e.activation(out=f[:, kc, :], in_=ps[bank][:, (kc % 4) * 128:(kc % 4 + 1) * 128], func=AF.Identity, scale=k.gs2[:, kc:kc + 1], bias=k.sh2[:, kc:kc + 1]),
                 reads=["ps%d" % bank, "gs", "modf"], writes=[fk])
        P.op("dve", lambda e, f=f, t=t: e.tensor_copy(h2T[:, :, t * 128:(t + 1) * 128], f), reads=[fk], writes=["h2T"])
        bank = 4 + t % 2
        for kc in range(8):
            P.op("pe", lambda e, kc=kc, f=f, bank=bank: e.matmul(ps[bank][:, 0:36], lhsT=f[:, kc, :], rhs=wrt[:, kc, :], start=(kc == 0), stop=(kc == 7)), reads=[fk, "wrt"], writes=["ps%d" % bank])
        P.op("dve", lambda e, t=t, bank=bank: e.tensor_tensor(lg[:, t, :], ps[bank][:, 0:36], rb, op=ALU.add), reads=["ps%d" % bank, "rbF"], writes=["lg"])
    g4 = A.alloc(NT * 4, F32).rearrange("p (t c) -> p t c", t=NT)
    oh = A.alloc(NT * 4, F32).rearrange("p (t c) -> p t c", t=NT)
    s1 = A.alloc(NT * 8, F32)
    le = A.alloc(NT * 32, F32).rearrange("p (t c) -> p t c", t=NT)
    o1 = A.alloc(NT * 32, F32).rearrange("p (t c) -> p t c", t=NT)
    o2 = A.alloc(NT * 32, F32).rearrange("p (t c) -> p t c", t=NT)
    mx = s1[:, 0:NT]; gs_ = s1[:, NT:2 * NT]; mA = s1[:, 2 * NT:3 * NT]; mB = s1[:, 3 * NT:4 * NT]; w1 = s1[:, 4 * NT:5 * NT]; w2 = s1[:, 5 * NT:6 * NT]
    R = ["lg", "g4", "oh", "s1", "le", "o1", "o2", "gate"]
    def D(fn):
        P.op("dve", fn, reads=R, writes=R)
    bc4 = lambda v: v.unsqueeze(2).to_broadcast([128, NT, 4])
    bc32 = lambda v: v.unsqueeze(2).to_broadcast([128, NT, 32])
    D(lambda e: e.tensor_reduce(out=mx, in_=lg[:, :, 0:4], axis=AX.X, op=ALU.max))
    D(lambda e: e.tensor_tensor(oh, lg[:, :, 0:4], bc4(mx), op=ALU.is_ge))
    D(lambda e: e.tensor_tensor(g4, lg[:, :, 0:4], bc4(mx), op=ALU.subtract))
    P.op("act", lambda e: e.activation(out=g4, in_=g4, func=AF.Exp), reads=R, writes=R)
    D(lambda e: e.tensor_reduce(out=gs_, in_=g4, axis=AX.X, op=ALU.add))
    D(lambda e: e.reciprocal(gs_, gs_))
    lev = le.rearrange("p t (g x) -> p t g x", g=4)
    D(lambda e: e.tensor_tensor(lev, lg[:, :, 4:36].rearrange("p t (g x) -> p t g x", g=4), oh.unsqueeze(3).to_broadcast([128, NT, 4, 8]), op=ALU.mult))
    D(lambda e: e.tensor_scalar(o1.rearrange("p t (g x) -> p t g x", g=4), oh.unsqueeze(3).to_broadcast([128, NT, 4, 8]), -1.0, 1e30, op0=ALU.add, op1=ALU.mult))
    D(lambda e: e.tensor_tensor(le, le, o1, op=ALU.add))
    D(lambda e: e.tensor_reduce(out=mA, in_=le, axis=AX.X, op=ALU.max))
    D(lambda e: e.tensor_tensor(o1, le, bc32(mA), op=ALU.is_ge))
    D(lambda e: e.scalar_tensor_tensor(out=le, in0=o1, scalar=-1e30, in1=le, op0=ALU.mult, op1=ALU.add))
    D(lambda e: e.tensor_reduce(out=mB, in_=le, axis=AX.X, op=ALU.max))
    D(lambda e: e.tensor_tensor(o2, le, bc32(mB), op=ALU.is_ge))
    D(lambda e: e.tensor_tensor(w1, mB, mA, op=ALU.subtract))
    P.op("act", lambda e: e.activation(out=w1, in_=w1, func=AF.Exp), reads=R, writes=R)
    D(lambda e: e.tensor_scalar(w1, w1, 1.0, None, op0=ALU.add))
    D(lambda e: e.reciprocal(w1, w1))
    D(lambda e: e.tensor_scalar(w2, w1, -1.0, 1.0, op0=ALU.mult, op1=ALU.add))
    D(lambda e: e.tensor_tensor(w1, w1, gs_, op=ALU.mult))
    D(lambda e: e.tensor_tensor(w2, w2, gs_, op=ALU.mult))
    D(lambda e: e.tensor_tensor(o1, o1, bc32(w1), op=ALU.mult))
    D(lambda e: e.tensor_tensor(o2, o2, bc32(w2), op=ALU.mult))
    D(lambda e: e.tensor_tensor(gate, o1, o2, op=ALU.add))
    P.barrier()
    A.release(m1_)
    TB = 1024
    acc = A.alloc(8 * 1024, F32).rearrange("p (t c) -> p t c", t=8)
    wg = [A.alloc(8 * 512, BF16).rearrange("p (k c) -> p k c", k=8) for _ in range(2)]
    wu = [A.alloc(8 * 512, BF16).rearrange("p (k c) -> p k c", k=8) for _ in range(2)]
    wd = [A.alloc(4 * 1024, BF16).rearrange("p (k c) -> p k c", k=4) for _ in range(2)]
    hid = [A.alloc(4 * 512, BF16).rearrange("p (k t) -> p k t", k=4) for _ in range(2)]
    sg = [A.alloc(512, F32) for _ in range(2)]
    ec = 0; hc_ = 0; sc_ = 0; yc = 0
    for tb in range(S // TB):
        P.op("pool", lambda e: e.memset(acc, 0.0), writes=["acc"])
        for ex in range(32):
            i2 = ec % 2
            P.dma("pool", lambda e, i2=i2, ex=ex: e.dma_start(out=wg[i2], in_=I["w_gate"][l, ex].rearrange("(k p) c -> p k c", p=128)), writes=["wg%d" % i2], key="wg%d" % i2)
            P.dma("pool", lambda e, i2=i2, ex=ex: e.dma_start(out=wu[i2], in_=I["w_up"][l, ex].rearrange("(k p) c -> p k c", p=128)), writes=["wu%d" % i2], key="wu%d" % i2)
            P.dma("pool", lambda e, i2=i2, ex=ex: e.dma_start(out=wd[i2], in_=I["w_down"][l, ex].rearrange("(k p) c -> p k c", p=128)), writes=["wd%d" % i2], key="wd%d" % i2)
            for hb in range(TB // 512):
                tok0 = tb * TB + hb * 512
                hd = hid[hc_ % 2]; hk = "hid%d" % (hc_ % 2)
                for cb in range(4):
                    bg = (sc_ % 2) * 2; bu = bg + 1
                    for kc in range(8):
                        P.op("pe", lambda e, kc=kc, cb=cb, bg=bg, i2=i2, tok0=tok0: e.matmul(ps[bg][:, :], lhsT=wg[i2][:, kc, cb * 128:(cb + 1) * 128], rhs=h2T[:, kc, tok0:tok0 + 512], start=(kc == 0), stop=(kc == 7)),
                             reads=["wg%d" % i2, "h2T"], writes=["ps%d" % bg])
                    for kc in range(8):
                        P.op("pe", lambda e, kc=kc, cb=cb, bu=bu, i2=i2, tok0=tok0: e.matmul(ps[bu][:, :], lhsT=wu[i2][:, kc, cb * 128:(cb + 1) * 128], rhs=h2T[:, kc, tok0:tok0 + 512], start=(kc == 0), stop=(kc == 7)),
                             reads=["wu%d" % i2, "h2T"], writes=["ps%d" % bu])
                    s = sg[sc_ % 2]; sk = "sgF%d" % (sc_ % 2)
                    P.op("act", lambda e, s=s, bg=bg: e.activation(out=s, in_=ps[bg][:, :], func=AF.Exp, scale=-1.0), reads=["ps%d" % bg], writes=[sk])
                    P.op("pool", lambda e, s=s: e.tensor_scalar(s, s, 1.0, None, op0=ALU.add), reads=[sk], writes=[sk])
                    P.op("dve", lambda e, s=s: e.reciprocal(s, s), reads=[sk], writes=[sk])
                    P.op("dve", lambda e, s=s, bg=bg: e.tensor_tensor(s, s, ps[bg][:, :], op=ALU.mult), reads=[sk, "ps%d" % bg], writes=[sk])
                    P.op("dve", lambda e, s=s, bu=bu, hd=hd, cb=cb: e.tensor_tensor(hd[:, cb, :], s, ps[bu][:, :], op=ALU.mult), reads=[sk, "ps%d" % bu], writes=[hk])
                    sc_ += 1
                for tt in range(4):
                    tl = hb * 4 + tt
                    tg = tb * 8 + tl
                    for half in range(2):
                        bank = 4 + yc % 4
                        for kc in range(4):
                            P.op("pe", lambda e, kc=kc, half=half, bank=bank, tt=tt, hd=hd, i2=i2: e.matmul(ps[bank][:, :], lhsT=hd[:, kc, tt * 128:(tt + 1) * 128], rhs=wd[i2][:, kc, half * 512:(half + 1) * 512], start=(kc == 0), stop=(kc == 3)),
                                 reads=[hk, "wd%d" % i2], writes=["ps%d" % bank])
                        P.op("dve", lambda e, bank=bank, tl=tl, tg=tg, half=half, ex=ex: e.scalar_tensor_tensor(out=acc[:, tl, half * 512:(half + 1) * 512], in0=ps[bank][:, :], scalar=gate[:, tg, ex:ex + 1], in1=acc[:, tl, half * 512:(half + 1) * 512], op0=ALU.mult, op1=ALU.add),
                             reads=["ps%d" % bank, "gate", "acc"], writes=["acc"])
                        yc += 1
                hc_ += 1
            ec += 1
        for tl in range(8):
            tg = tb * 8 + tl
            x = k.xb[tg % 2]; xk = "xb%d" % (tg % 2)
            P.dma("sp", lambda e, x=x, tg=tg: e.dma_start(out=x, in_=xsrc[tg * 128:(tg + 1) * 128, :]), writes=[xk], key=xk)
            P.op("pool", lambda e, tl=tl: e.tensor_tensor(acc[:, tl, :], acc[:, tl, :], k.gtbc[1], op=ALU.mult), reads=["acc", "gtbc1"], writes=["acc"])
            P.op("pool", lambda e, tl=tl, x=x: e.tensor_tensor(x, x, acc[:, tl, :], op=ALU.add), reads=["acc", xk], writes=[xk])
            P.dma("sp", lambda e, x=x, tg=tg: e.dma_start(out=xdst[tg * 128:(tg + 1) * 128, :], in_=x), reads=[xk], key=xk)
    P.barrier()
    A.release(m0)


def phaseG(k, I, xsrc, out):
    A, P, ps = k.A, k.P, k.ps
    m0 = A.mark()
    gb = A.alloc(1024, F32)
    P.dma("sp", lambda e: e.dma_start(out=gb, in_=I["g_final"].partition_broadcast(128)), writes=["gbG"], key="gbG")
    xb = [A.alloc(1024, F32) for _ in range(2)]
    xn = [A.alloc(1024, F32) for _ in range(2)]
    st = [A.alloc(2, F32) for _ in range(2)]
    for t in range(NT):
        x = xb[t % 2]; xk = "xbG%d" % (t % 2); n = xn[t % 2]; nk = "xnG%d" % (t % 2); s = st[t % 2]; sk = "stG%d" % (t % 2)
        P.dma("sp", lambda e, x=x, t=t: e.dma_start(out=x, in_=xsrc[t * 128:(t + 1) * 128, :]), writes=[xk], key=xk)
        P.op("act", lambda e, x=x, n=n, s=s: e.activation(out=n, in_=x, func=AF.Square, accum_out=s[:, 0:1]), reads=[xk], writes=[nk, sk])
        P.op("dve", lambda e, s=s: e.tensor_scalar(s[:, 1:2], s[:, 0:1], 1.0 / D, EPS, op0=ALU.mult, op1=ALU.add), reads=[sk], writes=[sk])
        P.op("act", lambda e, s=s: e.activation(out=s[:, 1:2], in_=s[:, 1:2], func=AF.Ln), reads=[sk], writes=[sk])
        P.op("act", lambda e, s=s: e.activation(out=s[:, 1:2], in_=s[:, 1:2], func=AF.Exp, scale=-0.5), reads=[sk], writes=[sk])
        P.op("dve", lambda e, x=x, n=n, s=s: e.scalar_tensor_tensor(out=n, in0=x, scalar=s[:, 1:2], in1=gb, op0=ALU.mult, op1=ALU.mult), reads=[xk, sk, "gbG"], writes=[nk])
        P.dma("sp", lambda e, n=n, t=t: e.dma_start(out=out[t * 128:(t + 1) * 128, :], in_=n), reads=[nk], key=nk)
    P.barrier()
    A.release(m0)


CAP = 768
NSL = 32 * CAP


def phaseF2(k, l, I, xsrc, xdst):
    A, P, ps = k.A, k.P, k.ps
    Xbuf = dram(k, "Xbuf", [NSL + 128, D], BF16)
    Ybuf = dram(k, "Ybuf", [NSL + 128, D], F32)
    m0 = A.mark()
    idx1 = A.alloc(NT, I32); idx2 = A.alloc(NT, I32)
    g12 = A.alloc(2 * NT, F32)
    g1 = g12[:, 0:NT]; g2 = g12[:, NT:2 * NT]
    k.xb = [A.alloc(1024, F32) for _ in range(2)]
    mH = A.mark()
    h2tm = A.alloc(NT * 1024, BF16).rearrange("p (t c) -> p t c", t=NT)
    m1_ = A.mark()
    hf = [A.alloc(8 * 128, F32).rearrange("p (k t) -> p k t", k=8) for _ in range(2)]
    wrt = A.alloc(8 * 36, F32).rearrange("p (k c) -> p k c", k=8)
    rb = A.alloc(36, F32)
    lg = A.alloc(NT * 36, F32).rearrange("p (t c) -> p t c", t=NT)
    k.xn = [A.alloc(1024, F32) for _ in range(2)]
    k.nst = [A.alloc(2, F32) for _ in range(2)]
    P.dma("sp", lambda e: e.dma_start(out=wrt[:, :, 0:4], in_=I["w_grp"][l].rearrange("(k p) c -> p k c", p=128)), writes=["wrt"], key="wrt")
    P.dma("sp", lambda e: e.dma_start(out=wrt[:, :, 4:36], in_=I["w_exp_router"][l].rearrange("(k p) c -> p k c", p=128)), writes=["wrt"], key="wrt")
    P.dma("sp", lambda e: e.dma_start(out=rb[:, 0:4], in_=I["b_grp"][l].partition_broadcast(128)), writes=["rbF"], key="rbF")
    P.dma("sp", lambda e: e.dma_start(out=rb[:, 4:36], in_=I["b_exp_router"][l].partition_broadcast(128)), writes=["rbF"], key="rbF")
    def EVt(t):
        b0 = (t % 2) * 2
        f = hf[t % 2]; fk = "hfF%d" % (t % 2)
        for kc in range(8):
            bank = b0 + kc // 4
            P.op("act", lambda e, kc=kc, bank=bank, f=f: e.activation(out=f[:, kc, :], in_=ps[bank][:, (kc % 4) * 128:(kc % 4 + 1) * 128], func=AF.Identity, scale=k.gs2[:, kc:kc + 1], bias=k.sh2[:, kc:kc + 1]),
                 reads=["ps%d" % bank, "gs", "modf"], writes=[fk])
        bank = 4 + t % 2
        for kc in range(8):
            P.op("pe", lambda e, kc=kc, f=f, bank=bank: e.matmul(ps[bank][:, 0:36], lhsT=f[:, kc, :], rhs=wrt[:, kc, :], start=(kc == 0), stop=(kc == 7)), reads=[fk, "wrt"], writes=["ps%d" % bank])
        P.op("dve", lambda e, t=t, bank=bank: e.tensor_tensor(lg[:, t, :], ps[bank][:, 0:36], rb, op=ALU.add), reads=["ps%d" % bank, "rbF"], writes=["lg"])
        for kc in range(8):
            bank = 6 + kc // 4
            P.op("pe", lambda e, kc=kc, f=f, bank=bank: e.transpose(ps[bank][:, (kc % 4) * 128:(kc % 4 + 1) * 128], f[:, kc, :], k.ident), reads=[fk, "ident"], writes=["ps%d" % bank])
        P.op("dve", lambda e, t=t: e.tensor_copy(h2tm[:, t, 0:512], ps[6][:, :]), reads=["ps6"], writes=["h2tm"])
        P.op("pool" if False else "dve", lambda e, t=t: e.tensor_copy(h2tm[:, t, 512:1024], ps[7][:, :]), reads=["ps7"], writes=["h2tm"])
    norm_transpose(k, xsrc, 0, None, None, None, None, None)
    for t in range(NT):
        if t + 1 < NT:
            norm_transpose(k, xsrc, t + 1, None, None, None, None, None)
        EVt(t)
    def T32():
        return A.alloc(NT * 32, F32).rearrange("p (t c) -> p t c", t=NT)
    g4 = A.alloc(NT * 4, F32).rearrange("p (t c) -> p t c", t=NT)
    oh = A.alloc(NT * 4, F32).rearrange("p (t c) -> p t c", t=NT)
    s1 = A.alloc(NT * 8, F32)
    le = T32(); o1 = T32(); o2 = T32(); pos = T32(); tmp = T32(); offs = T32()
    indb = A.alloc(NT * 32, BF16)
    SU = A.alloc(128, BF16)
    suf = A.alloc(128, F32)
    mx = s1[:, 0:NT]; gs_ = s1[:, NT:2 * NT]; mA = s1[:, 2 * NT:3 * NT]; mB = s1[:, 3 * NT:4 * NT]; w1 = s1[:, 4 * NT:5 * NT]; w2 = s1[:, 5 * NT:6 * NT]
    v1 = s1[:, 6 * NT:7 * NT]; v2 = s1[:, 7 * NT:8 * NT]
    R = ["lg", "g4", "oh", "s1", "le", "o1", "o2", "pos", "tmp", "offs", "g12", "idx"]
    def Dv(fn):
        P.op("dve", fn, reads=R, writes=R)
    bc4 = lambda v: v.unsqueeze(2).to_broadcast([128, NT, 4])
    bc32 = lambda v: v.unsqueeze(2).to_broadcast([128, NT, 32])
    P.op("pool", lambda e: e.memset(suf, 1.0), writes=["suf"])
    P.op("pool", lambda e: e.tensor_tensor(suf, k.cneg_u, k.cneg_u, op=ALU.mult), reads=["cneg_u"], writes=["suf"])
    P.op("dve", lambda e: e.tensor_copy(SU, suf), reads=["suf"], writes=["SU"])
    Dv(lambda e: e.tensor_reduce(out=mx, in_=lg[:, :, 0:4], axis=AX.X, op=ALU.max))
    Dv(lambda e: e.tensor_tensor(oh, lg[:, :, 0:4], bc4(mx), op=ALU.is_ge))
    Dv(lambda e: e.tensor_tensor(g4, lg[:, :, 0:4], bc4(mx), op=ALU.subtract))
    P.op("act", lambda e: e.activation(out=g4, in_=g4, func=AF.Exp), reads=R, writes=R)
    Dv(lambda e: e.tensor_reduce(out=gs_, in_=g4, axis=AX.X, op=ALU.add))
    Dv(lambda e: e.reciprocal(gs_, gs_))
    lev = le.rearrange("p t (g x) -> p t g x", g=4)
    Dv(lambda e: e.tensor_tensor(lev, lg[:, :, 4:36].rearrange("p t (g x) -> p t g x", g=4), oh.unsqueeze(3).to_broadcast([128, NT, 4, 8]), op=ALU.mult))
    Dv(lambda e: e.tensor_scalar(o1.rearrange("p t (g x) -> p t g x", g=4), oh.unsqueeze(3).to_broadcast([128, NT, 4, 8]), -1.0, 1e30, op0=ALU.add, op1=ALU.mult))
    Dv(lambda e: e.tensor_tensor(le, le, o1, op=ALU.add))
    Dv(lambda e: e.tensor_reduce(out=mA, in_=le, axis=AX.X, op=ALU.max))
    Dv(lambda e: e.tensor_tensor(o1, le, bc32(mA), op=ALU.is_ge))
    Dv(lambda e: e.scalar_tensor_tensor(out=le, in0=o1, scalar=-1e30, in1=le, op0=ALU.mult, op1=ALU.add))
    Dv(lambda e: e.tensor_reduce(out=mB, in_=le, axis=AX.X, op=ALU.max))
    Dv(lambda e: e.tensor_tensor(o2, le, bc32(mB), op=ALU.is_ge))
    Dv(lambda e: e.tensor_tensor(w1, mB, mA, op=ALU.subtract))
    P.op("act", lambda e: e.activation(out=w1, in_=w1, func=AF.Exp), reads=R, writes=R)
    Dv(lambda e: e.tensor_scalar(w1, w1, 1.0, None, op0=ALU.add))
    Dv(lambda e: e.reciprocal(w1, w1))
    Dv(lambda e: e.tensor_scalar(w2, w1, -1.0, 1.0, op0=ALU.mult, op1=ALU.add))
    Dv(lambda e: e.tensor_tensor(w1, w1, gs_, op=ALU.mult))
    Dv(lambda e: e.tensor_tensor(w2, w2, gs_, op=ALU.mult))
    Dv(lambda e: e.tensor_tensor(tmp, o1, o2, op=ALU.add))
    P.op("dve", lambda e: e.tensor_copy(indb, tmp.rearrange("p t c -> p (t c)")), reads=R, writes=["indb"])
    for c in range(2):
        P.op("pe", lambda e, c=c: e.matmul(ps[c][:, :], lhsT=SU, rhs=indb[:, c * 512:(c + 1) * 512], start=True, stop=True), reads=["SU", "indb"], writes=["ps%d" % c])
        P.op("pe", lambda e, c=c: e.matmul(ps[2 + c][:, :], lhsT=k.ones_b, rhs=indb[:, c * 512:(c + 1) * 512], start=True, stop=True), reads=["ones_b", "indb"], writes=["ps%d" % (2 + c)])
    for c in range(2):
        P.op("dve", lambda e, c=c: e.tensor_copy(pos.rearrange("p t c -> p (t c)")[:, c * 512:(c + 1) * 512], ps[c][:, :]), reads=["ps%d" % c] + R, writes=R)
        P.op("dve", lambda e, c=c: e.tensor_copy(tmp.rearrange("p t c -> p (t c)")[:, c * 512:(c + 1) * 512], ps[2 + c][:, :]), reads=["ps%d" % (2 + c)] + R, writes=R)
    Dv(lambda e: e.memset(offs[:, 0, :], 0.0))
    for t in range(1, NT):
        Dv(lambda e, t=t: e.tensor_tensor(offs[:, t, :], offs[:, t - 1, :], tmp[:, t - 1, :], op=ALU.add))
    Dv(lambda e: e.tensor_tensor(pos, pos, offs, op=ALU.add))
    Dv(lambda e: e.tensor_scalar(tmp, pos, float(CAP), None, op0=ALU.is_lt))
    Dv(lambda e: e.tensor_tensor(pos, pos, k.ecap.unsqueeze(1).to_broadcast([128, NT, 32]), op=ALU.add))
    Dv(lambda e: e.tensor_tensor(pos, pos, tmp, op=ALU.mult))
    Dv(lambda e: e.tensor_scalar(offs, tmp, -1.0, k.trash[:, 0:1], op0=ALU.add, op1=ALU.mult))
    Dv(lambda e: e.tensor_tensor(pos, pos, offs, op=ALU.add))
    Dv(lambda e: e.tensor_tensor(offs, o1, pos, op=ALU.mult))
    Dv(lambda e: e.tensor_reduce(out=v1, in_=offs, axis=AX.X, op=ALU.add))
    P.op("dve", lambda e: e.tensor_copy(idx1, v1), reads=R, writes=R)
    Dv(lambda e: e.tensor_tensor(offs, o2, pos, op=ALU.mult))
    Dv(lambda e: e.tensor_reduce(out=v2, in_=offs, axis=AX.X, op=ALU.add))
    P.op("dve", lambda e: e.tensor_copy(idx2, v2), reads=R, writes=R)
    Dv(lambda e: e.tensor_tensor(offs, o1, tmp, op=ALU.mult))
    Dv(lambda e: e.tensor_reduce(out=v1, in_=offs, axis=AX.X, op=ALU.add))
    Dv(lambda e: e.tensor_tensor(g1, w1, v1, op=ALU.mult))
    Dv(lambda e: e.tensor_tensor(offs, o2, tmp, op=ALU.mult))
    Dv(lambda e: e.tensor_reduce(out=v2, in_=offs, axis=AX.X, op=ALU.add))
    Dv(lambda e: e.tensor_tensor(g2, w2, v2, op=ALU.mult))
    P.op("pool", lambda e: e.memset(k.xb[0], 0.0), writes=["xb0"])
    P.dma("sp", lambda e: e.dma_start(out=Ybuf[NSL:NSL + 128, :], in_=k.xb[0]), reads=["xb0"], key="xb0")
    if l == 0:
        zb = k.xb[0].bitcast(BF16)[:, 0:1024]
        nrt = (NSL + 128) // 128
        Xv = Xbuf.rearrange("(n p) c -> p n c", p=128)
        for n0 in range(0, nrt, 16):
            nn = min(16, nrt - n0)
            P.dma("sp", lambda e, n0=n0, nn=nn: e.dma_start(out=Xv[:, n0:n0 + nn, :], in_=zb.unsqueeze(1).to_broadcast([128, nn, 1024])), reads=["xb0"], writes=["XbufZ"], key="xbz")
    if "dbg_idx" in k.debug:
        dbi = dram(k, "dbg_idx", [128, 2 * NT], I32)
        P.dma("sp", lambda e: e.dma_start(out=dbi[:, 0:NT], in_=idx1), reads=R, key="dbgi")
        P.dma("sp", lambda e: e.dma_start(out=dbi[:, NT:2 * NT], in_=idx2), reads=R, key="dbgi")
        dbg_ = dram(k, "dbg_g", [128, 2 * NT], F32)
        P.dma("sp", lambda e: e.dma_start(out=dbg_, in_=g12), reads=R, key="dbgg")
    for t in range(NT):
        for (ix, nm) in ((idx1, "a"), (idx2, "b")):
            P.dma("pool", lambda e, t=t, ix=ix: e.indirect_dma_start(out=Xbuf, out_offset=bass.IndirectOffsetOnAxis(ap=ix[:, t:t + 1], axis=0), in_=h2tm[:, t, :], in_offset=None),
                  reads=["h2tm", "XbufZ"] + R, writes=[], key="sc%s%d" % (nm, t % 4))
    P.barrier()
    A.release(mH)
    wg = [A.alloc(8 * 512, BF16).rearrange("p (k c) -> p k c", k=8) for _ in range(2)]
    wu = [A.alloc(8 * 512, BF16).rearrange("p (k c) -> p k c", k=8) for _ in range(2)]
    wd = [A.alloc(4 * 1024, BF16).rearrange("p (k c) -> p k c", k=4) for _ in range(2)]
    XT = [A.alloc(8 * CAP, BF16).rearrange("p (k s) -> p k s", k=8) for _ in range(2)]
    xr = [A.alloc(1024, BF16) for _ in range(3)]
    hid = [A.alloc(4 * CAP, BF16).rearrange("p (k s) -> p k s", k=4) for _ in range(2)]
    sg = [A.alloc(512, F32) for _ in range(2)]
    yrow = [A.alloc(1024, F32) for _ in range(3)]
    NST = CAP // 128
    if "dbg_X0" in k.debug:
        dx = dram(k, "dbg_X0", [128, 1024], BF16)
        P.dma("sp", lambda e: e.dma_start(out=xr[2], in_=Xbuf[0:128, :]), writes=["xr2"], key="xr2")
        P.dma("sp", lambda e: e.dma_start(out=dx, in_=xr[2]), reads=["xr2"], key="dbgx0")
    chunks = [(0, 512), (512, CAP - 512)] if CAP > 512 else [(0, CAP)]
    cnts = {"xc": 0, "cc": 0, "yc": 0, "scn": 0}
    NEXP = 32

    def Wload(ex):
        i2 = ex % 2
        P.dma("pool", lambda e: e.dma_start(out=wg[i2], in_=I["w_gate"][l, ex].rearrange("(k p) c -> p k c", p=128)), writes=["wg%d" % i2], key="wg%d" % i2)
        P.dma("pool", lambda e: e.dma_start(out=wu[i2], in_=I["w_up"][l, ex].rearrange("(k p) c -> p k c", p=128)), writes=["wu%d" % i2], key="wu%d" % i2)
        P.dma("pool", lambda e: e.dma_start(out=wd[i2], in_=I["w_down"][l, ex].rearrange("(k p) c -> p k c", p=128)), writes=["wd%d" % i2], key="wd%d" % i2)

    def Tphase(ex):
        i2 = ex % 2
        xt = XT[i2]; xtk = "XT%d" % i2
        for st in range(NST):
            xc = cnts["xc"]
            r = xr[xc % 3]; rk = "xr%d" % (xc % 3)
            P.dma("sp", lambda e, r=r, st=st: e.dma_start(out=r, in_=Xbuf[ex * CAP + st * 128:ex * CAP + (st + 1) * 128, :]), writes=[rk], key=rk)
            bank = 6 + xc % 2
            tb = ps[bank][:, :].bitcast(BF16)
            for kc in range(8):
                P.op("pe", lambda e, kc=kc, r=r, tb=tb: e.transpose(tb[:, kc * 128:(kc + 1) * 128], r[:, kc * 128:(kc + 1) * 128], k.identb), reads=[rk, "identb"], writes=["ps%d" % bank])
            if xc % 2 == 0:
                P.op("act", lambda e, tb=tb, st=st: e.activation(out=xt[:, :, st * 128:(st + 1) * 128], in_=tb.rearrange("p (k s) -> p k s", k=8), func=AF.Copy), reads=["ps%d" % bank], writes=[xtk])
            else:
                P.op("dve", lambda e, tb=tb, st=st: e.tensor_copy(xt[:, :, st * 128:(st + 1) * 128], tb.rearrange("p (k s) -> p k s", k=8)), reads=["ps%d" % bank], writes=[xtk])
            cnts["xc"] += 1

    def Hphase(ex):
        i2 = ex % 2
        xt = XT[i2]; xtk = "XT%d" % i2
        hd = hid[i2]; hk = "hid%d" % i2
        for hb in range(4):
            for (c0, cn) in chunks:
                cc = cnts["cc"]; scn = cnts["scn"]
                bg = (cc % 2) * 2; bu = bg + 1
                for kc in range(8):
                    P.op("pe", lambda e, kc=kc, hb=hb, bg=bg, c0=c0, cn=cn: e.matmul(ps[bg][:, 0:cn], lhsT=wg[i2][:, kc, hb * 128:(hb + 1) * 128], rhs=xt[:, kc, c0:c0 + cn], start=(kc == 0), stop=(kc == 7)),
                         reads=["wg%d" % i2, xtk], writes=["ps%d" % bg])
                for kc in range(8):
                    P.op("pe", lambda e, kc=kc, hb=hb, bu=bu, c0=c0, cn=cn: e.matmul(ps[bu][:, 0:cn], lhsT=wu[i2][:, kc, hb * 128:(hb + 1) * 128], rhs=xt[:, kc, c0:c0 + cn], start=(kc == 0), stop=(kc == 7)),
                         reads=["wu%d" % i2, xtk], writes=["ps%d" % bu])
                s_ = sg[scn % 2]; sk = "sgF%d" % (scn % 2)
                P.op("act", lambda e, s_=s_, bg=bg, cn=cn: e.activation(out=s_[:, 0:cn], in_=ps[bg][:, 0:cn], func=AF.Silu), reads=["ps%d" % bg], writes=[sk])
                P.op("dve", lambda e, s_=s_, bu=bu, hb=hb, c0=c0, cn=cn: e.tensor_tensor(hd[:, hb, c0:c0 + cn], s_[:, 0:cn], ps[bu][:, 0:cn], op=ALU.mult), reads=[sk, "ps%d" % bu], writes=[hk])
                cnts["scn"] += 1; cnts["cc"] += 1

    def Dphase(ex):
        i2 = ex % 2
        hd = hid[i2]; hk = "hid%d" % i2
        for st in range(NST):
            yc = cnts["yc"]
            y = yrow[yc % 3]; yk = "yrow%d" % (yc % 3)
            for half in range(2):
                bank = 4 + half
                for kc in range(4):
                    P.op("pe", lambda e, kc=kc, half=half, bank=bank, st=st: e.matmul(ps[bank][:, :], lhsT=hd[:, kc, st * 128:(st + 1) * 128], rhs=wd[i2][:, kc, half * 512:(half + 1) * 512], start=(kc == 0), stop=(kc == 3)),
                         reads=[hk, "wd%d" % i2], writes=["ps%d" % bank])
                if half == 0:
                    P.op("act", lambda e, y=y, bank=bank: e.activation(out=y[:, 0:512], in_=ps[bank][:, :], func=AF.Copy), reads=["ps%d" % bank], writes=[yk])
                else:
                    P.op("dve", lambda e, y=y, bank=bank: e.tensor_copy(y[:, 512:1024], ps[bank][:, :]), reads=["ps%d" % bank], writes=[yk])
            P.dma("sp", lambda e, y=y, st=st: e.dma_start(out=Ybuf[ex * CAP + st * 128:ex * CAP + (st + 1) * 128, :], in_=y), reads=[yk], key=yk)
            cnts["yc"] += 1

    Wload(0)
    Tphase(0)
    for ex in range(NEXP):
        if ex + 1 < NEXP:
            Wload(ex + 1)
        Hphase(ex)
        if ex + 1 < NEXP:
            Tphase(ex + 1)
        Dphase(ex)
    P.barrier()
    A.release(mH)
    Y1 = [A.alloc(1024, F32) for _ in range(2)]
    Y2 = [A.alloc(1024, F32) for _ in range(2)]
    for t in range(NT):
        b = t % 2
        x = k.xb[b]; xk = "xb%d" % b
        P.dma("sp", lambda e, x=x, t=t: e.dma_start(out=x, in_=xsrc[t * 128:(t + 1) * 128, :]), writes=[xk], key=xk)
        P.dma("pool", lambda e, t=t, b=b: e.indirect_dma_start(out=Y1[b], out_offset=None, in_=Ybuf, in_offset=bass.IndirectOffsetOnAxis(ap=idx1[:, t:t + 1], axis=0)), reads=R, writes=["Y1%d" % b], key="Y1%d" % b)
        P.dma("pool", lambda e, t=t, b=b: e.indirect_dma_start(out=Y2[b], out_offset=None, in_=Ybuf, in_offset=bass.IndirectOffsetOnAxis(ap=idx2[:, t:t + 1], axis=0)), reads=R, writes=["Y2%d" % b], key="Y2%d" % b)
        P.op("act", lambda e, t=t, b=b: e.activation(out=Y1[b], in_=Y1[b], func=AF.Copy, scale=g1[:, t:t + 1]), reads=["Y1%d" % b] + R, writes=["Y1%d" % b])
        P.op("dve", lambda e, t=t, b=b: e.scalar_tensor_tensor(out=Y1[b], in0=Y2[b], scalar=g2[:, t:t + 1], in1=Y1[b], op0=ALU.mult, op1=ALU.add), reads=["Y1%d" % b, "Y2%d" % b] + R, writes=["Y1%d" % b])
        P.op("dve", lambda e, b=b: e.tensor_tensor(Y1[b], Y1[b], k.gtbc[1], op=ALU.mult), reads=["Y1%d" % b, "gtbc1"], writes=["Y1%d" % b])
        P.op("dve", lambda e, x=x, b=b: e.tensor_tensor(x, x, Y1[b], op=ALU.add), reads=["Y1%d" % b, xk], writes=[xk])
        P.dma("sp", lambda e, x=x, t=t: e.dma_start(out=xdst[t * 128:(t + 1) * 128, :], in_=x), reads=[xk], key=xk)
    P.barrier()
    A.release(m0)


from concourse.bass_utils import run_bass_kernel_spmd

_WNAMES = ["w_mod", "b_mod", "g_norm1", "g_norm2", "w_in", "g_cq", "g_ckv", "g_kidx", "w_q_up", "w_idx_q", "w_v_up",
           "lb_logits", "g_rec", "w_branch_a", "w_branch_r", "w_out", "w_grp", "b_grp", "w_exp_router", "b_exp_router",
           "w_gate", "w_up", "w_down", "g_final"]


def _build(shapes, depth=4):
    nc = bass.Bass("TRN2", target_bir_lowering=False)
    es = ExitStack()
    with es:
        I = {}
        I["x"] = nc.dram_tensor("x", [S, D], F32, kind="ExternalInput").ap()
        I["c"] = nc.dram_tensor("c", [1, D], F32, kind="ExternalInput").ap()
        for n in _WNAMES:
            I[n] = nc.dram_tensor(n, list(shapes[n]), F32, kind="ExternalInput").ap()
        out = nc.dram_tensor("out", [S, D], F32, kind="ExternalOutput").ap()
        k = mkctx(nc, es)
        setup_consts(k)
        pm = k.A.mark()
        xa = dram(k, "xres_a", [S, D], F32)
        xb = dram(k, "xres_b", [S, D], F32)
        xcur = I["x"]
        for l in range(depth):
            k.A.release(pm)
            phase0(k, l, I)
            m_after0 = k.A.mark()
            phaseA(k, l, I, xcur)
            phaseB(k, l, I)
            phaseC(k, l, I)
            k.A.release(m_after0)
            phaseD(k, l, I)
            phaseE(k, l, I, xcur, xa)
            phaseF2(k, l, I, xa, xb)
            xcur = xb
        phaseG(k, I, xcur, out)
        k.P.finish(k.A.alloc(2, F32))
        k.P.emit(es)
    return nc


def kernel(**inputs):
    x = np.ascontiguousarray(inputs["x"], dtype=np.float32)
    c = np.ascontiguousarray(inputs["c"], dtype=np.float32)
    shapes = {n: inputs[n].shape for n in _WNAMES}
    nc = _build(shapes)
    w = {n: np.ascontiguousarray(inputs[n], dtype=np.float32) for n in _WNAMES}
    in_maps = []
    for b in range(8):
        m = {"x": x[b], "c": c[b:b + 1]}
        m.update(w)
        in_maps.append(m)
    res = run_bass_kernel_spmd(nc, in_maps, core_ids=list(range(8)))
    return np.stack([np.asarray(r["out"], dtype=np.float32) for r in res.results], axis=0)
```

```python
import numpy as np
import concourse.bass as bass
import concourse.mybir as mybir
from contextlib import ExitStack

F32 = mybir.dt.float32
BF16 = mybir.dt.bfloat16
I32 = mybir.dt.int32
AF = mybir.ActivationFunctionType
ALU = mybir.AluOpType
AX = mybir.AxisListType


class Op:
    __slots__ = ("eng", "fn", "deps", "inc", "cnt", "dma", "key", "consumed", "idx")

    def __init__(self, eng, fn, dma=False, key=None):
        self.eng = eng
        self.fn = fn
        self.deps = []
        self.inc = dma
        self.cnt = 0
        self.dma = dma
        self.key = key
        self.consumed = False


class Prog:
    ENGS = ("pe", "act", "dve", "pool", "sp")

    def __init__(self, nc):
        self.nc = nc
        self.ops = {e: [] for e in self.ENGS}
        self.last_w = {}
        self.readers = {}
        self.since_barrier = []
        self.pending_barrier = {e: [] for e in self.ENGS}
        self.nops = 0

    def _add(self, op, reads, writes):
        deps = []
        seen = set()
        for k in list(reads) + list(writes):
            w = self.last_w.get(k)
            if w is not None and id(w) not in seen:
                seen.add(id(w)); deps.append(w)
        for k in writes:
            for r in self.readers.get(k, ()):
                if id(r) not in seen:
                    seen.add(id(r)); deps.append(r)
        pb = self.pending_barrier[op.eng]
        if pb:
            for d in pb:
                if id(d) not in seen:
                    seen.add(id(d)); deps.append(d)
            self.pending_barrier[op.eng] = []
        op.deps = [d for d in deps if d is not op]
        for d in op.deps:
            d.consumed = True
        for k in writes:
            self.last_w[k] = op
            self.readers[k] = []
        for k in reads:
            if k not in writes:
                self.readers.setdefault(k, []).append(op)
        self.ops[op.eng].append(op)
        self.since_barrier.append(op)
        self.nops += 1
        return op

    def op(self, eng, fn, reads=(), writes=()):
        return self._add(Op(eng, fn), reads, writes)

    def dma(self, eng, fn, reads=(), writes=(), key=None):
        assert key is not None
        return self._add(Op(eng, fn, dma=True, key=key), reads, writes)

    def barrier(self):
        lst = []
        last = {}
        for o in self.since_barrier:
            if o.dma:
                if not o.consumed:
                    lst.append(o)
            else:
                last[o.eng] = o
        lst.extend(last.values())
        for e in self.ENGS:
            self.pending_barrier[e] = self.pending_barrier[e] + lst
        self.since_barrier = []

    def finish(self, scratch):
        self.barrier()
        self.op("pool", lambda e: e.memset(scratch, 0.0), writes=["__fin"])

    def emit(self, es):
        nc = self.nc
        for e in self.ENGS:
            for o in self.ops[e]:
                for d in o.deps:
                    if d.dma:
                        continue
                    if d.eng == "pe" and o.eng == "pe" and not o.dma:
                        continue
                    d.inc = True
        esem = {}
        for e in self.ENGS:
            esem[e] = es.enter_context(nc.semaphore("s_" + e))
        dsem = {}
        dcnt = {}
        for e in self.ENGS:
            c = 0
            for o in self.ops[e]:
                if o.dma:
                    if o.key not in dsem:
                        dsem[o.key] = es.enter_context(nc.semaphore("d_%d" % len(dsem)))
                        dcnt[o.key] = 0
                    dcnt[o.key] += 16
                    o.cnt = dcnt[o.key]
                elif o.inc:
                    c += 1
                    o.cnt = c
        self.n_dsem = len(dsem)
        block = es.enter_context(nc.Block())

        def run(ename, h):
            waited = {}
            for o in self.ops[ename]:
                need = {}
                for d in o.deps:
                    if d.dma:
                        s = dsem[d.key]
                    else:
                        if d.eng == "pe" and ename == "pe" and not o.dma:
                            continue
                        s = esem[d.eng]
                    sid = id(s)
                    if need.get(sid, (None, 0))[1] < d.cnt:
                        need[sid] = (s, d.cnt)
                for sid, (s, v) in need.items():
                    if waited.get(sid, 0) < v:
                        h.wait_ge(s, v)
                        waited[sid] = v
                ins = o.fn(h)
                if o.dma:
                    ins.then_inc(dsem[o.key], 16)
                elif o.inc:
                    ins.then_inc(esem[ename], 1)

        @block.tensor
        def _(h):
            run("pe", h)

        @block.scalar
        def _(h):
            run("act", h)

        @block.vector
        def _(h):
            run("dve", h)

        @block.gpsimd
        def _(h):
            run("pool", h)

        @block.sync
        def _(h):
            run("sp", h)


class Arena:
    def __init__(self, nc, es, ncols, name="arena"):
        self.t = es.enter_context(nc.sbuf_tensor(name, [128, ncols], F32))
        self.ncols = ncols
        self.off = 0
        self.uid = 0

    def mark(self):
        return self.off

    def release(self, m):
        self.off = m

    def alloc(self, cols, dtype=F32, parts=128):
        if dtype == BF16:
            w = (cols + 1) // 2
        else:
            w = cols
        assert self.off + w <= self.ncols, ("arena overflow", self.off, w, self.ncols)
        v = self.t[0:parts, self.off:self.off + w]
        self.off += w
        if dtype != F32:
            v = v.bitcast(dtype)
            if dtype == BF16 and cols % 2:
                v = v[:, 0:cols]
        return v


S = 4096
D = 1024
NT = S // 128
DIN = 4552
EPS = 1e-6
O_CQ, O_CKV, O_KIDX, O_WIDX, O_QREC, O_FREC, O_IREC, O_OG, O_GA, O_GR = 0, 256, 384, 448, 456, 968, 1480, 1992, 2504, 3528


class K:
    pass


def mkctx(nc, es, debug=()):
    k = K()
    k.nc = nc
    k.es = es
    k.P = Prog(nc)
    k.A = Arena(nc, es, 52800)
    k.ps = [es.enter_context(nc.psum_tensor("bank%d" % i, [128, 512], F32)) for i in range(8)]
    k.debug = set(debug)
    k.dr = {}
    k.uid = 0
    return k


def dram(k, name, shape, dtype):
    if name in k.dr:
        return k.dr[name]
    kind = "ExternalOutput" if name in k.debug else "Internal"
    t = k.nc.dram_tensor(name, list(shape), dtype, kind=kind).ap()
    k.dr[name] = t
    return t


def setup_consts(k):
    A, P = k.A, k.P
    k.ident = A.alloc(128, F32)
    k.identb = A.alloc(128, BF16)
    k.ones_f = A.alloc(128, F32)
    k.ones_b = A.alloc(128, BF16)
    P.op("pool", lambda e: e.memset(k.ident, 0.0), writes=["ident"])
    P.op("pool", lambda e: e.affine_select(out=k.ident, in_=k.ident, pattern=[[-1, 128]], compare_op=ALU.not_equal,
                                           fill=1.0, base=0, channel_multiplier=1), reads=["ident"], writes=["ident"])
    P.op("dve", lambda e: e.tensor_copy(k.identb, k.ident), reads=["ident"], writes=["identb"])
    P.op("dve", lambda e: e.memset(k.ones_f, 1.0), writes=["ones_f"])
    k.cneg = A.alloc(128, F32)
    P.op("pool", lambda e: e.memset(k.cneg, 0.0), writes=["cneg"])
    P.op("pool", lambda e: e.affine_select(out=k.cneg, in_=k.cneg, pattern=[[-1, 128]], compare_op=ALU.is_ge,
                                           fill=-1e30, base=0, channel_multiplier=1), reads=["cneg"], writes=["cneg"])
    k.ecap = A.alloc(32, F32)
    for e_ in range(32):
        P.op("dve", lambda e, e_=e_: e.memset(k.ecap[:, e_:e_ + 1], float(e_ * 768)), writes=["ecap"])
    k.cneg_u = A.alloc(128, F32)
    P.op("pool", lambda e: e.memset(k.cneg_u, 1.0), writes=["cneg_u"])
    P.op("pool", lambda e: e.affine_select(out=k.cneg_u, in_=k.cneg_u, pattern=[[1, 128]], compare_op=ALU.is_gt, fill=0.0, base=0, channel_multiplier=-1), reads=["cneg_u"], writes=["cneg_u"])
    k.trash = A.alloc(2, F32)
    P.op("dve", lambda e: e.tensor_reduce(out=k.trash[:, 0:1], in_=k.cneg_u, axis=AX.X, op=ALU.add), reads=["cneg_u"], writes=["trash"])
    P.op("dve", lambda e: e.tensor_scalar(k.trash[:, 0:1], k.trash[:, 0:1], 1.0, -(32.0 * 768 + 127.0), op0=ALU.mult, op1=ALU.add), reads=["trash"], writes=["trash"])
    k.cmf = A.alloc(128, F32)
    P.op("pool", lambda e: e.memset(k.cmf, 1.0), writes=["cmf"])
    P.op("pool", lambda e: e.affine_select(out=k.cmf, in_=k.cmf, pattern=[[1, 128]], compare_op=ALU.is_ge, fill=0.0, base=0, channel_multiplier=-1), reads=["cmf"], writes=["cmf"])
    P.op("pool", lambda e: e.memset(k.cmf[0:64, 64:128], 0.0), reads=["cmf"], writes=["cmf"])
    P.op("dve", lambda e: e.memset(k.ones_b, 1.0), writes=["ones_b"])


def phase0(k, l, I):
    A, P, nc = k.A, k.P, k.nc
    ps = k.ps
    stg = A.alloc(128, F32)
    k.par = A.alloc(128, F32)
    par = k.par
    P.op("dve", lambda e: e.memset(stg, 0.0), writes=["stg"])
    rows = [
        (0, 48, I["b_mod"][l].rearrange("(r c) -> r c", c=128), 128),
        (48, 8, I["c"][0].rearrange("(r c) -> r c", c=128), 128),
        (56, 8, I["g_norm1"][l].rearrange("(r c) -> r c", c=128), 128),
        (64, 8, I["g_norm2"][l].rearrange("(r c) -> r c", c=128), 128),
        (72, 2, I["g_cq"][l].rearrange("(r c) -> r c", c=128), 128),
        (74, 1, I["g_ckv"][l].rearrange("(r c) -> r c", c=128), 128),
        (75, 1, I["g_kidx"][l].rearrange("(r c) -> r c", c=64), 64),
        (76, 16, I["lb_logits"].rearrange("l (r c) -> (l r) c", c=128), 128),
        (92, 8, I["g_final"].rearrange("(r c) -> r c", c=128), 128),
    ]
    for (r0, n, src, w) in rows:
        P.dma("sp", lambda e, r0=r0, n=n, src=src, w=w: e.dma_start(out=stg[r0:r0 + n, 0:w], in_=src),
              reads=[], writes=["stg"], key="p0stg")
    P.dma("sp", lambda e: e.dma_start(out=stg[75:76, 64:128], in_=I["g_kidx"][l].rearrange("(r c) -> r c", c=64)),
          writes=["stg"], key="p0stg")
    P.op("pe", lambda e: e.transpose(ps[0][:, 0:128], stg, k.ident), reads=["stg", "ident"], writes=["ps0"])
    P.op("dve", lambda e: e.tensor_copy(par, ps[0][:, 0:128]), reads=["ps0"], writes=["par"])
    k.g1 = par[:, 56:64]; k.g2 = par[:, 64:72]; k.gcq = par[:, 72:74]; k.gckv = par[:, 74:75]
    k.gkidx = par[:, 75:76]; k.gfin = par[:, 92:100]
    sm = A.alloc(64, F32)
    k.sm = sm
    cact = sm[:, 0:8]
    t1 = sm[:, 8:16]
    P.op("act", lambda e: e.activation(out=t1, in_=par[:, 48:56], func=AF.Exp, scale=-1.0), reads=["par"], writes=["sm"])
    P.op("dve", lambda e: e.tensor_scalar(t1, t1, 1.0, None, op0=ALU.add), reads=["sm"], writes=["sm"])
    P.op("dve", lambda e: e.reciprocal(t1, t1), reads=["sm"], writes=["sm"])
    P.op("dve", lambda e: e.tensor_tensor(cact, par[:, 48:56], t1, op=ALU.mult), reads=["sm", "par"], writes=["sm"])
    lbl = par[:, 76:92].rearrange("p (l c) -> p l c", l=4)
    el = sm[:, 16:32].rearrange("p (l c) -> p l c", l=4)
    mx = sm[:, 32:36]
    P.op("dve", lambda e: e.tensor_reduce(out=mx, in_=par[:, 76:92].rearrange("p (l c) -> p c l", l=4), axis=AX.X, op=ALU.max),
         reads=["par"], writes=["sm"])
    P.op("dve", lambda e: e.tensor_tensor(el, lbl, mx.unsqueeze(1).to_broadcast([128, 4, 4]), op=ALU.subtract),
         reads=["sm", "par"], writes=["sm"])
    P.op("act", lambda e: e.activation(out=sm[:, 16:32], in_=sm[:, 16:32], func=AF.Exp), reads=["sm"], writes=["sm"])
    ssum = sm[:, 36:40]
    P.op("dve", lambda e: e.tensor_reduce(out=ssum, in_=sm[:, 16:32].rearrange("p (l c) -> p c l", l=4), axis=AX.X, op=ALU.add),
         reads=["sm"], writes=["sm"])
    P.op("dve", lambda e: e.reciprocal(ssum, ssum), reads=["sm"], writes=["sm"])
    k.lb = sm[:, 40:44]
    k.oml = sm[:, 44:48]
    P.op("dve", lambda e: e.memset(k.lb, 0.0), reads=["sm"], writes=["sm"])
    for j in range(1, l + 1):
        P.op("dve", lambda e, j=j: e.tensor_tensor(k.lb, k.lb, el[:, j, :], op=ALU.add), reads=["sm"], writes=["sm"])
    P.op("dve", lambda e: e.tensor_tensor(k.lb, k.lb, ssum, op=ALU.mult), reads=["sm"], writes=["sm"])
    P.op("dve", lambda e: e.tensor_scalar(k.lb, k.lb, 0.0, 1.0, op0=ALU.max, op1=ALU.min), reads=["sm"], writes=["sm"])
    P.op("dve", lambda e: e.tensor_scalar(k.oml, k.lb, -1.0, 1.0, op0=ALU.mult, op1=ALU.add), reads=["sm"], writes=["sm"])
    cact_rep = A.alloc(8 * 128, F32).rearrange("p (k m) -> p k m", k=8)
    P.op("dve", lambda e: e.tensor_copy(cact_rep, cact.unsqueeze(2).to_broadcast([128, 8, 128])), reads=["sm"], writes=["crep"])
    k.gtbc = [A.alloc(1024, F32), A.alloc(1024, F32)]
    bmbc = A.alloc(1024, F32)
    modf = k.A.alloc(32, F32)
    gs = k.A.alloc(16, F32)
    m = A.mark()
    wm = [A.alloc(8 * 1024, F32).rearrange("p (k c) -> p k c", k=8) for _ in range(2)]
    groups = [(0, "fm", 0), (1, "fm", 8), (3, "fm", 16), (4, "fm", 24), (2, "tm", 0), (5, "tm", 1)]
    wsrc = I["w_mod"][l].rearrange("(k p) c -> p k c", p=128)
    for gi, (g, kind, o) in enumerate(groups):
        w = wm[gi % 2]
        wk = "wm%d" % (gi % 2)
        P.dma("sp", lambda e, w=w, g=g: e.dma_start(out=w, in_=wsrc[:, :, g * 1024:(g + 1) * 1024]), writes=[wk], key=wk)
        if kind == "fm":
            for cb in range(8):
                for kc in range(8):
                    P.op("pe", lambda e, w=w, cb=cb, kc=kc, o=o: e.matmul(ps[1][:, o + cb:o + cb + 1], lhsT=w[:, kc, cb * 128:(cb + 1) * 128],
                                                                       rhs=cact[:, kc:kc + 1], start=(kc == 0), stop=(kc == 7)),
                         reads=[wk, "sm"], writes=["ps1"])
        else:
            P.dma("sp", lambda e, g=g: e.dma_start(out=bmbc, in_=I["b_mod"][l, g * 1024:(g + 1) * 1024].partition_broadcast(128)),
                  writes=["bmbc"], key="bmbc")
            for nb in range(2):
                pk = "ps%d" % (2 + nb)
                for kc in range(8):
                    P.op("pe", lambda e, w=w, nb=nb, kc=kc: e.matmul(ps[2 + nb][:, :], lhsT=cact_rep[:, kc, :], rhs=w[:, kc, nb * 512:(nb + 1) * 512],
                                                                  start=(kc == 0), stop=(kc == 7)),
                         reads=[wk, "crep"], writes=[pk])
                P.op("dve", lambda e, nb=nb, o=o: e.tensor_tensor(k.gtbc[o][:, nb * 512:(nb + 1) * 512], ps[2 + nb][:, :], bmbc[:, nb * 512:(nb + 1) * 512], op=ALU.add),
                     reads=[pk, "bmbc"], writes=["gtbc%d" % o])
    bsel = [0, 8, 24, 32]
    for i, b0 in enumerate(bsel):
        P.op("dve", lambda e, i=i, b0=b0: e.tensor_tensor(modf[:, i * 8:(i + 1) * 8], ps[1][:, i * 8:(i + 1) * 8], par[:, b0:b0 + 8], op=ALU.add),
             reads=["ps1", "par"], writes=["modf"])
    k.sh1 = modf[:, 0:8]; k.sh2 = modf[:, 16:24]
    k.gs1 = gs[:, 0:8]; k.gs2 = gs[:, 8:16]
    P.op("dve", lambda e: e.scalar_tensor_tensor(out=k.gs1, in0=modf[:, 8:16], scalar=1.0, in1=k.g1, op0=ALU.add, op1=ALU.mult),
         reads=["modf", "par"], writes=["gs"])
    P.op("dve", lambda e: e.scalar_tensor_tensor(out=k.gs2, in0=modf[:, 24:32], scalar=1.0, in1=k.g2, op0=ALU.add, op1=ALU.mult),
         reads=["modf", "par"], writes=["gs"])
    P.barrier()
    A.release(m)


def norm_transpose(k, xsrc, t, hT, gs, sh, tag, hkey):
    A, P, ps = k.A, k.P, k.ps
    xb = k.xb[t % 2]; xk = "xb%d" % (t % 2)
    xn = k.xn[t % 2]; nk = "xn%d" % (t % 2)
    st = k.nst[t % 2]; sk = "nst%d" % (t % 2)
    P.dma("sp", lambda e: e.dma_start(out=xb, in_=xsrc[t * 128:(t + 1) * 128, :]), writes=[xk], key=xk)
    P.op("act", lambda e: e.activation(out=xn, in_=xb, func=AF.Square, accum_out=st[:, 0:1]), reads=[xk], writes=[nk, sk])
    P.op("dve", lambda e: e.tensor_scalar(st[:, 1:2], st[:, 0:1], 1.0 / D, EPS, op0=ALU.mult, op1=ALU.add), reads=[sk], writes=[sk])
    P.op("act", lambda e: e.activation(out=st[:, 1:2], in_=st[:, 1:2], func=AF.Ln), reads=[sk], writes=[sk])
    P.op("act", lambda e: e.activation(out=st[:, 1:2], in_=st[:, 1:2], func=AF.Exp, scale=-0.5), reads=[sk], writes=[sk])
    P.op("dve", lambda e: e.tensor_scalar(xn, xb, st[:, 1:2], None, op0=ALU.mult), reads=[xk, sk], writes=[nk])
    b0 = (t % 2) * 2
    for kc in range(8):
        bank = b0 + kc // 4
        P.op("pe", lambda e, kc=kc, bank=bank: e.transpose(ps[bank][:, (kc % 4) * 128:(kc % 4 + 1) * 128], xn[:, kc * 128:(kc + 1) * 128], k.ident),
             reads=[nk, "ident"], writes=["ps%d" % bank])
    return b0


def phaseA(k, l, I, xsrc):
    A, P, nc, ps = k.A, k.P, k.nc, k.ps
    m0 = A.mark()
    WC = DIN + 64
    w = A.alloc(8 * WC, BF16).rearrange("p (k c) -> p k c", k=8)
    wsrc = I["w_in"][l].rearrange("(k p) c -> p k c", p=128)
    wchunks = [(0, 0, 448), (448, 384, 64), (512, 448, 1024), (1536, 1472, 1024), (2560, 2496, 1024), (3584, 3520, 1032)]
    for ci, (d0, s0, n) in enumerate(wchunks):
        P.dma("pool", lambda e, d0=d0, s0=s0, n=n: e.dma_start(out=w[:, :, d0:d0 + n], in_=wsrc[:, :, s0:s0 + n]), writes=["w_in%d" % ci], key="w_in%d" % ci)

    def wk(c0, n):
        return ["w_in%d" % ci for ci, (d0, _s, nn) in enumerate(wchunks) if d0 < c0 + n and c0 < d0 + nn]
    xbA = [A.alloc(1024, F32) for _ in range(4)]
    xnA = [A.alloc(1024, F32) for _ in range(2)]
    nstA = [A.alloc(8, F32) for _ in range(2)]
    hT = [A.alloc(8 * 512, BF16).rearrange("p (k t) -> p k t", k=8) for _ in range(2)]
    fo = [A.alloc(512, F32) for _ in range(4)]
    gb = [A.alloc(512, BF16) for _ in range(4)]
    to = [A.alloc(1672, F32) for _ in range(2)]
    cq_fm = dram(k, "cq_fm", [256, S], F32)
    ckv_fm = dram(k, "ckv_fm", [128, S], F32)
    kidx_fm = dram(k, "kidx_fm", [128, S], F32)
    qrec_fm = dram(k, "qrec_fm", [512, S], F32)
    frec_fm = dram(k, "frec_fm", [512, S], F32)
    gates_fm = dram(k, "gates_fm", [2048, S], BF16)
    tm_out = dram(k, "tm_out", [S, 1672], F32)
    fmb = [(0, cq_fm, 0, 0), (128, cq_fm, 128, 0), (256, ckv_fm, 0, 0), (384, kidx_fm, 0, 0)]
    for j in range(4):
        fmb.append((O_QREC + 64 + j * 128, qrec_fm, j * 128, 0))
    for j in range(4):
        fmb.append((O_FREC + 64 + j * 128, frec_fm, j * 128, 0))
    for j in range(16):
        fmb.append((O_GA + 64 + j * 128, gates_fm, j * 128, 1))
    tmg = [(256, 128, 0), (O_WIDX + 64, 8, 128), (O_IREC + 64, 512, 136), (O_OG + 64, 512, 648), (O_FREC + 64, 512, 1160)]
    fcount = 0
    def NORM(st):
        h = hT[st % 2]; hk = "hT%d" % (st % 2)
        sA = nstA[st % 2]; sAk = "nstA%d" % (st % 2)
        for tt in range(4):
            t = st * 4 + tt
            xb_ = xbA[tt]; xk_ = "xbA%d" % tt
            P.dma("sp", lambda e, xb_=xb_, t=t: e.dma_start(out=xb_, in_=xsrc[t * 128:(t + 1) * 128, :]), writes=[xk_], key=xk_)
            xn_ = xnA[tt % 2]; nk_ = "xnA%d" % (tt % 2)
            P.op("act", lambda e, xb_=xb_, xn_=xn_, sA=sA, tt=tt: e.activation(out=xn_, in_=xb_, func=AF.Square, accum_out=sA[:, tt:tt + 1]), reads=[xk_], writes=[nk_, sAk])
        P.op("dve", lambda e, sA=sA: e.tensor_scalar(sA[:, 4:8], sA[:, 0:4], 1.0 / D, EPS, op0=ALU.mult, op1=ALU.add), reads=[sAk], writes=[sAk])
        P.op("act", lambda e, sA=sA: e.activation(out=sA[:, 4:8], in_=sA[:, 4:8], func=AF.Ln), reads=[sAk], writes=[sAk])
        P.op("act", lambda e, sA=sA: e.activation(out=sA[:, 4:8], in_=sA[:, 4:8], func=AF.Exp, scale=-0.5), reads=[sAk], writes=[sAk])
        for tt in range(4):
            t = st * 4 + tt
            xb_ = xbA[tt]; xk_ = "xbA%d" % tt
            xn_ = xnA[tt % 2]; nk_ = "xnA%d" % (tt % 2)
            P.op("dve", lambda e, xb_=xb_, xn_=xn_, sA=sA, tt=tt: e.tensor_scalar(xn_, xb_, sA[:, 4 + tt:5 + tt], None, op0=ALU.mult), reads=[xk_, sAk], writes=[nk_])
            b0 = (t % 2) * 2
            for kc in range(8):
                bank = b0 + kc // 4
                P.op("pe", lambda e, kc=kc, bank=bank, xn_=xn_: e.transpose(ps[bank][:, (kc % 4) * 128:(kc % 4 + 1) * 128], xn_[:, kc * 128:(kc + 1) * 128], k.ident),
                     reads=[nk_, "ident"], writes=["ps%d" % bank])
            for kc in range(8):
                bank = b0 + kc // 4
                P.op("act", lambda e, kc=kc, bank=bank, tt=tt, h=h: e.activation(out=h[:, kc, tt * 128:(tt + 1) * 128], in_=ps[bank][:, (kc % 4) * 128:(kc % 4 + 1) * 128],
                                                                                 func=AF.Identity, scale=k.gs1[:, kc:kc + 1], bias=k.sh1[:, kc:kc + 1]),
                     reads=["ps%d" % bank, "gs", "modf"], writes=[hk])
    def MMS(st):
        nonlocal fcount
        h = hT[st % 2]; hk = "hT%d" % (st % 2)
        for bi, (c0, dst, r0, act) in enumerate(fmb):
            bank = 4 + fcount % 4
            pk = "ps%d" % bank
            for kc in range(8):
                P.op("pe", lambda e, kc=kc, c0=c0, bank=bank, h=h: e.matmul(ps[bank][:, :], lhsT=w[:, kc, c0:c0 + 128], rhs=h[:, kc, :], start=(kc == 0), stop=(kc == 7)),
                     reads=wk(c0, 128) + [hk], writes=[pk])
            f = fo[fcount % 4]; fk = "fo%d" % (fcount % 4)
            if act == 0:
                P.op("dve", lambda e, f=f, bank=bank: e.tensor_copy(f, ps[bank][:, :]), reads=[pk], writes=[fk])
                P.dma("sp", lambda e, f=f, dst=dst, r0=r0, st=st: e.dma_start(out=dst[r0:r0 + 128, st * 512:(st + 1) * 512], in_=f), reads=[fk], writes=[], key=fk)
            else:
                fb = gb[fcount % 4]
                P.op("act", lambda e, fb=fb, bank=bank: e.activation(out=fb, in_=ps[bank][:, :], func=AF.Sigmoid), reads=[pk], writes=[fk])
                P.dma("sp", lambda e, fb=fb, dst=dst, r0=r0, st=st: e.dma_start(out=dst[r0:r0 + 128, st * 512:(st + 1) * 512], in_=fb), reads=[fk], writes=[], key=fk)
            fcount += 1
        for tt in range(4):
            t = st * 4 + tt
            tb = to[t % 2]; tk = "to%d" % (t % 2)
            for gi, (c0, n, o0) in enumerate(tmg):
                if gi == 1:
                    continue
                bank = 4 + fcount % 4
                pk = "ps%d" % bank
                subs = [(c0, n, 0)]
                if gi == 0:
                    subs = [(c0, n, 0), (tmg[1][0], 8, 128)]
                for (cc, nn, po) in subs:
                    for kc in range(8):
                        P.op("pe", lambda e, kc=kc, cc=cc, nn=nn, po=po, bank=bank, h=h, tt=tt: e.matmul(ps[bank][:, po:po + nn], lhsT=h[:, kc, tt * 128:(tt + 1) * 128], rhs=w[:, kc, cc:cc + nn],
                                                                                                  start=(kc == 0), stop=(kc == 7)),
                             reads=wk(cc, nn) + [hk], writes=[pk])
                tot = n + (8 if gi == 0 else 0)
                eng = "dve" if gi % 2 == 0 else "act"
                if eng == "dve":
                    P.op("dve", lambda e, tb=tb, o0=o0, tot=tot, bank=bank: e.tensor_copy(tb[:, o0:o0 + tot], ps[bank][:, 0:tot]), reads=[pk], writes=[tk])
                else:
                    P.op("act", lambda e, tb=tb, o0=o0, tot=tot, bank=bank: e.activation(out=tb[:, o0:o0 + tot], in_=ps[bank][:, 0:tot], func=AF.Copy), reads=[pk], writes=[tk])
                fcount += 1
            P.dma("sp", lambda e, tb=tb, t=t: e.dma_start(out=tm_out[t * 128:(t + 1) * 128, :], in_=tb), reads=[tk], writes=[], key=tk)
    NORM(0)
    for st in range(S // 512):
        if st + 1 < S // 512:
            NORM(st + 1)
        MMS(st)
    P.barrier()
    A.release(m0)


ATTN_SCALE = 128 ** -0.5
NEG = -1e30
MASKV = -30000.0


def phaseB(k, l, I):
    A, P, ps = k.A, k.P, k.ps
    cq_fm = k.dr["cq_fm"]; ckv_fm = k.dr["ckv_fm"]; kidx_fm = k.dr["kidx_fm"]; tm_out = k.dr["tm_out"]
    qT_d = dram(k, "qT_d", [NT, 128, 8, 128], BF16)
    qidx_d = dram(k, "qidx_d", [NT, 128, 4, 128], BF16)
    k.kvT = A.alloc(S, BF16)
    k.kidxT = A.alloc(S, BF16)
    k.kvaug = A.alloc(NT * 132, BF16).rearrange("p (t c) -> p t c", t=NT)
    k.widx = A.alloc(NT * 8, F32).rearrange("p (t c) -> p t c", t=NT)
    k.wv = A.alloc(8 * 128, BF16).rearrange("p (h c) -> p h c", h=8)
    m0 = A.mark()
    wq = A.alloc(2 * 1024, BF16).rearrange("p (k c) -> p k c", k=2)
    wi = A.alloc(2 * 512, BF16).rearrange("p (k c) -> p k c", k=2)
    P.dma("pool", lambda e: e.dma_start(out=wq, in_=I["w_q_up"][l].rearrange("(k p) c -> p k c", p=128)), writes=["wq"], key="wq")
    P.dma("pool", lambda e: e.dma_start(out=wi, in_=I["w_idx_q"][l].rearrange("(k p) c -> p k c", p=128)), writes=["wi"], key="wi")
    P.op("dve", lambda e: e.memset(k.wv, 0.0), writes=["wv"])
    for par in range(2):
        P.dma("pool", lambda e, par=par: e.dma_start(out=k.wv.rearrange("p (j two) c -> p j two c", two=2)[:, :, par, par * 64:(par + 1) * 64],
                                                     in_=I["w_v_up"][l].rearrange("(j two) r v -> r j two v", two=2)[:, :, par, :]),
              writes=["wv"], key="wvd")
    ckt = A.alloc(NT * 128, F32).rearrange("p (t c) -> p t c", t=NT)
    sq = A.alloc(NT * 128, F32).rearrange("p (t c) -> p t c", t=NT)
    gb = A.alloc(128, F32)
    st = A.alloc(64, F32)
    P.dma("sp", lambda e: e.dma_start(out=ckt, in_=tm_out[:, 0:128].rearrange("(t p) c -> p t c", p=128)), writes=["ckt"], key="ckt")
    P.dma("sp", lambda e: e.dma_start(out=k.widx, in_=tm_out[:, 128:136].rearrange("(t p) c -> p t c", p=128)), writes=["widx"], key="widx")
    P.dma("sp", lambda e: e.dma_start(out=gb, in_=I["g_ckv"][l].partition_broadcast(128)), writes=["gb"], key="gb")
    P.op("dve", lambda e: e.tensor_tensor(sq, ckt, ckt, op=ALU.mult), reads=["ckt"], writes=["sq"])
    P.op("dve", lambda e: e.tensor_reduce(out=st[:, 0:32], in_=sq, axis=AX.X, op=ALU.add), reads=["sq"], writes=["stB"])
    P.op("dve", lambda e: e.tensor_scalar(st[:, 0:32], st[:, 0:32], 1.0 / 128, EPS, op0=ALU.mult, op1=ALU.add), reads=["stB"], writes=["stB"])
    P.op("act", lambda e: e.activation(out=st[:, 0:32], in_=st[:, 0:32], func=AF.Ln), reads=["stB"], writes=["stB"])
    P.op("act", lambda e: e.activation(out=st[:, 0:32], in_=st[:, 0:32], func=AF.Exp, scale=-0.5), reads=["stB"], writes=["stB"])
    P.op("dve", lambda e: e.tensor_tensor(sq, ckt, st[:, 0:32].unsqueeze(2).to_broadcast([128, NT, 128]), op=ALU.mult), reads=["ckt", "stB", "sq"], writes=["sq"])
    P.op("dve", lambda e: e.tensor_tensor(k.kvaug[:, :, 0:128], sq, gb.unsqueeze(1).to_broadcast([128, NT, 128]), op=ALU.mult), reads=["sq", "gb"], writes=["kvaug"])
    P.op("dve", lambda e: e.memset(k.kvaug[:, :, 128:132], 1.0), writes=["kvaug"])
    xin = [A.alloc(4 * 512, F32).rearrange("p (c t) -> p c t", c=4) for _ in range(2)]
    sqb = A.alloc(4 * 512, BF16).rearrange("p (c t) -> p c t", c=4)
    rs = A.alloc(3 * 512, F32).rearrange("p (c t) -> p c t", c=3)
    cqn2 = [A.alloc(2 * 512, BF16).rearrange("p (c t) -> p c t", c=2) for _ in range(2)]
    ob = [A.alloc(512, BF16) for _ in range(4)]
    oc = 0
    def NORMB(b):
        cqn = cqn2[b % 2]; cqk = "cqn%d" % (b % 2)
        x = xin[b % 2]; xk = "xinB%d" % (b % 2)
        sl = slice(b * 512, (b + 1) * 512)
        P.dma("sp", lambda e, x=x, sl=sl: e.dma_start(out=x[:, 0:2, :], in_=cq_fm[:, sl].rearrange("(c p) t -> p c t", p=128)), writes=[xk], key=xk)
        P.dma("sp", lambda e, x=x, sl=sl: e.dma_start(out=x[:, 2, :], in_=ckv_fm[:, sl]), writes=[xk], key=xk)
        P.dma("sp", lambda e, x=x, sl=sl: e.dma_start(out=x[:, 3, :], in_=kidx_fm[:, sl]), writes=[xk], key=xk)
        P.op("act", lambda e, x=x: e.activation(out=sqb, in_=x, func=AF.Square), reads=[xk], writes=["sqb"])
        P.op("pe", lambda e: e.matmul(ps[0][:, :], lhsT=k.ones_b, rhs=sqb[:, 0, :], start=True, stop=False), reads=["sqb", "ones_b"], writes=["ps0"])
        P.op("pe", lambda e: e.matmul(ps[0][:, :], lhsT=k.ones_b, rhs=sqb[:, 1, :], start=False, stop=True), reads=["sqb", "ones_b"], writes=["ps0"])
        P.op("pe", lambda e: e.matmul(ps[1][:, :], lhsT=k.ones_b, rhs=sqb[:, 2, :], start=True, stop=True), reads=["sqb", "ones_b"], writes=["ps1"])
        P.op("pe", lambda e: e.matmul(ps[2][:, :], lhsT=k.ones_b, rhs=sqb[:, 3, :], start=True, stop=True), reads=["sqb", "ones_b"], writes=["ps2"])
        for i, n in enumerate([256.0, 128.0, 128.0]):
            P.op("dve", lambda e, i=i, n=n: e.tensor_scalar(rs[:, i, :], ps[i][:, :], 1.0 / n, EPS, op0=ALU.mult, op1=ALU.add), reads=["ps%d" % i], writes=["rs"])
        P.op("act", lambda e: e.activation(out=rs, in_=rs, func=AF.Ln), reads=["rs"], writes=["rs"])
        P.op("act", lambda e: e.activation(out=rs, in_=rs, func=AF.Exp, scale=-0.5), reads=["rs"], writes=["rs"])
        for c in range(2):
            P.op("dve", lambda e, c=c, x=x: e.scalar_tensor_tensor(out=cqn[:, c, :], in0=x[:, c, :], scalar=k.gcq[:, c:c + 1], in1=rs[:, 0, :], op0=ALU.mult, op1=ALU.mult),
                 reads=[xk, "rs", "par"], writes=[cqk])
        P.op("dve", lambda e, x=x, sl=sl: e.scalar_tensor_tensor(out=k.kvT[:, sl], in0=x[:, 2, :], scalar=k.gckv[:, 0:1], in1=rs[:, 1, :], op0=ALU.mult, op1=ALU.mult),
             reads=[xk, "rs", "par"], writes=["kvT"])
        P.op("dve", lambda e, x=x, sl=sl: e.scalar_tensor_tensor(out=k.kidxT[:, sl], in0=x[:, 3, :], scalar=k.gkidx[:, 0:1], in1=rs[:, 2, :], op0=ALU.mult, op1=ALU.mult),
             reads=[xk, "rs", "par"], writes=["kidxT"])
    def PROJB(b):
        nonlocal oc
        cqn = cqn2[b % 2]; cqk = "cqn%d" % (b % 2)
        for h in range(8):
            bank = 4 + oc % 4; pk = "ps%d" % bank
            for c in range(2):
                P.op("pe", lambda e, h=h, c=c, bank=bank: e.matmul(ps[bank][:, :], lhsT=wq[:, c, h * 128:(h + 1) * 128], rhs=cqn[:, c, :], start=(c == 0), stop=(c == 1)),
                     reads=["wq", cqk], writes=[pk])
            o = ob[oc % 4]; ok = "obB%d" % (oc % 4)
            P.op("act", lambda e, o=o, bank=bank: e.activation(out=o, in_=ps[bank][:, :], func=AF.Copy, scale=ATTN_SCALE), reads=[pk], writes=[ok])
            P.dma("sp", lambda e, o=o, h=h, b=b: e.dma_start(out=qT_d[b * 4:(b + 1) * 4, :, h, :].rearrange("t r q -> r t q"), in_=o.rearrange("p (t q) -> p t q", t=4)),
                  reads=[ok], key=ok)
            oc += 1
        for j in range(4):
            bank = 4 + oc % 4; pk = "ps%d" % bank
            for c in range(2):
                P.op("pe", lambda e, j=j, c=c, bank=bank: e.matmul(ps[bank][:, :], lhsT=wi[:, c, j * 128:(j + 1) * 128], rhs=cqn[:, c, :], start=(c == 0), stop=(c == 1)),
                     reads=["wi", cqk], writes=[pk])
            o = ob[oc % 4]; ok = "obB%d" % (oc % 4)
            P.op("dve", lambda e, o=o, bank=bank: e.tensor_copy(o, ps[bank][:, :]), reads=[pk], writes=[ok])
            P.dma("sp", lambda e, o=o, j=j, b=b: e.dma_start(out=qidx_d[b * 4:(b + 1) * 4, :, j, :].rearrange("t r q -> r t q"), in_=o.rearrange("p (t q) -> p t q", t=4)),
                  reads=[ok], key=ok)
            oc += 1
    NORMB(0)
    for b in range(S // 512):
        if b + 1 < S // 512:
            NORMB(b + 1)
        PROJB(b)
    P.barrier()
    A.release(m0)


def phaseC(k, l, I, NITER=10):
    A, P, ps = k.A, k.P, k.ps
    qT_d = k.dr["qT_d"]; qidx_d = k.dr["qidx_d"]
    yaT_d = dram(k, "yaT_d", [4, 128, S], BF16)
    m0 = A.mark()
    qs = [A.alloc(8 * 128, BF16) for _ in range(4)]
    qi = [A.alloc(4 * 128, BF16).rearrange("p (j q) -> p j q", j=4) for _ in range(2)]
    score = [A.alloc(S, F32) for _ in range(2)]
    mb = [A.alloc(S, BF16) for _ in range(4)]
    junk = [A.alloc(S, BF16) for _ in range(2)]
    rb = [A.alloc(512, F32) for _ in range(3)]
    pT = [A.alloc(512, BF16) for _ in range(4)]
    on = A.alloc(8 * 128, BF16).rearrange("p (h r) -> p h r", h=8)
    onT = A.alloc(8 * 128, BF16).rearrange("p (h q) -> p h q", h=8)
    yo = [A.alloc(4 * 128, BF16).rearrange("p (j q) -> p j q", j=4) for _ in range(2)]
    ident4 = A.alloc(512, BF16)
    bs = [A.alloc(64, F32) for _ in range(2)]
    pw = A.alloc(NITER, F32)
    rden = A.alloc(8, F32)
    cm = A.alloc(2, F32)
    P.op("dve", lambda e: e.memset(cm, MASKV), writes=["cm"])
    for i in range(4):
        P.op("dve", lambda e, i=i: e.tensor_copy(ident4[:, i * 128:(i + 1) * 128], k.identb), reads=["identb"], writes=["ident4"])
    for i in range(NITER):
        P.op("dve", lambda e, i=i: e.memset(pw[:, i:i + 1], 0.5 ** (i + 1)), writes=["pw"])
    def oacc(h):
        return ps[h // 3][:, (h % 3) * 129:(h % 3) * 129 + 129]
    rc = [0]

    def prep(qt):
        b = qt % 2; b4 = qt % 4
        sc = score[b]; sk = "score%d" % b
        nk = (qt + 1) * 128
        P.dma("sp", lambda e: e.dma_start(out=qs[b4], in_=qT_d[qt].rearrange("r h q -> r (h q)")), writes=["qs%d" % b4], key="qs%d" % b4)
        P.dma("sp", lambda e: e.dma_start(out=qi[b], in_=qidx_d[qt]), writes=["qi%d" % b], key="qi%d" % b)
        for c0 in range(0, nk, 512):
            wd = min(512, nk - c0)
            for h in range(8):
                bank = 3 + rc[0] % 2; pk = "ps%d" % bank
                p0 = (h % 2) * 64
                P.op("pe", lambda e, h=h, p0=p0, bank=bank, c0=c0, wd=wd: e.matmul(ps[bank][:, 0:wd], lhsT=qi[b][p0:p0 + 64, h // 2, :], rhs=k.kidxT[p0:p0 + 64, c0:c0 + wd], start=True, stop=True),
                     reads=["qi%d" % b, "kidxT"], writes=[pk])
                r = rb[rc[0] % 3]; rk = "rb%d" % (rc[0] % 3)
                P.op("act", lambda e, r=r, bank=bank, wd=wd: e.activation(out=r[:, 0:wd], in_=ps[bank][:, 0:wd], func=AF.Relu), reads=[pk], writes=[rk])
                if h == 0:
                    P.op("dve", lambda e, r=r, c0=c0, wd=wd, h=h: e.tensor_scalar(sc[:, c0:c0 + wd], r[:, 0:wd], k.widx[:, qt, h:h + 1], None, op0=ALU.mult),
                         reads=[rk, "widx"], writes=[sk])
                else:
                    P.op("dve", lambda e, r=r, c0=c0, wd=wd, h=h: e.scalar_tensor_tensor(out=sc[:, c0:c0 + wd], in0=r[:, 0:wd], scalar=k.widx[:, qt, h:h + 1], in1=sc[:, c0:c0 + wd], op0=ALU.mult, op1=ALU.add),
                         reads=[rk, "widx", sk], writes=[sk])
                rc[0] += 1
        P.op("pool", lambda e: e.tensor_tensor(sc[:, qt * 128:(qt + 1) * 128], sc[:, qt * 128:(qt + 1) * 128], k.cneg, op=ALU.add), reads=[sk, "cneg"], writes=[sk])
        s_ = bs[b]; bk = "bs%d" % b
        lo = s_[:, 0:1]; w0 = s_[:, 1:2]; mid = s_[:, 2:3]; wi_ = s_[:, 8:8 + NITER]
        if qt < 2:
            P.op("dve", lambda e: e.memset(lo, -1e29), writes=[bk])
        else:
            P.op("dve", lambda e: e.tensor_reduce(out=w0, in_=sc[:, 0:nk], axis=AX.X, op=ALU.max), reads=[sk], writes=[bk])
            P.op("dve", lambda e: e.tensor_reduce(out=lo, in_=sc[:, 0:qt * 128], axis=AX.X, op=ALU.min), reads=[sk], writes=[bk])
            P.op("dve", lambda e: e.tensor_scalar(lo, lo, -1.0, None, op0=ALU.add), reads=[bk], writes=[bk])
            P.op("dve", lambda e: e.tensor_tensor(w0, w0, lo, op=ALU.subtract), reads=[bk], writes=[bk])
            P.op("dve", lambda e: e.tensor_scalar(wi_, pw, w0, None, op0=ALU.mult), reads=[bk, "pw"], writes=[bk])
            P.op("dve", lambda e: e.tensor_tensor(mid, lo, wi_[:, 0:1], op=ALU.add), reads=[bk], writes=[bk])

    def bis_iter(qt, it):
        b = qt % 2
        sc = score[b]; sk = "score%d" % b
        nk = (qt + 1) * 128
        s_ = bs[b]; bk = "bs%d" % b
        lo = s_[:, 0:1]; mid = s_[:, 2:3]; cnt = s_[:, 3:4]; tmp = s_[:, 4:5]; wi_ = s_[:, 8:8 + NITER]
        jk = junk[b]; jkk = "junk%d" % b
        P.op("dve", lambda e: e.tensor_scalar(jk[:, 0:nk], sc[:, 0:nk], mid, None, op0=ALU.is_gt, op1=ALU.add, accum_out=cnt), reads=[sk, bk], writes=[bk, jkk])
        P.op("dve", lambda e: e.scalar_tensor_tensor(out=tmp, in0=cnt, scalar=256.0, in1=wi_[:, it:it + 1], op0=ALU.is_ge, op1=ALU.mult), reads=[bk], writes=[bk])
        if it < NITER - 1:
            P.op("dve", lambda e: e.scalar_tensor_tensor(out=mid, in0=mid, scalar=wi_[:, it + 1:it + 2], in1=tmp, op0=ALU.subtract, op1=ALU.add), reads=[bk], writes=[bk])
        else:
            P.op("dve", lambda e: e.scalar_tensor_tensor(out=lo, in0=mid, scalar=wi_[:, it:it + 1], in1=tmp, op0=ALU.subtract, op1=ALU.add), reads=[bk], writes=[bk])

    def fin_mask(qt):
        b = qt % 2; b4 = qt % 4
        nk = (qt + 1) * 128
        lo = bs[b][:, 0:1]
        P.op("dve", lambda e: e.tensor_scalar(mb[b4][:, 0:nk], score[b][:, 0:nk], lo, cm[:, 0:1], op0=ALU.is_le, op1=ALU.mult), reads=["score%d" % b, "bs%d" % b, "cm"], writes=["mb%d" % b4])

    def bis(qts):
        for it in range(NITER):
            for qt in qts:
                if qt >= 2:
                    bis_iter(qt, it)
        for qt in qts:
            fin_mask(qt)

    pc = [0]

    def attention(qt):
        b = qt % 4
        q = qs[b]
        steps = [(kb, hg) for kb in range(qt + 1) for hg in range(2)]
        base = pc[0]

        def qk(i_):
            kb, hg = steps[i_]
            bank = 5 + (base + i_) % 2; pk = "ps%d" % bank
            P.op("pe", lambda e: e.matmul(ps[bank][:, :], lhsT=k.kvT[:, kb * 128:(kb + 1) * 128], rhs=q[:, hg * 512:(hg + 1) * 512], start=True, stop=False),
                 reads=["kvT", "qs%d" % b], writes=[pk])
            P.op("pe", lambda e: e.matmul(ps[bank][:, :], lhsT=mb[b][:, kb * 128:(kb + 1) * 128], rhs=ident4, start=False, stop=True),
                 reads=["mb%d" % b, "ident4"], writes=[pk])

        qk(0)
        for i_, (kb, hg) in enumerate(steps):
            if i_ + 1 < len(steps):
                qk(i_ + 1)
            bank = 5 + (base + i_) % 2; pk = "ps%d" % bank
            p = pT[(base + i_) % 4]; pk2 = "pT%d" % ((base + i_) % 4)
            P.op("act", lambda e, p=p, bank=bank: e.activation(out=p, in_=ps[bank][:, :], func=AF.Exp), reads=[pk], writes=[pk2])
            for hh in range(4):
                h = hg * 4 + hh
                P.op("pe", lambda e, p=p, hh=hh, h=h, kb=kb: e.matmul(oacc(h), lhsT=p[:, hh * 128:(hh + 1) * 128], rhs=k.kvaug[:, kb, 0:129], start=(kb == 0 and h % 3 == 0), stop=(kb == qt), skip_group_check=True),
                     reads=[pk2, "kvaug"], writes=["ps%d" % (h // 3)])
        pc[0] += len(steps)
        for bnk in range(3):
            nh = 3 if bnk < 2 else 2
            v = ps[bnk][:, 0:nh * 129].rearrange("p (h c) -> p h c", c=129)
            P.op("dve", lambda e, v=v, bnk=bnk, nh=nh: e.reciprocal(rden[:, bnk * 3:bnk * 3 + nh], v[:, :, 128]), reads=["ps%d" % bnk], writes=["rden"])
            P.op("dve", lambda e, v=v, bnk=bnk, nh=nh: e.tensor_tensor(on[:, bnk * 3:bnk * 3 + nh, :], v[:, :, 0:128], rden[:, bnk * 3:bnk * 3 + nh].unsqueeze(2).to_broadcast([128, nh, 128]), op=ALU.mult),
                 reads=["ps%d" % bnk, "rden"], writes=["on"])
        tb = ps[7][:, :].bitcast(BF16)
        for h in range(8):
            P.op("pe", lambda e, h=h: e.transpose(tb[:, h * 128:(h + 1) * 128], on[:, h, :], k.identb), reads=["on", "identb"], writes=["ps7"])
        P.op("act", lambda e: e.activation(out=onT.rearrange("p h q -> p (h q)"), in_=tb, func=AF.Copy), reads=["ps7"], writes=["onT"])
        for j in range(4):
            for two in range(2):
                h = j * 2 + two
                P.op("pe", lambda e, j=j, two=two, h=h: e.matmul(ps[7][:, j * 128:(j + 1) * 128], lhsT=k.wv[:, h, :], rhs=onT[:, h, :], start=(two == 0), stop=(two == 1)),
                     reads=["wv", "onT"], writes=["ps7"])
        y = yo[qt % 2]; yk = "yo%d" % (qt % 2)
        P.op("dve", lambda e: e.tensor_copy(y.rearrange("p j q -> p (j q)"), ps[7][:, :]), reads=["ps7"], writes=[yk])
        P.dma("sp", lambda e: e.dma_start(out=yaT_d[:, :, qt * 128:(qt + 1) * 128].rearrange("j p q -> p j q"), in_=y), reads=[yk], key=yk)

    prep(0); bis([0])
    for qt in range(NT):
        if qt + 1 < NT:
            prep(qt + 1); bis([qt + 1])
        attention(qt)
    P.barrier()
    A.release(m0)


def phaseD(k, l, I):
    A, P, ps = k.A, k.P, k.ps
    qrec_fm = k.dr["qrec_fm"]; frec_fm = k.dr["frec_fm"]; tm_out = k.dr["tm_out"]
    yrT_d = dram(k, "yrT_d", [4, 128, S], BF16)
    m0 = A.mark()
    NB = 512
    def fm(dt=F32):
        return A.alloc(4 * NB, dt).rearrange("p (j t) -> p j t", j=4)
    z = fm(); qr = fm(); e = fm(); t1 = fm(); t2 = fm(); Acum = fm(); kk = fm(); qq = fm()
    cmf = k.cmf
    def mkset():
        return dict(qt_=fm(BF16), kt_=fm(BF16), qhA=fm(BF16), qhB=fm(BF16), kh=fm(BF16),
                    vt=A.alloc(4 * 512, BF16).rearrange("p (t c) -> p t c", t=4), ogt=A.alloc(4 * 512, F32).rearrange("p (t c) -> p t c", t=4),
                    sog=A.alloc(4 * 512, F32).rearrange("p (t c) -> p t c", t=4), decay=A.alloc(32, F32))
    sets = [mkset(), mkset()]
    SETN = ["qt_", "kt_", "qhA", "qhB", "kh", "vt", "ogt", "sog", "decay"]
    grb = A.alloc(512, F32)
    vtf = A.alloc(4 * 512, F32).rearrange("p (t c) -> p t c", t=4)
    khT2 = [A.alloc(512, BF16) for _ in range(2)]
    Pm2 = [A.alloc(8 * 128, BF16).rearrange("p (h t) -> p h t", h=8) for _ in range(2)]
    state = A.alloc(4 * 64, F32).rearrange("p (j v) -> p j v", j=4)
    stmp = A.alloc(4 * 64, F32).rearrange("p (j v) -> p j v", j=4)
    sbf = [A.alloc(4 * 64, BF16).rearrange("p (j v) -> p j v", j=4) for _ in range(4)]
    osb = A.alloc(512, F32); osq = A.alloc(512, F32); oss = A.alloc(16, F32)
    sg = A.alloc(512, F32)
    yb = A.alloc(512, BF16)
    yT = [A.alloc(512, BF16) for _ in range(2)]
    P.dma("sp", lambda e_: e_.dma_start(out=osb[:, 0:64], in_=I["g_rec"][l].partition_broadcast(128)), writes=["osb"], key="grb")
    P.op("dve", lambda e_: e_.tensor_copy(grb.rearrange("p (h v) -> p h v", h=8), osb[:, 0:64].unsqueeze(1).to_broadcast([128, 8, 64])), reads=["osb"], writes=["grb"])
    P.op("dve", lambda e_: e_.memset(state, 0.0), writes=["state"])
    P.op("dve", lambda e_: e_.memset(sbf[0], 0.0), writes=["sbf0"])
    for bp_ in range(2):
        P.op("dve", lambda e_, bp_=bp_: e_.memset(sets[bp_]["qhA"], 0.0), writes=["qhA%d" % bp_])
        P.op("dve", lambda e_, bp_=bp_: e_.memset(sets[bp_]["qhB"], 0.0), writes=["qhB%d" % bp_])
    sv = [0]
    z2 = z.rearrange("p j t -> p (j t)"); e2 = e.rearrange("p j t -> p (j t)"); t12 = t1.rearrange("p j t -> p (j t)"); t22 = t2.rearrange("p j t -> p (j t)")
    A2 = Acum.rearrange("p j t -> p (j t)")
    def ch(x):
        return x.rearrange("p j (c t) -> p (j c) t", t=64)
    def prep_gen(b):
        bp = b % 2
        qt_, kt_, qhA, qhB, kh, vt, ogt, sog, decay = (sets[bp][n_] for n_ in SETN)
        sl = slice(b * NB, (b + 1) * NB)
        P.dma("sp", lambda e_, sl=sl: e_.dma_start(out=z, in_=frec_fm[:, sl].rearrange("(j p) t -> p j t", p=128)), writes=["z"], key="zD")
        yield
        P.dma("sp", lambda e_, sl=sl: e_.dma_start(out=qr, in_=qrec_fm[:, sl].rearrange("(j p) t -> p j t", p=128)), writes=["qr"], key="qrD")
        yield
        P.dma("sp", lambda e_, sl=sl: e_.dma_start(out=vtf, in_=tm_out[sl, 136:648].rearrange("(t p) c -> p t c", p=128)), writes=["vtf"], key="vtD")
        yield
        P.op("act", lambda e_: e_.activation(out=vt, in_=vtf, func=AF.Copy), reads=["vtf"], writes=[("vt%d" % bp)])
        yield
        P.dma("sp", lambda e_, sl=sl: e_.dma_start(out=ogt, in_=tm_out[sl, 648:1160].rearrange("(t p) c -> p t c", p=128)), writes=[("ogt%d" % bp)], key="ogD")
        yield
        P.op("act", lambda e_: e_.activation(out=kk, in_=z, func=AF.Sigmoid, scale=-1.0), reads=["z"], writes=["kk"])
        yield
        P.op("act", lambda e_: e_.activation(out=qq, in_=qr, func=AF.Sigmoid), reads=["qr"], writes=["qq"])
        yield
        P.op("act", lambda e_: e_.activation(out=sog, in_=ogt, func=AF.Sigmoid), reads=[("ogt%d" % bp)], writes=[("sog%d" % bp)])
        yield
        P.op("act", lambda e_: e_.activation(out=e, in_=z, func=AF.Exp, scale=-1.0), reads=["z"], writes=["e"])
        yield
        for j in range(4):
            P.op("dve", lambda e_, j=j: e_.tensor_scalar(t1[:, j, :], e[:, j, :], k.lb[:, j:j + 1], 1.0, op0=ALU.mult, op1=ALU.add), reads=["e", "sm"], writes=["t1"])
            yield
        P.op("dve", lambda e_: e_.tensor_scalar(t2, e, 1.0, None, op0=ALU.add), reads=["e"], writes=["t2"])
        yield
        P.op("act", lambda e_: e_.activation(out=t1, in_=t1, func=AF.Ln), reads=["t1"], writes=["t1"])
        yield
        P.op("act", lambda e_: e_.activation(out=z, in_=t2, func=AF.Ln), reads=["t2", "z"], writes=["z"])
        yield
        P.op("dve", lambda e_: e_.tensor_tensor(t1, t1, z, op=ALU.subtract), reads=["t1", "z"], writes=["t1"])
        yield
        for j in range(4):
            P.op("dve", lambda e_, j=j: e_.tensor_scalar(kk[:, j, :], kk[:, j, :], k.oml[:, j:j + 1], None, op0=ALU.mult), reads=["kk", "sm"], writes=["kk"])
            yield
        srcs = [t1, Acum]
        for si, sh in enumerate([1, 2, 4, 8, 16, 32]):
            a_ = ch(srcs[si % 2]); b_ = ch(srcs[(si + 1) % 2])
            P.op("dve", lambda e_, a_=a_, b_=b_, sh=sh: e_.tensor_tensor(b_[:, :, sh:64], a_[:, :, sh:64], a_[:, :, 0:64 - sh], op=ALU.add), reads=["t1", "Acum"], writes=["t1", "Acum"])
            yield
            P.op("act", lambda e_, a_=a_, b_=b_, sh=sh: e_.activation(out=b_[:, :, 0:sh], in_=a_[:, :, 0:sh], func=AF.Copy), reads=["t1", "Acum"], writes=["t1", "Acum"])
            yield
        P.op("act", lambda e_: e_.activation(out=Acum, in_=t1, func=AF.Copy), reads=["t1", "Acum"], writes=["t1", "Acum"])
        yield
        P.op("dve", lambda e_: e_.tensor_tensor(qq, qq, qr, op=ALU.mult), reads=["qq", "qr"], writes=["qq"])
        yield
        Ac = ch(Acum)
        P.op("dve", lambda e_: e_.tensor_tensor(ch(t1), Ac, Ac[:, :, 31:32].to_broadcast([128, 32, 64]), op=ALU.subtract), reads=["Acum", "t1"], writes=["t1"])
        yield
        P.op("dve", lambda e_: e_.tensor_scalar(t1, t1, -40.0, 40.0, op0=ALU.max, op1=ALU.min), reads=["t1"], writes=["t1"])
        yield
        P.op("act", lambda e_: e_.activation(out=t2, in_=t1, func=AF.Exp), reads=["t1"], writes=["t2"])
        yield
        P.op("dve", lambda e_: e_.tensor_tensor(qt_, qq, t2, op=ALU.mult), reads=["qq", "t2"], writes=[("qt_%d" % bp)])
        yield
        P.op("act", lambda e_: e_.activation(out=t2, in_=t1, func=AF.Exp, scale=-1.0), reads=["t1", ("qt_%d" % bp)], writes=["t2"])
        yield
        P.op("dve", lambda e_: e_.tensor_tensor(kt_, kk, t2, op=ALU.mult), reads=["kk", "t2"], writes=[("kt_%d" % bp)])
        yield
        P.op("act", lambda e_: e_.activation(out=t2, in_=Acum, func=AF.Exp), reads=["Acum", ("kt_%d" % bp)], writes=["t2"])
        yield
        def eo(x, par):
            return x.rearrange("p j (c two t) -> p j c two t", two=2, t=64)[:, :, :, par, :]
        P.op("dve", lambda e_: e_.tensor_tensor(eo(qhA, 0), eo(qq, 0), eo(t2, 0), op=ALU.mult), reads=["qq", "t2"], writes=[("qhA%d" % bp)])
        yield
        P.op("dve", lambda e_: e_.tensor_tensor(eo(qhB, 1), eo(qq, 1), eo(t2, 1), op=ALU.mult), reads=["qq", "t2"], writes=[("qhB%d" % bp)])
        yield
        P.op("act", lambda e_: e_.activation(out=decay, in_=Ac[:, :, 63], func=AF.Exp), reads=["Acum"], writes=[("decay%d" % bp)])
        yield
        P.op("dve", lambda e_: e_.tensor_tensor(ch(t1), Ac[:, :, 63:64].to_broadcast([128, 32, 64]), Ac, op=ALU.subtract), reads=["Acum", "t1"], writes=["t1"])
        yield
        P.op("act", lambda e_: e_.activation(out=t2, in_=t1, func=AF.Exp), reads=["t1", ("qhA%d" % bp), ("qhB%d" % bp)], writes=["t2"])
        yield
        P.op("dve", lambda e_: e_.tensor_tensor(kh, kk, t2, op=ALU.mult), reads=["kk", "t2"], writes=[("kh%d" % bp)])
        yield
    def tiles(b, filler, quota):
        bp = b % 2
        qt_, kt_, qhA, qhB, kh, vt, ogt, sog, decay = (sets[bp][n_] for n_ in SETN)
        tb = ps[7][:, :].bitcast(BF16)

        def stage1(tt):
            tsl = slice(tt * 128, (tt + 1) * 128)
            par = tt % 2
            khT = khT2[par]; khk = "khT%d" % par
            Pm = Pm2[par]; pmk = "Pm%d" % par
            for j in range(4):
                P.op("pe", lambda e_, j=j: e_.transpose(tb[:, j * 128:(j + 1) * 128], kh[:, j, tsl], k.identb), reads=[("kh%d" % bp), "identb"], writes=["ps7"])
            P.op("act", lambda e_: e_.activation(out=khT, in_=tb[:, 0:512], func=AF.Copy), reads=["ps7"], writes=[khk])
            for h in range(8):
                j = h // 2; p0 = (h % 2) * 64
                bank = 5 + h % 2
                P.op("pe", lambda e_, j=j, p0=p0, bank=bank: e_.matmul(ps[bank][:, j * 128:(j + 1) * 128], lhsT=kt_[p0:p0 + 64, j, tsl], rhs=qt_[p0:p0 + 64, j, tsl], start=True, stop=True),
                     reads=[("kt_%d" % bp), ("qt_%d" % bp)], writes=["ps%d" % bank])
            for g in range(2):
                P.op("dve", lambda e_, g=g: e_.tensor_tensor(Pm[:, g * 4:(g + 1) * 4, :], ps[5 + g][:, :].rearrange("p (h t) -> p h t", h=4), cmf.unsqueeze(1).to_broadcast([128, 4, 128]), op=ALU.mult),
                     reads=["ps%d" % (5 + g), "cmf"], writes=[pmk])
            for half in range(2):
                hs = slice(half * 64, (half + 1) * 64)
                bank = (4, 2)[half] if par == 0 else (0, 1)[half]
                for j in range(4):
                    P.op("pe", lambda e_, j=j, hs=hs, bank=bank: e_.matmul(ps[bank][:, j * 128:(j + 1) * 128], lhsT=khT[hs, j * 128:(j + 1) * 128], rhs=vt[hs, tt, j * 128:(j + 1) * 128], start=True, stop=True),
                         reads=[khk, ("vt%d" % bp)], writes=["ps%d" % bank])

        def stage2(tt):
            tsl = slice(tt * 128, (tt + 1) * 128)
            par = tt % 2
            Pm = Pm2[par]; pmk = "Pm%d" % par
            svA = sv[0]
            for half in range(2):
                c = tt * 2 + half
                bank = (4, 2)[half] if par == 0 else (0, 1)[half]
                dv = decay.rearrange("p (j c) -> p j c", j=4)[:, :, c:c + 1]
                P.op("dve", lambda e_, dv=dv: e_.tensor_tensor(stmp, state, dv.to_broadcast([128, 4, 64]), op=ALU.mult), reads=["state", ("decay%d" % bp)], writes=["stmp"])
                pv = ps[bank][:, :].rearrange("p (j x) -> p j x", j=4)
                P.op("dve", lambda e_, pv=pv: e_.tensor_tensor(state[0:64], stmp[0:64], pv[0:64, :, 0:64], op=ALU.add), reads=["stmp", "ps%d" % bank], writes=["state"])
                P.op("dve", lambda e_, pv=pv: e_.tensor_tensor(state[64:128], stmp[64:128], pv[64:128, :, 64:128], op=ALU.add), reads=["stmp", "ps%d" % bank], writes=["state"])
                sv[0] += 1
                sb_ = sbf[sv[0] % 4]
                P.op("act", lambda e_, sb_=sb_: e_.activation(out=sb_, in_=state, func=AF.Copy), reads=["state"], writes=["sbf%d" % (sv[0] % 4)])
            s0 = sbf[svA % 4]; s0k = "sbf%d" % (svA % 4)
            s1 = sbf[(svA + 1) % 4]; s1k = "sbf%d" % ((svA + 1) % 4)
            for h in range(8):
                j = h // 2; p0 = (h % 2) * 64
                oo = ps[3][:, h * 64:(h + 1) * 64]
                P.op("pe", lambda e_, h=h, oo=oo: e_.matmul(oo, lhsT=Pm[:, (h % 2) * 4 + h // 2, :], rhs=vt[:, tt, h * 64:(h + 1) * 64], start=True, stop=False), reads=[pmk, ("vt%d" % bp)], writes=["ps3"])
                P.op("pe", lambda e_, j=j, p0=p0, oo=oo: e_.matmul(oo, lhsT=qhA[p0:p0 + 64, j, tsl], rhs=s0[p0:p0 + 64, j, :], start=False, stop=False), reads=[("qhA%d" % bp), s0k], writes=["ps3"])
                P.op("pe", lambda e_, j=j, p0=p0, oo=oo: e_.matmul(oo, lhsT=qhB[p0:p0 + 64, j, tsl], rhs=s1[p0:p0 + 64, j, :], start=False, stop=True), reads=[("qhB%d" % bp), s1k], writes=["ps3"])
            P.op("act", lambda e_: e_.activation(out=osb, in_=ps[3][:, :], func=AF.Copy), reads=["ps3"], writes=["osb"])
            P.op("dve", lambda e_: e_.tensor_tensor(osq, osb, osb, op=ALU.mult), reads=["osb"], writes=["osq"])
            P.op("dve", lambda e_: e_.tensor_reduce(out=oss[:, 0:8], in_=osq.rearrange("p (h v) -> p h v", h=8), axis=AX.X, op=ALU.add), reads=["osq"], writes=["oss"])
            P.op("dve", lambda e_: e_.tensor_scalar(oss[:, 0:8], oss[:, 0:8], 1.0 / 64, EPS, op0=ALU.mult, op1=ALU.add), reads=["oss"], writes=["oss"])
            P.op("act", lambda e_: e_.activation(out=oss[:, 0:8], in_=oss[:, 0:8], func=AF.Ln), reads=["oss"], writes=["oss"])
            P.op("act", lambda e_: e_.activation(out=oss[:, 0:8], in_=oss[:, 0:8], func=AF.Exp, scale=-0.5), reads=["oss"], writes=["oss"])
            P.op("dve", lambda e_: e_.tensor_tensor(osq.rearrange("p (h v) -> p h v", h=8), osb.rearrange("p (h v) -> p h v", h=8), oss[:, 0:8].unsqueeze(2).to_broadcast([128, 8, 64]), op=ALU.mult),
                 reads=["osb", "oss", "osq"], writes=["osq"])
            P.op("dve", lambda e_: e_.tensor_tensor(osq, osq, grb, op=ALU.mult), reads=["osq", "grb"], writes=["osq"])
            P.op("dve", lambda e_: e_.tensor_tensor(sg, sog[:, tt, :], ogt[:, tt, :], op=ALU.mult), reads=[("sog%d" % bp), ("ogt%d" % bp)], writes=["sg"])
            P.op("dve", lambda e_: e_.tensor_tensor(yb, osq, sg, op=ALU.mult), reads=["osq", "sg"], writes=["yb"])
            for j in range(4):
                P.op("pe", lambda e_, j=j: e_.transpose(tb[:, 512 + j * 128:512 + (j + 1) * 128], yb[:, j * 128:(j + 1) * 128], k.identb), reads=["yb", "identb"], writes=["ps7"])
            t = b * 4 + tt
            y = yT[t % 2]; yk = "yTD%d" % (t % 2)
            P.op("act", lambda e_, y=y: e_.activation(out=y, in_=tb[:, 512:1024], func=AF.Copy), reads=["ps7"], writes=[yk])
            P.dma("sp", lambda e_, y=y, t=t: e_.dma_start(out=yrT_d[:, :, t * 128:(t + 1) * 128].rearrange("j p q -> p j q"), in_=y.rearrange("p (j q) -> p j q", j=4)), reads=[yk], key=yk)

        stage1(0)
        for tt in range(4):
            if filler is not None:
                for _ in range(quota):
                    next(filler, None)
            if tt + 1 < 4:
                stage1(tt + 1)
            stage2(tt)

    for _ in prep_gen(0):
        pass
    for b in range(S // NB):
        filler = prep_gen(b + 1) if b + 1 < S // NB else None
        tiles(b, filler, 14)
        if filler is not None:
            for _ in filler:
                pass
    P.barrier()
    A.release(m0)


def phaseE(k, l, I, xsrc, xdst):
    A, P, ps = k.A, k.P, k.ps
    yaT_d = k.dr["yaT_d"]; yrT_d = k.dr["yrT_d"]; gates_fm = k.dr["gates_fm"]
    m0 = A.mark()
    wa = A.alloc(4 * 1024, BF16).rearrange("p (k c) -> p k c", k=4)
    wr = A.alloc(4 * 1024, BF16).rearrange("p (k c) -> p k c", k=4)
    wo = A.alloc(8 * 1024, BF16).rearrange("p (k c) -> p k c", k=8)
    P.dma("pool", lambda e: e.dma_start(out=wa, in_=I["w_branch_a"][l].rearrange("(k p) c -> p k c", p=128)), writes=["wa"], key="wa")
    P.dma("pool", lambda e: e.dma_start(out=wr, in_=I["w_branch_r"][l].rearrange("(k p) c -> p k c", p=128)), writes=["wr"], key="wr")
    P.dma("pool", lambda e: e.dma_start(out=wo, in_=I["w_out"][l].rearrange("(k p) c -> p k c", p=128)), writes=["wo"], key="wo")
    ya = [A.alloc(4 * 512, BF16).rearrange("p (k t) -> p k t", k=4) for _ in range(2)]
    yr = [A.alloc(4 * 512, BF16).rearrange("p (k t) -> p k t", k=4) for _ in range(2)]
    gt = [A.alloc(16 * 512, BF16).rearrange("p (k t) -> p k t", k=16) for _ in range(2)]
    mT2 = [A.alloc(8 * 512, BF16).rearrange("p (k t) -> p k t", k=8) for _ in range(2)]
    m1 = [A.alloc(512, F32) for _ in range(2)]
    m2 = [A.alloc(512, F32) for _ in range(2)]
    xb = [A.alloc(1024, F32) for _ in range(2)]
    tb_ = [A.alloc(1024, F32) for _ in range(2)]
    cnt = 0
    for b in range(S // 512):
        sl = slice(b * 512, (b + 1) * 512)
        i2 = b % 2
        mT = mT2[i2]; mTk = "mT%d" % i2
        P.dma("sp", lambda e, i2=i2, sl=sl: e.dma_start(out=ya[i2], in_=yaT_d[:, :, sl].rearrange("j p t -> p j t")), writes=["yaE%d" % i2], key="yaE%d" % i2)
        P.dma("sp", lambda e, i2=i2, sl=sl: e.dma_start(out=yr[i2], in_=yrT_d[:, :, sl].rearrange("j p t -> p j t")), writes=["yrE%d" % i2], key="yrE%d" % i2)
        P.dma("sp", lambda e, i2=i2, sl=sl: e.dma_start(out=gt[i2], in_=gates_fm[:, sl].rearrange("(j p) t -> p j t", p=128)), writes=["gtE%d" % i2], key="gtE%d" % i2)
        for cb in range(8):
            ba = cnt % 2; bb = 2 + cnt % 2
            for kc in range(4):
                P.op("pe", lambda e, kc=kc, cb=cb, ba=ba, i2=i2: e.matmul(ps[ba][:, :], lhsT=wa[:, kc, cb * 128:(cb + 1) * 128], rhs=ya[i2][:, kc, :], start=(kc == 0), stop=(kc == 3)),
                     reads=["wa", "yaE%d" % i2], writes=["ps%d" % ba])
            for kc in range(4):
                P.op("pe", lambda e, kc=kc, cb=cb, bb=bb, i2=i2: e.matmul(ps[bb][:, :], lhsT=wr[:, kc, cb * 128:(cb + 1) * 128], rhs=yr[i2][:, kc, :], start=(kc == 0), stop=(kc == 3)),
                     reads=["wr", "yrE%d" % i2], writes=["ps%d" % bb])
            a1 = m1[cnt % 2]; a2 = m2[cnt % 2]
            P.op("dve", lambda e, a1=a1, ba=ba, cb=cb, i2=i2: e.tensor_tensor(a1, ps[ba][:, :], gt[i2][:, cb, :], op=ALU.mult), reads=["ps%d" % ba, "gtE%d" % i2], writes=["m1%d" % (cnt % 2)])
            P.op("dve", lambda e, a2=a2, bb=bb, cb=cb, i2=i2: e.tensor_tensor(a2, ps[bb][:, :], gt[i2][:, 8 + cb, :], op=ALU.mult), reads=["ps%d" % bb, "gtE%d" % i2], writes=["m2%d" % (cnt % 2)])
            P.op("dve", lambda e, a1=a1, a2=a2, cb=cb, mT=mT: e.tensor_tensor(mT[:, cb, :], a1, a2, op=ALU.add), reads=["m1%d" % (cnt % 2), "m2%d" % (cnt % 2)], writes=[mTk])
            cnt += 1
        for tt in range(4):
            t = b * 4 + tt
            x = xb[t % 2]; xk = "xbE%d" % (t % 2)
            tq = tb_[t % 2]; tk = "tbE%d" % (t % 2)
            P.dma("sp", lambda e, x=x, t=t: e.dma_start(out=x, in_=xsrc[t * 128:(t + 1) * 128, :]), writes=[xk], key=xk)
            for half in range(2):
                bank = 4 + (t * 2 + half) % 4
                for kc in range(8):
                    P.op("pe", lambda e, kc=kc, half=half, bank=bank, tt=tt, mT=mT: e.matmul(ps[bank][:, :], lhsT=mT[:, kc, tt * 128:(tt + 1) * 128], rhs=wo[:, kc, half * 512:(half + 1) * 512], start=(kc == 0), stop=(kc == 7)),
                         reads=[mTk, "wo"], writes=["ps%d" % bank])
                P.op("dve", lambda e, tq=tq, half=half, bank=bank: e.tensor_tensor(tq[:, half * 512:(half + 1) * 512], ps[bank][:, :], k.gtbc[0][:, half * 512:(half + 1) * 512], op=ALU.mult),
                     reads=["ps%d" % bank, "gtbc0"], writes=[tk])
            P.op("dve", lambda e, tq=tq, x=x: e.tensor_tensor(tq, tq, x, op=ALU.add), reads=[tk, xk], writes=[tk])
            P.dma("sp", lambda e, tq=tq, t=t: e.dma_start(out=xdst[t * 128:(t + 1) * 128, :], in_=tq), reads=[tk], key=tk)
    P.barrier()
    A.release(m0)


def phaseF(k, l, I, xsrc, xdst):
    A, P, ps = k.A, k.P, k.ps
    m0 = A.mark()
    h2T = A.alloc(8 * S, BF16).rearrange("p (k t) -> p k t", k=8)
    gate = A.alloc(NT * 32, F32).rearrange("p (t c) -> p t c", t=NT)
    k.xb = [A.alloc(1024, F32) for _ in range(2)]
    m1_ = A.mark()
    hf = [A.alloc(8 * 128, F32).rearrange("p (k t) -> p k t", k=8) for _ in range(2)]
    wrt = A.alloc(8 * 36, F32).rearrange("p (k c) -> p k c", k=8)
    rb = A.alloc(36, F32)
    lg = A.alloc(NT * 36, F32).rearrange("p (t c) -> p t c", t=NT)
    k.xn = [A.alloc(1024, F32) for _ in range(2)]
    k.nst = [A.alloc(2, F32) for _ in range(2)]
    P.dma("sp", lambda e: e.dma_start(out=wrt[:, :, 0:4], in_=I["w_grp"][l].rearrange("(k p) c -> p k c", p=128)), writes=["wrt"], key="wrt")
    P.dma("sp", lambda e: e.dma_start(out=wrt[:, :, 4:36], in_=I["w_exp_router"][l].rearrange("(k p) c -> p k c", p=128)), writes=["wrt"], key="wrt")
    P.dma("sp", lambda e: e.dma_start(out=rb[:, 0:4], in_=I["b_grp"][l].partition_broadcast(128)), writes=["rbF"], key="rbF")
    P.dma("sp", lambda e: e.dma_start(out=rb[:, 4:36], in_=I["b_exp_router"][l].partition_broadcast(128)), writes=["rbF"], key="rbF")
    for t in range(NT):
        b0 = norm_transpose(k, xsrc, t, None, None, None, None, None)
        f = hf[t % 2]; fk = "hfF%d" % (t % 2)
        for kc in range(8):
            bank = b0 + kc // 4
            P.op("act", lambda e, kc=kc, bank=bank, f=f: e.activation(out=f[:, kc, :], in_=ps[bank][:, (kc % 4) * 128:(kc % 4 + 1) * 128], func=AF.Identity, scale=k.gs2[:, kc:kc + 1], bias=k.sh2[:, kc:kc + 1]),
                 reads=["ps%d" % bank, "gs", "modf"], writes=[fk])
        P.op("dve", lambda e, f=f, t=t: e.tensor_copy(h2T[:, :, t * 128:(t + 1) * 128], f), reads=[fk], writes=["h2T"])
        bank = 4 + t % 2
        for kc in range(8):
            P.op("pe", lambda e, kc=kc, f=f, bank=bank: e.matmul(ps[bank][:, 0:36], lhsT=f[:, kc, :], rhs=wrt[:, kc, :], start=(kc == 0), stop=(kc == 7)), reads=[fk, "wrt"], writes=["ps%d" % bank])
        P.op("dve", lambda e, t=t, bank=bank: e.tensor_tensor(lg[:, t, :], ps[bank][:, 0:36], rb, op=ALU.add), reads=["ps%d" % bank, "rbF"], writes=["lg"])
    g4 = A.alloc(NT * 4, F32).rearrange("p (t c) -> p t c", t=NT)
    oh = A.alloc(NT * 4, F32).rearrange("p (t c) -> p t c", t=NT)
    s1 = A.alloc(NT * 8, F32)
    le = A.alloc(NT * 32, F32).rearrange("p (t c) -> p t c", t=NT)
    o1 = A.alloc(NT * 32, F32).rearrange("p (t c) -> p t c", t=NT)
    o2 = A.alloc(NT * 32, F32).rearrange("p (t c) -> p t c", t=NT)
    mx = s1[:, 0:NT]; gs_ = s1[:, NT:2 * NT]; mA = s1[:, 2 * NT:3 * NT]; mB = s1[:, 3 * NT:4 * NT]; w1 = s1[:, 4 * NT:5 * NT]; w2 = s1[:, 5 * NT:6 * NT]
    R = ["lg", "g4", "oh", "s1", "le", "o1", "o2", "gate"]
    def D(fn):
        P.op("dve", fn, reads=R, writes=R)
    bc4 = lambda v: v.unsqueeze(2).to_broadcast([128, NT, 4])
    bc32 = lambda v: v.unsqueeze(2).to_broadcast([128, NT, 32])
    D(lambda e: e.tensor_reduce(out=mx, in_=lg[:, :, 0:4], axis=AX.X, op=ALU.max))
    D(lambda e: e.tensor_tensor(oh, lg[:, :, 0:4], bc4(mx), op=ALU.is_ge))
    D(lambda e: e.tensor_tensor(g4, lg[:, :, 0:4], bc4(mx), op=ALU.subtract))
    P.op("act", lambda e: e.activation(out=g4, in_=g4, func=AF.Exp), reads=R, writes=R)
    D(lambda e: e.tensor_reduce(out=gs_, in_=g4, axis=AX.X, op=ALU.add))
    D(lambda e: e.reciprocal(gs_, gs_))
    lev = le.rearrange("p t (g x) -> p t g x", g=4)
    D(lambda e: e.tensor_tensor(lev, lg[:, :, 4:36].rearrange("p t (g x) -> p t g x", g=4), oh.unsqueeze(3).to_broadcast([128, NT, 4, 8]), op=ALU.mult))
    D(lambda e: e.tensor_scalar(o1.rearrange("p t (g x) -> p t g x", g=4), oh.unsqueeze(3).to_broadcast([128, NT, 4, 8]), -1.0, 1e30, op0=ALU.add, op1=ALU.mult))
    D(lambda e: e.tensor_tensor(le, le, o1, op=ALU.add))
    D(lambda e: e.tensor_reduce(out=mA, in_=le, axis=AX.X, op=ALU.max))
    D(lambda e: e.tensor_tensor(o1, le, bc32(mA), op=ALU.is_ge))
    D(lambda e: e.scalar_tensor_tensor(out=le, in0=o1, scalar=-1e30, in1=le, op0=ALU.mult, op1=ALU.add))
    D(lambda e: e.tensor_reduce(out=mB, in_=le, axis=AX.X, op=ALU.max))
    D(lambda e: e.tensor_tensor(o2, le, bc32(mB), op=ALU.is_ge))
    D(lambda e: e.tensor_tensor(w1, mB, mA, op=ALU.subtract))
    P.op("act", lambda e: e.activation(out=w1, in_=w1, func=AF.Exp), reads=R, writes=R)
    D(lambda e: e.tensor_scalar(w1, w1, 1.0, None, op0=ALU.add))
    D(lambda e: e.reciprocal(w1, w1))
    D(lambda e: e.tensor_scalar(w2, w1, -1.0, 1.0, op0=ALU.mult, op1=ALU.add))
    D(lambda e: e.tensor_tensor(w1, w1, gs_, op=ALU.mult))
    D(lambda e: e.tensor_tensor(w2, w2, gs_, op=ALU.mult))
    D(lambda e: e.tensor_tensor(o1, o1, bc32(w1), op=ALU.mult))
    D(lambda e: e.tensor_tensor(o2, o2, bc32(w2), op=ALU.mult))
    D(lambda e: e.tensor_tensor(gate, o1, o2, op=ALU.add))
    P.barrier()
    A.release(m1_)
    TB = 1024
    acc = A.alloc(8 * 1024, F32).rearrange("p (t c) -> p t c", t=8)
    wg = [A.alloc(8 * 512, BF16).rearrange("p (k c) -> p k c", k=8) for _ in range(2)]
    wu = [A.alloc(8 * 512, BF16).rearrange("p (k c) -> p k c", k=8) for _ in range(2)]
    wd = [A.alloc(4 * 1024, BF16).rearrange("p (k c) -> p k c", k=4) for _ in range(2)]
    hid = [A.alloc(4 * 512, BF16).rearrange("p (k t) -> p k t", k=4) for _ in range(2)]
    sg = [A.alloc(512, F32) for _ in range(2)]
    ec = 0; hc_ = 0; sc_ = 0; yc = 0
    for tb in range(S // TB):
        P.op("pool", lambda e: e.memset(acc, 0.0), writes=["acc"])
        for ex in range(32):
            i2 = ec % 2
            P.dma("pool", lambda e, i2=i2, ex=ex: e.dma_start(out=wg[i2], in_=I["w_gate"][l, ex].rearrange("(k p) c -> p k c", p=128)), writes=["wg%d" % i2], key="wg%d" % i2)
            P.dma("pool", lambda e, i2=i2, ex=ex: e.dma_start(out=wu[i2], in_=I["w_up"][l, ex].rearrange("(k p) c -> p k c", p=128)), writes=["wu%d" % i2], key="wu%d" % i2)
            P.dma("pool", lambda e, i2=i2, ex=ex: e.dma_start(out=wd[i2], in_=I["w_down"][l, ex].rearrange("(k p) c -> p k c", p=128)), writes=["wd%d" % i2], key="wd%d" % i2)
            for hb in range(TB // 512):
                tok0 = tb * TB + hb * 512
                hd = hid[hc_ % 2]; hk = "hid%d" % (hc_ % 2)
                for cb in range(4):
                    bg = (sc_ % 2) * 2; bu = bg + 1
                    for kc in range(8):
                        P.op("pe", lambda e, kc=kc, cb=cb, bg=bg, i2=i2, tok0=tok0: e.matmul(ps[bg][:, :], lhsT=wg[i2][:, kc, cb * 128:(cb + 1) * 128], rhs=h2T[:, kc, tok0:tok0 + 512], start=(kc == 0), stop=(kc == 7)),
                             reads=["wg%d" % i2, "h2T"], writes=["ps%d" % bg])
                    for kc in range(8):
                        P.op("pe", lambda e, kc=kc, cb=cb, bu=bu, i2=i2, tok0=tok0: e.matmul(ps[bu][:, :], lhsT=wu[i2][:, kc, cb * 128:(cb + 1) * 128], rhs=h2T[:, kc, tok0:tok0 + 512], start=(kc == 0), stop=(kc == 7)),
                             reads=["wu%d" % i2, "h2T"], writes=["ps%d" % bu])
                    s = sg[sc_ % 2]; sk = "sgF%d" % (sc_ % 2)
                    P.op("act", lambda e, s=s, bg=bg: e.activation(out=s, in_=ps[bg][:, :], func=AF.Exp, scale=-1.0), reads=["ps%d" % bg], writes=[sk])
                    P.op("pool", lambda e, s=s: e.tensor_scalar(s, s, 1.0, None, op0=ALU.add), reads=[sk], writes=[sk])
                    P.op("dve", lambda e, s=s: e.reciprocal(s, s), reads=[sk], writes=[sk])
                    P.op("dve", lambda e, s=s, bg=bg: e.tensor_tensor(s, s, ps[bg][:, :], op=ALU.mult), reads=[sk, "ps%d" % bg], writes=[sk])
                    P.op("dve", lambda e, s=s, bu=bu, hd=hd, cb=cb: e.tensor_tensor(hd[:, cb, :], s, ps[bu][:, :], op=ALU.mult), reads=[sk, "ps%d" % bu], writes=[hk])
                    sc_ += 1
                for tt in range(4):
                    tl = hb * 4 + tt
                    tg = tb * 8 + tl
                    for half in range(2):
                        bank = 4 + yc % 4
                        for kc in range(4):
                            P.op("pe", lambda e, kc=kc, half=half, bank=bank, tt=tt, hd=hd, i2=i2: e.matmul(ps[bank][:, :], lhsT=hd[:, kc, tt * 128:(tt + 1) * 128], rhs=wd[i2][:, kc, half * 512:(half + 1) * 512], start=(kc == 0), stop=(kc == 3)),
                                 reads=[hk, "wd%d" % i2], writes=["ps%d" % bank])
                        P.op("dve", lambda e, bank=bank, tl=tl, tg=tg, half=half, ex=ex: e.scalar_tensor_tensor(out=acc[:, tl, half * 512:(half + 1) * 512], in0=ps[bank][:, :], scalar=gate[:, tg, ex:ex + 1], in1=acc[:, tl, half * 512:(half + 1) * 512], op0=ALU.mult, op1=ALU.add),
                             reads=["ps%d" % bank, "gate", "acc"], writes=["acc"])
                        yc += 1
                hc_ += 1
            ec += 1
        for tl in range(8):
            tg = tb * 8 + tl
            x = k.xb[tg % 2]; xk = "xb%d" % (tg % 2)
            P.dma("sp", lambda e, x=x, tg=tg: e.dma_start(out=x, in_=xsrc[tg * 128:(tg + 1) * 128, :]), writes=[xk], key=xk)
            P.op("pool", lambda e, tl=tl: e.tensor_tensor(acc[:, tl, :], acc[:, tl, :], k.gtbc[1], op=ALU.mult), reads=["acc", "gtbc1"], writes=["acc"])
            P.op("pool", lambda e, tl=tl, x=x: e.tensor_tensor(x, x, acc[:, tl, :], op=ALU.add), reads=["acc", xk], writes=[xk])
            P.dma("sp", lambda e, x=x, tg=tg: e.dma_start(out=xdst[tg * 128:(tg + 1) * 128, :], in_=x), reads=[xk], key=xk)
    P.barrier()
    A.release(m0)


def phaseG(k, I, xsrc, out):
    A, P, ps = k.A, k.P, k.ps
    m0 = A.mark()
    gb = A.alloc(1024, F32)
    P.dma("sp", lambda e: e.dma_start(out=gb, in_=I["g_final"].partition_broadcast(128)), writes=["gbG"], key="gbG")
    xb = [A.alloc(1024, F32) for _ in range(2)]
    xn = [A.alloc(1024, F32) for _ in range(2)]
    st = [A.alloc(2, F32) for _ in range(2)]
    for t in range(NT):
        x = xb[t % 2]; xk = "xbG%d" % (t % 2); n = xn[t % 2]; nk = "xnG%d" % (t % 2); s = st[t % 2]; sk = "stG%d" % (t % 2)
        P.dma("sp", lambda e, x=x, t=t: e.dma_start(out=x, in_=xsrc[t * 128:(t + 1) * 128, :]), writes=[xk], key=xk)
        P.op("act", lambda e, x=x, n=n, s=s: e.activation(out=n, in_=x, func=AF.Square, accum_out=s[:, 0:1]), reads=[xk], writes=[nk, sk])
        P.op("dve", lambda e, s=s: e.tensor_scalar(s[:, 1:2], s[:, 0:1], 1.0 / D, EPS, op0=ALU.mult, op1=ALU.add), reads=[sk], writes=[sk])
        P.op("act", lambda e, s=s: e.activation(out=s[:, 1:2], in_=s[:, 1:2], func=AF.Ln), reads=[sk], writes=[sk])
        P.op("act", lambda e, s=s: e.activation(out=s[:, 1:2], in_=s[:, 1:2], func=AF.Exp, scale=-0.5), reads=[sk], writes=[sk])
        P.op("dve", lambda e, x=x, n=n, s=s: e.scalar_tensor_tensor(out=n, in0=x, scalar=s[:, 1:2], in1=gb, op0=ALU.mult, op1=ALU.mult), reads=[xk, sk, "gbG"], writes=[nk])
        P.dma("sp", lambda e, n=n, t=t: e.dma_start(out=out[t * 128:(t + 1) * 128, :], in_=n), reads=[nk], key=nk)
    P.barrier()
    A.release(m0)


CAP = 768
NSL = 32 * CAP


def phaseF2(k, l, I, xsrc, xdst):
    A, P, ps = k.A, k.P, k.ps
    Xbuf = dram(k, "Xbuf", [NSL + 128, D], BF16)
    Ybuf = dram(k, "Ybuf", [NSL + 128, D], F32)
    m0 = A.mark()
    idx1 = A.alloc(NT, I32); idx2 = A.alloc(NT, I32)
    g12 = A.alloc(2 * NT, F32)
    g1 = g12[:, 0:NT]; g2 = g12[:, NT:2 * NT]
    k.xb = [A.alloc(1024, F32) for _ in range(2)]
    mH = A.mark()
    h2tm = A.alloc(NT * 1024, BF16).rearrange("p (t c) -> p t c", t=NT)
    m1_ = A.mark()
    hf = [A.alloc(8 * 128, F32).rearrange("p (k t) -> p k t", k=8) for _ in range(2)]
    wrt = A.alloc(8 * 36, F32).rearrange("p (k c) -> p k c", k=8)
    rb = A.alloc(36, F32)
    lg = A.alloc(NT * 36, F32).rearrange("p (t c) -> p t c", t=NT)
    k.xn = [A.alloc(1024, F32) for _ in range(2)]
    k.nst = [A.alloc(2, F32) for _ in range(2)]
    P.dma("sp", lambda e: e.dma_start(out=wrt[:, :, 0:4], in_=I["w_grp"][l].rearrange("(k p) c -> p k c", p=128)), writes=["wrt"], key="wrt")
    P.dma("sp", lambda e: e.dma_start(out=wrt[:, :, 4:36], in_=I["w_exp_router"][l].rearrange("(k p) c -> p k c", p=128)), writes=["wrt"], key="wrt")
    P.dma("sp", lambda e: e.dma_start(out=rb[:, 0:4], in_=I["b_grp"][l].partition_broadcast(128)), writes=["rbF"], key="rbF")
    P.dma("sp", lambda e: e.dma_start(out=rb[:, 4:36], in_=I["b_exp_router"][l].partition_broadcast(128)), writes=["rbF"], key="rbF")
    def EVt(t):
        b0 = (t % 2) * 2
        f = hf[t % 2]; fk = "hfF%d" % (t % 2)
        for kc in range(8):
            bank = b0 + kc // 4
            P.op("act", lambda e, kc=kc, bank=bank, f=f: e.activation(out=f[:, kc, :], in_=ps[bank][:, (kc % 4) * 128:(kc % 4 + 1) * 128], func=AF.Identity, scale=k.gs2[:, kc:kc + 1], bias=k.sh2[:, kc:kc + 1]),
                 reads=["ps%d" % bank, "gs", "modf"], writes=[fk])
        bank = 4 + t % 2
        for kc in range(8):
            P.op("pe", lambda e, kc=kc, f=f, bank=bank: e.matmul(ps[bank][:, 0:36], lhsT=f[:, kc, :], rhs=wrt[:, kc, :], start=(kc == 0), stop=(kc == 7)), reads=[fk, "wrt"], writes=["ps%d" % bank])
        P.op("dve", lambda e, t=t, bank=bank: e.tensor_tensor(lg[:, t, :], ps[bank][:, 0:36], rb, op=ALU.add), reads=["ps%d" % bank, "rbF"], writes=["lg"])
        for kc in range(8):
            bank = 6 + kc // 4
            P.op("pe", lambda e, kc=kc, f=f, bank=bank: e.transpose(ps[bank][:, (kc % 4) * 128:(kc % 4 + 1) * 128], f[:, kc, :], k.ident), reads=[fk, "ident"], writes=["ps%d" % bank])
        P.op("dve", lambda e, t=t: e.tensor_copy(h2tm[:, t, 0:512], ps[6][:, :]), reads=["ps6"], writes=["h2tm"])
        P.op("pool" if False else "dve", lambda e, t=t: e.tensor_copy(h2tm[:, t, 512:1024], ps[7][:, :]), reads=["ps7"], writes=["h2tm"])
    norm_transpose(k, xsrc, 0, None, None, None, None, None)
    for t in range(NT):
        if t + 1 < NT:
            norm_transpose(k, xsrc, t + 1, None, None, None, None, None)
        EVt(t)
    def T32():
        return A.alloc(NT * 32, F32).rearrange("p (t c) -> p t c", t=NT)
    g4 = A.alloc(NT * 4, F32).rearrange("p (t c) -> p t c", t=NT)
    oh = A.alloc(NT * 4, F32).rearrange("p (t c) -> p t c", t=NT)
    s1 = A.alloc(NT * 8, F32)
    le = T32(); o1 = T32(); o2 = T32(); pos = T32(); tmp = T32(); offs = T32()
    indb = A.alloc(NT * 32, BF16)
    SU = A.alloc(128, BF16)
    suf = A.alloc(128, F32)
    mx = s1[:, 0:NT]; gs_ = s1[:, NT:2 * NT]; mA = s1[:, 2 * NT:3 * NT]; mB = s1[:, 3 * NT:4 * NT]; w1 = s1[:, 4 * NT:5 * NT]; w2 = s1[:, 5 * NT:6 * NT]
    v1 = s1[:, 6 * NT:7 * NT]; v2 = s1[:, 7 * NT:8 * NT]
    R = ["lg", "g4", "oh", "s1", "le", "o1", "o2", "pos", "tmp", "offs", "g12", "idx"]
    def Dv(fn):
        P.op("dve", fn, reads=R, writes=R)
    bc4 = lambda v: v.unsqueeze(2).to_broadcast([128, NT, 4])
    bc32 = lambda v: v.unsqueeze(2).to_broadcast([128, NT, 32])
    P.op("pool", lambda e: e.memset(suf, 1.0), writes=["suf"])
    P.op("pool", lambda e: e.tensor_tensor(suf, k.cneg_u, k.cneg_u, op=ALU.mult), reads=["cneg_u"], writes=["suf"])
    P.op("dve", lambda e: e.tensor_copy(SU, suf), reads=["suf"], writes=["SU"])
    Dv(lambda e: e.tensor_reduce(out=mx, in_=lg[:, :, 0:4], axis=AX.X, op=ALU.max))
    Dv(lambda e: e.tensor_tensor(oh, lg[:, :, 0:4], bc4(mx), op=ALU.is_ge))
    Dv(lambda e: e.tensor_tensor(g4, lg[:, :, 0:4], bc4(mx), op=ALU.subtract))
    P.op("act", lambda e: e.activation(out=g4, in_=g4, func=AF.Exp), reads=R, writes=R)
    Dv(lambda e: e.tensor_reduce(out=gs_, in_=g4, axis=AX.X, op=ALU.add))
    Dv(lambda e: e.reciprocal(gs_, gs_))
    lev = le.rearrange("p t (g x) -> p t g x", g=4)
    Dv(lambda e: e.tensor_tensor(lev, lg[:, :, 4:36].rearrange("p t (g x) -> p t g x", g=4), oh.unsqueeze(3).to_broadcast([128, NT, 4, 8]), op=ALU.mult))
    Dv(lambda e: e.tensor_scalar(o1.rearrange("p t (g x) -> p t g x", g=4), oh.unsqueeze(3).to_broadcast([128, NT, 4, 8]), -1.0, 1e30, op0=ALU.add, op1=ALU.mult))
    Dv(lambda e: e.tensor_tensor(le, le, o1, op=ALU.add))
    Dv(lambda e: e.tensor_reduce(out=mA, in_=le, axis=AX.X, op=ALU.max))
    Dv(lambda e: e.tensor_tensor(o1, le, bc32(mA), op=ALU.is_ge))
    Dv(lambda e: e.scalar_tensor_tensor(out=le, in0=o1, scalar=-1e30, in1=le, op0=ALU.mult, op1=ALU.add))
    Dv(lambda e: e.tensor_reduce(out=mB, in_=le, axis=AX.X, op=ALU.max))
    Dv(lambda e: e.tensor_tensor(o2, le, bc32(mB), op=ALU.is_ge))
    Dv(lambda e: e.tensor_tensor(w1, mB, mA, op=ALU.subtract))
    P.op("act", lambda e: e.activation(out=w1, in_=w1, func=AF.Exp), reads=R, writes=R)
    Dv(lambda e: e.tensor_scalar(w1, w1, 1.0, None, op0=ALU.add))
    Dv(lambda e: e.reciprocal(w1, w1))
    Dv(lambda e: e.tensor_scalar(w2, w1, -1.0, 1.0, op0=ALU.mult, op1=ALU.add))
    Dv(lambda e: e.tensor_tensor(w1, w1, gs_, op=ALU.mult))
    Dv(lambda e: e.tensor_tensor(w2, w2, gs_, op=ALU.mult))
    Dv(lambda e: e.tensor_tensor(tmp, o1, o2, op=ALU.add))
    P.op("dve", lambda e: e.tensor_copy(indb, tmp.rearrange("p t c -> p (t c)")), reads=R, writes=["indb"])
    for c in range(2):
        P.op("pe", lambda e, c=c: e.matmul(ps[c][:, :], lhsT=SU, rhs=indb[:, c * 512:(c + 1) * 512], start=True, stop=True), reads=["SU", "indb"], writes=["ps%d" % c])
        P.op("pe", lambda e, c=c: e.matmul(ps[2 + c][:, :], lhsT=k.ones_b, rhs=indb[:, c * 512:(c + 1) * 512], start=True, stop=True), reads=["ones_b", "indb"], writes=["ps%d" % (2 + c)])
    for c in range(2):
        P.op("dve", lambda e, c=c: e.tensor_copy(pos.rearrange("p t c -> p (t c)")[:, c * 512:(c + 1) * 512], ps[c][:, :]), reads=["ps%d" % c] + R, writes=R)
        P.op("dve", lambda e, c=c: e.tensor_copy(tmp.rearrange("p t c -> p (t c)")[:, c * 512:(c + 1) * 512], ps[2 + c][:, :]), reads=["ps%d" % (2 + c)] + R, writes=R)
    Dv(lambda e: e.memset(offs[:, 0, :], 0.0))
    for t in range(1, NT):
        Dv(lambda e, t=t: e.tensor_tensor(offs[:, t, :], offs[:, t - 1, :], tmp[:, t - 1, :], op=ALU.add))
    Dv(lambda e: e.tensor_tensor(pos, pos, offs, op=ALU.add))
    Dv(lambda e: e.tensor_scalar(tmp, pos, float(CAP), None, op0=ALU.is_lt))
    Dv(lambda e: e.tensor_tensor(pos, pos, k.ecap.unsqueeze(1).to_broadcast([128, NT, 32]), op=ALU.add))
    Dv(lambda e: e.tensor_tensor(pos, pos, tmp, op=ALU.mult))
    Dv(lambda e: e.tensor_scalar(offs, tmp, -1.0, k.trash[:, 0:1], op0=ALU.add, op1=ALU.mult))
    Dv(lambda e: e.tensor_tensor(pos, pos, offs, op=ALU.add))
    Dv(lambda e: e.tensor_tensor(offs, o1, pos, op=ALU.mult))
    Dv(lambda e: e.tensor_reduce(out=v1, in_=offs, axis=AX.X, op=ALU.add))
    P.op("dve", lambda e: e.tensor_copy(idx1, v1), reads=R, writes=R)
    Dv(lambda e: e.tensor_tensor(offs, o2, pos, op=ALU.mult))
    Dv(lambda e: e.tensor_reduce(out=v2, in_=offs, axis=AX.X, op=ALU.add))
    P.op("dve", lambda e: e.tensor_copy(idx2, v2), reads=R, writes=R)
    Dv(lambda e: e.tensor_tensor(offs, o1, tmp, op=ALU.mult))
    Dv(lambda e: e.tensor_reduce(out=v1, in_=offs, axis=AX.X, op=ALU.add))
    Dv(lambda e: e.tensor_tensor(g1, w1, v1, op=ALU.mult))
    Dv(lambda e: e.tensor_tensor(offs, o2, tmp, op=ALU.mult))
    Dv(lambda e: e.tensor_reduce(out=v2, in_=offs, axis=AX.X, op=ALU.add))
    Dv(lambda e: e.tensor_tensor(g2, w2, v2, op=ALU.mult))
    P.op("pool", lambda e: e.memset(k.xb[0], 0.0), writes=["xb0"])
    P.dma("sp", lambda e: e.dma_start(out=Ybuf[NSL:NSL + 128, :], in_=k.xb[0]), reads=["xb0"], key="xb0")
    if l == 0:
        zb = k.xb[0].bitcast(BF16)[:, 0:1024]
        nrt = (NSL + 128) // 128
        Xv = Xbuf.rearrange("(n p) c -> p n c", p=128)
        for n0 in range(0, nrt, 16):
            nn = min(16, nrt - n0)
            P.dma("sp", lambda e, n0=n0, nn=nn: e.dma_start(out=Xv[:, n0:n0 + nn, :], in_=zb.unsqueeze(1).to_broadcast([128, nn, 1024])), reads=["xb0"], writes=["XbufZ"], key="xbz")
    if "dbg_idx" in k.debug:
        dbi = dram(k, "dbg_idx", [128, 2 * NT], I32)
        P.dma("sp", lambda e: e.dma_start(out=dbi[:, 0:NT], in_=idx1), reads=R, key="dbgi")
        P.dma("sp", lambda e: e.dma_start(out=dbi[:, NT:2 * NT], in_=idx2), reads=R, key="dbgi")
        dbg_ = dram(k, "dbg_g", [128, 2 * NT], F32)
        P.dma("sp", lambda e: e.dma_start(out=dbg_, in_=g12), reads=R, key="dbgg")
    for t in range(NT):
        for (ix, nm) in ((idx1, "a"), (idx2, "b")):
            P.dma("pool", lambda e, t=t, ix=ix: e.indirect_dma_start(out=Xbuf, out_offset=bass.IndirectOffsetOnAxis(ap=ix[:, t:t + 1], axis=0), in_=h2tm[:, t, :], in_offset=None),
                  reads=["h2tm", "XbufZ"] + R, writes=[], key="sc%s%d" % (nm, t % 4))
    P.barrier()
    A.release(mH)
    wg = [A.alloc(8 * 512, BF16).rearrange("p (k c) -> p k c", k=8) for _ in range(2)]
    wu = [A.alloc(8 * 512, BF16).rearrange("p (k c) -> p k c", k=8) for _ in range(2)]
    wd = [A.alloc(4 * 1024, BF16).rearrange("p (k c) -> p k c", k=4) for _ in range(2)]
    XT = [A.alloc(8 * CAP, BF16).rearrange("p (k s) -> p k s", k=8) for _ in range(2)]
    xr = [A.alloc(1024, BF16) for _ in range(3)]
    hid = [A.alloc(4 * CAP, BF16).rearrange("p (k s) -> p k s", k=4) for _ in range(2)]
    sg = [A.alloc(512, F32) for _ in range(2)]
    yrow = [A.alloc(1024, F32) for _ in range(3)]
    NST = CAP // 128
    if "dbg_X0" in k.debug:
        dx = dram(k, "dbg_X0", [128, 1024], BF16)
        P.dma("sp", lambda e: e.dma_start(out=xr[2], in_=Xbuf[0:128, :]), writes=["xr2"], key="xr2")
        P.dma("sp", lambda e: e.dma_start(out=dx, in_=xr[2]), reads=["xr2"], key="dbgx0")
    chunks = [(0, 512), (512, CAP - 512)] if CAP > 512 else [(0, CAP)]
    cnts = {"xc": 0, "cc": 0, "yc": 0, "scn": 0}
    NEXP = 32

    def Wload(ex):
        i2 = ex % 2
        P.dma("pool", lambda e: e.dma_start(out=wg[i2], in_=I["w_gate"][l, ex].rearrange("(k p) c -> p k c", p=128)), writes=["wg%d" % i2], key="wg%d" % i2)
        P.dma("pool", lambda e: e.dma_start(out=wu[i2], in_=I["w_up"][l, ex].rearrange("(k p) c -> p k c", p=128)), writes=["wu%d" % i2], key="wu%d" % i2)
        P.dma("pool", lambda e: e.dma_start(out=wd[i2], in_=I["w_down"][l, ex].rearrange("(k p) c -> p k c", p=128)), writes=["wd%d" % i2], key="wd%d" % i2)

    def Tphase(ex):
        i2 = ex % 2
        xt = XT[i2]; xtk = "XT%d" % i2
        for st in range(NST):
            xc = cnts["xc"]
            r = xr[xc % 3]; rk = "xr%d" % (xc % 3)
            P.dma("sp", lambda e, r=r, st=st: e.dma_start(out=r, in_=Xbuf[ex * CAP + st * 128:ex * CAP + (st + 1) * 128, :]), writes=[rk], key=rk)
            bank = 6 + xc % 2
            tb = ps[bank][:, :].bitcast(BF16)
            for kc in range(8):
                P.op("pe", lambda e, kc=kc, r=r, tb=tb: e.transpose(tb[:, kc * 128:(kc + 1) * 128], r[:, kc * 128:(kc + 1) * 128], k.identb), reads=[rk, "identb"], writes=["ps%d" % bank])
            if xc % 2 == 0:
                P.op("act", lambda e, tb=tb, st=st: e.activation(out=xt[:, :, st * 128:(st + 1) * 128], in_=tb.rearrange("p (k s) -> p k s", k=8), func=AF.Copy), reads=["ps%d" % bank], writes=[xtk])
            else:
                P.op("dve", lambda e, tb=tb, st=st: e.tensor_copy(xt[:, :, st * 128:(st + 1) * 128], tb.rearrange("p (k s) -> p k s", k=8)), reads=["ps%d" % bank], writes=[xtk])
            cnts["xc"] += 1

    def Hphase(ex):
        i2 = ex % 2
        xt = XT[i2]; xtk = "XT%d" % i2
        hd = hid[i2]; hk = "hid%d" % i2
        for hb in range(4):
            for (c0, cn) in chunks:
                cc = cnts["cc"]; scn = cnts["scn"]
                bg = (cc % 2) * 2; bu = bg + 1
                for kc in range(8):
                    P.op("pe", lambda e, kc=kc, hb=hb, bg=bg, c0=c0, cn=cn: e.matmul(ps[bg][:, 0:cn], lhsT=wg[i2][:, kc, hb * 128:(hb + 1) * 128], rhs=xt[:, kc, c0:c0 + cn], start=(kc == 0), stop=(kc == 7)),
                         reads=["wg%d" % i2, xtk], writes=["ps%d" % bg])
                for kc in range(8):
                    P.op("pe", lambda e, kc=kc, hb=hb, bu=bu, c0=c0, cn=cn: e.matmul(ps[bu][:, 0:cn], lhsT=wu[i2][:, kc, hb * 128:(hb + 1) * 128], rhs=xt[:, kc, c0:c0 + cn], start=(kc == 0), stop=(kc == 7)),
                         reads=["wu%d" % i2, xtk], writes=["ps%d" % bu])
                s_ = sg[scn % 2]; sk = "sgF%d" % (scn % 2)
                P.op("act", lambda e, s_=s_, bg=bg, cn=cn: e.activation(out=s_[:, 0:cn], in_=ps[bg][:, 0:cn], func=AF.Silu), reads=["ps%d" % bg], writes=[sk])
                P.op("dve", lambda e, s_=s_, bu=bu, hb=hb, c0=c0, cn=cn: e.tensor_tensor(hd[:, hb, c0:c0 + cn], s_[:, 0:cn], ps[bu][:, 0:cn], op=ALU.mult), reads=[sk, "ps%d" % bu], writes=[hk])
                cnts["scn"] += 1; cnts["cc"] += 1

    def Dphase(ex):
        i2 = ex % 2
        hd = hid[i2]; hk = "hid%d" % i2
        for st in range(NST):
            yc = cnts["yc"]
            y = yrow[yc % 3]; yk = "yrow%d" % (yc % 3)
            for half in range(2):
                bank = 4 + half
                for kc in range(4):
                    P.op("pe", lambda e, kc=kc, half=half, bank=bank, st=st: e.matmul(ps[bank][:, :], lhsT=hd[:, kc, st * 128:(st + 1) * 128], rhs=wd[i2][:, kc, half * 512:(half + 1) * 512], start=(kc == 0), stop=(kc == 3)),
                         reads=[hk, "wd%d" % i2], writes=["ps%d" % bank])
                if half == 0:
                    P.op("act", lambda e, y=y, bank=bank: e.activation(out=y[:, 0:512], in_=ps[bank][:, :], func=AF.Copy), reads=["ps%d" % bank], writes=[yk])
                else:
                    P.op("dve", lambda e, y=y, bank=bank: e.tensor_copy(y[:, 512:1024], ps[bank][:, :]), reads=["ps%d" % bank], writes=[yk])
            P.dma("sp", lambda e, y=y, st=st: e.dma_start(out=Ybuf[ex * CAP + st * 128:ex * CAP + (st + 1) * 128, :], in_=y), reads=[yk], key=yk)
            cnts["yc"] += 1

    Wload(0)
    Tphase(0)
    for ex in range(NEXP):
        if ex + 1 < NEXP:
            Wload(ex + 1)
        Hphase(ex)
        if ex + 1 < NEXP:
            Tphase(ex + 1)
        Dphase(ex)
    P.barrier()
    A.release(mH)
    Y1 = [A.alloc(1024, F32) for _ in range(2)]
    Y2 = [A.alloc(1024, F32) for _ in range(2)]
    for t in range(NT):
        b = t % 2
        x = k.xb[b]; xk = "xb%d" % b
        P.dma("sp", lambda e, x=x, t=t: e.dma_start(out=x, in_=xsrc[t * 128:(t + 1) * 128, :]), writes=[xk], key=xk)
        P.dma("pool", lambda e, t=t, b=b: e.indirect_dma_start(out=Y1[b], out_offset=None, in_=Ybuf, in_offset=bass.IndirectOffsetOnAxis(ap=idx1[:, t:t + 1], axis=0)), reads=R, writes=["Y1%d" % b], key="Y1%d" % b)
        P.dma("pool", lambda e, t=t, b=b: e.indirect_dma_start(out=Y2[b], out_offset=None, in_=Ybuf, in_offset=bass.IndirectOffsetOnAxis(ap=idx2[:, t:t + 1], axis=0)), reads=R, writes=["Y2%d" % b], key="Y2%d" % b)
        P.op("act", lambda e, t=t, b=b: e.activation(out=Y1[b], in_=Y1[b], func=AF.Copy, scale=g1[:, t:t + 1]), reads=["Y1%d" % b] + R, writes=["Y1%d" % b])
        P.op("dve", lambda e, t=t, b=b: e.scalar_tensor_tensor(out=Y1[b], in0=Y2[b], scalar=g2[:, t:t + 1], in1=Y1[b], op0=ALU.mult, op1=ALU.add), reads=["Y1%d" % b, "Y2%d" % b] + R, writes=["Y1%d" % b])
        P.op("dve", lambda e, b=b: e.tensor_tensor(Y1[b], Y1[b], k.gtbc[1], op=ALU.mult), reads=["Y1%d" % b, "gtbc1"], writes=["Y1%d" % b])
        P.op("dve", lambda e, x=x, b=b: e.tensor_tensor(x, x, Y1[b], op=ALU.add), reads=["Y1%d" % b, xk], writes=[xk])
        P.dma("sp", lambda e, x=x, t=t: e.dma_start(out=xdst[t * 128:(t + 1) * 128, :], in_=x), reads=[xk], key=xk)
    P.barrier()
    A.release(m0)


from concourse.bass_utils import run_bass_kernel_spmd

_WNAMES = ["w_mod", "b_mod", "g_norm1", "g_norm2", "w_in", "g_cq", "g_ckv", "g_kidx", "w_q_up", "w_idx_q", "w_v_up",
           "lb_logits", "g_rec", "w_branch_a", "w_branch_r", "w_out", "w_grp", "b_grp", "w_exp_router", "b_exp_router",
           "w_gate", "w_up", "w_down", "g_final"]


def _build(shapes, depth=4):
    nc = bass.Bass("TRN2", target_bir_lowering=False)
    es = ExitStack()
    with es:
        I = {}
        I["x"] = nc.dram_tensor("x", [S, D], F32, kind="ExternalInput").ap()
        I["c"] = nc.dram_tensor("c", [1, D], F32, kind="ExternalInput").ap()
        for n in _WNAMES:
            I[n] = nc.dram_tensor(n, list(shapes[n]), F32, kind="ExternalInput").ap()
        out = nc.dram_tensor("out", [S, D], F32, kind="ExternalOutput").ap()
        k = mkctx(nc, es)
        setup_consts(k)
        pm = k.A.mark()
        xa = dram(k, "xres_a", [S, D], F32)
        xb = dram(k, "xres_b", [S, D], F32)
        xcur = I["x"]
        for l in range(depth):
            k.A.release(pm)
            phase0(k, l, I)
            m_after0 = k.A.mark()
            phaseA(k, l, I, xcur)
            phaseB(k, l, I)
            phaseC(k, l, I)
            k.A.release(m_after0)
            phaseD(k, l, I)
            phaseE(k, l, I, xcur, xa)
            phaseF2(k, l, I, xa, xb)
            xcur = xb
        phaseG(k, I, xcur, out)
        k.P.finish(k.A.alloc(2, F32))
        k.P.emit(es)
    return nc


def kernel(**inputs):
    x = np.ascontiguousarray(inputs["x"], dtype=np.float32)
    c = np.ascontiguousarray(inputs["c"], dtype=np.float32)
    shapes = {n: inputs[n].shape for n in _WNAMES}
    nc = _build(shapes)
    w = {n: np.ascontiguousarray(inputs[n], dtype=np.float32) for n in _WNAMES}
    in_maps = []
    for b in range(8):
        m = {"x": x[b], "c": c[b:b + 1]}
        m.update(w)
        in_maps.append(m)
    res = run_bass_kernel_spmd(nc, in_maps, core_ids=list(range(8)))
    return np.stack([np.asarray(r["out"], dtype=np.float32) for r in res.results], axis=0)
```
